# Optimizing a Trainium2 kernel written in Bass

```python
import jax
import jax.numpy as jnp
from jax import lax
import numpy as np

D_MODEL = 1024
BATCH = 8
SEQ = 2048
DEPTH = 2

GRID_W = 64
CTX_LEN = 256
N_MIXERS = 2
RET_HEADS = 4
RET_DK = 256
RET_DV = 512
RET_QK = RET_HEADS * RET_DK
RET_V = RET_HEADS * RET_DV
RET_IN = 2 * RET_QK + 2 * RET_V
RET_CHUNK = 128
HG_HEADS = 8
HG_DK = 128
HG_DV = D_MODEL // HG_HEADS
HG_K = HG_HEADS * HG_DK
HG_V = HG_HEADS * HG_DV
HG_IN = 3 * HG_K + 2 * HG_V
HG_CHUNK = 64
N_EXPERTS = 16
EC_CAPACITY = 2
EXPERT_FF = 2816
ROPE_THETA = 10000.0
EPS = 1e-6
F32 = jnp.float32

kernel_name = 'hybrid_retention_hgrn2_ec_moe_dit'


def rmsnorm(x, w=None):
    xf = x.astype(F32)
    y = xf * lax.rsqrt(jnp.mean(xf * xf, axis=-1, keepdims=True) + EPS)
    if w is not None:
        y = y * w.astype(F32)
    return y.astype(x.dtype)


def modulate(h, shift, scale):
    return h * (1 + scale) + shift


def split_heads(a, n_heads, d):
    B, T, _ = a.shape
    return a.reshape(B, T, n_heads, d).transpose(0, 2, 1, 3)


def merge_heads(a):
    B, H, T, d = a.shape
    return a.transpose(0, 2, 1, 3).reshape(B, T, H * d)


def grid_angles(n):
    rows = n // GRID_W
    r = jnp.repeat(jnp.arange(rows), GRID_W).astype(F32)
    col = jnp.tile(jnp.arange(GRID_W), rows).astype(F32)
    half = RET_DK // 2
    inv = ROPE_THETA ** (-jnp.arange(0, half, 2, dtype=F32) / half)
    return r[:, None] * inv, col[:, None] * inv


def rope(x, ang):
    x1, x2 = jnp.split(x, 2, axis=-1)
    cos = jnp.cos(ang).astype(x.dtype)
    sin = jnp.sin(ang).astype(x.dtype)
    return jnp.concatenate([x1 * cos - x2 * sin, x1 * sin + x2 * cos], axis=-1)


def rope2d(x, ang_r, ang_c):
    xr, xc = jnp.split(x, 2, axis=-1)
    return jnp.concatenate([rope(xr, ang_r), rope(xc, ang_c)], axis=-1)


def retention_log_decay():
    j = jnp.arange(2 * RET_HEADS, dtype=F32)
    lg = jnp.log1p(-jnp.exp2(-5.0 - j / 2))
    return lg[0::2], lg[1::2]


def chunk_recurrence(q, k, v, g, s0, chunk):
    B, H, T, dk = q.shape
    dv = v.shape[-1]
    dg = g.shape[-1]
    n = T // chunk

    def blocks(a):
        return a.reshape(B, H, n, chunk, a.shape[-1]).transpose(2, 0, 1, 3, 4)

    lower = jnp.tril(jnp.ones((chunk, chunk), dtype=bool))[:, :, None]

    def step(S, blk):
        qc, kc, vc, gc = blk
        b = jnp.cumsum(gc.astype(F32), axis=2)
        b_end = b[:, :, -1:, :]
        o = jnp.einsum('bhik,bhkv->bhiv', qc * jnp.exp(b), S)
        decay = jnp.exp(jnp.where(lower, b[:, :, :, None, :] - b[:, :, None, :, :], -jnp.inf))
        if dg == 1:
            scores = jnp.einsum('bhik,bhjk->bhij', qc, kc) * decay[..., 0]
        else:
            scores = jnp.einsum('bhijk,bhjk->bhij', qc[:, :, :, None, :] * decay, kc)
        o = o + jnp.einsum('bhij,bhjv->bhiv', scores, vc)
        S = S * jnp.exp(jnp.swapaxes(b_end, -1, -2)) + jnp.einsum('bhjk,bhjv->bhkv', kc * jnp.exp(b_end - b), vc)
        return S, o

    S, o = lax.scan(step, s0, (blocks(q), blocks(k), blocks(v), blocks(g)))
    return o.transpose(1, 2, 0, 3, 4).reshape(B, H, T, dv), S


def recurrence_final_state(k, v, g):
    b = jnp.cumsum(g.astype(F32), axis=2)
    return jnp.einsum('bhtk,bhtv->bhkv', k * jnp.exp(b[:, :, -1:, :] - b), v)


def bidirectional_recurrence(lat, ctx, chunk, need_ctx_out):
    rev = lambda a: jnp.flip(a, axis=2)
    q, kf, gf, kb, gb, v = ctx
    B, H, _, dk = q.shape
    dv = v.shape[-1]
    if need_ctx_out:
        s0 = jnp.zeros((B, H, dk, dv), F32)
        oc_f, sf = chunk_recurrence(q, kf, v, gf, s0, chunk)
        oc_b, sb = chunk_recurrence(rev(q), rev(kb), rev(v), rev(gb), s0, chunk)
        oc = oc_f + rev(oc_b)
    else:
        sf = recurrence_final_state(kf, v, gf)
        sb = recurrence_final_state(rev(kb), rev(v), rev(gb))
        oc = None
    q, kf, gf, kb, gb, v = lat
    ox_f, _ = chunk_recurrence(q, kf, v, gf, sf, chunk)
    ox_b, _ = chunk_recurrence(rev(q), rev(kb), rev(v), rev(gb), sb, chunk)
    return ox_f + rev(ox_b), oc


def retention_mixer(hx, hc, w_in, w_out, need_ctx_out):
    ang_r, ang_c = grid_angles(hx.shape[1])
    log_gf, log_gb = retention_log_decay()

    def project(h, rotate):
        B, T, _ = h.shape
        q, k, v, gt = jnp.split(h @ w_in, [RET_QK, 2 * RET_QK, 2 * RET_QK + RET_V], axis=-1)
        q = split_heads(q, RET_HEADS, RET_DK)
        k = split_heads(k, RET_HEADS, RET_DK) * (RET_DK ** -0.5)
        v = split_heads(v, RET_HEADS, RET_DV)
        if rotate:
            q = rope2d(q, ang_r, ang_c)
            k = rope2d(k, ang_r, ang_c)
        gf = jnp.broadcast_to(log_gf[None, :, None, None], (B, RET_HEADS, T, 1))
        gb = jnp.broadcast_to(log_gb[None, :, None, None], (B, RET_HEADS, T, 1))
        return (q, k, gf, k, gb, v), gt

    def readout(o, gt):
        o = merge_heads(rmsnorm(o))
        return (jax.nn.silu(gt) * o.astype(gt.dtype)) @ w_out

    lat, gt_x = project(hx, True)
    ctx, gt_c = project(hc, False)
    ox, oc = bidirectional_recurrence(lat, ctx, RET_CHUNK, need_ctx_out)
    yx = readout(ox, gt_x)
    yc = readout(oc, gt_c) if need_ctx_out else None
    return yx, yc


def hgrn2_mixer(hx, hc, w_in, lb_f, lb_b, g_norm, w_out, need_ctx_out):
    def project(h):
        q, ff, fb, i, gt = jnp.split(h @ w_in, [HG_K, 2 * HG_K, 3 * HG_K, 3 * HG_K + HG_V], axis=-1)
        q = split_heads(jax.nn.silu(q), HG_HEADS, HG_DK)
        v = split_heads(i, HG_HEADS, HG_DV)

        def gate(fl, lb):
            f = lb + (1 - lb) * jax.nn.sigmoid(fl.astype(F32))
            return split_heads(1 - f, HG_HEADS, HG_DK), split_heads(jnp.log(f), HG_HEADS, HG_DK)

        kf, gf = gate(ff, lb_f)
        kb, gb = gate(fb, lb_b)
        return (q, kf, gf, kb, gb, v), gt

    def readout(o, gt):
        o = merge_heads(rmsnorm(o, g_norm))
        return (jax.nn.silu(gt) * o.astype(gt.dtype)) @ w_out

    lat, gt_x = project(hx)
    ctx, gt_c = project(hc)
    ox, oc = bidirectional_recurrence(lat, ctx, HG_CHUNK, need_ctx_out)
    yx = readout(ox, gt_x)
    yc = readout(oc, gt_c) if need_ctx_out else None
    return yx, yc


def expert_choice_ffn(h, w_router, w_gate, w_up, w_down):
    B, N, D = h.shape
    cap = EC_CAPACITY * N // N_EXPERTS
    aff = jax.nn.softmax(jnp.einsum('bnd,de->bne', h, w_router).astype(F32), axis=-1)
    gate, idx = lax.top_k(jnp.swapaxes(aff, 1, 2), cap)
    xe = jax.vmap(lambda hb, ib: hb[ib])(h, idx)
    a = jnp.einsum('becd,edf->becf', xe, w_gate)
    u = jnp.einsum('becd,edf->becf', xe, w_up)
    y = jnp.einsum('becf,efd->becd', jax.nn.silu(a) * u, w_down) * gate[..., None].astype(h.dtype)
    out = jax.vmap(lambda ib, yb: jnp.zeros((N, D), yb.dtype).at[ib].add(yb))(idx, y)
    return out.astype(h.dtype)


def setup_inputs(seed: int = 0) -> dict:
    key = jax.random.key(seed)
    ks = jax.random.split(key, 20)
    n_ret = (DEPTH + N_MIXERS - 1) // N_MIXERS
    n_hg = (DEPTH + N_MIXERS - 2) // N_MIXERS
    nrm = lambda k, shape, scale: jax.random.normal(k, shape, F32) * scale
    D = D_MODEL
    return {
        'x': nrm(ks[0], (BATCH, SEQ, D), 1.0),
        'c': nrm(ks[1], (BATCH, D), 1.0),
        'ctx': nrm(ks[2], (BATCH, CTX_LEN, D), 1.0),
        'c_ctx': nrm(ks[3], (D,), 1.0),
        'w_ada': nrm(ks[4], (DEPTH, D, 6 * D), 0.5 * D ** -0.5),
        'b_ada': nrm(ks[5], (DEPTH, 6 * D), 0.02),
        'norm_mix': 1.0 + nrm(ks[6], (DEPTH, D), 0.02),
        'norm_ffn': 1.0 + nrm(ks[7], (DEPTH, D), 0.02),
        'ret_w_in': nrm(ks[8], (n_ret, D, RET_IN), D ** -0.5),
        'ret_w_out': nrm(ks[9], (n_ret, RET_V, D), RET_V ** -0.5),
        'hg_w_in': nrm(ks[10], (n_hg, D, HG_IN), D ** -0.5),
        'hg_g_norm': 1.0 + nrm(ks[11], (n_hg, HG_DV), 0.02),
        'hg_lower_bounds': nrm(ks[12], (DEPTH, 2, HG_K), 0.1),
        'hg_w_out': nrm(ks[13], (n_hg, HG_V, D), HG_V ** -0.5),
        'moe_router': nrm(ks[14], (DEPTH, D, N_EXPERTS), D ** -0.5),
        'moe_w_gate': nrm(ks[15], (DEPTH, N_EXPERTS, D, EXPERT_FF), D ** -0.5),
        'moe_w_up': nrm(ks[16], (DEPTH, N_EXPERTS, D, EXPERT_FF), D ** -0.5),
        'moe_w_down': nrm(ks[17], (DEPTH, N_EXPERTS, EXPERT_FF, D), EXPERT_FF ** -0.5),
        'norm_final': 1.0 + nrm(ks[18], (D,), 0.02),
    }


def reference(x, c, ctx, c_ctx, w_ada, b_ada, norm_mix, norm_ffn, ret_w_in, ret_w_out,
              hg_w_in, hg_g_norm, hg_lower_bounds, hg_w_out, moe_router, moe_w_gate,
              moe_w_up, moe_w_down, norm_final):
    lb_w = jax.nn.softmax(hg_lower_bounds.astype(F32), axis=0)
    lower_bound = jnp.cumsum(lb_w, axis=0) - lb_w[0]
    cx = ctx
    for layer in range(DEPTH):
        last = layer == DEPTH - 1
        mix_id, slot = layer % N_MIXERS, layer // N_MIXERS
        mod_x = jnp.split((jax.nn.silu(c) @ w_ada[layer] + b_ada[layer])[:, None, :], 6, axis=-1)
        mod_c = jnp.split(jax.nn.silu(c_ctx) @ w_ada[layer] + b_ada[layer], 6, axis=-1)
        hx = modulate(rmsnorm(x, norm_mix[layer]), mod_x[0], mod_x[1])
        hc = modulate(rmsnorm(cx, norm_mix[layer]), mod_c[0], mod_c[1])
        if mix_id == 0:
            ox, oc = retention_mixer(hx, hc, ret_w_in[slot], ret_w_out[slot], not last)
        else:
            ox, oc = hgrn2_mixer(hx, hc, hg_w_in[slot], lower_bound[layer, 0], lower_bound[layer, 1],
                                 hg_g_norm[slot], hg_w_out[slot], not last)
        x = x + mod_x[2] * ox
        hx = modulate(rmsnorm(x, norm_ffn[layer]), mod_x[3], mod_x[4])
        x = x + mod_x[5] * expert_choice_ffn(hx, moe_router[layer], moe_w_gate[layer],
                                              moe_w_up[layer], moe_w_down[layer])
        if not last:
            cx = cx + mod_c[2] * oc
            hc = modulate(rmsnorm(cx, norm_ffn[layer]), mod_c[3], mod_c[4])
            cx = cx + mod_c[5] * expert_choice_ffn(hc, moe_router[layer], moe_w_gate[layer],
                                                    moe_w_up[layer], moe_w_down[layer])
    return rmsnorm(x, norm_final)
```

```python
import math
from contextlib import ExitStack

import numpy as np
import concourse.bass as bass
import concourse.mybir as mybir
from concourse.bass_utils import run_bass_kernel_spmd

F32 = mybir.dt.float32
BF16 = mybir.dt.bfloat16
ALU = mybir.AluOpType
AF = mybir.ActivationFunctionType
AX = mybir.AxisListType

D = 1024
KD = 8
T_LAT = 2048
T_CTX = 256
T_ALL = T_LAT + T_CTX
NT = T_ALL // 128
EPS = 1e-6
N_EXP = 16
FF = 2816
NFC = FF // 128
RET_H = 4
HG_H = 8

BLOCKS = [(0, 512, False), (512, 512, False), (1024, 512, False), (1536, 512, False), (2048, 256, True)]


def interleave(*gens):
    gens = list(gens)
    while gens:
        for g in list(gens):
            try:
                next(g)
            except StopIteration:
                gens.remove(g)


class Buf:
    __slots__ = ("name", "w", "rs")

    def __init__(self, name):
        self.name = name
        self.w = None
        self.rs = {}


class Sched:
    NDMA = 12

    def __init__(self, nc, es):
        self.nc = nc
        self.h = {"pe": nc.tensor, "act": nc.scalar, "dve": nc.vector, "pool": nc.gpsimd, "sp": nc.sync}
        self.sem = {}
        self.cnt = {}
        self.seen = {}
        for e in self.h:
            self.sem[e] = es.enter_context(nc.semaphore("s_" + e))
            self.cnt[e] = 0
            self.seen[e] = {}
        self.dsem = {}
        self.dval = {}
        self.dnext = {}
        for q in ("sp", "pool"):
            self.dsem[q] = [es.enter_context(nc.semaphore(f"d_{q}{i}")) for i in range(self.NDMA)]
            self.dval[q] = [0] * self.NDMA
            self.dnext[q] = 0
        self.ninst = 0

    def _wait(self, eng, tk):
        if tk is None:
            return
        kind = tk[0]
        if kind == "c":
            _, src, n = tk
            if src == eng and eng in ("pe", "sp"):
                return
            if self.seen[eng].get(src, 0) >= n:
                return
            self.h[eng].wait_ge(self.sem[src], n)
            self.seen[eng][src] = n
        else:
            _, q, idx, val = tk
            key = (q, idx)
            if self.seen[eng].get(key, 0) >= val:
                return
            self.h[eng].wait_ge(self.dsem[q][idx], val)
            self.seen[eng][key] = val
        self.ninst += 1

    def _deps(self, eng, R, W):
        for b in R:
            self._wait(eng, b.w)
        for b in W:
            self._wait(eng, b.w)
            for tk in b.rs.values():
                self._wait(eng, tk)

    def _record(self, tk, R, W):
        for b in W:
            b.w = tk
            b.rs = {}
        for b in R:
            if b in W:
                continue
            key = tk[1] if tk[0] == "c" else (tk[1], tk[2])
            b.rs[key] = tk

    @staticmethod
    def _flat(L):
        out = []
        for b in L:
            if isinstance(b, (list, tuple)):
                out.extend(Sched._flat(b))
            else:
                out.append(b)
        return out

    def op(self, eng, fn, R=(), W=(), signal=True):
        R, W = self._flat(R), self._flat(W)
        self._deps(eng, R, W)
        ins = fn(self.h[eng])
        self.ninst += 1
        if signal:
            ins.then_inc(self.sem[eng], 1)
            self.cnt[eng] += 1
            tk = ("c", eng, self.cnt[eng])
            if eng not in ("pe",):
                pass
        else:
            tk = ("c", eng, self.cnt[eng] + 1)
        self._record(tk, R, W)
        return tk

    def dma(self, q, out, in_, R=(), W=()):
        R, W = self._flat(R), self._flat(W)
        self._deps(q, R, W)
        idx = self.dnext[q]
        self.dnext[q] = (idx + 1) % self.NDMA
        prev = self.dval[q][idx]
        if prev > 0:
            self._wait(q, ("d", q, idx, prev))
        val = prev + 16
        self.dval[q][idx] = val
        self.h[q].dma_start(out=out, in_=in_).then_inc(self.dsem[q][idx], 16)
        self.ninst += 1
        tk = ("d", q, idx, val)
        self._record(tk, R, W)
        return tk

    def barrier(self):
        for e in self.h:
            for src in self.h:
                if src != e and self.cnt[src] > 0:
                    self._wait(e, ("c", src, self.cnt[src]))
            for q in ("sp", "pool"):
                for idx in range(self.NDMA):
                    if self.dval[q][idx] > 0:
                        self._wait(e, ("d", q, idx, self.dval[q][idx]))


def _ret_gammas():
    j = np.arange(8, dtype=np.float64)
    g = 1.0 - np.exp2(-5.0 - j / 2)
    return g[0::2], g[1::2]


def _const_tables():
    t = {}
    half = 128
    inv = 10000.0 ** (-np.arange(0, half, 2, dtype=np.float64) / half)
    p = np.arange(128)
    sign = np.where(p < 64, -1.0, 1.0)
    rows = np.arange(T_LAT // 64, dtype=np.float64)
    cols = np.arange(64, dtype=np.float64)
    ang_r = rows[None, :] * inv[p % 64][:, None]
    ang_c = cols[None, :] * inv[p % 64][:, None]
    t["rope"] = np.concatenate(
        [np.cos(ang_r), np.sin(ang_r) * sign[:, None], np.cos(ang_c), np.sin(ang_c) * sign[:, None]], axis=1
    ).astype(np.float32)
    gf, gb = _ret_gammas()
    b = np.arange(128, dtype=np.float64)[:, None]
    a = np.arange(512, dtype=np.float64)[None, :]
    tabs = np.zeros((RET_H, 128, 1920), dtype=np.float64)
    xs = np.arange(896, dtype=np.float64)[None, :] - 384.0
    for h in range(RET_H):
        tabs[h, :, 0:512] = gf[h] ** (a - b)
        tabs[h, :, 512:1024] = gb[h] ** (b + 511 - a)
        dl = xs - b
        tabs[h, :, 1024:1920] = np.where(dl > 0, gf[h] ** np.maximum(dl, 0),
                                         np.where(dl < 0, gb[h] ** np.maximum(-dl, 0), 2.0))
    t["rdec"] = tabs.astype(np.float32)
    t["ident"] = np.eye(128, dtype=np.float32)
    t["hgmask"] = np.concatenate([np.triu(np.ones((128, 128))), np.tril(np.ones((128, 128)))], axis=1).astype(np.float32)
    t["iota1"] = np.tile(np.arange(1, 257, dtype=np.float32)[None, :], (128, 1))
    return t


class Prog:
    def __init__(self, dbg=None, stop_after=None):
        self.dbg = dbg or []
        self.stop_after = stop_after
        self.nc = bass.Bass("TRN2", target_bir_lowering=False)
        self.es = ExitStack()
        self.S = Sched(self.nc, self.es)
        self.dram = {}
        self.dbg_out = {}

    def din(self, name, shape, dt=F32):
        self.dram[name] = self.nc.dram_tensor(name, list(shape), dt, kind="ExternalInput").ap()
        return self.dram[name]

    def dout(self, name, shape, dt=F32):
        self.dram[name] = self.nc.dram_tensor(name, list(shape), dt, kind="ExternalOutput").ap()
        return self.dram[name]

    def sb(self, es, name, shape, dt):
        self._uid = getattr(self, "_uid", 0) + 1
        return es.enter_context(self.nc.sbuf_tensor(f"{name}_u{self._uid}", list(shape), dt))

    def ps(self, es, name, shape, dt=F32):
        return es.enter_context(self.nc.psum_tensor(name, list(shape), dt))


def build_program(dbg=(), stop_after=None, parts=("ret", "moe0", "hg", "moe1", "final"), n_exp_run=N_EXP):
    P = Prog(list(dbg), stop_after)
    P.parts = parts
    P.n_exp_run = n_exp_run
    nc, S, es = P.nc, P.S, P.es
    op, dma = S.op, S.dma

    xT_d = P.din("xT", [D, T_ALL])
    cvec_d = P.din("cvec", [128, 2 * KD])
    wada_d = P.din("w_ada", [2, D, 6 * D])
    bada_d = P.din("b_ada", [2, 12 * D])
    nrm_d = P.din("norms", [128, 5 * KD])
    retin_d = P.din("ret_w_in", [D, 6144])
    retout_d = P.din("ret_w_out", [2048, D])
    router_d = P.din("moe_router", [2, D, N_EXP])
    rope_d = P.din("rope", [128, 192])
    rdec_d = P.din("rdec", [RET_H, 128, 1920])
    ident_d = P.din("ident", [128, 128])
    out_d = P.dout("outT", [D, T_LAT])
    for name, shape, dt_ in P.dbg:
        P.dbg_out[name] = P.dout("dbg_" + name, shape, dt_)

    xT = P.sb(es, "xT_sb", [128, KD, T_ALL], F32)
    B_x = [[Buf(f"x{k}_{b}") for b in range(5)] for k in range(KD)]
    cvec = P.sb(es, "cvec_sb", [128, 2 * KD], F32)
    scc = P.sb(es, "scc", [128, KD, 2], F32)
    nrm = P.sb(es, "nrm", [128, 5 * KD], F32)
    mod = P.sb(es, "mod", [128, 2, 48, 2], F32)
    modA = P.sb(es, "modA", [128, 2, 2, KD, 2], F32)
    ident_f = P.sb(es, "ident_f", [128, 128], F32)
    ident_b = P.sb(es, "ident_b", [128, 128], BF16)
    ones_b = P.sb(es, "ones_b", [128, 128], BF16)
    B_const = Buf("const")
    B_mod = Buf("mod")

    psb = [P.ps(es, f"psb{i}", [128, 512], F32) for i in range(8)]
    B_ps = [Buf(f"ps{i}") for i in range(8)]

    def sl(b):
        s, n, _ = BLOCKS[b]
        return slice(s, s + n)

    xsrc = xT_d.rearrange("(k p) t -> p k t", p=128)
    for k in range(KD):
        dma("sp", xT[:, k, :], xsrc[:, k, :], W=B_x[k])
    dma("sp", cvec[:], cvec_d, W=[B_const])
    dma("sp", nrm[:], nrm_d, W=[B_const])
    dma("sp", ident_f[:], ident_d, W=[B_const])
    op("act", lambda e: e.copy(out=ident_b[:], in_=ident_f[:]), R=[B_const], W=[B_const])
    op("dve", lambda e: e.memset(ones_b[:], 1.0), W=[B_const])
    op("act", lambda e: e.activation(out=scc[:].rearrange("p k c -> p c k"),
                                     in_=cvec[:].rearrange("p (c k) -> p c k", c=2), func=AF.Silu),
       R=[B_const], W=[B_const])

    with ExitStack() as pes:
        NPIECE = 12
        NST = 4
        wa = [P.sb(pes, f"wa{i}", [128, KD, 512], F32) for i in range(NST)]
        B_wa = [Buf(f"wa{i}") for i in range(NST)]
        HALF = 3 * D
        modrow = P.sb(pes, "modrow", [2, HALF], F32)
        B_mr = Buf("modrow")
        bada2 = P.sb(pes, "bada2", [2, HALF], F32)
        B_b2 = Buf("bada2")
        pi = 0
        for l in range(2):
            wsrc = wada_d[l].rearrange("(k p) f -> p k f", p=128)
            for hf in range(2):
                dma("sp", bada2[:], bada_d[:, l * 6 * D + hf * HALF: l * 6 * D + (hf + 1) * HALF], W=[B_b2])
                for pc6 in range(6):
                    pc = hf * 6 + pc6
                    slot = pi % NST
                    dma("sp", wa[slot][:], wsrc[:, :, pc * 512:(pc + 1) * 512], W=[B_wa[slot]])
                    pb = pi % 2
                    for k in range(KD):
                        op("pe", lambda e: e.matmul(psb[pb][0:2, :], lhsT=scc[:, k, :], rhs=wa[slot][:, k, :],
                                                    start=(k == 0), stop=(k == KD - 1)),
                           R=[B_wa[slot], B_const], W=[B_ps[pb]], signal=(k == KD - 1))
                    op("dve", lambda e: e.tensor_tensor(out=modrow[:, pc6 * 512:(pc6 + 1) * 512], in0=psb[pb][0:2, :],
                                                        in1=bada2[:, pc6 * 512:(pc6 + 1) * 512],
                                                        op=ALU.add), R=[B_ps[pb], B_b2], W=[B_mr])
                    pi += 1
                pt_ = 2 + (l * 2 + hf) % 2
                for j in range(24):
                    op("pe", lambda e: e.transpose(out=psb[pt_][:, j * 2:(j + 1) * 2], in_=modrow[:, j * 128:(j + 1) * 128],
                                                   identity=ident_f[0:2, 0:2]),
                       R=[B_mr, B_const], W=[B_ps[pt_]], signal=(j == 23))
                op("dve", lambda e: e.tensor_copy(out=mod[:, l, hf * 24:(hf + 1) * 24, :].rearrange("p j c -> p (j c)"),
                                                  in_=psb[pt_][:, 0:48]), R=[B_ps[pt_]], W=[B_mod])
        for l in range(2):
            for site in range(2):
                sc_j = (1 if site == 0 else 4) * KD
                nw = nrm[:, (site * 2 + l) * KD:(site * 2 + l + 1) * KD]
                op("dve", lambda e, l=l, site=site, sc_j=sc_j, nw=nw: e.scalar_tensor_tensor(
                    out=modA[:, l, site, :, :], in0=mod[:, l, sc_j:sc_j + KD, :], scalar=1.0,
                    in1=nw.unsqueeze(2).to_broadcast([128, KD, 2]), op0=ALU.add, op1=ALU.mult),
                   R=[B_mod, B_const], W=[B_mod])
        S.barrier()

    def modv(l, chunk, k, c):
        return mod[:, l, chunk * KD + k, c:c + 1]

    def norm_block(l, site, b, dst_fn, tmp, B_tmp, rstd, B_rstd, sq, B_sq, psn_i, final=False, after=None):
        s, n, isc = BLOCKS[b]
        c = 1 if isc else 0
        for k in range(KD):
            q = k % 2
            op("act", lambda e, k=k, q=q: e.activation(out=sq[q][:, :n], in_=xT[:, k, s:s + n], func=AF.Square),
               R=[B_x[k][b]], W=[B_sq[q]])
            op("pe", lambda e, k=k, q=q: e.matmul(psb[psn_i][:, :n], lhsT=ones_b[:], rhs=sq[q][:, :n],
                                                 start=(k == 0), stop=(k == KD - 1)),
               R=[B_sq[q], B_const], W=[B_ps[psn_i]], signal=True)
        op("act", lambda e: e.activation(out=rstd[:, :n], in_=psb[psn_i][:, :n], func=AF.Sqrt, bias=EPS,
                                         scale=1.0 / D), R=[B_ps[psn_i]], W=[B_rstd])
        op("dve", lambda e: e.reciprocal(out=rstd[:, :n], in_=rstd[:, :n]), R=[B_rstd], W=[B_rstd])
        for k in range(KD):
            q = k % 2
            out_ap, obufs = dst_fn(k)
            if final:
                a_ap = nrm[:, 4 * KD + k:4 * KD + k + 1]
                op("dve", lambda e, k=k, a_ap=a_ap, out_ap=out_ap: e.scalar_tensor_tensor(
                    out=out_ap, in0=xT[:, k, s:s + n], scalar=a_ap, in1=rstd[:, :n], op0=ALU.mult, op1=ALU.mult),
                   R=[B_x[k][b], B_rstd, B_const], W=obufs)
                if after is not None:
                    after(k)
            else:
                a_ap = modA[:, l, site, k, c:c + 1]
                sh_ap = modv(l, 0 if site == 0 else 3, k, c)
                op("dve", lambda e, k=k, q=q, a_ap=a_ap: e.scalar_tensor_tensor(
                    out=tmp[q][:, :n], in0=xT[:, k, s:s + n], scalar=a_ap, in1=rstd[:, :n],
                    op0=ALU.mult, op1=ALU.mult), R=[B_x[k][b], B_rstd, B_mod], W=[B_tmp[q]])
                op("act", lambda e, q=q, sh_ap=sh_ap, out_ap=out_ap: e.activation(
                    out=out_ap, in_=tmp[q][:, :n], func=AF.Identity, bias=sh_ap, scale=1.0),
                   R=[B_tmp[q], B_mod], W=obufs)

    def debug_dump(name, ap_fn):
        if name in P.dbg_out:
            S.barrier()
            tk = ap_fn(P.dbg_out[name])
            S.barrier()

    NSLOT = 4
    wring = [P.sb(es, f"wring{i}", [128, KD, 512], BF16) for i in range(NSLOT)]
    B_wr = [Buf(f"wr{i}") for i in range(NSLOT)]
    wr_state = {"n": 0}

    def wslot():
        i = wr_state["n"] % NSLOT
        wr_state["n"] += 1
        return i

    def retention_layer(l):
        with ExitStack() as les:
            hT = P.sb(les, "hT", [128, KD, T_ALL], BF16)
            B_h = [[Buf(f"h{k}_{b}") for b in range(5)] for k in range(KD)]
            tmp = [P.sb(les, f"ntmp{i}", [128, 512], F32) for i in range(2)]
            B_tmp = [Buf("ntmp0"), Buf("ntmp1")]
            sg = [P.sb(les, f"sg{i}", [128, 512], F32) for i in range(2)]
            B_sg = [Buf("sg0"), Buf("sg1")]
            rstd = P.sb(les, "rstd", [128, 512], F32)
            B_rstd = Buf("rstd")
            sq = [P.sb(les, f"sq{i}", [128, 512], BF16) for i in range(2)]
            B_sq = [Buf("sq0"), Buf("sq1")]
            rope_sb = P.sb(les, "rope_sb", [128, 192], F32)
            B_rope = Buf("rope")
            dma("sp", rope_sb[:], rope_d, W=[B_rope])
            for b in range(5):
                s, n, _ = BLOCKS[b]
                norm_block(l, 0, b, lambda k, b=b, s=s, n=n: (hT[:, k, s:s + n], [B_h[k][b]]),
                           tmp, B_tmp, rstd, B_rstd, sq, B_sq, psn_i=7)
            debug_dump("hT0", lambda o: [dma("sp", o.rearrange("(k p) t -> p k t", p=128)[:, k, :], hT[:, k, :],
                                             R=B_h[k]) for k in range(KD)])
            if P.stop_after == "hT0":
                return

            qr = P.sb(les, "qr", [128, 2, 2, 512], BF16)
            kr = P.sb(les, "kr", [128, 2, T_ALL], BF16)
            vv = P.sb(les, "vv", [128, NT, 512], BF16)
            B_q = [[Buf(f"q{i}_{m}") for m in range(2)] for i in range(2)]
            B_k = [[Buf(f"k{m}_{t}") for t in range(NT)] for m in range(2)]
            B_v = [Buf(f"v{t}") for t in range(NT)]
            rdec = P.sb(les, "rdec_sb", [128, 1920], F32)
            B_rdec = Buf("rdec")
            Ff = rdec[:, 0:512]
            Fb = rdec[:, 512:1024]

            def Dg(kk, n):
                return rdec[:, 1024 + 384 - 128 * kk: 1024 + 384 - 128 * kk + n]
            rt1, B_rt1 = tmp, B_tmp
            rt2, B_rt2 = sg, B_sg
            ctab, B_ctab = tmp, B_tmp
            AT = [P.sb(les, f"AT{i}", [128, 512], BF16) for i in range(3)]
            B_AT = [Buf(f"AT{i}") for i in range(3)]
            ogT = P.sb(les, "ogT", [128, 4, 512], BF16)
            B_og = [Buf(f"og{c}") for c in range(4)]
            gf, gb = _ret_gammas()
            rcount = {"rope": 0, "at": 0, "q": 0}

            win_src = retin_d.rearrange("(k p) f -> p k f", p=128)
            wout_src = retout_d.rearrange("(c p) f -> p c f", p=128)

            def proj_rope(sA, qk, m, b, dst_ap, wb):
                s, n, isc = BLOCKS[b]
                pi_ = rcount["rope"] % 2
                rcount["rope"] += 1
                pst = psb[pi_]
                for k in range(KD):
                    op("pe", lambda e, k=k: e.matmul(
                        pst[:, :n], lhsT=wring[sA][:, k, qk * 256 + m * 128: qk * 256 + (m + 1) * 128],
                        rhs=hT[:, k, s:s + n], start=(k == 0), stop=(k == KD - 1)),
                       R=[B_wr[sA], B_h[k][b]], W=[B_ps[pi_]], signal=(k == KD - 1))
                scale = 1.0 if qk == 0 else 1.0 / 16.0
                if isc:
                    op("act", lambda e: e.activation(out=dst_ap, in_=pst[:, :n], func=AF.Copy, scale=scale),
                       R=[B_ps[pi_]], W=wb)
                    return
                g0 = s // 64
                if m == 0:
                    cos_ap = rope_sb[:, g0:g0 + 8].unsqueeze(2).to_broadcast([128, 8, 64])
                    sin_lo = rope_sb[0:64, 32 + g0:32 + g0 + 8].unsqueeze(2).to_broadcast([64, 8, 64])
                    sin_hi = rope_sb[64:128, 32 + g0:32 + g0 + 8].unsqueeze(2).to_broadcast([64, 8, 64])
                else:
                    cos_ap = rope_sb[:, 64:128].unsqueeze(1).to_broadcast([128, 8, 64])
                    sin_lo = rope_sb[0:64, 128:192].unsqueeze(1).to_broadcast([64, 8, 64])
                    sin_hi = rope_sb[64:128, 128:192].unsqueeze(1).to_broadcast([64, 8, 64])
                ti = pi_
                p3 = pst[:].rearrange("p (g t) -> p g t", t=64)
                t1v = rt1[ti][:].rearrange("p (g t) -> p g t", t=64)
                t2v = rt2[ti][:].rearrange("p (g t) -> p g t", t=64)
                op("dve", lambda e: e.scalar_tensor_tensor(
                    out=t1v, in0=p3, scalar=scale, in1=cos_ap, op0=ALU.mult, op1=ALU.mult),
                   R=[B_ps[pi_], B_rope], W=[B_rt1[ti]])
                op("dve", lambda e: e.scalar_tensor_tensor(
                    out=t2v[0:64], in0=p3[64:128], scalar=scale, in1=sin_lo, op0=ALU.mult, op1=ALU.mult),
                   R=[B_ps[pi_], B_rope], W=[B_rt2[ti]])
                op("dve", lambda e: e.scalar_tensor_tensor(
                    out=t2v[64:128], in0=p3[0:64], scalar=scale, in1=sin_hi, op0=ALU.mult, op1=ALU.mult),
                   R=[B_ps[pi_], B_rope, B_rt2[ti]], W=[B_rt2[ti]])
                op("pool", lambda e: e.tensor_tensor(
                    out=dst_ap, in0=rt1[ti][:, :n], in1=rt2[ti][:, :n], op=ALU.add),
                   R=[B_rt1[ti], B_rt2[ti]], W=wb)

            for h in range(RET_H):
                sA, sB, sC, sD = wslot(), wslot(), wslot(), wslot()
                dma("pool", wring[sA][:, :, 0:256], win_src[:, :, h * 256:(h + 1) * 256], W=[B_wr[sA]])
                dma("pool", wring[sA][:, :, 256:512], win_src[:, :, 1024 + h * 256:1024 + (h + 1) * 256], W=[B_wr[sA]])
                dma("pool", wring[sB][:], win_src[:, :, 2048 + h * 512:2048 + (h + 1) * 512], W=[B_wr[sB]])
                dma("pool", wring[sC][:], win_src[:, :, 4096 + h * 512:4096 + (h + 1) * 512], W=[B_wr[sC]])
                wD = wring[sD][:].rearrange("p k f -> p (k f)").rearrange("p (c f) -> p c f", c=4)
                dma("pool", wD, wout_src[:, h * 4:(h + 1) * 4, :], W=[B_wr[sD]])
                dma("sp", rdec[:], rdec_d[h], W=[B_rdec])

                for m in range(2):
                    for b in range(5):
                        s, n, isc = BLOCKS[b]
                        proj_rope(sA, 1, m, b, kr[:, m, s:s + n], [B_k[m][t] for t in range(s // 128, (s + n) // 128)])
                for t in range(NT):
                    b = min(t // 4, 4)
                    pi_ = t % 2
                    for k in range(KD):
                        op("pe", lambda e, k=k, t=t, pi_=pi_: e.matmul(
                            psb[pi_][:, :], lhsT=hT[:, k, t * 128:(t + 1) * 128], rhs=wring[sB][:, k, :],
                            start=(k == 0), stop=(k == KD - 1)),
                           R=[B_wr[sB], B_h[k][b]], W=[B_ps[pi_]], signal=(k == KD - 1))
                    op("act", lambda e, t=t, pi_=pi_: e.copy(out=vv[:, t, :], in_=psb[pi_][:, :]),
                       R=[B_ps[pi_]], W=[B_v[t]])
                if h == 0:
                    debug_dump("kr0", lambda o: [dma("sp", o[:, m, :], kr[:, m, :], R=B_k[m]) for m in range(2)])
                    debug_dump("vv0", lambda o: [dma("sp", o, vv[:], R=B_v)])
                    if P.stop_after == "proj0":
                        return

                for b in range(5):
                    s, n, isc = BLOCKS[b]
                    c = 1 if isc else 0
                    qi = rcount["q"] % 2
                    rcount["q"] += 1
                    for m in range(2):
                        proj_rope(sA, 0, m, b, qr[:, qi, m, :n], [B_q[qi][m]])
                    keys = [16, 17] if isc else list(range(NT))
                    pso = [psb[2 + cc] for cc in range(4)]
                    B_pso = [B_ps[2 + cc] for cc in range(4)]

                    def emit_scores(j, idx):
                        pi_ = 6 + (idx % 2)
                        for m in range(2):
                            op("pe", lambda e, m=m: e.matmul(
                                psb[pi_][:, :n], lhsT=kr[:, m, j * 128:(j + 1) * 128], rhs=qr[:, qi, m, :n],
                                start=(m == 0), stop=(m == 1)),
                               R=[B_k[m][j], B_q[qi][m]], W=[B_ps[pi_]], signal=(m == 1))
                        ai = rcount["at"] % 3
                        rcount["at"] += 1
                        pst = psb[pi_]
                        if isc:
                            tab = Dg(j - 16, n)
                            op("dve", lambda e: e.tensor_tensor(out=AT[ai][:, :n], in0=pst[:, :n], in1=tab, op=ALU.mult),
                               R=[B_ps[pi_], B_rdec], W=[B_AT[ai]])
                        elif j >= 16:
                            jc = j - 16
                            s1 = float(gf[h] ** (s + 256 - 128 * jc))
                            s2 = float(gb[h] ** (T_LAT - s - 511 + 128 * jc))
                            ci = jc
                            op("pool", lambda e: e.tensor_scalar(
                                out=ctab[ci][:], in0=Ff, scalar1=s1, scalar2=None, op0=ALU.mult),
                               R=[B_rdec], W=[B_ctab[ci]])
                            op("dve", lambda e: e.scalar_tensor_tensor(
                                out=ctab[ci][:], in0=Fb, scalar=s2, in1=ctab[ci][:], op0=ALU.mult, op1=ALU.add),
                               R=[B_rdec, B_ctab[ci]], W=[B_ctab[ci]])
                            op("dve", lambda e: e.tensor_tensor(
                                out=AT[ai][:, :n], in0=pst[:, :n], in1=ctab[ci][:, :n], op=ALU.mult),
                               R=[B_ps[pi_], B_ctab[ci]], W=[B_AT[ai]])
                        else:
                            rel = j - 4 * b
                            if 0 <= rel < 4:
                                tab = Dg(rel, 512)
                                op("dve", lambda e: e.tensor_tensor(out=AT[ai][:], in0=pst[:], in1=tab, op=ALU.mult),
                                   R=[B_ps[pi_], B_rdec], W=[B_AT[ai]])
                            elif rel < 0:
                                sc = float(gf[h] ** (s - 128 * j))
                                op("dve", lambda e: e.scalar_tensor_tensor(
                                    out=AT[ai][:], in0=pst[:], scalar=sc, in1=Ff, op0=ALU.mult, op1=ALU.mult),
                                   R=[B_ps[pi_], B_rdec], W=[B_AT[ai]])
                            else:
                                sc = float(gb[h] ** (128 * j - s - 511))
                                op("dve", lambda e: e.scalar_tensor_tensor(
                                    out=AT[ai][:], in0=pst[:], scalar=sc, in1=Fb, op0=ALU.mult, op1=ALU.mult),
                                   R=[B_ps[pi_], B_rdec], W=[B_AT[ai]])
                        return ai

                    def emit_av(j, ai, first, last):
                        for cc in range(4):
                            op("pe", lambda e, cc=cc: e.matmul(
                                pso[cc][:, :n], lhsT=vv[:, j, cc * 128:(cc + 1) * 128], rhs=AT[ai][:, :n],
                                start=first, stop=last),
                               R=[B_v[j], B_AT[ai]], W=[B_pso[cc]], signal=(cc == 3))

                    pend = None
                    for idx, j in enumerate(keys):
                        ai = emit_scores(j, idx)
                        if pend is not None:
                            emit_av(*pend)
                        pend = (j, ai, idx == 0, idx == len(keys) - 1)
                    emit_av(*pend)

                    for cc in range(4):
                        q2 = cc % 2
                        op("act", lambda e, cc=cc, q2=q2: e.activation(out=sq[q2][:, :n], in_=pso[cc][:, :n],
                                                                      func=AF.Square),
                           R=[B_pso[cc]], W=[B_sq[q2]])
                        op("pe", lambda e, cc=cc, q2=q2: e.matmul(psb[6][:, :n], lhsT=ones_b[:], rhs=sq[q2][:, :n],
                                                                 start=(cc == 0), stop=(cc == 3)),
                           R=[B_sq[q2], B_const], W=[B_ps[6]], signal=True)
                    op("act", lambda e: e.activation(out=rstd[:, :n], in_=psb[6][:, :n], func=AF.Sqrt, bias=EPS,
                                                     scale=1.0 / 512), R=[B_ps[6]], W=[B_rstd])
                    op("dve", lambda e: e.reciprocal(out=rstd[:, :n], in_=rstd[:, :n]), R=[B_rstd], W=[B_rstd])
                    for cc in range(4):
                        q2 = cc % 2
                        for k in range(KD):
                            op("pe", lambda e, k=k, cc=cc: e.matmul(
                                psb[7][:, :n], lhsT=wring[sC][:, k, cc * 128:(cc + 1) * 128], rhs=hT[:, k, s:s + n],
                                start=(k == 0), stop=(k == KD - 1)),
                               R=[B_wr[sC], B_h[k][b]], W=[B_ps[7]], signal=(k == KD - 1))
                        op("act", lambda e, q2=q2: e.activation(out=sg[q2][:, :n], in_=psb[7][:, :n], func=AF.Silu),
                           R=[B_ps[7]], W=[B_sg[q2]])
                        op("dve", lambda e, cc=cc, q2=q2: e.tensor_tensor(
                            out=tmp[q2][:, :n], in0=pso[cc][:, :n], in1=rstd[:, :n], op=ALU.mult),
                           R=[B_pso[cc], B_rstd], W=[B_tmp[q2]])
                        op("pool", lambda e, cc=cc, q2=q2: e.tensor_tensor(
                            out=ogT[:, cc, :n], in0=tmp[q2][:, :n], in1=sg[q2][:, :n], op=ALU.mult),
                           R=[B_tmp[q2], B_sg[q2]], W=[B_og[cc]])
                    for mch in range(KD):
                        pi_ = 6 + (mch % 2)
                        for cc in range(4):
                            op("pe", lambda e, cc=cc, mch=mch, pi_=pi_: e.matmul(
                                psb[pi_][:, :n], lhsT=wD[:, cc, mch * 128:(mch + 1) * 128], rhs=ogT[:, cc, :n],
                                start=(cc == 0), stop=(cc == 3)),
                               R=[B_wr[sD], B_og[cc]], W=[B_ps[pi_]], signal=(cc == 3))
                        g_ap = modv(l, 2, mch, c)
                        op("dve", lambda e, mch=mch, pi_=pi_, g_ap=g_ap: e.scalar_tensor_tensor(
                            out=xT[:, mch, s:s + n], in0=psb[pi_][:, :n], scalar=g_ap, in1=xT[:, mch, s:s + n],
                            op0=ALU.mult, op1=ALU.add),
                           R=[B_ps[pi_], B_mod, B_x[mch][b]], W=[B_x[mch][b]])
            S.barrier()

    iota_d2 = P.din("iota1", [128, 256])

    def moe_layer(l, with_ctx, n_exp_run=N_EXP):
        nblk = 5 if with_ctx else 4
        ntile = NT if with_ctx else 16
        NS = 288 if with_ctx else 256
        nst = 3 if with_ctx else 2
        st_sz = [128, 128, 32][:nst]
        st_off = [0, 128, 256][:nst]
        with ExitStack() as mes:
            h2tok = P.sb(mes, "h2tok", [128, NT, D], BF16)
            B_h2 = [Buf(f"h2t{t}") for t in range(NT)]
            wr_sb = P.sb(mes, "wr_sb", [128, KD, N_EXP], F32)
            B_wrt = Buf("wrt")
            dma("sp", wr_sb[:], router_d[l].rearrange("(k p) e -> p k e", p=128), W=[B_wrt])
            iota1 = P.sb(mes, "iota1_sb", [128, 256], F32)
            dma("sp", iota1[:], iota_d2, W=[B_wrt])
            aff = P.sb(mes, "aff", [128, NT, N_EXP], F32)
            B_aff = Buf("aff")
            aff_hl = P.sb(mes, "aff_hl", [128, NT, N_EXP, 2], BF16)
            posm_tok = P.sb(mes, "posm_tok", [128, NT, N_EXP], F32)
            B_posm = Buf("posm")

            with ExitStack() as r1:
                tmp = [P.sb(r1, f"mtmp{i}", [128, 512], F32) for i in range(2)]
                B_tmp = [Buf("mtmp0"), Buf("mtmp1")]
                rstd = P.sb(r1, "mrstd", [128, 512], F32)
                B_rstd = Buf("mrstd")
                sq = [P.sb(r1, f"msq{i}", [128, 512], BF16) for i in range(2)]
                B_sq = [Buf("msq0"), Buf("msq1")]
                h2f = P.sb(r1, "h2f", [128, KD, 512], F32)
                B_h2f = [Buf(f"h2f{k}") for k in range(KD)]
                h2b = P.sb(r1, "h2b", [128, KD, 512], BF16)
                B_h2b = [Buf(f"h2b{k}") for k in range(KD)]
                for b in range(nblk):
                    s, n, isc = BLOCKS[b]
                    norm_block(l, 1, b, lambda k, n=n: (h2f[:, k, :n], [B_h2f[k]]),
                               tmp, B_tmp, rstd, B_rstd, sq, B_sq, psn_i=7)
                    for k in range(KD):
                        op("pool", lambda e, k=k: e.tensor_copy(out=h2b[:, k, :n], in_=h2f[:, k, :n]),
                           R=[B_h2f[k]], W=[B_h2b[k]])
                    for tt in range(n // 128):
                        t = s // 128 + tt
                        for k in range(KD):
                            op("pe", lambda e, k=k: e.matmul(psb[6][:, 0:N_EXP], lhsT=h2f[:, k, tt * 128:(tt + 1) * 128],
                                                            rhs=wr_sb[:, k, :], start=(k == 0), stop=(k == KD - 1)),
                               R=[B_h2f[k], B_wrt], W=[B_ps[6]], signal=(k == KD - 1))
                        op("act", lambda e: e.copy(out=aff[:, t, :], in_=psb[6][:, 0:N_EXP]), R=[B_ps[6]], W=[B_aff])
                        pi_ = t % 2
                        pbf = psb[pi_][:].bitcast(BF16)
                        for k in range(KD):
                            op("pe", lambda e, k=k: e.transpose(out=pbf[:, k * 128:(k + 1) * 128],
                                                               in_=h2b[:, k, tt * 128:(tt + 1) * 128], identity=ident_b[:]),
                               R=[B_h2b[k], B_const], W=[B_ps[pi_]], signal=(k == KD - 1))
                        op("act", lambda e: e.copy(out=h2tok[:, t, :], in_=pbf), R=[B_ps[pi_]], W=[B_h2[t]])
                mx = P.sb(r1, "smx", [128, NT], F32)
                op("dve", lambda e: e.tensor_reduce(out=mx[:, :ntile], in_=aff[:, :ntile, :], axis=AX.X, op=ALU.max),
                   R=[B_aff], W=[B_rstd])
                op("dve", lambda e: e.tensor_tensor(out=aff[:, :ntile, :], in0=aff[:, :ntile, :],
                                                    in1=mx[:, :ntile].unsqueeze(2).to_broadcast([128, ntile, N_EXP]),
                                                    op=ALU.subtract), R=[B_rstd, B_aff], W=[B_aff])
                op("act", lambda e: e.activation(out=aff[:, :ntile, :], in_=aff[:, :ntile, :], func=AF.Exp),
                   R=[B_aff], W=[B_aff])
                op("dve", lambda e: e.tensor_reduce(out=mx[:, :ntile], in_=aff[:, :ntile, :], axis=AX.X, op=ALU.add),
                   R=[B_aff], W=[B_rstd])
                op("dve", lambda e: e.reciprocal(out=mx[:, :ntile], in_=mx[:, :ntile]), R=[B_rstd], W=[B_rstd])
                op("dve", lambda e: e.tensor_tensor(out=aff[:, :ntile, :], in0=aff[:, :ntile, :],
                                                    in1=mx[:, :ntile].unsqueeze(2).to_broadcast([128, ntile, N_EXP]),
                                                    op=ALU.mult), R=[B_rstd, B_aff], W=[B_aff])
                op("dve", lambda e: e.tensor_copy(out=aff_hl[:, :ntile, :, 0], in_=aff[:, :ntile, :]), R=[B_aff], W=[B_posm])
                op("dve", lambda e: e.tensor_tensor(out=aff_hl[:, :ntile, :, 1], in0=aff[:, :ntile, :],
                                                    in1=aff_hl[:, :ntile, :, 0], op=ALU.subtract),
                   R=[B_aff, B_posm], W=[B_posm])
                S.barrier()
            debug_dump(f"aff{l}", lambda o: [dma("sp", o, aff[:], R=[B_aff])])
            debug_dump(f"h2tok{l}", lambda o: [dma("sp", o, h2tok[:], R=B_h2)])

            with ExitStack() as r2:
                affT = P.sb(r2, "affT", [16, T_ALL], F32)
                mk = P.sb(r2, "mkT", [16, T_ALL], F32)
                cum = P.sb(r2, "cumT", [16, T_ALL], F32)
                sm = P.sb(r2, "bis", [16, 16], F32)
                B_affT, B_mk, B_cum, B_sm = Buf("affT"), Buf("mk"), Buf("cum"), Buf("sm")
                for t in range(ntile):
                    pi_ = t % 2
                    op("pe", lambda e: e.transpose(out=psb[pi_][0:16, 0:128], in_=aff[:, t, :], identity=ident_f[:]),
                       R=[B_aff, B_const], W=[B_ps[pi_]])
                    op("act", lambda e: e.copy(out=affT[:, t * 128:(t + 1) * 128], in_=psb[pi_][0:16, 0:128]),
                       R=[B_ps[pi_]], W=[B_affT])
                sets = [(0, T_LAT, 256)] + ([(T_LAT, T_CTX, 32)] if with_ctx else [])
                one = sm[:, 15:16]
                op("dve", lambda e: e.memset(one, 1.0), W=[B_sm])
                B_smx = [Buf("smA"), Buf("smB")]

                def bisect(si, s0, ns, cap):
                    lo, mid, gs = [sm[:, si * 6 + i: si * 6 + i + 1] for i in range(3)]
                    cnts = [sm[:, si * 6 + 3 + i: si * 6 + 4 + i] for i in range(2)]
                    Bs = B_smx[si]
                    a_ap = affT[:, s0:s0 + ns]
                    op("dve", lambda e: e.memset(lo, 0.0), W=[Bs])
                    op("dve", lambda e: e.memset(mid, 0.5), W=[Bs])
                    NIT = 30
                    for it in range(NIT):
                        step = 0.5 ** (it + 1)
                        cnt = cnts[it % 2]
                        op("dve", lambda e: e.memset(cnt, 0.0), W=[Bs])
                        yield
                        op("dve", lambda e: e.tensor_scalar(out=mk[:, s0:s0 + ns], in0=a_ap, scalar1=mid, scalar2=0.0,
                                                            op0=ALU.is_ge, op1=ALU.add, accum_out=cnt),
                           R=[B_affT, Bs], W=[B_mk, Bs])
                        yield
                        op("dve", lambda e: e.tensor_scalar(out=gs, in0=cnt, scalar1=float(cap), scalar2=step,
                                                            op0=ALU.is_ge, op1=ALU.mult), R=[Bs], W=[Bs])
                        yield
                        if it < NIT - 1:
                            op("dve", lambda e: e.scalar_tensor_tensor(out=mid, in0=gs, scalar=step * 0.5, in1=lo,
                                                                       op0=ALU.add, op1=ALU.add), R=[Bs], W=[Bs])
                            yield
                        op("dve", lambda e: e.tensor_tensor(out=lo, in0=lo, in1=gs, op=ALU.add), R=[Bs], W=[Bs])
                        yield
                    op("dve", lambda e: e.tensor_scalar(out=mk[:, s0:s0 + ns], in0=a_ap, scalar1=lo, scalar2=None,
                                                        op0=ALU.is_ge), R=[B_affT, Bs], W=[B_mk])
                    yield
                    op("dve", lambda e: e.tensor_tensor_scan(out=cum[:, s0:s0 + ns],
                                                             data0=one.to_broadcast([16, ns]), data1=mk[:, s0:s0 + ns],
                                                             initial=0.0, op0=ALU.mult, op1=ALU.add),
                       R=[B_mk, B_sm], W=[B_cum])
                    yield
                    op("dve", lambda e: e.tensor_tensor(out=cum[:, s0:s0 + ns], in0=cum[:, s0:s0 + ns],
                                                        in1=mk[:, s0:s0 + ns], op=ALU.mult), R=[B_mk, B_cum], W=[B_cum])
                    yield

                interleave(*[bisect(si, s0, ns, cap) for si, (s0, ns, cap) in enumerate(sets)])
                for t in range(ntile):
                    pi_ = t % 2
                    op("pe", lambda e: e.transpose(out=psb[pi_][:, 0:16], in_=cum[:, t * 128:(t + 1) * 128],
                                                   identity=ident_f[0:16, 0:16]),
                       R=[B_cum, B_const], W=[B_ps[pi_]])
                    op("act", lambda e: e.copy(out=posm_tok[:, t, :], in_=psb[pi_][:, 0:16]), R=[B_ps[pi_]], W=[B_posm])
                S.barrier()
            debug_dump(f"posm{l}", lambda o: [dma("sp", o, posm_tok[:], R=[B_posm])])
            if P.stop_after == f"route{l}":
                return

            with ExitStack() as xs:
                Pm = P.sb(xs, "Pm", [128, NT, NS], BF16)
                B_Pm = Buf("Pm")
                PTl = P.sb(xs, "PTl", [128, 2, T_LAT], BF16)
                PTc = P.sb(xs, "PTc", [128, T_CTX], BF16)
                B_PT = Buf("PT")

                def PTv(st, a, b2, sz):
                    return PTl[0:sz, st, a:b2] if st < 2 else PTc[0:sz, a - T_LAT:b2 - T_LAT]
                xeT = P.sb(xs, "xeT", [128, KD, NS], BF16)
                B_xe = [Buf(f"xe{k}") for k in range(KD)]
                hmid = P.sb(xs, "hmid", [128, 2, NS], BF16)
                B_hm = [Buf("hm0"), Buf("hm1")]
                sil = P.sb(xs, "sil", [128, 2, NS], BF16)
                B_sil = [Buf("sil0"), Buf("sil1")]
                y_sb = P.sb(xs, "y_sb", [128, nst, D], BF16)
                B_y = [Buf(f"y{i}") for i in range(nst)]
                gsl = P.sb(xs, "gsl", [128, 4], F32)
                B_gsl = Buf("gsl")
                NGU = 5
                NDN = 5 if with_ctx else 6
                wgu = wring + [P.sb(xs, f"wgu{i}", [128, KD, 512], BF16) for i in range(NGU - NSLOT)]
                B_gu = [Buf(f"gu{i}") for i in range(NGU)]
                wdn = [P.sb(xs, f"wdn{i}", [128, 2, D], BF16) for i in range(NDN)]
                B_dn = [Buf(f"dn{i}") for i in range(NDN)]
                op("dve", lambda e: e.memset(Pm[:], 0.0), W=[B_Pm])

                NFB = NFC // 2
                sched = [(e_, fb) for e_ in range(n_exp_run) for fb in range(NFB)]
                issued = {"n": 0}

                def issue_weights(upto):
                    while issued["n"] < min(upto, len(sched)):
                        i = issued["n"]
                        e_, fb = sched[i]
                        if f"wg_{l}_{e_}" not in P.dram:
                            P.din(f"wg_{l}_{e_}", [D, FF])
                            P.din(f"wu_{l}_{e_}", [D, FF])
                            P.din(f"wd_{l}_{e_}", [FF, D])
                        wg_ap, wu_ap, wd_ap = P.dram[f"wg_{l}_{e_}"], P.dram[f"wu_{l}_{e_}"], P.dram[f"wd_{l}_{e_}"]
                        gi, di = i % NGU, i % NDN
                        dma("pool", wgu[gi][:, :, 0:256],
                            wg_ap.rearrange("(k p) f -> p k f", p=128)[:, :, fb * 256:(fb + 1) * 256], W=[B_gu[gi]])
                        dma("pool", wgu[gi][:, :, 256:512],
                            wu_ap.rearrange("(k p) f -> p k f", p=128)[:, :, fb * 256:(fb + 1) * 256], W=[B_gu[gi]])
                        dma("pool", wdn[di][:],
                            wd_ap.rearrange("(c p) d -> p c d", p=128)[:, fb * 2:(fb + 1) * 2, :], W=[B_dn[di]])
                        issued["n"] += 1

                LA = 4 if with_ctx else 5
                issue_weights(LA)
                blk_i = 0
                for e_ in range(n_exp_run):
                    op("dve", lambda e: e.tensor_tensor(
                        out=Pm[:, 0:16, 0:256], in0=iota1[:, :].unsqueeze(1).to_broadcast([128, 16, 256]),
                        in1=posm_tok[:, 0:16, e_:e_ + 1].to_broadcast([128, 16, 256]), op=ALU.is_equal),
                       R=[B_posm, B_wrt], W=[B_Pm])
                    if with_ctx:
                        op("dve", lambda e: e.tensor_tensor(
                            out=Pm[:, 16:18, 256:288], in0=iota1[:, 0:32].unsqueeze(1).to_broadcast([128, 2, 32]),
                            in1=posm_tok[:, 16:18, e_:e_ + 1].to_broadcast([128, 2, 32]), op=ALU.is_equal),
                           R=[B_posm, B_wrt], W=[B_Pm])
                    for st in range(nst):
                        tiles = range(16) if st < 2 else range(16, 18)
                        tl = list(tiles)
                        for ti, t in enumerate(tl):
                            op("pe", lambda e: e.matmul(psb[6][0:st_sz[st], st * 2:st * 2 + 2],
                                                        lhsT=Pm[:, t, st_off[st]:st_off[st] + st_sz[st]],
                                                        rhs=aff_hl[:, t, e_, :], start=(ti == 0), stop=(ti == len(tl) - 1)),
                               R=[B_Pm, B_posm], W=[B_ps[6]], signal=(ti == len(tl) - 1))
                    for st in range(nst):
                        op("dve", lambda e: e.tensor_reduce(out=gsl[0:st_sz[st], st:st + 1],
                                                            in_=psb[6][0:st_sz[st], st * 2:st * 2 + 2], axis=AX.X, op=ALU.add),
                           R=[B_ps[6]], W=[B_gsl])
                    for st in range(nst):
                        tl = list(range(16)) if st < 2 else [16, 17]
                        for g0 in range(0, len(tl), 8):
                            grp = tl[g0:g0 + 8]
                            pi_ = 7
                            pbf = psb[pi_][:].bitcast(BF16)
                            for gi_, t in enumerate(grp):
                                op("pe", lambda e: e.transpose(
                                    out=pbf[0:st_sz[st], gi_ * 128:(gi_ + 1) * 128],
                                    in_=Pm[:, t, st_off[st]:st_off[st] + st_sz[st]], identity=ident_b[:]),
                                   R=[B_Pm, B_const], W=[B_ps[pi_]], signal=(gi_ == len(grp) - 1))
                            op("act", lambda e: e.copy(out=PTv(st, grp[0] * 128, (grp[-1] + 1) * 128, st_sz[st]),
                                                       in_=pbf[0:st_sz[st], 0:len(grp) * 128]),
                               R=[B_ps[pi_]], W=[B_PT])
                    for k in range(KD):
                        pi_ = 6 + (k % 2)
                        for t in range(ntile):
                            op("pe", lambda e: e.matmul(psb[pi_][:, 0:NS], lhsT=h2tok[:, t, k * 128:(k + 1) * 128],
                                                        rhs=Pm[:, t, :], start=(t == 0), stop=(t == ntile - 1)),
                               R=[B_h2[t], B_Pm], W=[B_ps[pi_]], signal=(t == ntile - 1))
                        op("act", lambda e: e.copy(out=xeT[:, k, :], in_=psb[pi_][:, 0:NS]), R=[B_ps[pi_]], W=[B_xe[k]])
                    psY = [[psb[st * 2 + nh] for nh in range(2)] for st in range(nst)]
                    B_psY = [[B_ps[st * 2 + nh] for nh in range(2)] for st in range(nst)]

                    def emit_down(fc, hb, di, first, last):
                        for st in range(nst):
                            for nh in range(2):
                                op("pe", lambda e: e.matmul(
                                    psY[st][nh][0:st_sz[st], :], lhsT=hmid[:, hb, st_off[st]:st_off[st] + st_sz[st]],
                                    rhs=wdn[di][:, fc % 2, nh * 512:(nh + 1) * 512], start=first, stop=last),
                                   R=[B_hm[hb], B_dn[di]], W=[B_psY[st][nh]], signal=(st == nst - 1 and nh == 1))

                    pend = None
                    for fb in range(NFB):
                        i = blk_i
                        blk_i += 1
                        issue_weights(i + LA)
                        gi, di = i % NGU, i % NDN
                        for f2 in range(2):
                            fc = fb * 2 + f2
                            hb = fc % 2
                            if with_ctx or (fc % 2 == 0):
                                ia, iu = 6, 7
                            else:
                                ia, iu = 4, 5
                            pa, pu = psb[ia], psb[iu]
                            for k in range(KD):
                                op("pe", lambda e: e.matmul(pa[:, 0:NS], lhsT=wgu[gi][:, k, f2 * 128:(f2 + 1) * 128],
                                                            rhs=xeT[:, k, :], start=(k == 0), stop=(k == KD - 1)),
                                   R=[B_gu[gi], B_xe[k]], W=[B_ps[ia]], signal=(k == KD - 1))
                            for k in range(KD):
                                op("pe", lambda e: e.matmul(pu[:, 0:NS],
                                                            lhsT=wgu[gi][:, k, 256 + f2 * 128:256 + (f2 + 1) * 128],
                                                            rhs=xeT[:, k, :], start=(k == 0), stop=(k == KD - 1)),
                                   R=[B_gu[gi], B_xe[k]], W=[B_ps[iu]], signal=(k == KD - 1))
                            op("act", lambda e: e.activation(out=sil[:, hb, :], in_=pa[:, 0:NS], func=AF.Silu),
                               R=[B_ps[ia]], W=[B_sil[hb]])
                            op("dve", lambda e: e.tensor_tensor(out=hmid[:, hb, :], in0=pu[:, 0:NS], in1=sil[:, hb, :],
                                                                op=ALU.mult), R=[B_ps[iu], B_sil[hb]], W=[B_hm[hb]])
                            if pend is not None:
                                emit_down(*pend)
                            pend = (fc, hb, di, fc == 0, fc == NFC - 1)
                    emit_down(*pend)
                    for st in range(nst):
                        for nh in range(2):
                            op("act", lambda e: e.activation(out=y_sb[0:st_sz[st], st, nh * 512:(nh + 1) * 512],
                                                             in_=psY[st][nh][0:st_sz[st], :], func=AF.Copy,
                                                             scale=gsl[0:st_sz[st], st:st + 1]),
                               R=[B_psY[st][nh], B_gsl], W=[B_y[st]])
                    gi_s = 0
                    for b in range(nblk):
                        s, n, isc = BLOCKS[b]
                        c = 1 if isc else 0
                        sts = [2] if isc else [0, 1]
                        for mch in range(KD):
                            pi_ = 6 + (gi_s % 2)
                            gi_s += 1
                            for si, st in enumerate(sts):
                                op("pe", lambda e: e.matmul(psb[pi_][:, :n], lhsT=y_sb[0:st_sz[st], st, mch * 128:(mch + 1) * 128],
                                                            rhs=PTv(st, s, s + n, st_sz[st]), start=(si == 0),
                                                            stop=(si == len(sts) - 1)),
                                   R=[B_y[st], B_PT], W=[B_ps[pi_]], signal=(si == len(sts) - 1))
                            g_ap = modv(l, 5, mch, c)
                            op("dve", lambda e: e.scalar_tensor_tensor(
                                out=xT[:, mch, s:s + n], in0=psb[pi_][:, :n], scalar=g_ap, in1=xT[:, mch, s:s + n],
                                op0=ALU.mult, op1=ALU.add),
                               R=[B_ps[pi_], B_mod, B_x[mch][b]], W=[B_x[mch][b]])
                S.barrier()

    def hgrn2_layer(l):
        hgin_d = P.din("hg_w_in", [D, 5120])
        hgout_d = P.din("hg_w_out", [D, D])
        hgmisc_d = P.din("hg_misc", [128, 1 + 4 * KD])
        hgmask_d = P.din("hgmask", [128, 256])
        PT_ = 384
        NPART = 6
        NCH = 3
        with ExitStack() as les:
            hT = P.sb(les, "hT1", [128, KD, T_ALL], BF16)
            B_h = [[Buf(f"h1{k}_{b}") for b in range(5)] for k in range(KD)]
            B_hT = Buf("hT1all")
            hgm = P.sb(les, "hgm", [128, 1 + 4 * KD], F32)
            msk = P.sb(les, "hgmask_sb", [128, 256], F32)
            lbs = P.sb(les, "lbs", [128, 2, 2, HG_H], F32)
            B_hc = Buf("hgconst")
            dma("sp", hgm[:], hgmisc_d, W=[B_hc])
            dma("sp", msk[:], hgmask_d, W=[B_hc])
            AA = P.sb(les, "hgAA", [128, 10 * PT_], F32)
            Aset = [[AA[:, (st_ * 5 + i) * PT_:(st_ * 5 + i + 1) * PT_] for i in range(5)] for st_ in range(2)]
            B_Aset = [[Buf(f"hgA{st_}{i}") for i in range(5)] for st_ in range(2)]
            tmp = [AA[:, 0:512], AA[:, 768:1280]]
            B_tmp = [[B_Aset[0][0], B_Aset[0][1]], [B_Aset[0][2], B_Aset[0][3]]]
            rstds = [AA[:, 1536:2048], AA[:, 2304:2816]]
            B_rstds = [[B_Aset[0][4], B_Aset[1][0]], [B_Aset[1][1], B_Aset[1][2]]]
            rstd, B_rstd = rstds[0], B_rstds[0]
            sq = [P.sb(les, f"hsq{i}", [128, 512], BF16) for i in range(2)]
            B_sq = [Buf("hsq0"), Buf("hsq1")]

            class _V:
                def __init__(self, ap):
                    self.ap = ap

                def __getitem__(self, key):
                    return self.ap[key]
            for b in range(5):
                s, n, _ = BLOCKS[b]
                norm_block(l, 0, b, lambda k, b=b, s=s, n=n: (hT[:, k, s:s + n], [B_h[k][b]]),
                           [_V(tmp[0]), _V(tmp[1])], B_tmp, _V(rstd), B_rstd, sq, B_sq, psn_i=7)
            for d_ in range(2):
                op("dve", lambda e: e.tensor_tensor(out=lbs[:, 0, d_, :], in0=hgm[:, 1 + (2 + d_) * 8:1 + (3 + d_) * 8],
                                                    in1=hgm[:, 1 + d_ * 8:1 + (d_ + 1) * 8], op=ALU.subtract),
                   R=[B_hc], W=[B_hc])
            op("act", lambda e: e.activation(out=lbs[:, 0, :, :], in_=lbs[:, 0, :, :], func=AF.Sigmoid), R=[B_hc], W=[B_hc])
            op("dve", lambda e: e.tensor_scalar(out=lbs[:, 1, :, :], in0=lbs[:, 0, :, :], scalar1=-1.0, scalar2=1.0,
                                                op0=ALU.mult, op1=ALU.add), R=[B_hc], W=[B_hc])
            S.barrier()
            debug_dump("hT1", lambda o: [dma("sp", o.rearrange("(k p) t -> p k t", p=128)[:, k, :], hT[:, k, :],
                                             R=[B_hT]) for k in range(KD)])

            qs2 = P.sb(les, "hqs", [128, 2, PT_], BF16)
            B_qs2 = [Buf("hqs0"), Buf("hqs1")]
            arr = [[P.sb(les, f"harr{d_}{i}", [128, T_ALL], BF16) for i in range(3)] for d_ in range(2)]
            B_arr = [[Buf(f"harr{d_}{i}") for i in range(3)] for d_ in range(2)]
            sgT, B_sg = arr[0][2], B_arr[0][2]
            ogT, B_og = arr[1][2], B_arr[1][2]
            B_ogb = [[B_og, Buf(f"ogb{b}")] for b in range(4)]
            vv = P.sb(les, "hvv", [128, NT, 128], BF16)
            B_v = Buf("hvv")
            EF = P.sb(les, "hEF", [128, 2, NT], F32)
            EM = P.sb(les, "hEM", [128, 2, NT], F32)
            t6b = P.sb(les, "ht6", [128, 2, 8], F32)
            B_t6 = [Buf("t6a"), Buf("t6b")]
            B_E = Buf("hE")
            Sst = P.sb(les, "hS", [128, 2, 128], F32)
            B_S = [Buf("hS0"), Buf("hS1")]
            Suse = P.sb(les, "hSuse", [128, 2, 16, 128], BF16)
            B_Su = Buf("hSuse")
            kht = [P.sb(les, f"hkht{i}", [128, 128], BF16) for i in range(2)]
            B_kht = [Buf("kht0"), Buf("kht1")]
            ATs = [P.sb(les, f"hAT{i}", [128, 128], BF16) for i in range(8)]
            B_ATs = [Buf(f"hAT{i}") for i in range(8)]
            one_col = hgm[:, 0:1]
            ones_f = P.sb(les, "hones", [128, 1], F32)
            op("dve", lambda e: e.memset(ones_f[:], 1.0), W=[B_hc])

            win_src = hgin_d.rearrange("(k p) f -> p k f", p=128)
            cnt_ = {"ps": 0, "kht": 0, "at": 0}

            def view3(ap):
                return ap.rearrange("p (c i) -> p c i", i=128)

            for h in range(HG_H):
                sX, sY = wslot(), wslot()
                for gi_, off in enumerate((0, 1024, 2048, 3072)):
                    dma("pool", wring[sX][:, :, gi_ * 128:(gi_ + 1) * 128], win_src[:, :, off + h * 128: off + (h + 1) * 128],
                        W=[B_wr[sX]])
                yflat = wring[sY][:].rearrange("p k f -> p (k f)")
                wG = yflat[:, 0:1024].rearrange("p (k f) -> p k f", k=KD)
                wO = yflat[:, 1024:2048]
                dma("pool", wG, win_src[:, :, 4096 + h * 128:4096 + (h + 1) * 128], W=[B_wr[sY]])
                dma("pool", wO, hgout_d[h * 128:(h + 1) * 128, :], W=[B_wr[sY]])

                def proj(gi_, s, n, pi_):
                    for k in range(KD):
                        op("pe", lambda e: e.matmul(psb[pi_][:, :n], lhsT=wring[sX][:, k, gi_ * 128:(gi_ + 1) * 128],
                                                    rhs=hT[:, k, s:s + n], start=(k == 0), stop=(k == KD - 1)),
                           R=[B_wr[sX], B_hT], W=[B_ps[pi_]], signal=(k == KD - 1))

                for part in range(NPART):
                    p0 = part * PT_
                    c0 = part * NCH
                    qi = part % 2
                    qs = qs2[:, qi, :]
                    B_qs = B_qs2[qi]
                    pi_ = cnt_["ps"] % 2
                    cnt_["ps"] += 1
                    proj(0, p0, PT_, pi_)
                    op("act", lambda e: e.activation(out=qs, in_=psb[pi_][:, :PT_], func=AF.Silu), R=[B_ps[pi_]], W=[B_qs])
                    for tt in range(NCH):
                        t = c0 + tt
                        pi_ = cnt_["ps"] % 2
                        cnt_["ps"] += 1
                        for k in range(KD):
                            op("pe", lambda e: e.matmul(psb[pi_][:, 0:128], lhsT=hT[:, k, t * 128:(t + 1) * 128],
                                                        rhs=wring[sX][:, k, 384:512], start=(k == 0), stop=(k == KD - 1)),
                               R=[B_wr[sX], B_hT], W=[B_ps[pi_]], signal=(k == KD - 1))
                        op("act", lambda e: e.copy(out=vv[:, t, :], in_=psb[pi_][:, 0:128]), R=[B_ps[pi_]], W=[B_v])
                    def chain(d_):
                        si_ = d_
                        A1, A2, A3, A4, A5 = Aset[si_]
                        BA = B_Aset[si_]
                        t6 = t6b[:, si_, :]
                        B_T = B_t6[si_]
                        pi_ = cnt_["ps"] % 2
                        cnt_["ps"] += 1
                        proj(1 + d_, p0, PT_, pi_)
                        oml_ap = lbs[:, 1, d_, h:h + 1]
                        op("act", lambda e: e.activation(out=A2, in_=psb[pi_][:, :PT_], func=AF.Sigmoid),
                           R=[B_ps[pi_]], W=[BA[1]])
                        yield
                        op("act", lambda e: e.activation(out=A1, in_=psb[pi_][:, :PT_], func=AF.Sigmoid, scale=-1.0),
                           R=[B_ps[pi_]], W=[BA[0]])
                        yield
                        op("act", lambda e: e.activation(out=A2, in_=A2, func=AF.Ln, bias=lbs[:, 0, d_, h:h + 1],
                                                         scale=oml_ap), R=[BA[1], B_hc], W=[BA[1]])
                        yield
                        op("dve", lambda e: e.tensor_tensor_scan(out=A3, data0=ones_f[:, 0:1].to_broadcast([128, PT_]),
                                                                 data1=A2, initial=0.0, op0=ALU.mult, op1=ALU.add),
                           R=[BA[1], B_hc], W=[BA[2]])
                        G3, g3 = view3(A3), view3(A2)
                        dst = [arr[d_][i][:, p0:p0 + PT_] for i in range(3)]
                        Bd = B_arr[d_]
                        bc = [128, NCH, 128]
                        if d_ == 0:
                            yield
                            op("dve", lambda e: e.tensor_tensor(out=view3(A4), in0=G3, in1=G3[:, :, 63:64].to_broadcast(bc),
                                                                op=ALU.subtract), R=[BA[2]], W=[BA[3]])
                            yield
                            op("act", lambda e: e.activation(out=A5, in_=A4, func=AF.Exp), R=[BA[3]], W=[BA[4]])
                            yield
                            op("pool", lambda e: e.tensor_tensor(out=dst[0], in0=qs, in1=A5, op=ALU.mult),
                               R=[B_qs, BA[4]], W=[Bd[0]])
                            yield
                            op("dve", lambda e: e.tensor_tensor(out=t6[:, 0:NCH], in0=G3[:, :, 0], in1=g3[:, :, 0], op=ALU.subtract),
                               R=[BA[2], BA[1]], W=[B_T])
                            yield
                            op("dve", lambda e: e.tensor_tensor(out=EF[:, 0, c0:c0 + NCH], in0=G3[:, :, 127], in1=t6[:, 0:NCH],
                                                                op=ALU.subtract), R=[BA[2], B_T], W=[B_E])
                            yield
                            op("dve", lambda e: e.tensor_tensor(out=EM[:, 0, c0:c0 + NCH], in0=G3[:, :, 63], in1=t6[:, 0:NCH],
                                                                op=ALU.subtract), R=[BA[2], B_T], W=[B_E])
                            yield
                            op("act", lambda e: e.activation(out=A2, in_=A4, func=AF.Exp, scale=-1.0), R=[BA[3]], W=[BA[1]])
                            yield
                            op("dve", lambda e: e.scalar_tensor_tensor(out=dst[1], in0=A1, scalar=oml_ap, in1=A2, op0=ALU.mult, op1=ALU.mult),
                               R=[BA[0], BA[1]], W=[Bd[1]])
                            yield
                            op("dve", lambda e: e.tensor_tensor(out=view3(A4), in0=G3, in1=G3[:, :, 127:128].to_broadcast(bc),
                                                                op=ALU.subtract), R=[BA[2]], W=[BA[3]])
                            yield
                            op("act", lambda e: e.activation(out=A5, in_=A4, func=AF.Exp, scale=-1.0), R=[BA[3]], W=[BA[4]])
                            yield
                            op("dve", lambda e: e.scalar_tensor_tensor(out=dst[2], in0=A1, scalar=oml_ap, in1=A5, op0=ALU.mult, op1=ALU.mult),
                               R=[BA[0], BA[4]], W=[Bd[2]])
                        else:
                            yield
                            op("dve", lambda e: e.tensor_copy(out=t6[:, 0:NCH], in_=G3[:, :, 127]), R=[BA[2]], W=[B_T])
                            yield
                            op("pool", lambda e: e.tensor_tensor(out=A2, in0=A3, in1=A2, op=ALU.subtract),
                               R=[BA[2], BA[1]], W=[BA[1]])
                            H3 = view3(A2)
                            yield
                            op("dve", lambda e: e.tensor_tensor(out=view3(A4), in0=H3, in1=H3[:, :, 64:65].to_broadcast(bc),
                                                                op=ALU.subtract), R=[BA[1]], W=[BA[3]])
                            yield
                            op("act", lambda e: e.activation(out=A5, in_=A4, func=AF.Exp, scale=-1.0), R=[BA[3]], W=[BA[4]])
                            yield
                            op("pool", lambda e: e.tensor_tensor(out=dst[0], in0=qs, in1=A5, op=ALU.mult),
                               R=[B_qs, BA[4]], W=[Bd[0]])
                            yield
                            op("act", lambda e: e.activation(out=A3, in_=A4, func=AF.Exp), R=[BA[3]], W=[BA[2]])
                            yield
                            op("dve", lambda e: e.scalar_tensor_tensor(out=dst[1], in0=A1, scalar=oml_ap, in1=A3, op0=ALU.mult, op1=ALU.mult),
                               R=[BA[0], BA[2]], W=[Bd[1]])
                            yield
                            op("dve", lambda e: e.tensor_tensor(out=EF[:, 1, c0:c0 + NCH], in0=t6[:, 0:NCH], in1=H3[:, :, 0],
                                                                op=ALU.subtract), R=[BA[1], B_T], W=[B_E])
                            yield
                            op("dve", lambda e: e.tensor_tensor(out=EM[:, 1, c0:c0 + NCH], in0=t6[:, 0:NCH], in1=H3[:, :, 64],
                                                                op=ALU.subtract), R=[BA[1], B_T], W=[B_E])
                            yield
                            op("dve", lambda e: e.tensor_tensor(out=view3(A4), in0=H3, in1=H3[:, :, 0:1].to_broadcast(bc),
                                                                op=ALU.subtract), R=[BA[1]], W=[BA[3]])
                            yield
                            op("act", lambda e: e.activation(out=A5, in_=A4, func=AF.Exp), R=[BA[3]], W=[BA[4]])
                            yield
                            op("dve", lambda e: e.scalar_tensor_tensor(out=dst[2], in0=A1, scalar=oml_ap, in1=A5, op0=ALU.mult, op1=ALU.mult),
                               R=[BA[0], BA[4]], W=[Bd[2]])
                        yield
                    interleave(chain(0), chain(1))
                op("act", lambda e: e.activation(out=EF[:], in_=EF[:], func=AF.Exp), R=[B_E], W=[B_E])
                op("act", lambda e: e.activation(out=EM[:], in_=EM[:], func=AF.Exp), R=[B_E], W=[B_E])

                orders = [[16, 17] + list(range(16)), [17, 16] + list(range(15, -1, -1))]
                for d_ in range(2):
                    op("dve", lambda e: e.memset(Sst[:, d_, :], 0.0), W=[B_S[d_]])
                items = [(step, d_) for step in range(NT - 1) for d_ in range(2)]

                def emit_T(i):
                    step, d_ = items[i]
                    c = orders[d_][step]
                    ki = i % 2
                    pq = 4 + (i % 2)
                    pbf = psb[pq][:].bitcast(BF16)
                    op("pe", lambda e: e.transpose(out=pbf[:, 0:128], in_=arr[d_][2][:, c * 128:(c + 1) * 128],
                                                   identity=ident_b[:]), R=[B_arr[d_][2], B_const], W=[B_ps[pq]])
                    op("act", lambda e: e.copy(out=kht[ki][:], in_=pbf[:, 0:128]), R=[B_ps[pq]], W=[B_kht[ki]])

                def emit_suse(step, d_):
                    c = orders[d_][step]
                    if c < 16:
                        op("act", lambda e: e.activation(out=Suse[:, d_, c, :], in_=Sst[:, d_, :], func=AF.Copy,
                                                         scale=EM[:, d_, c:c + 1]), R=[B_S[d_], B_E], W=[B_Su])

                emit_T(0)
                for i, (step, d_) in enumerate(items):
                    c = orders[d_][step]
                    if i + 1 < len(items):
                        emit_T(i + 1)
                    emit_suse(step, d_)
                    ki = i % 2
                    pd = 2 + (i % 2)
                    op("pe", lambda e: e.matmul(psb[pd][:, 0:128], lhsT=kht[ki][:], rhs=vv[:, c, :], start=True, stop=True),
                       R=[B_kht[ki], B_v], W=[B_ps[pd]])
                    op("dve", lambda e: e.scalar_tensor_tensor(out=Sst[:, d_, :], in0=Sst[:, d_, :], scalar=EF[:, d_, c:c + 1],
                                                               in1=psb[pd][:, 0:128], op0=ALU.mult, op1=ALU.add),
                       R=[B_S[d_], B_E, B_ps[pd]], W=[B_S[d_]])
                for d_ in range(2):
                    emit_suse(NT - 1, d_)

                for b in range(4):
                    s, n, _ = BLOCKS[b]
                    pi_ = b % 2
                    for k in range(KD):
                        op("pe", lambda e: e.matmul(psb[pi_][:, :n], lhsT=wG[:, k, :], rhs=hT[:, k, s:s + n],
                                                    start=(k == 0), stop=(k == KD - 1)),
                           R=[B_wr[sY], B_hT], W=[B_ps[pi_]], signal=(k == KD - 1))
                    op("act", lambda e: e.activation(out=sgT[:, s:s + n], in_=psb[pi_][:, :n], func=AF.Silu),
                       R=[B_ps[pi_]], W=[B_sg])

                def out_block(b):
                    s, n, _ = BLOCKS[b]
                    bp = b % 2
                    iO = 6 if bp == 0 else 4
                    pO = psb[iO]

                    def scores(cc):
                        c = b * 4 + cc
                        cs = slice(c * 128, (c + 1) * 128)
                        ais = []
                        for d_ in range(2):
                            pa = 2 + (cnt_["at"] % 2)
                            ai = cnt_["at"] % 8
                            cnt_["at"] += 1
                            op("pe", lambda e: e.matmul(psb[pa][:, 0:128], lhsT=arr[d_][1][:, cs], rhs=arr[d_][0][:, cs],
                                                        start=True, stop=True),
                               R=[B_arr[d_][1], B_arr[d_][0]], W=[B_ps[pa]])
                            op("dve", lambda e: e.tensor_tensor(out=ATs[ai][:], in0=psb[pa][:, 0:128],
                                                                in1=msk[:, d_ * 128:(d_ + 1) * 128], op=ALU.mult),
                               R=[B_ps[pa], B_hc], W=[B_ATs[ai]])
                            ais.append(ai)
                        return ais

                    def outs(cc, ais):
                        c = b * 4 + cc
                        cs = slice(c * 128, (c + 1) * 128)
                        oc = pO[:, cc * 128:(cc + 1) * 128]
                        op("pe", lambda e: e.matmul(oc, lhsT=Suse[:, 0, c, :], rhs=arr[0][0][:, cs], start=True, stop=False),
                           R=[B_Su, B_arr[0][0]], W=[B_ps[iO]], signal=False)
                        op("pe", lambda e: e.matmul(oc, lhsT=Suse[:, 1, c, :], rhs=arr[1][0][:, cs], start=False, stop=False),
                           R=[B_Su, B_arr[1][0]], W=[B_ps[iO]], signal=False)
                        op("pe", lambda e: e.matmul(oc, lhsT=vv[:, c, :], rhs=ATs[ais[0]][:], start=False, stop=False),
                           R=[B_v, B_ATs[ais[0]]], W=[B_ps[iO]], signal=False)
                        op("pe", lambda e: e.matmul(oc, lhsT=vv[:, c, :], rhs=ATs[ais[1]][:], start=False, stop=True),
                           R=[B_v, B_ATs[ais[1]]], W=[B_ps[iO]], signal=True)

                    pend = None
                    for cc in range(4):
                        ais = scores(cc)
                        if pend is not None:
                            outs(*pend)
                        pend = (cc, ais)
                    outs(*pend)

                def post_block(b):
                    s, n, _ = BLOCKS[b]
                    bp = b % 2
                    iO, iN = (6, 7) if bp == 0 else (4, 5)
                    pO = psb[iO]
                    rstd, B_rstd = rstds[bp], B_rstds[bp]
                    op("act", lambda e: e.activation(out=sq[bp][:, :n], in_=pO[:, :n], func=AF.Square), R=[B_ps[iO]], W=[B_sq[bp]])
                    op("pe", lambda e: e.matmul(psb[iN][:, :n], lhsT=ones_b[:], rhs=sq[bp][:, :n], start=True, stop=True),
                       R=[B_sq[bp], B_const], W=[B_ps[iN]])
                    op("act", lambda e: e.activation(out=rstd[:, :n], in_=psb[iN][:, :n], func=AF.Sqrt, bias=EPS,
                                                     scale=1.0 / 128), R=[B_ps[iN]], W=[B_rstd])
                    op("dve", lambda e: e.reciprocal(out=rstd[:, :n], in_=rstd[:, :n]), R=[B_rstd], W=[B_rstd])
                    op("dve", lambda e: e.scalar_tensor_tensor(out=tmp[bp][:, :n], in0=pO[:, :n], scalar=hgm[:, 0:1],
                                                               in1=rstd[:, :n], op0=ALU.mult, op1=ALU.mult),
                       R=[B_ps[iO], B_rstd, B_hc], W=[B_tmp[bp]])
                    op("pool", lambda e: e.tensor_tensor(out=ogT[:, s:s + n], in0=tmp[bp][:, :n], in1=sgT[:, s:s + n],
                                                         op=ALU.mult), R=[B_tmp[bp], B_sg], W=[B_ogb[b][1]])

                def proj_block(b):
                    s, n, _ = BLOCKS[b]
                    for mch in range(KD):
                        pi_ = mch % 2
                        op("pe", lambda e: e.matmul(psb[pi_][:, :n], lhsT=wO[:, mch * 128:(mch + 1) * 128],
                                                    rhs=ogT[:, s:s + n], start=True, stop=True),
                           R=[B_wr[sY], B_ogb[b]], W=[B_ps[pi_]])
                        g_ap = modv(l, 2, mch, 0)
                        op("dve", lambda e: e.scalar_tensor_tensor(
                            out=xT[:, mch, s:s + n], in0=psb[pi_][:, :n], scalar=g_ap, in1=xT[:, mch, s:s + n],
                            op0=ALU.mult, op1=ALU.add),
                           R=[B_ps[pi_], B_mod, B_x[mch][b]], W=[B_x[mch][b]])

                out_block(0)
                out_block(1)
                post_block(0)
                out_block(2)
                proj_block(0)
                post_block(1)
                out_block(3)
                proj_block(1)
                post_block(2)
                post_block(3)
                proj_block(2)
                proj_block(3)
            S.barrier()

    if "ret" in P.parts:
        retention_layer(0)
    debug_dump("x_mix0", lambda o: [dma("sp", o.rearrange("(k p) t -> p k t", p=128)[:, k, :], xT[:, k, :],
                                        R=B_x[k]) for k in range(KD)])

    if "moe0" in P.parts:
        moe_layer(0, True, P.n_exp_run)
    debug_dump("x_ffn0", lambda o: [dma("sp", o.rearrange("(k p) t -> p k t", p=128)[:, k, :], xT[:, k, :],
                                        R=B_x[k]) for k in range(KD)])
    if "hg" in P.parts:
        hgrn2_layer(1)
    debug_dump("x_mix1", lambda o: [dma("sp", o.rearrange("(k p) t -> p k t", p=128)[:, k, :], xT[:, k, 0:T_LAT],
                                        R=B_x[k][:4]) for k in range(KD)])
    if "moe1" in P.parts:
        moe_layer(1, False, P.n_exp_run)
    debug_dump("x_ffn1", lambda o: [dma("sp", o.rearrange("(k p) t -> p k t", p=128)[:, k, :], xT[:, k, 0:T_LAT],
                                        R=B_x[k][:4]) for k in range(KD)])
    if "final" in P.parts:
        with ExitStack() as fes:
            ftmp = [P.sb(fes, f"ftmp{i}", [128, 512], F32) for i in range(2)]
            B_ft = [Buf("ft0"), Buf("ft1")]
            frs = P.sb(fes, "frs", [128, 512], F32)
            B_frs = Buf("frs")
            fsq = [P.sb(fes, f"fsq{i}", [128, 512], BF16) for i in range(2)]
            B_fsq = [Buf("fsq0"), Buf("fsq1")]
            fo = [P.sb(fes, f"fo{i}", [128, 512], F32) for i in range(4)]
            B_fo = [Buf(f"fo{i}") for i in range(4)]
            osrc = out_d.rearrange("(k p) t -> p k t", p=128)
            oc_ = {"n": 0}
            for b in range(4):
                s, n, _ = BLOCKS[b]

                def dst(k):
                    i = oc_["n"] % 4
                    oc_["n"] += 1
                    dst.last = i
                    return fo[i][:, :n], [B_fo[i]]
                norm_block(1, 0, b, dst, ftmp, B_ft, frs, B_frs, fsq, B_fsq, psn_i=7, final=True,
                           after=lambda k, b=b, s=s, n=n: dma("sp", osrc[:, k, s:s + n], fo[dst.last][:, :n], R=[B_fo[dst.last]]))
    if P.stop_after is not None:
        osrc = out_d.rearrange("(k p) t -> p k t", p=128)
        for k in range(KD):
            dma("sp", osrc[:, k, :], xT[:, k, 0:T_LAT], R=B_x[k][:4])
    S.barrier()
    es.close()
    return P


def _prep_inputs(inp, b, consts, names=None):
    f = np.float32
    m = {}
    m["xT"] = np.ascontiguousarray(np.concatenate([inp["x"][b].T, inp["ctx"][b].T], axis=1)).astype(f)
    cv = np.stack([inp["c"][b], inp["c_ctx"]], axis=0)
    m["cvec"] = np.ascontiguousarray(cv.reshape(2, KD, 128).transpose(2, 0, 1).reshape(128, 2 * KD))
    m["w_ada"] = inp["w_ada"]
    m["b_ada"] = np.ascontiguousarray(np.tile(inp["b_ada"].reshape(1, 12 * D), (2, 1)))
    nr = np.stack([inp["norm_mix"][0], inp["norm_mix"][1], inp["norm_ffn"][0], inp["norm_ffn"][1],
                   inp["norm_final"]], axis=0)
    m["norms"] = np.ascontiguousarray(nr.reshape(5, KD, 128).transpose(2, 0, 1).reshape(128, 5 * KD))
    m["ret_w_in"] = inp["ret_w_in"][0]
    m["ret_w_out"] = inp["ret_w_out"][0]
    m["hg_w_in"] = inp["hg_w_in"][0]
    m["hg_w_out"] = inp["hg_w_out"][0]
    lb = inp["hg_lower_bounds"].reshape(4, KD, 128).transpose(2, 0, 1).reshape(128, 4 * KD)
    m["hg_misc"] = np.ascontiguousarray(np.concatenate([inp["hg_g_norm"][0].reshape(128, 1), lb], axis=1)).astype(f)
    m["moe_router"] = inp["moe_router"]
    m.update(consts)
    for l in range(2):
        for e in range(N_EXP):
            m[f"wg_{l}_{e}"] = inp["moe_w_gate"][l, e]
            m[f"wu_{l}_{e}"] = inp["moe_w_up"][l, e]
            m[f"wd_{l}_{e}"] = inp["moe_w_down"][l, e]
    if names is not None:
        m = {k: v for k, v in m.items() if k in names}
    return m


def kernel(**inputs):
    inp = {k: np.asarray(v) for k, v in inputs.items()}
    consts = _const_tables()
    P = build_program()
    names = set(P.dram.keys())
    in_maps = [_prep_inputs(inp, b, consts, names) for b in range(8)]
    res = run_bass_kernel_spmd(P.nc, in_maps, core_ids=list(range(8)))
    out = np.stack([np.ascontiguousarray(res.results[b]["outT"].T) for b in range(8)], axis=0)
    return out.astype(np.float32)
```

```python
import math
from contextlib import ExitStack

import numpy as np
import concourse.bass as bass
import concourse.mybir as mybir
from concourse.bass_utils import run_bass_kernel_spmd

F32 = mybir.dt.float32
BF16 = mybir.dt.bfloat16
ALU = mybir.AluOpType
AF = mybir.ActivationFunctionType
AX = mybir.AxisListType

D = 1024
KD = 8
T_LAT = 2048
T_CTX = 256
T_ALL = T_LAT + T_CTX
NT = T_ALL // 128
EPS = 1e-6
N_EXP = 16
FF = 2816
NFC = FF // 128
RET_H = 4
HG_H = 8

BLOCKS = [(0, 512, False), (512, 512, False), (1024, 512, False), (1536, 512, False), (2048, 256, True)]


def interleave(*gens):
    gens = list(gens)
    while gens:
        for g in list(gens):
            try:
                next(g)
            except StopIteration:
                gens.remove(g)


class Buf:
    __slots__ = ("name", "w", "rs")

    def __init__(self, name):
        self.name = name
        self.w = None
        self.rs = {}


class Sched:
    NDMA = 12

    def __init__(self, nc, es):
        self.nc = nc
        self.h = {"pe": nc.tensor, "act": nc.scalar, "dve": nc.vector, "pool": nc.gpsimd, "sp": nc.sync}
        self.sem = {}
        self.cnt = {}
        self.seen = {}
        for e in self.h:
            self.sem[e] = es.enter_context(nc.semaphore("s_" + e))
            self.cnt[e] = 0
            self.seen[e] = {}
        self.dsem = {}
        self.dval = {}
        self.dnext = {}
        for q in ("sp", "pool"):
            self.dsem[q] = [es.enter_context(nc.semaphore(f"d_{q}{i}")) for i in range(self.NDMA)]
            self.dval[q] = [0] * self.NDMA
            self.dnext[q] = 0
        self.ninst = 0

    def _wait(self, eng, tk):
        if tk is None:
            return
        kind = tk[0]
        if kind == "c":
            _, src, n = tk
            if src == eng and eng in ("pe", "sp"):
                return
            if self.seen[eng].get(src, 0) >= n:
                return
            self.h[eng].wait_ge(self.sem[src], n)
            self.seen[eng][src] = n
        else:
            _, q, idx, val = tk
            key = (q, idx)
            if self.seen[eng].get(key, 0) >= val:
                return
            self.h[eng].wait_ge(self.dsem[q][idx], val)
            self.seen[eng][key] = val
        self.ninst += 1

    def _deps(self, eng, R, W):
        for b in R:
            self._wait(eng, b.w)
        for b in W:
            self._wait(eng, b.w)
            for tk in b.rs.values():
                self._wait(eng, tk)

    def _record(self, tk, R, W):
        for b in W:
            b.w = tk
            b.rs = {}
        for b in R:
            if b in W:
                continue
            key = tk[1] if tk[0] == "c" else (tk[1], tk[2])
            b.rs[key] = tk

    @staticmethod
    def _flat(L):
        out = []
        for b in L:
            if isinstance(b, (list, tuple)):
                out.extend(Sched._flat(b))
            else:
                out.append(b)
        return out

    def op(self, eng, fn, R=(), W=(), signal=True):
        R, W = self._flat(R), self._flat(W)
        self._deps(eng, R, W)
        ins = fn(self.h[eng])
        self.ninst += 1
        if signal:
            ins.then_inc(self.sem[eng], 1)
            self.cnt[eng] += 1
            tk = ("c", eng, self.cnt[eng])
            if eng not in ("pe",):
                pass
        else:
            tk = ("c", eng, self.cnt[eng] + 1)
        self._record(tk, R, W)
        return tk

    def dma(self, q, out, in_, R=(), W=()):
        R, W = self._flat(R), self._flat(W)
        self._deps(q, R, W)
        idx = self.dnext[q]
        self.dnext[q] = (idx + 1) % self.NDMA
        prev = self.dval[q][idx]
        if prev > 0:
            self._wait(q, ("d", q, idx, prev))
        val = prev + 16
        self.dval[q][idx] = val
        self.h[q].dma_start(out=out, in_=in_).then_inc(self.dsem[q][idx], 16)
        self.ninst += 1
        tk = ("d", q, idx, val)
        self._record(tk, R, W)
        return tk

    def barrier(self):
        for e in self.h:
            for src in self.h:
                if src != e and self.cnt[src] > 0:
                    self._wait(e, ("c", src, self.cnt[src]))
            for q in ("sp", "pool"):
                for idx in range(self.NDMA):
                    if self.dval[q][idx] > 0:
                        self._wait(e, ("d", q, idx, self.dval[q][idx]))


def _ret_gammas():
    j = np.arange(8, dtype=np.float64)
    g = 1.0 - np.exp2(-5.0 - j / 2)
    return g[0::2], g[1::2]


def _const_tables():
    t = {}
    half = 128
    inv = 10000.0 ** (-np.arange(0, half, 2, dtype=np.float64) / half)
    p = np.arange(128)
    sign = np.where(p < 64, -1.0, 1.0)
    rows = np.arange(T_LAT // 64, dtype=np.float64)
    cols = np.arange(64, dtype=np.float64)
    ang_r = rows[None, :] * inv[p % 64][:, None]
    ang_c = cols[None, :] * inv[p % 64][:, None]
    t["rope"] = np.concatenate(
        [np.cos(ang_r), np.sin(ang_r) * sign[:, None], np.cos(ang_c), np.sin(ang_c) * sign[:, None]], axis=1
    ).astype(np.float32)
    gf, gb = _ret_gammas()
    b = np.arange(128, dtype=np.float64)[:, None]
    a = np.arange(512, dtype=np.float64)[None, :]
    tabs = np.zeros((RET_H, 128, 1920), dtype=np.float64)
    xs = np.arange(896, dtype=np.float64)[None, :] - 384.0
    for h in range(RET_H):
        tabs[h, :, 0:512] = gf[h] ** (a - b)
        tabs[h, :, 512:1024] = gb[h] ** (b + 511 - a)
        dl = xs - b
        tabs[h, :, 1024:1920] = np.where(dl > 0, gf[h] ** np.maximum(dl, 0),
                                         np.where(dl < 0, gb[h] ** np.maximum(-dl, 0), 2.0))
    t["rdec"] = tabs.astype(np.float32)
    t["ident"] = np.eye(128, dtype=np.float32)
    t["hgmask"] = np.concatenate([np.triu(np.ones((128, 128))), np.tril(np.ones((128, 128)))], axis=1).astype(np.float32)
    t["iota1"] = np.tile(np.arange(1, 257, dtype=np.float32)[None, :], (128, 1))
    return t


class Prog:
    def __init__(self, dbg=None, stop_after=None):
        self.dbg = dbg or []
        self.stop_after = stop_after
        self.nc = bass.Bass("TRN2", target_bir_lowering=False)
        self.es = ExitStack()
        self.S = Sched(self.nc, self.es)
        self.dram = {}
        self.dbg_out = {}

    def din(self, name, shape, dt=F32):
        self.dram[name] = self.nc.dram_tensor(name, list(shape), dt, kind="ExternalInput").ap()
        return self.dram[name]

    def dout(self, name, shape, dt=F32):
        self.dram[name] = self.nc.dram_tensor(name, list(shape), dt, kind="ExternalOutput").ap()
        return self.dram[name]

    def sb(self, es, name, shape, dt):
        self._uid = getattr(self, "_uid", 0) + 1
        return es.enter_context(self.nc.sbuf_tensor(f"{name}_u{self._uid}", list(shape), dt))

    def ps(self, es, name, shape, dt=F32):
        return es.enter_context(self.nc.psum_tensor(name, list(shape), dt))


def build_program(dbg=(), stop_after=None, parts=("ret", "moe0", "hg", "moe1", "final"), n_exp_run=N_EXP):
    P = Prog(list(dbg), stop_after)
    P.parts = parts
    P.n_exp_run = n_exp_run
    nc, S, es = P.nc, P.S, P.es
    op, dma = S.op, S.dma

    xT_d = P.din("xT", [D, T_ALL])
    cvec_d = P.din("cvec", [128, 2 * KD])
    wada_d = P.din("w_ada", [2, D, 6 * D])
    bada_d = P.din("b_ada", [2, 12 * D])
    nrm_d = P.din("norms", [128, 5 * KD])
    retin_d = P.din("ret_w_in", [D, 6144])
    retout_d = P.din("ret_w_out", [2048, D])
    router_d = P.din("moe_router", [2, D, N_EXP])
    rope_d = P.din("rope", [128, 192])
    rdec_d = P.din("rdec", [RET_H, 128, 1920])
    ident_d = P.din("ident", [128, 128])
    out_d = P.dout("outT", [D, T_LAT])
    for name, shape, dt_ in P.dbg:
        P.dbg_out[name] = P.dout("dbg_" + name, shape, dt_)

    xT = P.sb(es, "xT_sb", [128, KD, T_ALL], F32)
    B_x = [[Buf(f"x{k}_{b}") for b in range(5)] for k in range(KD)]
    cvec = P.sb(es, "cvec_sb", [128, 2 * KD], F32)
    scc = P.sb(es, "scc", [128, KD, 2], F32)
    nrm = P.sb(es, "nrm", [128, 5 * KD], F32)
    mod = P.sb(es, "mod", [128, 2, 48, 2], F32)
    modA = P.sb(es, "modA", [128, 2, 2, KD, 2], F32)
    ident_f = P.sb(es, "ident_f", [128, 128], F32)
    ident_b = P.sb(es, "ident_b", [128, 128], BF16)
    ones_b = P.sb(es, "ones_b", [128, 128], BF16)
    B_const = Buf("const")
    B_mod = Buf("mod")

    psb = [P.ps(es, f"psb{i}", [128, 512], F32) for i in range(8)]
    B_ps = [Buf(f"ps{i}") for i in range(8)]

    def sl(b):
        s, n, _ = BLOCKS[b]
        return slice(s, s + n)

    xsrc = xT_d.rearrange("(k p) t -> p k t", p=128)
    for k in range(KD):
        dma("sp", xT[:, k, :], xsrc[:, k, :], W=B_x[k])
    dma("sp", cvec[:], cvec_d, W=[B_const])
    dma("sp", nrm[:], nrm_d, W=[B_const])
    dma("sp", ident_f[:], ident_d, W=[B_const])
    op("act", lambda e: e.copy(out=ident_b[:], in_=ident_f[:]), R=[B_const], W=[B_const])
    op("dve", lambda e: e.memset(ones_b[:], 1.0), W=[B_const])
    op("act", lambda e: e.activation(out=scc[:].rearrange("p k c -> p c k"),
                                     in_=cvec[:].rearrange("p (c k) -> p c k", c=2), func=AF.Silu),
       R=[B_const], W=[B_const])

    with ExitStack() as pes:
        NPIECE = 12
        NST = 4
        wa = [P.sb(pes, f"wa{i}", [128, KD, 512], F32) for i in range(NST)]
        B_wa = [Buf(f"wa{i}") for i in range(NST)]
        HALF = 3 * D
        modrow = P.sb(pes, "modrow", [2, HALF], F32)
        B_mr = Buf("modrow")
        bada2 = P.sb(pes, "bada2", [2, HALF], F32)
        B_b2 = Buf("bada2")
        pi = 0
        for l in range(2):
            wsrc = wada_d[l].rearrange("(k p) f -> p k f", p=128)
            for hf in range(2):
                dma("sp", bada2[:], bada_d[:, l * 6 * D + hf * HALF: l * 6 * D + (hf + 1) * HALF], W=[B_b2])
                for pc6 in range(6):
                    pc = hf * 6 + pc6
                    slot = pi % NST
                    dma("sp", wa[slot][:], wsrc[:, :, pc * 512:(pc + 1) * 512], W=[B_wa[slot]])
                    pb = pi % 2
                    for k in range(KD):
                        op("pe", lambda e: e.matmul(psb[pb][0:2, :], lhsT=scc[:, k, :], rhs=wa[slot][:, k, :],
                                                    start=(k == 0), stop=(k == KD - 1)),
                           R=[B_wa[slot], B_const], W=[B_ps[pb]], signal=(k == KD - 1))
                    op("dve", lambda e: e.tensor_tensor(out=modrow[:, pc6 * 512:(pc6 + 1) * 512], in0=psb[pb][0:2, :],
                                                        in1=bada2[:, pc6 * 512:(pc6 + 1) * 512],
                                                        op=ALU.add), R=[B_ps[pb], B_b2], W=[B_mr])
                    pi += 1
                pt_ = 2 + (l * 2 + hf) % 2
                for j in range(24):
                    op("pe", lambda e: e.transpose(out=psb[pt_][:, j * 2:(j + 1) * 2], in_=modrow[:, j * 128:(j + 1) * 128],
                                                   identity=ident_f[0:2, 0:2]),
                       R=[B_mr, B_const], W=[B_ps[pt_]], signal=(j == 23))
                op("dve", lambda e: e.tensor_copy(out=mod[:, l, hf * 24:(hf + 1) * 24, :].rearrange("p j c -> p (j c)"),
                                                  in_=psb[pt_][:, 0:48]), R=[B_ps[pt_]], W=[B_mod])
        for l in range(2):
            for site in range(2):
                sc_j = (1 if site == 0 else 4) * KD
                nw = nrm[:, (site * 2 + l) * KD:(site * 2 + l + 1) * KD]
                op("dve", lambda e, l=l, site=site, sc_j=sc_j, nw=nw: e.scalar_tensor_tensor(
                    out=modA[:, l, site, :, :], in0=mod[:, l, sc_j:sc_j + KD, :], scalar=1.0,
                    in1=nw.unsqueeze(2).to_broadcast([128, KD, 2]), op0=ALU.add, op1=ALU.mult),
                   R=[B_mod, B_const], W=[B_mod])
        S.barrier()

    def modv(l, chunk, k, c):
        return mod[:, l, chunk * KD + k, c:c + 1]

    def norm_block(l, site, b, dst_fn, tmp, B_tmp, rstd, B_rstd, sq, B_sq, psn_i, final=False, after=None):
        s, n, isc = BLOCKS[b]
        c = 1 if isc else 0
        for k in range(KD):
            q = k % 2
            op("act", lambda e, k=k, q=q: e.activation(out=sq[q][:, :n], in_=xT[:, k, s:s + n], func=AF.Square),
               R=[B_x[k][b]], W=[B_sq[q]])
            op("pe", lambda e, k=k, q=q: e.matmul(psb[psn_i][:, :n], lhsT=ones_b[:], rhs=sq[q][:, :n],
                                                 start=(k == 0), stop=(k == KD - 1)),
               R=[B_sq[q], B_const], W=[B_ps[psn_i]], signal=True)
        op("act", lambda e: e.activation(out=rstd[:, :n], in_=psb[psn_i][:, :n], func=AF.Sqrt, bias=EPS,
                                         scale=1.0 / D), R=[B_ps[psn_i]], W=[B_rstd])
        op("dve", lambda e: e.reciprocal(out=rstd[:, :n], in_=rstd[:, :n]), R=[B_rstd], W=[B_rstd])
        for k in range(KD):
            q = k % 2
            out_ap, obufs = dst_fn(k)
            if final:
                a_ap = nrm[:, 4 * KD + k:4 * KD + k + 1]
                op("dve", lambda e, k=k, a_ap=a_ap, out_ap=out_ap: e.scalar_tensor_tensor(
                    out=out_ap, in0=xT[:, k, s:s + n], scalar=a_ap, in1=rstd[:, :n], op0=ALU.mult, op1=ALU.mult),
                   R=[B_x[k][b], B_rstd, B_const], W=obufs)
                if after is not None:
                    after(k)
            else:
                a_ap = modA[:, l, site, k, c:c + 1]
                sh_ap = modv(l, 0 if site == 0 else 3, k, c)
                op("dve", lambda e, k=k, q=q, a_ap=a_ap: e.scalar_tensor_tensor(
                    out=tmp[q][:, :n], in0=xT[:, k, s:s + n], scalar=a_ap, in1=rstd[:, :n],
                    op0=ALU.mult, op1=ALU.mult), R=[B_x[k][b], B_rstd, B_mod], W=[B_tmp[q]])
                op("act", lambda e, q=q, sh_ap=sh_ap, out_ap=out_ap: e.activation(
                    out=out_ap, in_=tmp[q][:, :n], func=AF.Identity, bias=sh_ap, scale=1.0),
                   R=[B_tmp[q], B_mod], W=obufs)

    def debug_dump(name, ap_fn):
        if name in P.dbg_out:
            S.barrier()
            tk = ap_fn(P.dbg_out[name])
            S.barrier()

    NSLOT = 4
    wring = [P.sb(es, f"wring{i}", [128, KD, 512], BF16) for i in range(NSLOT)]
    B_wr = [Buf(f"wr{i}") for i in range(NSLOT)]
    wr_state = {"n": 0}

    def wslot():
        i = wr_state["n"] % NSLOT
        wr_state["n"] += 1
        return i

    def retention_layer(l):
        with ExitStack() as les:
            hT = P.sb(les, "hT", [128, KD, T_ALL], BF16)
            B_h = [[Buf(f"h{k}_{b}") for b in range(5)] for k in range(KD)]
            tmp = [P.sb(les, f"ntmp{i}", [128, 512], F32) for i in range(2)]
            B_tmp = [Buf("ntmp0"), Buf("ntmp1")]
            sg = [P.sb(les, f"sg{i}", [128, 512], F32) for i in range(2)]
            B_sg = [Buf("sg0"), Buf("sg1")]
            rstd = P.sb(les, "rstd", [128, 512], F32)
            B_rstd = Buf("rstd")
            sq = [P.sb(les, f"sq{i}", [128, 512], BF16) for i in range(2)]
            B_sq = [Buf("sq0"), Buf("sq1")]
            rope_sb = P.sb(les, "rope_sb", [128, 192], F32)
            B_rope = Buf("rope")
            dma("sp", rope_sb[:], rope_d, W=[B_rope])
            for b in range(5):
                s, n, _ = BLOCKS[b]
                norm_block(l, 0, b, lambda k, b=b, s=s, n=n: (hT[:, k, s:s + n], [B_h[k][b]]),
                           tmp, B_tmp, rstd, B_rstd, sq, B_sq, psn_i=7)
            debug_dump("hT0", lambda o: [dma("sp", o.rearrange("(k p) t -> p k t", p=128)[:, k, :], hT[:, k, :],
                                             R=B_h[k]) for k in range(KD)])
            if P.stop_after == "hT0":
                return

            qr = P.sb(les, "qr", [128, 2, 2, 512], BF16)
            kr = P.sb(les, "kr", [128, 2, T_ALL], BF16)
            vv = P.sb(les, "vv", [128, NT, 512], BF16)
            B_q = [[Buf(f"q{i}_{m}") for m in range(2)] for i in range(2)]
            B_k = [[Buf(f"k{m}_{t}") for t in range(NT)] for m in range(2)]
            B_v = [Buf(f"v{t}") for t in range(NT)]
            rdec = P.sb(les, "rdec_sb", [128, 1920], F32)
            B_rdec = Buf("rdec")
            Ff = rdec[:, 0:512]
            Fb = rdec[:, 512:1024]

            def Dg(kk, n):
                return rdec[:, 1024 + 384 - 128 * kk: 1024 + 384 - 128 * kk + n]
            rt1, B_rt1 = tmp, B_tmp
            rt2, B_rt2 = sg, B_sg
            ctab, B_ctab = tmp, B_tmp
            AT = [P.sb(les, f"AT{i}", [128, 512], BF16) for i in range(3)]
            B_AT = [Buf(f"AT{i}") for i in range(3)]
            ogT = P.sb(les, "ogT", [128, 4, 512], BF16)
            B_og = [Buf(f"og{c}") for c in range(4)]
            sgb = P.sb(les, "sgb", [128, 4, 512], BF16)
            B_sgb = [Buf(f"sgb{c}") for c in range(4)]
            gf, gb = _ret_gammas()
            rcount = {"rope": 0, "at": 0, "q": 0}

            win_src = retin_d.rearrange("(k p) f -> p k f", p=128)
            wout_src = retout_d.rearrange("(c p) f -> p c f", p=128)

            def proj_rope(sA, qk, m, b, dst_ap, wb):
                s, n, isc = BLOCKS[b]
                pi_ = rcount["rope"] % 2
                rcount["rope"] += 1
                pst = psb[pi_]
                for k in range(KD):
                    op("pe", lambda e, k=k: e.matmul(
                        pst[:, :n], lhsT=wring[sA][:, k, qk * 256 + m * 128: qk * 256 + (m + 1) * 128],
                        rhs=hT[:, k, s:s + n], start=(k == 0), stop=(k == KD - 1)),
                       R=[B_wr[sA], B_h[k][b]], W=[B_ps[pi_]], signal=(k == KD - 1))
                scale = 1.0 if qk == 0 else 1.0 / 16.0
                if isc:
                    op("act", lambda e: e.activation(out=dst_ap, in_=pst[:, :n], func=AF.Copy, scale=scale),
                       R=[B_ps[pi_]], W=wb)
                    return
                g0 = s // 64
                if m == 0:
                    cos_ap = rope_sb[:, g0:g0 + 8].unsqueeze(2).to_broadcast([128, 8, 64])
                    sin_lo = rope_sb[0:64, 32 + g0:32 + g0 + 8].unsqueeze(2).to_broadcast([64, 8, 64])
                    sin_hi = rope_sb[64:128, 32 + g0:32 + g0 + 8].unsqueeze(2).to_broadcast([64, 8, 64])
                else:
                    cos_ap = rope_sb[:, 64:128].unsqueeze(1).to_broadcast([128, 8, 64])
                    sin_lo = rope_sb[0:64, 128:192].unsqueeze(1).to_broadcast([64, 8, 64])
                    sin_hi = rope_sb[64:128, 128:192].unsqueeze(1).to_broadcast([64, 8, 64])
                ti = pi_
                p3 = pst[:].rearrange("p (g t) -> p g t", t=64)
                t1v = rt1[ti][:].rearrange("p (g t) -> p g t", t=64)
                t2v = rt2[ti][:].rearrange("p (g t) -> p g t", t=64)
                op("dve", lambda e: e.scalar_tensor_tensor(
                    out=t1v, in0=p3, scalar=scale, in1=cos_ap, op0=ALU.mult, op1=ALU.mult),
                   R=[B_ps[pi_], B_rope], W=[B_rt1[ti]])
                op("dve", lambda e: e.scalar_tensor_tensor(
                    out=t2v[0:64], in0=p3[64:128], scalar=scale, in1=sin_lo, op0=ALU.mult, op1=ALU.mult),
                   R=[B_ps[pi_], B_rope], W=[B_rt2[ti]])
                op("dve", lambda e: e.scalar_tensor_tensor(
                    out=t2v[64:128], in0=p3[0:64], scalar=scale, in1=sin_hi, op0=ALU.mult, op1=ALU.mult),
                   R=[B_ps[pi_], B_rope, B_rt2[ti]], W=[B_rt2[ti]])
                op("dve", lambda e: e.tensor_tensor(
                    out=dst_ap, in0=rt1[ti][:, :n], in1=rt2[ti][:, :n], op=ALU.add),
                   R=[B_rt1[ti], B_rt2[ti]], W=wb)

            for h in range(RET_H):
                sA, sB, sC, sD = wslot(), wslot(), wslot(), wslot()
                dma("pool", wring[sA][:, :, 0:256], win_src[:, :, h * 256:(h + 1) * 256], W=[B_wr[sA]])
                dma("pool", wring[sA][:, :, 256:512], win_src[:, :, 1024 + h * 256:1024 + (h + 1) * 256], W=[B_wr[sA]])
                dma("pool", wring[sB][:], win_src[:, :, 2048 + h * 512:2048 + (h + 1) * 512], W=[B_wr[sB]])
                dma("pool", wring[sC][:], win_src[:, :, 4096 + h * 512:4096 + (h + 1) * 512], W=[B_wr[sC]])
                wD = wring[sD][:].rearrange("p k f -> p (k f)").rearrange("p (c f) -> p c f", c=4)
                dma("pool", wD, wout_src[:, h * 4:(h + 1) * 4, :], W=[B_wr[sD]])
                dma("sp", rdec[:], rdec_d[h], W=[B_rdec])

                for m in range(2):
                    for b in range(5):
                        s, n, isc = BLOCKS[b]
                        proj_rope(sA, 1, m, b, kr[:, m, s:s + n], [B_k[m][t] for t in range(s // 128, (s + n) // 128)])
                for t in range(NT):
                    b = min(t // 4, 4)
                    pi_ = t % 2
                    for k in range(KD):
                        op("pe", lambda e, k=k, t=t, pi_=pi_: e.matmul(
                            psb[pi_][:, :], lhsT=hT[:, k, t * 128:(t + 1) * 128], rhs=wring[sB][:, k, :],
                            start=(k == 0), stop=(k == KD - 1)),
                           R=[B_wr[sB], B_h[k][b]], W=[B_ps[pi_]], signal=(k == KD - 1))
                    op("act", lambda e, t=t, pi_=pi_: e.copy(out=vv[:, t, :], in_=psb[pi_][:, :]),
                       R=[B_ps[pi_]], W=[B_v[t]])
                if h == 0:
                    debug_dump("kr0", lambda o: [dma("sp", o[:, m, :], kr[:, m, :], R=B_k[m]) for m in range(2)])
                    debug_dump("vv0", lambda o: [dma("sp", o, vv[:], R=B_v)])
                    if P.stop_after == "proj0":
                        return

                def block_gen(b):
                    s, n, isc = BLOCKS[b]
                    c = 1 if isc else 0
                    qi = rcount["q"] % 2
                    rcount["q"] += 1
                    for m in range(2):
                        proj_rope(sA, 0, m, b, qr[:, qi, m, :n], [B_q[qi][m]])
                    yield
                    for cc in range(4):
                        pg = cc % 2
                        for k in range(KD):
                            op("pe", lambda e: e.matmul(
                                psb[pg][:, :n], lhsT=wring[sC][:, k, cc * 128:(cc + 1) * 128], rhs=hT[:, k, s:s + n],
                                start=(k == 0), stop=(k == KD - 1)),
                               R=[B_wr[sC], B_h[k][b]], W=[B_ps[pg]], signal=(k == KD - 1))
                        op("act", lambda e: e.activation(out=sgb[:, cc, :n], in_=psb[pg][:, :n], func=AF.Silu),
                           R=[B_ps[pg]], W=[B_sgb[cc]])
                    keys = [16, 17] if isc else list(range(NT))
                    pso = [psb[2 + cc] for cc in range(4)]
                    B_pso = [B_ps[2 + cc] for cc in range(4)]

                    def emit_scores(j, idx):
                        pi_ = 6 + (idx % 2)
                        for m in range(2):
                            op("pe", lambda e, m=m: e.matmul(
                                psb[pi_][:, :n], lhsT=kr[:, m, j * 128:(j + 1) * 128], rhs=qr[:, qi, m, :n],
                                start=(m == 0), stop=(m == 1)),
                               R=[B_k[m][j], B_q[qi][m]], W=[B_ps[pi_]], signal=(m == 1))
                        ai = rcount["at"] % 3
                        rcount["at"] += 1
                        pst = psb[pi_]
                        if isc:
                            tab = Dg(j - 16, n)
                            op("dve", lambda e: e.tensor_tensor(out=AT[ai][:, :n], in0=pst[:, :n], in1=tab, op=ALU.mult),
                               R=[B_ps[pi_], B_rdec], W=[B_AT[ai]])
                        elif j >= 16:
                            jc = j - 16
                            s1 = float(gf[h] ** (s + 256 - 128 * jc))
                            s2 = float(gb[h] ** (T_LAT - s - 511 + 128 * jc))
                            ci = jc
                            op("pool", lambda e: e.tensor_scalar(
                                out=ctab[ci][:], in0=Ff, scalar1=s1, scalar2=None, op0=ALU.mult),
                               R=[B_rdec], W=[B_ctab[ci]])
                            op("dve", lambda e: e.scalar_tensor_tensor(
                                out=ctab[ci][:], in0=Fb, scalar=s2, in1=ctab[ci][:], op0=ALU.mult, op1=ALU.add),
                               R=[B_rdec, B_ctab[ci]], W=[B_ctab[ci]])
                            op("dve", lambda e: e.tensor_tensor(
                                out=AT[ai][:, :n], in0=pst[:, :n], in1=ctab[ci][:, :n], op=ALU.mult),
                               R=[B_ps[pi_], B_ctab[ci]], W=[B_AT[ai]])
                        else:
                            rel = j - 4 * b
                            if 0 <= rel < 4:
                                tab = Dg(rel, 512)
                                op("dve", lambda e: e.tensor_tensor(out=AT[ai][:], in0=pst[:], in1=tab, op=ALU.mult),
                                   R=[B_ps[pi_], B_rdec], W=[B_AT[ai]])
                            elif rel < 0:
                                sc = float(gf[h] ** (s - 128 * j))
                                op("dve", lambda e: e.scalar_tensor_tensor(
                                    out=AT[ai][:], in0=pst[:], scalar=sc, in1=Ff, op0=ALU.mult, op1=ALU.mult),
                                   R=[B_ps[pi_], B_rdec], W=[B_AT[ai]])
                            else:
                                sc = float(gb[h] ** (128 * j - s - 511))
                                op("dve", lambda e: e.scalar_tensor_tensor(
                                    out=AT[ai][:], in0=pst[:], scalar=sc, in1=Fb, op0=ALU.mult, op1=ALU.mult),
                                   R=[B_ps[pi_], B_rdec], W=[B_AT[ai]])
                        return ai

                    def emit_av(j, ai, first, last):
                        for cc in range(4):
                            op("pe", lambda e, cc=cc: e.matmul(
                                pso[cc][:, :n], lhsT=vv[:, j, cc * 128:(cc + 1) * 128], rhs=AT[ai][:, :n],
                                start=first, stop=last),
                               R=[B_v[j], B_AT[ai]], W=[B_pso[cc]], signal=(cc == 3))

                    pend = None
                    for idx, j in enumerate(keys):
                        ai = emit_scores(j, idx)
                        if pend is not None:
                            emit_av(*pend)
                        pend = (j, ai, idx == 0, idx == len(keys) - 1)
                    emit_av(*pend)

                    yield
                    for cc in range(4):
                        q2 = cc % 2
                        op("act", lambda e, cc=cc, q2=q2: e.activation(out=sq[q2][:, :n], in_=pso[cc][:, :n],
                                                                      func=AF.Square),
                           R=[B_pso[cc]], W=[B_sq[q2]])
                        op("pe", lambda e, cc=cc, q2=q2: e.matmul(psb[6][:, :n], lhsT=ones_b[:], rhs=sq[q2][:, :n],
                                                                 start=(cc == 0), stop=(cc == 3)),
                           R=[B_sq[q2], B_const], W=[B_ps[6]], signal=True)
                    op("act", lambda e: e.activation(out=rstd[:, :n], in_=psb[6][:, :n], func=AF.Sqrt, bias=EPS,
                                                     scale=1.0 / 512), R=[B_ps[6]], W=[B_rstd])
                    op("dve", lambda e: e.reciprocal(out=rstd[:, :n], in_=rstd[:, :n]), R=[B_rstd], W=[B_rstd])
                    for cc in range(4):
                        q2 = cc % 2
                        op("dve", lambda e, cc=cc, q2=q2: e.tensor_tensor(
                            out=tmp[q2][:, :n], in0=pso[cc][:, :n], in1=rstd[:, :n], op=ALU.mult),
                           R=[B_pso[cc], B_rstd], W=[B_tmp[q2]])
                        op("dve", lambda e, cc=cc, q2=q2: e.tensor_tensor(
                            out=ogT[:, cc, :n], in0=tmp[q2][:, :n], in1=sgb[:, cc, :n], op=ALU.mult),
                           R=[B_tmp[q2], B_sgb[cc]], W=[B_og[cc]])
                    for mch in range(KD):
                        pi_ = 6 + (mch % 2)
                        for cc in range(4):
                            op("pe", lambda e, cc=cc, mch=mch, pi_=pi_: e.matmul(
                                psb[pi_][:, :n], lhsT=wD[:, cc, mch * 128:(mch + 1) * 128], rhs=ogT[:, cc, :n],
                                start=(cc == 0), stop=(cc == 3)),
                               R=[B_wr[sD], B_og[cc]], W=[B_ps[pi_]], signal=(cc == 3))
                        g_ap = modv(l, 2, mch, c)
                        op("dve", lambda e, mch=mch, pi_=pi_, g_ap=g_ap: e.scalar_tensor_tensor(
                            out=xT[:, mch, s:s + n], in0=psb[pi_][:, :n], scalar=g_ap, in1=xT[:, mch, s:s + n],
                            op0=ALU.mult, op1=ALU.add),
                           R=[B_ps[pi_], B_mod, B_x[mch][b]], W=[B_x[mch][b]])

                gens = [block_gen(b) for b in range(5)]
                next(gens[0])
                for b in range(5):
                    next(gens[b])
                    if b + 1 < 5:
                        next(gens[b + 1])
                    for _ in gens[b]:
                        pass
            S.barrier()

    iota_d2 = P.din("iota1", [128, 256])

    def moe_layer(l, with_ctx, n_exp_run=N_EXP):
        nblk = 5 if with_ctx else 4
        ntile = NT if with_ctx else 16
        NS = 288 if with_ctx else 256
        nst = 3 if with_ctx else 2
        st_sz = [128, 128, 32][:nst]
        st_off = [0, 128, 256][:nst]
        with ExitStack() as mes:
            h2tok = P.sb(mes, "h2tok", [128, NT, D], BF16)
            B_h2 = [Buf(f"h2t{t}") for t in range(NT)]
            wr_sb = P.sb(mes, "wr_sb", [128, KD, N_EXP], F32)
            B_wrt = Buf("wrt")
            dma("sp", wr_sb[:], router_d[l].rearrange("(k p) e -> p k e", p=128), W=[B_wrt])
            iota1 = P.sb(mes, "iota1_sb", [128, 256], F32)
            dma("sp", iota1[:], iota_d2, W=[B_wrt])
            aff = P.sb(mes, "aff", [128, NT, N_EXP], F32)
            B_aff = Buf("aff")
            aff_hl = P.sb(mes, "aff_hl", [128, NT, N_EXP, 2], BF16)
            posm_tok = P.sb(mes, "posm_tok", [128, NT, N_EXP], F32)
            B_posm = Buf("posm")

            with ExitStack() as r1:
                tmp = [P.sb(r1, f"mtmp{i}", [128, 512], F32) for i in range(2)]
                B_tmp = [Buf("mtmp0"), Buf("mtmp1")]
                rstd = P.sb(r1, "mrstd", [128, 512], F32)
                B_rstd = Buf("mrstd")
                sq = [P.sb(r1, f"msq{i}", [128, 512], BF16) for i in range(2)]
                B_sq = [Buf("msq0"), Buf("msq1")]
                h2f = P.sb(r1, "h2f", [128, KD, 512], F32)
                B_h2f = [Buf(f"h2f{k}") for k in range(KD)]
                h2b = P.sb(r1, "h2b", [128, KD, 512], BF16)
                B_h2b = [Buf(f"h2b{k}") for k in range(KD)]
                for b in range(nblk):
                    s, n, isc = BLOCKS[b]
                    norm_block(l, 1, b, lambda k, n=n: (h2f[:, k, :n], [B_h2f[k]]),
                               tmp, B_tmp, rstd, B_rstd, sq, B_sq, psn_i=7)
                    for k in range(KD):
                        eng_ = "act" if k % 2 == 0 else "dve"
                        if eng_ == "act":
                            op("act", lambda e, k=k: e.copy(out=h2b[:, k, :n], in_=h2f[:, k, :n]), R=[B_h2f[k]], W=[B_h2b[k]])
                        else:
                            op("dve", lambda e, k=k: e.tensor_copy(out=h2b[:, k, :n], in_=h2f[:, k, :n]),
                               R=[B_h2f[k]], W=[B_h2b[k]])
                    for tt in range(n // 128):
                        t = s // 128 + tt
                        for k in range(KD):
                            op("pe", lambda e, k=k: e.matmul(psb[6][:, 0:N_EXP], lhsT=h2f[:, k, tt * 128:(tt + 1) * 128],
                                                            rhs=wr_sb[:, k, :], start=(k == 0), stop=(k == KD - 1)),
                               R=[B_h2f[k], B_wrt], W=[B_ps[6]], signal=(k == KD - 1))
                        op("act", lambda e: e.copy(out=aff[:, t, :], in_=psb[6][:, 0:N_EXP]), R=[B_ps[6]], W=[B_aff])
                        pi_ = t % 2
                        pbf = psb[pi_][:].bitcast(BF16)
                        for k in range(KD):
                            op("pe", lambda e, k=k: e.transpose(out=pbf[:, k * 128:(k + 1) * 128],
                                                               in_=h2b[:, k, tt * 128:(tt + 1) * 128], identity=ident_b[:]),
                               R=[B_h2b[k], B_const], W=[B_ps[pi_]], signal=(k == KD - 1))
                        op("act", lambda e: e.copy(out=h2tok[:, t, :], in_=pbf), R=[B_ps[pi_]], W=[B_h2[t]])
                mx = P.sb(r1, "smx", [128, NT], F32)
                op("dve", lambda e: e.tensor_reduce(out=mx[:, :ntile], in_=aff[:, :ntile, :], axis=AX.X, op=ALU.max),
                   R=[B_aff], W=[B_rstd])
                op("dve", lambda e: e.tensor_tensor(out=aff[:, :ntile, :], in0=aff[:, :ntile, :],
                                                    in1=mx[:, :ntile].unsqueeze(2).to_broadcast([128, ntile, N_EXP]),
                                                    op=ALU.subtract), R=[B_rstd, B_aff], W=[B_aff])
                op("act", lambda e: e.activation(out=aff[:, :ntile, :], in_=aff[:, :ntile, :], func=AF.Exp),
                   R=[B_aff], W=[B_aff])
                op("dve", lambda e: e.tensor_reduce(out=mx[:, :ntile], in_=aff[:, :ntile, :], axis=AX.X, op=ALU.add),
                   R=[B_aff], W=[B_rstd])
                op("dve", lambda e: e.reciprocal(out=mx[:, :ntile], in_=mx[:, :ntile]), R=[B_rstd], W=[B_rstd])
                op("dve", lambda e: e.tensor_tensor(out=aff[:, :ntile, :], in0=aff[:, :ntile, :],
                                                    in1=mx[:, :ntile].unsqueeze(2).to_broadcast([128, ntile, N_EXP]),
                                                    op=ALU.mult), R=[B_rstd, B_aff], W=[B_aff])
                op("dve", lambda e: e.tensor_copy(out=aff_hl[:, :ntile, :, 0], in_=aff[:, :ntile, :]), R=[B_aff], W=[B_posm])
                op("dve", lambda e: e.tensor_tensor(out=aff_hl[:, :ntile, :, 1], in0=aff[:, :ntile, :],
                                                    in1=aff_hl[:, :ntile, :, 0], op=ALU.subtract),
                   R=[B_aff, B_posm], W=[B_posm])
                S.barrier()
            debug_dump(f"aff{l}", lambda o: [dma("sp", o, aff[:], R=[B_aff])])
            debug_dump(f"h2tok{l}", lambda o: [dma("sp", o, h2tok[:], R=B_h2)])

            with ExitStack() as r2:
                affT = P.sb(r2, "affT", [16, T_ALL], F32)
                mk = P.sb(r2, "mkT", [16, T_ALL], F32)
                cum = P.sb(r2, "cumT", [16, T_ALL], F32)
                sm = P.sb(r2, "bis", [16, 16], F32)
                B_affT, B_mk, B_cum, B_sm = Buf("affT"), Buf("mk"), Buf("cum"), Buf("sm")
                for t in range(ntile):
                    pi_ = t % 2
                    op("pe", lambda e: e.transpose(out=psb[pi_][0:16, 0:128], in_=aff[:, t, :], identity=ident_f[:]),
                       R=[B_aff, B_const], W=[B_ps[pi_]])
                    op("act", lambda e: e.copy(out=affT[:, t * 128:(t + 1) * 128], in_=psb[pi_][0:16, 0:128]),
                       R=[B_ps[pi_]], W=[B_affT])
                sets = [(0, T_LAT, 256)] + ([(T_LAT, T_CTX, 32)] if with_ctx else [])
                one = sm[:, 15:16]
                op("dve", lambda e: e.memset(one, 1.0), W=[B_sm])
                B_smx = [Buf("smA"), Buf("smB")]

                def bisect(si, s0, ns, cap):
                    lo, mid, gs = [sm[:, si * 6 + i: si * 6 + i + 1] for i in range(3)]
                    cnts = [sm[:, si * 6 + 3 + i: si * 6 + 4 + i] for i in range(2)]
                    Bs = B_smx[si]
                    a_ap = affT[:, s0:s0 + ns]
                    op("dve", lambda e: e.memset(lo, 0.0), W=[Bs])
                    op("dve", lambda e: e.memset(mid, 0.5), W=[Bs])
                    NIT = 30
                    for it in range(NIT):
                        step = 0.5 ** (it + 1)
                        cnt = cnts[it % 2]
                        op("dve", lambda e: e.memset(cnt, 0.0), W=[Bs])
                        yield
                        op("dve", lambda e: e.tensor_scalar(out=mk[:, s0:s0 + ns], in0=a_ap, scalar1=mid, scalar2=0.0,
                                                            op0=ALU.is_ge, op1=ALU.add, accum_out=cnt),
                           R=[B_affT, Bs], W=[B_mk, Bs])
                        yield
                        op("dve", lambda e: e.tensor_scalar(out=gs, in0=cnt, scalar1=float(cap), scalar2=step,
                                                            op0=ALU.is_ge, op1=ALU.mult), R=[Bs], W=[Bs])
                        yield
                        if it < NIT - 1:
                            op("dve", lambda e: e.scalar_tensor_tensor(out=mid, in0=gs, scalar=step * 0.5, in1=lo,
                                                                       op0=ALU.add, op1=ALU.add), R=[Bs], W=[Bs])
                            yield
                        op("dve", lambda e: e.tensor_tensor(out=lo, in0=lo, in1=gs, op=ALU.add), R=[Bs], W=[Bs])
                        yield
                    op("dve", lambda e: e.tensor_scalar(out=mk[:, s0:s0 + ns], in0=a_ap, scalar1=lo, scalar2=None,
                                                        op0=ALU.is_ge), R=[B_affT, Bs], W=[B_mk])
                    yield
                    op("dve", lambda e: e.tensor_tensor_scan(out=cum[:, s0:s0 + ns],
                                                             data0=one.to_broadcast([16, ns]), data1=mk[:, s0:s0 + ns],
                                                             initial=0.0, op0=ALU.mult, op1=ALU.add),
                       R=[B_mk, B_sm], W=[B_cum])
                    yield
                    op("dve", lambda e: e.tensor_tensor(out=cum[:, s0:s0 + ns], in0=cum[:, s0:s0 + ns],
                                                        in1=mk[:, s0:s0 + ns], op=ALU.mult), R=[B_mk, B_cum], W=[B_cum])
                    yield

                interleave(*[bisect(si, s0, ns, cap) for si, (s0, ns, cap) in enumerate(sets)])
                for t in range(ntile):
                    pi_ = t % 2
                    op("pe", lambda e: e.transpose(out=psb[pi_][:, 0:16], in_=cum[:, t * 128:(t + 1) * 128],
                                                   identity=ident_f[0:16, 0:16]),
                       R=[B_cum, B_const], W=[B_ps[pi_]])
                    op("act", lambda e: e.copy(out=posm_tok[:, t, :], in_=psb[pi_][:, 0:16]), R=[B_ps[pi_]], W=[B_posm])
                S.barrier()
            debug_dump(f"posm{l}", lambda o: [dma("sp", o, posm_tok[:], R=[B_posm])])
            if P.stop_after == f"route{l}":
                return

            with ExitStack() as xs:
                Pm = P.sb(xs, "Pm", [128, NT, NS], BF16)
                B_Pm = Buf("Pm")
                PTl = P.sb(xs, "PTl", [128, 2, T_LAT], BF16)
                PTc = P.sb(xs, "PTc", [128, T_CTX], BF16)
                B_PT = Buf("PT")

                def PTv(st, a, b2, sz):
                    return PTl[0:sz, st, a:b2] if st < 2 else PTc[0:sz, a - T_LAT:b2 - T_LAT]
                xeT = P.sb(xs, "xeT", [128, KD, NS], BF16)
                B_xe = [Buf(f"xe{k}") for k in range(KD)]
                hmid = P.sb(xs, "hmid", [128, 2, NS], BF16)
                B_hm = [Buf("hm0"), Buf("hm1")]
                sil = P.sb(xs, "sil", [128, 2, NS], BF16)
                B_sil = [Buf("sil0"), Buf("sil1")]
                y_sb = P.sb(xs, "y_sb", [128, nst, D], BF16)
                B_y = [Buf(f"y{i}") for i in range(nst)]
                gsl = P.sb(xs, "gsl", [128, 4], F32)
                B_gsl = Buf("gsl")
                NGU = 5
                NDN = 5 if with_ctx else 6
                wgu = wring + [P.sb(xs, f"wgu{i}", [128, KD, 512], BF16) for i in range(NGU - NSLOT)]
                B_gu = [Buf(f"gu{i}") for i in range(NGU)]
                wdn = [P.sb(xs, f"wdn{i}", [128, 2, D], BF16) for i in range(NDN)]
                B_dn = [Buf(f"dn{i}") for i in range(NDN)]
                op("dve", lambda e: e.memset(Pm[:], 0.0), W=[B_Pm])

                NFB = NFC // 2
                sched = [(e_, fb) for e_ in range(n_exp_run) for fb in range(NFB)]
                issued = {"n": 0}

                def issue_weights(upto):
                    while issued["n"] < min(upto, len(sched)):
                        i = issued["n"]
                        e_, fb = sched[i]
                        if f"wg_{l}_{e_}" not in P.dram:
                            P.din(f"wg_{l}_{e_}", [D, FF])
                            P.din(f"wu_{l}_{e_}", [D, FF])
                            P.din(f"wd_{l}_{e_}", [FF, D])
                        wg_ap, wu_ap, wd_ap = P.dram[f"wg_{l}_{e_}"], P.dram[f"wu_{l}_{e_}"], P.dram[f"wd_{l}_{e_}"]
                        gi, di = i % NGU, i % NDN
                        dma("pool", wgu[gi][:, :, 0:256],
                            wg_ap.rearrange("(k p) f -> p k f", p=128)[:, :, fb * 256:(fb + 1) * 256], W=[B_gu[gi]])
                        dma("pool", wgu[gi][:, :, 256:512],
                            wu_ap.rearrange("(k p) f -> p k f", p=128)[:, :, fb * 256:(fb + 1) * 256], W=[B_gu[gi]])
                        dma("pool", wdn[di][:],
                            wd_ap.rearrange("(c p) d -> p c d", p=128)[:, fb * 2:(fb + 1) * 2, :], W=[B_dn[di]])
                        issued["n"] += 1

                LA = 4 if with_ctx else 5
                issue_weights(LA)
                blk_i = 0
                for e_ in range(n_exp_run):
                    op("dve", lambda e: e.tensor_tensor(
                        out=Pm[:, 0:16, 0:256], in0=iota1[:, :].unsqueeze(1).to_broadcast([128, 16, 256]),
                        in1=posm_tok[:, 0:16, e_:e_ + 1].to_broadcast([128, 16, 256]), op=ALU.is_equal),
                       R=[B_posm, B_wrt], W=[B_Pm])
                    if with_ctx:
                        op("dve", lambda e: e.tensor_tensor(
                            out=Pm[:, 16:18, 256:288], in0=iota1[:, 0:32].unsqueeze(1).to_broadcast([128, 2, 32]),
                            in1=posm_tok[:, 16:18, e_:e_ + 1].to_broadcast([128, 2, 32]), op=ALU.is_equal),
                           R=[B_posm, B_wrt], W=[B_Pm])
                    for st in range(nst):
                        tiles = range(16) if st < 2 else range(16, 18)
                        tl = list(tiles)
                        for ti, t in enumerate(tl):
                            op("pe", lambda e: e.matmul(psb[6][0:st_sz[st], st * 2:st * 2 + 2],
                                                        lhsT=Pm[:, t, st_off[st]:st_off[st] + st_sz[st]],
                                                        rhs=aff_hl[:, t, e_, :], start=(ti == 0), stop=(ti == len(tl) - 1)),
                               R=[B_Pm, B_posm], W=[B_ps[6]], signal=(ti == len(tl) - 1))
                    for st in range(nst):
                        op("dve", lambda e: e.tensor_reduce(out=gsl[0:st_sz[st], st:st + 1],
                                                            in_=psb[6][0:st_sz[st], st * 2:st * 2 + 2], axis=AX.X, op=ALU.add),
                           R=[B_ps[6]], W=[B_gsl])
                    for st in range(nst):
                        tl = list(range(16)) if st < 2 else [16, 17]
                        for g0 in range(0, len(tl), 8):
                            grp = tl[g0:g0 + 8]
                            pi_ = 7
                            pbf = psb[pi_][:].bitcast(BF16)
                            for gi_, t in enumerate(grp):
                                op("pe", lambda e: e.transpose(
                                    out=pbf[0:st_sz[st], gi_ * 128:(gi_ + 1) * 128],
                                    in_=Pm[:, t, st_off[st]:st_off[st] + st_sz[st]], identity=ident_b[:]),
                                   R=[B_Pm, B_const], W=[B_ps[pi_]], signal=(gi_ == len(grp) - 1))
                            op("act", lambda e: e.copy(out=PTv(st, grp[0] * 128, (grp[-1] + 1) * 128, st_sz[st]),
                                                       in_=pbf[0:st_sz[st], 0:len(grp) * 128]),
                               R=[B_ps[pi_]], W=[B_PT])
                    for k in range(KD):
                        pi_ = 6 + (k % 2)
                        for t in range(ntile):
                            op("pe", lambda e: e.matmul(psb[pi_][:, 0:NS], lhsT=h2tok[:, t, k * 128:(k + 1) * 128],
                                                        rhs=Pm[:, t, :], start=(t == 0), stop=(t == ntile - 1)),
                               R=[B_h2[t], B_Pm], W=[B_ps[pi_]], signal=(t == ntile - 1))
                        op("act", lambda e: e.copy(out=xeT[:, k, :], in_=psb[pi_][:, 0:NS]), R=[B_ps[pi_]], W=[B_xe[k]])
                    psY = [[psb[st * 2 + nh] for nh in range(2)] for st in range(nst)]
                    B_psY = [[B_ps[st * 2 + nh] for nh in range(2)] for st in range(nst)]

                    def emit_down(fc, hb, di, first, last):
                        for st in range(nst):
                            for nh in range(2):
                                op("pe", lambda e: e.matmul(
                                    psY[st][nh][0:st_sz[st], :], lhsT=hmid[:, hb, st_off[st]:st_off[st] + st_sz[st]],
                                    rhs=wdn[di][:, fc % 2, nh * 512:(nh + 1) * 512], start=first, stop=last),
                                   R=[B_hm[hb], B_dn[di]], W=[B_psY[st][nh]], signal=(st == nst - 1 and nh == 1))

                    pend = None
                    for fb in range(NFB):
                        i = blk_i
                        blk_i += 1
                        issue_weights(i + LA)
                        gi, di = i % NGU, i % NDN
                        for f2 in range(2):
                            fc = fb * 2 + f2
                            hb = fc % 2
                            if with_ctx or (fc % 2 == 0):
                                ia, iu = 6, 7
                            else:
                                ia, iu = 4, 5
                            pa, pu = psb[ia], psb[iu]
                            for k in range(KD):
                                op("pe", lambda e: e.matmul(pa[:, 0:NS], lhsT=wgu[gi][:, k, f2 * 128:(f2 + 1) * 128],
                                                            rhs=xeT[:, k, :], start=(k == 0), stop=(k == KD - 1)),
                                   R=[B_gu[gi], B_xe[k]], W=[B_ps[ia]], signal=(k == KD - 1))
                            for k in range(KD):
                                op("pe", lambda e: e.matmul(pu[:, 0:NS],
                                                            lhsT=wgu[gi][:, k, 256 + f2 * 128:256 + (f2 + 1) * 128],
                                                            rhs=xeT[:, k, :], start=(k == 0), stop=(k == KD - 1)),
                                   R=[B_gu[gi], B_xe[k]], W=[B_ps[iu]], signal=(k == KD - 1))
                            op("act", lambda e: e.activation(out=sil[:, hb, :], in_=pa[:, 0:NS], func=AF.Silu),
                               R=[B_ps[ia]], W=[B_sil[hb]])
                            op("dve", lambda e: e.tensor_tensor(out=hmid[:, hb, :], in0=pu[:, 0:NS], in1=sil[:, hb, :],
                                                                op=ALU.mult), R=[B_ps[iu], B_sil[hb]], W=[B_hm[hb]])
                            if pend is not None:
                                emit_down(*pend)
                            pend = (fc, hb, di, fc == 0, fc == NFC - 1)
                    emit_down(*pend)
                    for st in range(nst):
                        for nh in range(2):
                            op("act", lambda e: e.activation(out=y_sb[0:st_sz[st], st, nh * 512:(nh + 1) * 512],
                                                             in_=psY[st][nh][0:st_sz[st], :], func=AF.Copy,
                                                             scale=gsl[0:st_sz[st], st:st + 1]),
                               R=[B_psY[st][nh], B_gsl], W=[B_y[st]])
                    gi_s = 0
                    for b in range(nblk):
                        s, n, isc = BLOCKS[b]
                        c = 1 if isc else 0
                        sts = [2] if isc else [0, 1]
                        for mch in range(KD):
                            pi_ = 6 + (gi_s % 2)
                            gi_s += 1
                            for si, st in enumerate(sts):
                                op("pe", lambda e: e.matmul(psb[pi_][:, :n], lhsT=y_sb[0:st_sz[st], st, mch * 128:(mch + 1) * 128],
                                                            rhs=PTv(st, s, s + n, st_sz[st]), start=(si == 0),
                                                            stop=(si == len(sts) - 1)),
                                   R=[B_y[st], B_PT], W=[B_ps[pi_]], signal=(si == len(sts) - 1))
                            g_ap = modv(l, 5, mch, c)
                            op("dve", lambda e: e.scalar_tensor_tensor(
                                out=xT[:, mch, s:s + n], in0=psb[pi_][:, :n], scalar=g_ap, in1=xT[:, mch, s:s + n],
                                op0=ALU.mult, op1=ALU.add),
                               R=[B_ps[pi_], B_mod, B_x[mch][b]], W=[B_x[mch][b]])
                S.barrier()

    def hgrn2_layer(l):
        hgin_d = P.din("hg_w_in", [D, 5120])
        hgout_d = P.din("hg_w_out", [D, D])
        hgmisc_d = P.din("hg_misc", [128, 1 + 4 * KD])
        hgmask_d = P.din("hgmask", [128, 256])
        PT_ = 384
        NPART = 6
        NCH = 3
        with ExitStack() as les:
            hT = P.sb(les, "hT1", [128, KD, T_ALL], BF16)
            B_h = [[Buf(f"h1{k}_{b}") for b in range(5)] for k in range(KD)]
            B_hT = Buf("hT1all")
            hgm = P.sb(les, "hgm", [128, 1 + 4 * KD], F32)
            msk = P.sb(les, "hgmask_sb", [128, 256], F32)
            lbs = P.sb(les, "lbs", [128, 2, 2, HG_H], F32)
            B_hc = Buf("hgconst")
            dma("sp", hgm[:], hgmisc_d, W=[B_hc])
            dma("sp", msk[:], hgmask_d, W=[B_hc])
            AA = P.sb(les, "hgAA", [128, 10 * PT_], F32)
            Aset = [[AA[:, (st_ * 5 + i) * PT_:(st_ * 5 + i + 1) * PT_] for i in range(5)] for st_ in range(2)]
            B_Aset = [[Buf(f"hgA{st_}{i}") for i in range(5)] for st_ in range(2)]
            tmp = [AA[:, 0:512], AA[:, 768:1280]]
            B_tmp = [[B_Aset[0][0], B_Aset[0][1]], [B_Aset[0][2], B_Aset[0][3]]]
            rstds = [AA[:, 1536:2048], AA[:, 2304:2816]]
            B_rstds = [[B_Aset[0][4], B_Aset[1][0]], [B_Aset[1][1], B_Aset[1][2]]]
            rstd, B_rstd = rstds[0], B_rstds[0]
            sq = [P.sb(les, f"hsq{i}", [128, 512], BF16) for i in range(2)]
            B_sq = [Buf("hsq0"), Buf("hsq1")]

            class _V:
                def __init__(self, ap):
                    self.ap = ap

                def __getitem__(self, key):
                    return self.ap[key]
            for b in range(5):
                s, n, _ = BLOCKS[b]
                norm_block(l, 0, b, lambda k, b=b, s=s, n=n: (hT[:, k, s:s + n], [B_h[k][b]]),
                           [_V(tmp[0]), _V(tmp[1])], B_tmp, _V(rstd), B_rstd, sq, B_sq, psn_i=7)
            for d_ in range(2):
                op("dve", lambda e: e.tensor_tensor(out=lbs[:, 0, d_, :], in0=hgm[:, 1 + (2 + d_) * 8:1 + (3 + d_) * 8],
                                                    in1=hgm[:, 1 + d_ * 8:1 + (d_ + 1) * 8], op=ALU.subtract),
                   R=[B_hc], W=[B_hc])
            op("act", lambda e: e.activation(out=lbs[:, 0, :, :], in_=lbs[:, 0, :, :], func=AF.Sigmoid), R=[B_hc], W=[B_hc])
            op("dve", lambda e: e.tensor_scalar(out=lbs[:, 1, :, :], in0=lbs[:, 0, :, :], scalar1=-1.0, scalar2=1.0,
                                                op0=ALU.mult, op1=ALU.add), R=[B_hc], W=[B_hc])
            S.barrier()
            debug_dump("hT1", lambda o: [dma("sp", o.rearrange("(k p) t -> p k t", p=128)[:, k, :], hT[:, k, :],
                                             R=[B_hT]) for k in range(KD)])

            qs2 = P.sb(les, "hqs", [128, 2, PT_], BF16)
            B_qs2 = [Buf("hqs0"), Buf("hqs1")]
            arr = [[P.sb(les, f"harr{d_}{i}", [128, T_ALL], BF16) for i in range(3)] for d_ in range(2)]
            B_arr = [[Buf(f"harr{d_}{i}") for i in range(3)] for d_ in range(2)]
            sgT, B_sg = arr[0][2], B_arr[0][2]
            ogT, B_og = arr[1][2], B_arr[1][2]
            B_ogb = [[B_og, Buf(f"ogb{b}")] for b in range(4)]
            vv = P.sb(les, "hvv", [128, NT, 128], BF16)
            B_v = Buf("hvv")
            EF = P.sb(les, "hEF", [128, 2, NT], F32)
            EM = P.sb(les, "hEM", [128, 2, NT], F32)
            t6b = P.sb(les, "ht6", [128, 2, 8], F32)
            B_t6 = [Buf("t6a"), Buf("t6b")]
            B_E = Buf("hE")
            Sst = P.sb(les, "hS", [128, 2, 128], F32)
            B_S = [Buf("hS0"), Buf("hS1")]
            Suse = P.sb(les, "hSuse", [128, 2, 16, 128], BF16)
            B_Su = Buf("hSuse")
            kht = [P.sb(les, f"hkht{i}", [128, 128], BF16) for i in range(2)]
            B_kht = [Buf("kht0"), Buf("kht1")]
            ATs = [P.sb(les, f"hAT{i}", [128, 128], BF16) for i in range(8)]
            B_ATs = [Buf(f"hAT{i}") for i in range(8)]
            one_col = hgm[:, 0:1]
            ones_f = P.sb(les, "hones", [128, 1], F32)
            op("dve", lambda e: e.memset(ones_f[:], 1.0), W=[B_hc])

            win_src = hgin_d.rearrange("(k p) f -> p k f", p=128)
            cnt_ = {"ps": 0, "kht": 0, "at": 0}

            def view3(ap):
                return ap.rearrange("p (c i) -> p c i", i=128)

            for h in range(HG_H):
                sX, sY = wslot(), wslot()
                for gi_, off in enumerate((0, 1024, 2048, 3072)):
                    dma("pool", wring[sX][:, :, gi_ * 128:(gi_ + 1) * 128], win_src[:, :, off + h * 128: off + (h + 1) * 128],
                        W=[B_wr[sX]])
                yflat = wring[sY][:].rearrange("p k f -> p (k f)")
                wG = yflat[:, 0:1024].rearrange("p (k f) -> p k f", k=KD)
                wO = yflat[:, 1024:2048]
                dma("pool", wG, win_src[:, :, 4096 + h * 128:4096 + (h + 1) * 128], W=[B_wr[sY]])
                dma("pool", wO, hgout_d[h * 128:(h + 1) * 128, :], W=[B_wr[sY]])

                def proj(gi_, s, n, pi_):
                    for k in range(KD):
                        op("pe", lambda e: e.matmul(psb[pi_][:, :n], lhsT=wring[sX][:, k, gi_ * 128:(gi_ + 1) * 128],
                                                    rhs=hT[:, k, s:s + n], start=(k == 0), stop=(k == KD - 1)),
                           R=[B_wr[sX], B_hT], W=[B_ps[pi_]], signal=(k == KD - 1))

                for part in range(NPART):
                    p0 = part * PT_
                    c0 = part * NCH
                    qi = part % 2
                    qs = qs2[:, qi, :]
                    B_qs = B_qs2[qi]
                    pi_ = cnt_["ps"] % 2
                    cnt_["ps"] += 1
                    proj(0, p0, PT_, pi_)
                    op("act", lambda e: e.activation(out=qs, in_=psb[pi_][:, :PT_], func=AF.Silu), R=[B_ps[pi_]], W=[B_qs])
                    for tt in range(NCH):
                        t = c0 + tt
                        pi_ = cnt_["ps"] % 2
                        cnt_["ps"] += 1
                        for k in range(KD):
                            op("pe", lambda e: e.matmul(psb[pi_][:, 0:128], lhsT=hT[:, k, t * 128:(t + 1) * 128],
                                                        rhs=wring[sX][:, k, 384:512], start=(k == 0), stop=(k == KD - 1)),
                               R=[B_wr[sX], B_hT], W=[B_ps[pi_]], signal=(k == KD - 1))
                        op("act", lambda e: e.copy(out=vv[:, t, :], in_=psb[pi_][:, 0:128]), R=[B_ps[pi_]], W=[B_v])
                    def chain(d_):
                        si_ = d_
                        A1, A2, A3, A4, A5 = Aset[si_]
                        BA = B_Aset[si_]
                        t6 = t6b[:, si_, :]
                        B_T = B_t6[si_]
                        pi_ = cnt_["ps"] % 2
                        cnt_["ps"] += 1
                        proj(1 + d_, p0, PT_, pi_)
                        oml_ap = lbs[:, 1, d_, h:h + 1]
                        op("act", lambda e: e.activation(out=A2, in_=psb[pi_][:, :PT_], func=AF.Sigmoid),
                           R=[B_ps[pi_]], W=[BA[1]])
                        yield
                        op("act", lambda e: e.activation(out=A1, in_=psb[pi_][:, :PT_], func=AF.Sigmoid, scale=-1.0),
                           R=[B_ps[pi_]], W=[BA[0]])
                        yield
                        op("act", lambda e: e.activation(out=A2, in_=A2, func=AF.Ln, bias=lbs[:, 0, d_, h:h + 1],
                                                         scale=oml_ap), R=[BA[1], B_hc], W=[BA[1]])
                        yield
                        op("dve", lambda e: e.tensor_tensor_scan(out=A3, data0=ones_f[:, 0:1].to_broadcast([128, PT_]),
                                                                 data1=A2, initial=0.0, op0=ALU.mult, op1=ALU.add),
                           R=[BA[1], B_hc], W=[BA[2]])
                        G3, g3 = view3(A3), view3(A2)
                        dst = [arr[d_][i][:, p0:p0 + PT_] for i in range(3)]
                        Bd = B_arr[d_]
                        bc = [128, NCH, 128]
                        if d_ == 0:
                            yield
                            op("dve", lambda e: e.tensor_tensor(out=view3(A4), in0=G3, in1=G3[:, :, 63:64].to_broadcast(bc),
                                                                op=ALU.subtract), R=[BA[2]], W=[BA[3]])
                            yield
                            op("act", lambda e: e.activation(out=A5, in_=A4, func=AF.Exp), R=[BA[3]], W=[BA[4]])
                            yield
                            op("pool", lambda e: e.tensor_tensor(out=dst[0], in0=qs, in1=A5, op=ALU.mult),
                               R=[B_qs, BA[4]], W=[Bd[0]])
                            yield
                            op("dve", lambda e: e.tensor_tensor(out=t6[:, 0:NCH], in0=G3[:, :, 0], in1=g3[:, :, 0], op=ALU.subtract),
                               R=[BA[2], BA[1]], W=[B_T])
                            yield
                            op("dve", lambda e: e.tensor_tensor(out=EF[:, 0, c0:c0 + NCH], in0=G3[:, :, 127], in1=t6[:, 0:NCH],
                                                                op=ALU.subtract), R=[BA[2], B_T], W=[B_E])
                            yield
                            op("dve", lambda e: e.tensor_tensor(out=EM[:, 0, c0:c0 + NCH], in0=G3[:, :, 63], in1=t6[:, 0:NCH],
                                                                op=ALU.subtract), R=[BA[2], B_T], W=[B_E])
                            yield
                            op("act", lambda e: e.activation(out=A2, in_=A4, func=AF.Exp, scale=-1.0), R=[BA[3]], W=[BA[1]])
                            yield
                            op("dve", lambda e: e.scalar_tensor_tensor(out=dst[1], in0=A1, scalar=oml_ap, in1=A2, op0=ALU.mult, op1=ALU.mult),
                               R=[BA[0], BA[1]], W=[Bd[1]])
                            yield
                            op("dve", lambda e: e.tensor_tensor(out=view3(A4), in0=G3, in1=G3[:, :, 127:128].to_broadcast(bc),
                                                                op=ALU.subtract), R=[BA[2]], W=[BA[3]])
                            yield
                            op("act", lambda e: e.activation(out=A5, in_=A4, func=AF.Exp, scale=-1.0), R=[BA[3]], W=[BA[4]])
                            yield
                            op("dve", lambda e: e.scalar_tensor_tensor(out=dst[2], in0=A1, scalar=oml_ap, in1=A5, op0=ALU.mult, op1=ALU.mult),
                               R=[BA[0], BA[4]], W=[Bd[2]])
                        else:
                            yield
                            op("dve", lambda e: e.tensor_copy(out=t6[:, 0:NCH], in_=G3[:, :, 127]), R=[BA[2]], W=[B_T])
                            yield
                            op("pool", lambda e: e.tensor_tensor(out=A2, in0=A3, in1=A2, op=ALU.subtract),
                               R=[BA[2], BA[1]], W=[BA[1]])
                            H3 = view3(A2)
                            yield
                            op("dve", lambda e: e.tensor_tensor(out=view3(A4), in0=H3, in1=H3[:, :, 64:65].to_broadcast(bc),
                                                                op=ALU.subtract), R=[BA[1]], W=[BA[3]])
                            yield
                            op("act", lambda e: e.activation(out=A5, in_=A4, func=AF.Exp, scale=-1.0), R=[BA[3]], W=[BA[4]])
                            yield
                            op("pool", lambda e: e.tensor_tensor(out=dst[0], in0=qs, in1=A5, op=ALU.mult),
                               R=[B_qs, BA[4]], W=[Bd[0]])
                            yield
                            op("act", lambda e: e.activation(out=A3, in_=A4, func=AF.Exp), R=[BA[3]], W=[BA[2]])
                            yield
                            op("dve", lambda e: e.scalar_tensor_tensor(out=dst[1], in0=A1, scalar=oml_ap, in1=A3, op0=ALU.mult, op1=ALU.mult),
                               R=[BA[0], BA[2]], W=[Bd[1]])
                            yield
                            op("dve", lambda e: e.tensor_tensor(out=EF[:, 1, c0:c0 + NCH], in0=t6[:, 0:NCH], in1=H3[:, :, 0],
                                                                op=ALU.subtract), R=[BA[1], B_T], W=[B_E])
                            yield
                            op("dve", lambda e: e.tensor_tensor(out=EM[:, 1, c0:c0 + NCH], in0=t6[:, 0:NCH], in1=H3[:, :, 64],
                                                                op=ALU.subtract), R=[BA[1], B_T], W=[B_E])
                            yield
                            op("dve", lambda e: e.tensor_tensor(out=view3(A4), in0=H3, in1=H3[:, :, 0:1].to_broadcast(bc),
                                                                op=ALU.subtract), R=[BA[1]], W=[BA[3]])
                            yield
                            op("act", lambda e: e.activation(out=A5, in_=A4, func=AF.Exp), R=[BA[3]], W=[BA[4]])
                            yield
                            op("dve", lambda e: e.scalar_tensor_tensor(out=dst[2], in0=A1, scalar=oml_ap, in1=A5, op0=ALU.mult, op1=ALU.mult),
                               R=[BA[0], BA[4]], W=[Bd[2]])
                        yield
                    interleave(chain(0), chain(1))
                op("act", lambda e: e.activation(out=EF[:], in_=EF[:], func=AF.Exp), R=[B_E], W=[B_E])
                op("act", lambda e: e.activation(out=EM[:], in_=EM[:], func=AF.Exp), R=[B_E], W=[B_E])

                orders = [[16, 17] + list(range(16)), [17, 16] + list(range(15, -1, -1))]
                for d_ in range(2):
                    op("dve", lambda e: e.memset(Sst[:, d_, :], 0.0), W=[B_S[d_]])
                items = [(step, d_) for step in range(NT - 1) for d_ in range(2)]

                def emit_T(i):
                    step, d_ = items[i]
                    c = orders[d_][step]
                    ki = i % 2
                    pq = 4 + (i % 2)
                    pbf = psb[pq][:].bitcast(BF16)
                    op("pe", lambda e: e.transpose(out=pbf[:, 0:128], in_=arr[d_][2][:, c * 128:(c + 1) * 128],
                                                   identity=ident_b[:]), R=[B_arr[d_][2], B_const], W=[B_ps[pq]])
                    op("act", lambda e: e.copy(out=kht[ki][:], in_=pbf[:, 0:128]), R=[B_ps[pq]], W=[B_kht[ki]])

                def emit_suse(step, d_):
                    c = orders[d_][step]
                    if c < 16:
                        op("act", lambda e: e.activation(out=Suse[:, d_, c, :], in_=Sst[:, d_, :], func=AF.Copy,
                                                         scale=EM[:, d_, c:c + 1]), R=[B_S[d_], B_E], W=[B_Su])

                emit_T(0)
                for i, (step, d_) in enumerate(items):
                    c = orders[d_][step]
                    if i + 1 < len(items):
                        emit_T(i + 1)
                    emit_suse(step, d_)
                    ki = i % 2
                    pd = 2 + (i % 2)
                    op("pe", lambda e: e.matmul(psb[pd][:, 0:128], lhsT=kht[ki][:], rhs=vv[:, c, :], start=True, stop=True),
                       R=[B_kht[ki], B_v], W=[B_ps[pd]])
                    op("dve", lambda e: e.scalar_tensor_tensor(out=Sst[:, d_, :], in0=Sst[:, d_, :], scalar=EF[:, d_, c:c + 1],
                                                               in1=psb[pd][:, 0:128], op0=ALU.mult, op1=ALU.add),
                       R=[B_S[d_], B_E, B_ps[pd]], W=[B_S[d_]])
                for d_ in range(2):
                    emit_suse(NT - 1, d_)

                for b in range(4):
                    s, n, _ = BLOCKS[b]
                    pi_ = b % 2
                    for k in range(KD):
                        op("pe", lambda e: e.matmul(psb[pi_][:, :n], lhsT=wG[:, k, :], rhs=hT[:, k, s:s + n],
                                                    start=(k == 0), stop=(k == KD - 1)),
                           R=[B_wr[sY], B_hT], W=[B_ps[pi_]], signal=(k == KD - 1))
                    op("act", lambda e: e.activation(out=sgT[:, s:s + n], in_=psb[pi_][:, :n], func=AF.Silu),
                       R=[B_ps[pi_]], W=[B_sg])

                def out_block(b):
                    s, n, _ = BLOCKS[b]
                    bp = b % 2
                    iO = 6 if bp == 0 else 4
                    pO = psb[iO]

                    def scores(cc):
                        c = b * 4 + cc
                        cs = slice(c * 128, (c + 1) * 128)
                        ais = []
                        for d_ in range(2):
                            pa = 2 + (cnt_["at"] % 2)
                            ai = cnt_["at"] % 8
                            cnt_["at"] += 1
                            op("pe", lambda e: e.matmul(psb[pa][:, 0:128], lhsT=arr[d_][1][:, cs], rhs=arr[d_][0][:, cs],
                                                        start=True, stop=True),
                               R=[B_arr[d_][1], B_arr[d_][0]], W=[B_ps[pa]])
                            op("dve", lambda e: e.tensor_tensor(out=ATs[ai][:], in0=psb[pa][:, 0:128],
                                                                in1=msk[:, d_ * 128:(d_ + 1) * 128], op=ALU.mult),
                               R=[B_ps[pa], B_hc], W=[B_ATs[ai]])
                            ais.append(ai)
                        return ais

                    def outs(cc, ais):
                        c = b * 4 + cc
                        cs = slice(c * 128, (c + 1) * 128)
                        oc = pO[:, cc * 128:(cc + 1) * 128]
                        op("pe", lambda e: e.matmul(oc, lhsT=Suse[:, 0, c, :], rhs=arr[0][0][:, cs], start=True, stop=False),
                           R=[B_Su, B_arr[0][0]], W=[B_ps[iO]], signal=False)
                        op("pe", lambda e: e.matmul(oc, lhsT=Suse[:, 1, c, :], rhs=arr[1][0][:, cs], start=False, stop=False),
                           R=[B_Su, B_arr[1][0]], W=[B_ps[iO]], signal=False)
                        op("pe", lambda e: e.matmul(oc, lhsT=vv[:, c, :], rhs=ATs[ais[0]][:], start=False, stop=False),
                           R=[B_v, B_ATs[ais[0]]], W=[B_ps[iO]], signal=False)
                        op("pe", lambda e: e.matmul(oc, lhsT=vv[:, c, :], rhs=ATs[ais[1]][:], start=False, stop=True),
                           R=[B_v, B_ATs[ais[1]]], W=[B_ps[iO]], signal=True)

                    pend = None
                    for cc in range(4):
                        ais = scores(cc)
                        if pend is not None:
                            outs(*pend)
                        pend = (cc, ais)
                    outs(*pend)

                def post_block(b):
                    s, n, _ = BLOCKS[b]
                    bp = b % 2
                    iO, iN = (6, 7) if bp == 0 else (4, 5)
                    pO = psb[iO]
                    rstd, B_rstd = rstds[bp], B_rstds[bp]
                    op("act", lambda e: e.activation(out=sq[bp][:, :n], in_=pO[:, :n], func=AF.Square), R=[B_ps[iO]], W=[B_sq[bp]])
                    op("pe", lambda e: e.matmul(psb[iN][:, :n], lhsT=ones_b[:], rhs=sq[bp][:, :n], start=True, stop=True),
                       R=[B_sq[bp], B_const], W=[B_ps[iN]])
                    op("act", lambda e: e.activation(out=rstd[:, :n], in_=psb[iN][:, :n], func=AF.Sqrt, bias=EPS,
                                                     scale=1.0 / 128), R=[B_ps[iN]], W=[B_rstd])
                    op("dve", lambda e: e.reciprocal(out=rstd[:, :n], in_=rstd[:, :n]), R=[B_rstd], W=[B_rstd])
                    op("dve", lambda e: e.scalar_tensor_tensor(out=tmp[bp][:, :n], in0=pO[:, :n], scalar=hgm[:, 0:1],
                                                               in1=rstd[:, :n], op0=ALU.mult, op1=ALU.mult),
                       R=[B_ps[iO], B_rstd, B_hc], W=[B_tmp[bp]])
                    op("dve", lambda e: e.tensor_tensor(out=ogT[:, s:s + n], in0=tmp[bp][:, :n], in1=sgT[:, s:s + n],
                                                         op=ALU.mult), R=[B_tmp[bp], B_sg], W=[B_ogb[b][1]])

                def proj_block(b):
                    s, n, _ = BLOCKS[b]
                    for mch in range(KD):
                        pi_ = mch % 2
                        op("pe", lambda e: e.matmul(psb[pi_][:, :n], lhsT=wO[:, mch * 128:(mch + 1) * 128],
                                                    rhs=ogT[:, s:s + n], start=True, stop=True),
                           R=[B_wr[sY], B_ogb[b]], W=[B_ps[pi_]])
                        g_ap = modv(l, 2, mch, 0)
                        op("dve", lambda e: e.scalar_tensor_tensor(
                            out=xT[:, mch, s:s + n], in0=psb[pi_][:, :n], scalar=g_ap, in1=xT[:, mch, s:s + n],
                            op0=ALU.mult, op1=ALU.add),
                           R=[B_ps[pi_], B_mod, B_x[mch][b]], W=[B_x[mch][b]])

                out_block(0)
                out_block(1)
                post_block(0)
                out_block(2)
                proj_block(0)
                post_block(1)
                out_block(3)
                proj_block(1)
                post_block(2)
                post_block(3)
                proj_block(2)
                proj_block(3)
            S.barrier()

    if "ret" in P.parts:
        retention_layer(0)
    debug_dump("x_mix0", lambda o: [dma("sp", o.rearrange("(k p) t -> p k t", p=128)[:, k, :], xT[:, k, :],
                                        R=B_x[k]) for k in range(KD)])

    if "moe0" in P.parts:
        moe_layer(0, True, P.n_exp_run)
    debug_dump("x_ffn0", lambda o: [dma("sp", o.rearrange("(k p) t -> p k t", p=128)[:, k, :], xT[:, k, :],
                                        R=B_x[k]) for k in range(KD)])
    if "hg" in P.parts:
        hgrn2_layer(1)
    debug_dump("x_mix1", lambda o: [dma("sp", o.rearrange("(k p) t -> p k t", p=128)[:, k, :], xT[:, k, 0:T_LAT],
                                        R=B_x[k][:4]) for k in range(KD)])
    if "moe1" in P.parts:
        moe_layer(1, False, P.n_exp_run)
    debug_dump("x_ffn1", lambda o: [dma("sp", o.rearrange("(k p) t -> p k t", p=128)[:, k, :], xT[:, k, 0:T_LAT],
                                        R=B_x[k][:4]) for k in range(KD)])
    if "final" in P.parts:
        with ExitStack() as fes:
            ftmp = [P.sb(fes, f"ftmp{i}", [128, 512], F32) for i in range(2)]
            B_ft = [Buf("ft0"), Buf("ft1")]
            frs = P.sb(fes, "frs", [128, 512], F32)
            B_frs = Buf("frs")
            fsq = [P.sb(fes, f"fsq{i}", [128, 512], BF16) for i in range(2)]
            B_fsq = [Buf("fsq0"), Buf("fsq1")]
            fo = [P.sb(fes, f"fo{i}", [128, 512], F32) for i in range(4)]
            B_fo = [Buf(f"fo{i}") for i in range(4)]
            osrc = out_d.rearrange("(k p) t -> p k t", p=128)
            oc_ = {"n": 0}
            for b in range(4):
                s, n, _ = BLOCKS[b]

                def dst(k):
                    i = oc_["n"] % 4
                    oc_["n"] += 1
                    dst.last = i
                    return fo[i][:, :n], [B_fo[i]]
                norm_block(1, 0, b, dst, ftmp, B_ft, frs, B_frs, fsq, B_fsq, psn_i=7, final=True,
                           after=lambda k, b=b, s=s, n=n: dma("sp", osrc[:, k, s:s + n], fo[dst.last][:, :n], R=[B_fo[dst.last]]))
    if P.stop_after is not None:
        osrc = out_d.rearrange("(k p) t -> p k t", p=128)
        for k in range(KD):
            dma("sp", osrc[:, k, :], xT[:, k, 0:T_LAT], R=B_x[k][:4])
    S.barrier()
    es.close()
    return P


def _prep_inputs(inp, b, consts, names=None):
    f = np.float32
    m = {}
    m["xT"] = np.ascontiguousarray(np.concatenate([inp["x"][b].T, inp["ctx"][b].T], axis=1)).astype(f)
    cv = np.stack([inp["c"][b], inp["c_ctx"]], axis=0)
    m["cvec"] = np.ascontiguousarray(cv.reshape(2, KD, 128).transpose(2, 0, 1).reshape(128, 2 * KD))
    m["w_ada"] = inp["w_ada"]
    m["b_ada"] = np.ascontiguousarray(np.tile(inp["b_ada"].reshape(1, 12 * D), (2, 1)))
    nr = np.stack([inp["norm_mix"][0], inp["norm_mix"][1], inp["norm_ffn"][0], inp["norm_ffn"][1],
                   inp["norm_final"]], axis=0)
    m["norms"] = np.ascontiguousarray(nr.reshape(5, KD, 128).transpose(2, 0, 1).reshape(128, 5 * KD))
    m["ret_w_in"] = inp["ret_w_in"][0]
    m["ret_w_out"] = inp["ret_w_out"][0]
    m["hg_w_in"] = inp["hg_w_in"][0]
    m["hg_w_out"] = inp["hg_w_out"][0]
    lb = inp["hg_lower_bounds"].reshape(4, KD, 128).transpose(2, 0, 1).reshape(128, 4 * KD)
    m["hg_misc"] = np.ascontiguousarray(np.concatenate([inp["hg_g_norm"][0].reshape(128, 1), lb], axis=1)).astype(f)
    m["moe_router"] = inp["moe_router"]
    m.update(consts)
    for l in range(2):
        for e in range(N_EXP):
            m[f"wg_{l}_{e}"] = inp["moe_w_gate"][l, e]
            m[f"wu_{l}_{e}"] = inp["moe_w_up"][l, e]
            m[f"wd_{l}_{e}"] = inp["moe_w_down"][l, e]
    if names is not None:
        m = {k: v for k, v in m.items() if k in names}
    return m


def kernel(**inputs):
    inp = {k: np.asarray(v) for k, v in inputs.items()}
    consts = _const_tables()
    P = build_program()
    names = set(P.dram.keys())
    in_maps = [_prep_inputs(inp, b, consts, names) for b in range(8)]
    res = run_bass_kernel_spmd(P.nc, in_maps, core_ids=list(range(8)))
    out = np.stack([np.ascontiguousarray(res.results[b]["outT"].T) for b in range(8)], axis=0)
    return out.astype(np.float32)
```

```python
import math
from contextlib import ExitStack

import numpy as np
import concourse.bass as bass
import concourse.mybir as mybir
from concourse.bass_utils import run_bass_kernel_spmd

F32 = mybir.dt.float32
BF16 = mybir.dt.bfloat16
ALU = mybir.AluOpType
AF = mybir.ActivationFunctionType
AX = mybir.AxisListType

D = 1024
KD = 8
T_LAT = 2048
T_CTX = 256
T_ALL = T_LAT + T_CTX
NT = T_ALL // 128
EPS = 1e-6
N_EXP = 16
FF = 2816
NFC = FF // 128
RET_H = 4
HG_H = 8

BLOCKS = [(0, 512, False), (512, 512, False), (1024, 512, False), (1536, 512, False), (2048, 256, True)]


def interleave(*gens):
    gens = list(gens)
    while gens:
        for g in list(gens):
            try:
                next(g)
            except StopIteration:
                gens.remove(g)


class Buf:
    __slots__ = ("name", "w", "rs")

    def __init__(self, name):
        self.name = name
        self.w = None
        self.rs = {}


class Sched:
    NDMA = 12

    def __init__(self, nc, es):
        self.nc = nc
        self.h = {"pe": nc.tensor, "act": nc.scalar, "dve": nc.vector, "pool": nc.gpsimd, "sp": nc.sync}
        self.sem = {}
        self.cnt = {}
        self.seen = {}
        for e in self.h:
            self.sem[e] = es.enter_context(nc.semaphore("s_" + e))
            self.cnt[e] = 0
            self.seen[e] = {}
        self.dsem = {}
        self.dval = {}
        self.dnext = {}
        for q in ("sp", "pool"):
            self.dsem[q] = [es.enter_context(nc.semaphore(f"d_{q}{i}")) for i in range(self.NDMA)]
            self.dval[q] = [0] * self.NDMA
            self.dnext[q] = 0
        self.ninst = 0

    def _wait(self, eng, tk):
        if tk is None:
            return
        kind = tk[0]
        if kind == "c":
            _, src, n = tk
            if src == eng and eng in ("pe", "sp"):
                return
            if self.seen[eng].get(src, 0) >= n:
                return
            self.h[eng].wait_ge(self.sem[src], n)
            self.seen[eng][src] = n
        else:
            _, q, idx, val = tk
            key = (q, idx)
            if self.seen[eng].get(key, 0) >= val:
                return
            self.h[eng].wait_ge(self.dsem[q][idx], val)
            self.seen[eng][key] = val
        self.ninst += 1

    def _deps(self, eng, R, W):
        for b in R:
            self._wait(eng, b.w)
        for b in W:
            self._wait(eng, b.w)
            for tk in b.rs.values():
                self._wait(eng, tk)

    def _record(self, tk, R, W):
        for b in W:
            b.w = tk
            b.rs = {}
        for b in R:
            if b in W:
                continue
            key = tk[1] if tk[0] == "c" else (tk[1], tk[2])
            b.rs[key] = tk

    @staticmethod
    def _flat(L):
        out = []
        for b in L:
            if isinstance(b, (list, tuple)):
                out.extend(Sched._flat(b))
            else:
                out.append(b)
        return out

    def op(self, eng, fn, R=(), W=(), signal=True):
        R, W = self._flat(R), self._flat(W)
        self._deps(eng, R, W)
        ins = fn(self.h[eng])
        self.ninst += 1
        if signal:
            ins.then_inc(self.sem[eng], 1)
            self.cnt[eng] += 1
            tk = ("c", eng, self.cnt[eng])
            if eng not in ("pe",):
                pass
        else:
            tk = ("c", eng, self.cnt[eng] + 1)
        self._record(tk, R, W)
        return tk

    def dma(self, q, out, in_, R=(), W=()):
        R, W = self._flat(R), self._flat(W)
        self._deps(q, R, W)
        idx = self.dnext[q]
        self.dnext[q] = (idx + 1) % self.NDMA
        prev = self.dval[q][idx]
        if prev > 0:
            self._wait(q, ("d", q, idx, prev))
        val = prev + 16
        self.dval[q][idx] = val
        self.h[q].dma_start(out=out, in_=in_).then_inc(self.dsem[q][idx], 16)
        self.ninst += 1
        tk = ("d", q, idx, val)
        self._record(tk, R, W)
        return tk

    def barrier(self):
        for e in self.h:
            for src in self.h:
                if src != e and self.cnt[src] > 0:
                    self._wait(e, ("c", src, self.cnt[src]))
            for q in ("sp", "pool"):
                for idx in range(self.NDMA):
                    if self.dval[q][idx] > 0:
                        self._wait(e, ("d", q, idx, self.dval[q][idx]))


def _ret_gammas():
    j = np.arange(8, dtype=np.float64)
    g = 1.0 - np.exp2(-5.0 - j / 2)
    return g[0::2], g[1::2]


def _const_tables():
    t = {}
    half = 128
    inv = 10000.0 ** (-np.arange(0, half, 2, dtype=np.float64) / half)
    p = np.arange(128)
    sign = np.where(p < 64, -1.0, 1.0)
    rows = np.arange(T_LAT // 64, dtype=np.float64)
    cols = np.arange(64, dtype=np.float64)
    ang_r = rows[None, :] * inv[p % 64][:, None]
    ang_c = cols[None, :] * inv[p % 64][:, None]
    t["rope"] = np.concatenate(
        [np.cos(ang_r), np.sin(ang_r) * sign[:, None], np.cos(ang_c), np.sin(ang_c) * sign[:, None]], axis=1
    ).astype(np.float32)
    gf, gb = _ret_gammas()
    b = np.arange(128, dtype=np.float64)[:, None]
    a = np.arange(512, dtype=np.float64)[None, :]
    tabs = np.zeros((RET_H, 128, 1920), dtype=np.float64)
    xs = np.arange(896, dtype=np.float64)[None, :] - 384.0
    for h in range(RET_H):
        tabs[h, :, 0:512] = gf[h] ** (a - b)
        tabs[h, :, 512:1024] = gb[h] ** (b + 511 - a)
        dl = xs - b
        tabs[h, :, 1024:1920] = np.where(dl > 0, gf[h] ** np.maximum(dl, 0),
                                         np.where(dl < 0, gb[h] ** np.maximum(-dl, 0), 2.0))
    t["rdec"] = tabs.astype(np.float32)
    t["ident"] = np.eye(128, dtype=np.float32)
    t["hgmask"] = np.concatenate([np.triu(np.ones((128, 128))), np.tril(np.ones((128, 128)))], axis=1).astype(np.float32)
    t["iota1"] = np.tile(np.arange(1, 257, dtype=np.float32)[None, :], (128, 1))
    return t


class Prog:
    def __init__(self, dbg=None, stop_after=None):
        self.dbg = dbg or []
        self.stop_after = stop_after
        self.nc = bass.Bass("TRN2", target_bir_lowering=False)
        self.es = ExitStack()
        self.S = Sched(self.nc, self.es)
        self.dram = {}
        self.dbg_out = {}

    def din(self, name, shape, dt=F32):
        self.dram[name] = self.nc.dram_tensor(name, list(shape), dt, kind="ExternalInput").ap()
        return self.dram[name]

    def dout(self, name, shape, dt=F32):
        self.dram[name] = self.nc.dram_tensor(name, list(shape), dt, kind="ExternalOutput").ap()
        return self.dram[name]

    def sb(self, es, name, shape, dt):
        self._uid = getattr(self, "_uid", 0) + 1
        return es.enter_context(self.nc.sbuf_tensor(f"{name}_u{self._uid}", list(shape), dt))

    def ps(self, es, name, shape, dt=F32):
        return es.enter_context(self.nc.psum_tensor(name, list(shape), dt))


def build_program(dbg=(), stop_after=None, parts=("ret", "moe0", "hg", "moe1", "final"), n_exp_run=N_EXP):
    P = Prog(list(dbg), stop_after)
    P.parts = parts
    P.n_exp_run = n_exp_run
    nc, S, es = P.nc, P.S, P.es
    op, dma = S.op, S.dma

    xT_d = P.din("xT", [D, T_ALL])
    cvec_d = P.din("cvec", [128, 2 * KD])
    wada_d = P.din("w_ada", [2, D, 6 * D])
    bada_d = P.din("b_ada", [2, 12 * D])
    nrm_d = P.din("norms", [128, 5 * KD])
    retin_d = P.din("ret_w_in", [D, 6144])
    retout_d = P.din("ret_w_out", [2048, D])
    router_d = P.din("moe_router", [2, D, N_EXP])
    rope_d = P.din("rope", [128, 192])
    rdec_d = P.din("rdec", [RET_H, 128, 1920])
    ident_d = P.din("ident", [128, 128])
    out_d = P.dout("outT", [D, T_LAT])
    for name, shape, dt_ in P.dbg:
        P.dbg_out[name] = P.dout("dbg_" + name, shape, dt_)

    xT = P.sb(es, "xT_sb", [128, KD, T_ALL], F32)
    B_x = [[Buf(f"x{k}_{b}") for b in range(5)] for k in range(KD)]
    cvec = P.sb(es, "cvec_sb", [128, 2 * KD], F32)
    scc = P.sb(es, "scc", [128, KD, 2], F32)
    nrm = P.sb(es, "nrm", [128, 5 * KD], F32)
    mod = P.sb(es, "mod", [128, 2, 48, 2], F32)
    modA = P.sb(es, "modA", [128, 2, 2, KD, 2], F32)
    ident_f = P.sb(es, "ident_f", [128, 128], F32)
    ident_b = P.sb(es, "ident_b", [128, 128], BF16)
    ones_b = P.sb(es, "ones_b", [128, 128], BF16)
    B_const = Buf("const")
    B_mod = Buf("mod")

    psb = [P.ps(es, f"psb{i}", [128, 512], F32) for i in range(8)]
    B_ps = [Buf(f"ps{i}") for i in range(8)]

    def sl(b):
        s, n, _ = BLOCKS[b]
        return slice(s, s + n)

    xsrc = xT_d.rearrange("(k p) t -> p k t", p=128)
    for k in range(KD):
        dma("sp", xT[:, k, :], xsrc[:, k, :], W=B_x[k])
    dma("sp", cvec[:], cvec_d, W=[B_const])
    dma("sp", nrm[:], nrm_d, W=[B_const])
    dma("sp", ident_f[:], ident_d, W=[B_const])
    op("act", lambda e: e.copy(out=ident_b[:], in_=ident_f[:]), R=[B_const], W=[B_const])
    op("dve", lambda e: e.memset(ones_b[:], 1.0), W=[B_const])
    op("act", lambda e: e.activation(out=scc[:].rearrange("p k c -> p c k"),
                                     in_=cvec[:].rearrange("p (c k) -> p c k", c=2), func=AF.Silu),
       R=[B_const], W=[B_const])

    with ExitStack() as pes:
        NPIECE = 12
        NST = 4
        wa = [P.sb(pes, f"wa{i}", [128, KD, 512], F32) for i in range(NST)]
        B_wa = [Buf(f"wa{i}") for i in range(NST)]
        HALF = 3 * D
        modrow = P.sb(pes, "modrow", [2, HALF], F32)
        B_mr = Buf("modrow")
        bada2 = P.sb(pes, "bada2", [2, HALF], F32)
        B_b2 = Buf("bada2")
        pi = 0
        for l in range(2):
            wsrc = wada_d[l].rearrange("(k p) f -> p k f", p=128)
            for hf in range(2):
                dma("sp", bada2[:], bada_d[:, l * 6 * D + hf * HALF: l * 6 * D + (hf + 1) * HALF], W=[B_b2])
                for pc6 in range(6):
                    pc = hf * 6 + pc6
                    slot = pi % NST
                    dma("sp", wa[slot][:], wsrc[:, :, pc * 512:(pc + 1) * 512], W=[B_wa[slot]])
                    pb = pi % 2
                    for k in range(KD):
                        op("pe", lambda e: e.matmul(psb[pb][0:2, :], lhsT=scc[:, k, :], rhs=wa[slot][:, k, :],
                                                    start=(k == 0), stop=(k == KD - 1)),
                           R=[B_wa[slot], B_const], W=[B_ps[pb]], signal=(k == KD - 1))
                    op("dve", lambda e: e.tensor_tensor(out=modrow[:, pc6 * 512:(pc6 + 1) * 512], in0=psb[pb][0:2, :],
                                                        in1=bada2[:, pc6 * 512:(pc6 + 1) * 512],
                                                        op=ALU.add), R=[B_ps[pb], B_b2], W=[B_mr])
                    pi += 1
                pt_ = 2 + (l * 2 + hf) % 2
                for j in range(24):
                    op("pe", lambda e: e.transpose(out=psb[pt_][:, j * 2:(j + 1) * 2], in_=modrow[:, j * 128:(j + 1) * 128],
                                                   identity=ident_f[0:2, 0:2]),
                       R=[B_mr, B_const], W=[B_ps[pt_]], signal=(j == 23))
                op("dve", lambda e: e.tensor_copy(out=mod[:, l, hf * 24:(hf + 1) * 24, :].rearrange("p j c -> p (j c)"),
                                                  in_=psb[pt_][:, 0:48]), R=[B_ps[pt_]], W=[B_mod])
        for l in range(2):
            for site in range(2):
                sc_j = (1 if site == 0 else 4) * KD
                nw = nrm[:, (site * 2 + l) * KD:(site * 2 + l + 1) * KD]
                op("dve", lambda e, l=l, site=site, sc_j=sc_j, nw=nw: e.scalar_tensor_tensor(
                    out=modA[:, l, site, :, :], in0=mod[:, l, sc_j:sc_j + KD, :], scalar=1.0,
                    in1=nw.unsqueeze(2).to_broadcast([128, KD, 2]), op0=ALU.add, op1=ALU.mult),
                   R=[B_mod, B_const], W=[B_mod])
        S.barrier()

    def modv(l, chunk, k, c):
        return mod[:, l, chunk * KD + k, c:c + 1]

    def norm_block(l, site, b, dst_fn, tmp, B_tmp, rstd, B_rstd, sq, B_sq, psn_i, final=False, after=None):
        s, n, isc = BLOCKS[b]
        c = 1 if isc else 0
        for k in range(KD):
            q = k % 2
            op("act", lambda e, k=k, q=q: e.activation(out=sq[q][:, :n], in_=xT[:, k, s:s + n], func=AF.Square),
               R=[B_x[k][b]], W=[B_sq[q]])
            op("pe", lambda e, k=k, q=q: e.matmul(psb[psn_i][:, :n], lhsT=ones_b[:], rhs=sq[q][:, :n],
                                                 start=(k == 0), stop=(k == KD - 1)),
               R=[B_sq[q], B_const], W=[B_ps[psn_i]], signal=True)
        op("act", lambda e: e.activation(out=rstd[:, :n], in_=psb[psn_i][:, :n], func=AF.Sqrt, bias=EPS,
                                         scale=1.0 / D), R=[B_ps[psn_i]], W=[B_rstd])
        op("dve", lambda e: e.reciprocal(out=rstd[:, :n], in_=rstd[:, :n]), R=[B_rstd], W=[B_rstd])
        for k in range(KD):
            q = k % 2
            out_ap, obufs = dst_fn(k)
            if final:
                a_ap = nrm[:, 4 * KD + k:4 * KD + k + 1]
                op("dve", lambda e, k=k, a_ap=a_ap, out_ap=out_ap: e.scalar_tensor_tensor(
                    out=out_ap, in0=xT[:, k, s:s + n], scalar=a_ap, in1=rstd[:, :n], op0=ALU.mult, op1=ALU.mult),
                   R=[B_x[k][b], B_rstd, B_const], W=obufs)
                if after is not None:
                    after(k)
            else:
                a_ap = modA[:, l, site, k, c:c + 1]
                sh_ap = modv(l, 0 if site == 0 else 3, k, c)
                op("dve", lambda e, k=k, q=q, a_ap=a_ap: e.scalar_tensor_tensor(
                    out=tmp[q][:, :n], in0=xT[:, k, s:s + n], scalar=a_ap, in1=rstd[:, :n],
                    op0=ALU.mult, op1=ALU.mult), R=[B_x[k][b], B_rstd, B_mod], W=[B_tmp[q]])
                op("act", lambda e, q=q, sh_ap=sh_ap, out_ap=out_ap: e.activation(
                    out=out_ap, in_=tmp[q][:, :n], func=AF.Identity, bias=sh_ap, scale=1.0),
                   R=[B_tmp[q], B_mod], W=obufs)

    def debug_dump(name, ap_fn):
        if name in P.dbg_out:
            S.barrier()
            tk = ap_fn(P.dbg_out[name])
            S.barrier()

    NSLOT = 4
    wring = [P.sb(es, f"wring{i}", [128, KD, 512], BF16) for i in range(NSLOT)]
    B_wr = [Buf(f"wr{i}") for i in range(NSLOT)]
    wr_state = {"n": 0}

    def wslot():
        i = wr_state["n"] % NSLOT
        wr_state["n"] += 1
        return i

    def retention_layer(l):
        with ExitStack() as les:
            hT = P.sb(les, "hT", [128, KD, T_ALL], BF16)
            B_h = [[Buf(f"h{k}_{b}") for b in range(5)] for k in range(KD)]
            tmp = [P.sb(les, f"ntmp{i}", [128, 512], F32) for i in range(2)]
            B_tmp = [Buf("ntmp0"), Buf("ntmp1")]
            sg = [P.sb(les, f"sg{i}", [128, 512], F32) for i in range(2)]
            B_sg = [Buf("sg0"), Buf("sg1")]
            rstd = P.sb(les, "rstd", [128, 512], F32)
            B_rstd = Buf("rstd")
            sq = [P.sb(les, f"sq{i}", [128, 512], BF16) for i in range(2)]
            B_sq = [Buf("sq0"), Buf("sq1")]
            rope_sb = P.sb(les, "rope_sb", [128, 192], F32)
            B_rope = Buf("rope")
            dma("sp", rope_sb[:], rope_d, W=[B_rope])
            for b in range(5):
                s, n, _ = BLOCKS[b]
                norm_block(l, 0, b, lambda k, b=b, s=s, n=n: (hT[:, k, s:s + n], [B_h[k][b]]),
                           tmp, B_tmp, rstd, B_rstd, sq, B_sq, psn_i=7)
            debug_dump("hT0", lambda o: [dma("sp", o.rearrange("(k p) t -> p k t", p=128)[:, k, :], hT[:, k, :],
                                             R=B_h[k]) for k in range(KD)])
            if P.stop_after == "hT0":
                return

            qr = P.sb(les, "qr", [128, 2, 2, 512], BF16)
            kr = P.sb(les, "kr", [128, 2, T_ALL], BF16)
            vv = P.sb(les, "vv", [128, NT, 512], BF16)
            B_q = [[Buf(f"q{i}_{m}") for m in range(2)] for i in range(2)]
            B_k = [[Buf(f"k{m}_{t}") for t in range(NT)] for m in range(2)]
            B_v = [Buf(f"v{t}") for t in range(NT)]
            rdec = P.sb(les, "rdec_sb", [128, 1920], F32)
            B_rdec = Buf("rdec")
            Ff = rdec[:, 0:512]
            Fb = rdec[:, 512:1024]

            def Dg(kk, n):
                return rdec[:, 1024 + 384 - 128 * kk: 1024 + 384 - 128 * kk + n]
            rt1, B_rt1 = tmp, B_tmp
            rt2, B_rt2 = sg, B_sg
            ctab, B_ctab = tmp, B_tmp
            AT = [P.sb(les, f"AT{i}", [128, 512], BF16) for i in range(3)]
            B_AT = [Buf(f"AT{i}") for i in range(3)]
            ogT = P.sb(les, "ogT", [128, 4, 512], BF16)
            B_og = [Buf(f"og{c}") for c in range(4)]
            sgb = P.sb(les, "sgb", [128, 4, 512], BF16)
            B_sgb = [Buf(f"sgb{c}") for c in range(4)]
            gf, gb = _ret_gammas()
            rcount = {"rope": 0, "at": 0, "q": 0}

            win_src = retin_d.rearrange("(k p) f -> p k f", p=128)
            wout_src = retout_d.rearrange("(c p) f -> p c f", p=128)

            def proj_rope(sA, qk, m, b, dst_ap, wb):
                s, n, isc = BLOCKS[b]
                pi_ = rcount["rope"] % 2
                rcount["rope"] += 1
                pst = psb[pi_]
                for k in range(KD):
                    op("pe", lambda e, k=k: e.matmul(
                        pst[:, :n], lhsT=wring[sA][:, k, qk * 256 + m * 128: qk * 256 + (m + 1) * 128],
                        rhs=hT[:, k, s:s + n], start=(k == 0), stop=(k == KD - 1)),
                       R=[B_wr[sA], B_h[k][b]], W=[B_ps[pi_]], signal=(k == KD - 1))
                scale = 1.0 if qk == 0 else 1.0 / 16.0
                if isc:
                    op("act", lambda e: e.activation(out=dst_ap, in_=pst[:, :n], func=AF.Copy, scale=scale),
                       R=[B_ps[pi_]], W=wb)
                    return
                g0 = s // 64
                if m == 0:
                    cos_ap = rope_sb[:, g0:g0 + 8].unsqueeze(2).to_broadcast([128, 8, 64])
                    sin_lo = rope_sb[0:64, 32 + g0:32 + g0 + 8].unsqueeze(2).to_broadcast([64, 8, 64])
                    sin_hi = rope_sb[64:128, 32 + g0:32 + g0 + 8].unsqueeze(2).to_broadcast([64, 8, 64])
                else:
                    cos_ap = rope_sb[:, 64:128].unsqueeze(1).to_broadcast([128, 8, 64])
                    sin_lo = rope_sb[0:64, 128:192].unsqueeze(1).to_broadcast([64, 8, 64])
                    sin_hi = rope_sb[64:128, 128:192].unsqueeze(1).to_broadcast([64, 8, 64])
                ti = pi_
                p3 = pst[:].rearrange("p (g t) -> p g t", t=64)
                t1v = rt1[ti][:].rearrange("p (g t) -> p g t", t=64)
                t2v = rt2[ti][:].rearrange("p (g t) -> p g t", t=64)
                op("dve", lambda e: e.scalar_tensor_tensor(
                    out=t1v, in0=p3, scalar=scale, in1=cos_ap, op0=ALU.mult, op1=ALU.mult),
                   R=[B_ps[pi_], B_rope], W=[B_rt1[ti]])
                op("dve", lambda e: e.scalar_tensor_tensor(
                    out=t2v[0:64], in0=p3[64:128], scalar=scale, in1=sin_lo, op0=ALU.mult, op1=ALU.mult),
                   R=[B_ps[pi_], B_rope], W=[B_rt2[ti]])
                op("dve", lambda e: e.scalar_tensor_tensor(
                    out=t2v[64:128], in0=p3[0:64], scalar=scale, in1=sin_hi, op0=ALU.mult, op1=ALU.mult),
                   R=[B_ps[pi_], B_rope, B_rt2[ti]], W=[B_rt2[ti]])
                op("dve", lambda e: e.tensor_tensor(
                    out=dst_ap, in0=rt1[ti][:, :n], in1=rt2[ti][:, :n], op=ALU.add),
                   R=[B_rt1[ti], B_rt2[ti]], W=wb)

            for h in range(RET_H):
                sA, sB, sC, sD = wslot(), wslot(), wslot(), wslot()
                dma("pool", wring[sA][:, :, 0:256], win_src[:, :, h * 256:(h + 1) * 256], W=[B_wr[sA]])
                dma("pool", wring[sA][:, :, 256:512], win_src[:, :, 1024 + h * 256:1024 + (h + 1) * 256], W=[B_wr[sA]])
                dma("pool", wring[sB][:], win_src[:, :, 2048 + h * 512:2048 + (h + 1) * 512], W=[B_wr[sB]])
                dma("pool", wring[sC][:], win_src[:, :, 4096 + h * 512:4096 + (h + 1) * 512], W=[B_wr[sC]])
                wD = wring[sD][:].rearrange("p k f -> p (k f)").rearrange("p (c f) -> p c f", c=4)
                dma("pool", wD, wout_src[:, h * 4:(h + 1) * 4, :], W=[B_wr[sD]])
                dma("sp", rdec[:], rdec_d[h], W=[B_rdec])

                for m in range(2):
                    for b in range(5):
                        s, n, isc = BLOCKS[b]
                        proj_rope(sA, 1, m, b, kr[:, m, s:s + n], [B_k[m][t] for t in range(s // 128, (s + n) // 128)])
                for t in range(NT):
                    b = min(t // 4, 4)
                    pi_ = t % 2
                    for k in range(KD):
                        op("pe", lambda e, k=k, t=t, pi_=pi_: e.matmul(
                            psb[pi_][:, :], lhsT=hT[:, k, t * 128:(t + 1) * 128], rhs=wring[sB][:, k, :],
                            start=(k == 0), stop=(k == KD - 1)),
                           R=[B_wr[sB], B_h[k][b]], W=[B_ps[pi_]], signal=(k == KD - 1))
                    op("act", lambda e, t=t, pi_=pi_: e.copy(out=vv[:, t, :], in_=psb[pi_][:, :]),
                       R=[B_ps[pi_]], W=[B_v[t]])
                if h == 0:
                    debug_dump("kr0", lambda o: [dma("sp", o[:, m, :], kr[:, m, :], R=B_k[m]) for m in range(2)])
                    debug_dump("vv0", lambda o: [dma("sp", o, vv[:], R=B_v)])
                    if P.stop_after == "proj0":
                        return

                def block_gen(b):
                    s, n, isc = BLOCKS[b]
                    c = 1 if isc else 0
                    qi = rcount["q"] % 2
                    rcount["q"] += 1
                    for m in range(2):
                        proj_rope(sA, 0, m, b, qr[:, qi, m, :n], [B_q[qi][m]])
                    yield
                    for cc in range(4):
                        pg = cc % 2
                        for k in range(KD):
                            op("pe", lambda e: e.matmul(
                                psb[pg][:, :n], lhsT=wring[sC][:, k, cc * 128:(cc + 1) * 128], rhs=hT[:, k, s:s + n],
                                start=(k == 0), stop=(k == KD - 1)),
                               R=[B_wr[sC], B_h[k][b]], W=[B_ps[pg]], signal=(k == KD - 1))
                        op("act", lambda e: e.activation(out=sgb[:, cc, :n], in_=psb[pg][:, :n], func=AF.Silu),
                           R=[B_ps[pg]], W=[B_sgb[cc]])
                    keys = [16, 17] if isc else list(range(NT))
                    pso = [psb[2 + cc] for cc in range(4)]
                    B_pso = [B_ps[2 + cc] for cc in range(4)]

                    def emit_scores(j, idx):
                        pi_ = 6 + (idx % 2)
                        for m in range(2):
                            op("pe", lambda e, m=m: e.matmul(
                                psb[pi_][:, :n], lhsT=kr[:, m, j * 128:(j + 1) * 128], rhs=qr[:, qi, m, :n],
                                start=(m == 0), stop=(m == 1)),
                               R=[B_k[m][j], B_q[qi][m]], W=[B_ps[pi_]], signal=(m == 1))
                        ai = rcount["at"] % 3
                        rcount["at"] += 1
                        pst = psb[pi_]
                        if isc:
                            tab = Dg(j - 16, n)
                            op("dve", lambda e: e.tensor_tensor(out=AT[ai][:, :n], in0=pst[:, :n], in1=tab, op=ALU.mult),
                               R=[B_ps[pi_], B_rdec], W=[B_AT[ai]])
                        elif j >= 16:
                            jc = j - 16
                            s1 = float(gf[h] ** (s + 256 - 128 * jc))
                            s2 = float(gb[h] ** (T_LAT - s - 511 + 128 * jc))
                            ci = jc
                            op("pool", lambda e: e.tensor_scalar(
                                out=ctab[ci][:], in0=Ff, scalar1=s1, scalar2=None, op0=ALU.mult),
                               R=[B_rdec], W=[B_ctab[ci]])
                            op("dve", lambda e: e.scalar_tensor_tensor(
                                out=ctab[ci][:], in0=Fb, scalar=s2, in1=ctab[ci][:], op0=ALU.mult, op1=ALU.add),
                               R=[B_rdec, B_ctab[ci]], W=[B_ctab[ci]])
                            op("dve", lambda e: e.tensor_tensor(
                                out=AT[ai][:, :n], in0=pst[:, :n], in1=ctab[ci][:, :n], op=ALU.mult),
                               R=[B_ps[pi_], B_ctab[ci]], W=[B_AT[ai]])
                        else:
                            rel = j - 4 * b
                            if 0 <= rel < 4:
                                tab = Dg(rel, 512)
                                op("dve", lambda e: e.tensor_tensor(out=AT[ai][:], in0=pst[:], in1=tab, op=ALU.mult),
                                   R=[B_ps[pi_], B_rdec], W=[B_AT[ai]])
                            elif rel < 0:
                                sc = float(gf[h] ** (s - 128 * j))
                                op("dve", lambda e: e.scalar_tensor_tensor(
                                    out=AT[ai][:], in0=pst[:], scalar=sc, in1=Ff, op0=ALU.mult, op1=ALU.mult),
                                   R=[B_ps[pi_], B_rdec], W=[B_AT[ai]])
                            else:
                                sc = float(gb[h] ** (128 * j - s - 511))
                                op("dve", lambda e: e.scalar_tensor_tensor(
                                    out=AT[ai][:], in0=pst[:], scalar=sc, in1=Fb, op0=ALU.mult, op1=ALU.mult),
                                   R=[B_ps[pi_], B_rdec], W=[B_AT[ai]])
                        return ai

                    def emit_av(j, ai, first, last):
                        for cc in range(4):
                            op("pe", lambda e, cc=cc: e.matmul(
                                pso[cc][:, :n], lhsT=vv[:, j, cc * 128:(cc + 1) * 128], rhs=AT[ai][:, :n],
                                start=first, stop=last),
                               R=[B_v[j], B_AT[ai]], W=[B_pso[cc]], signal=(cc == 3))

                    pend = None
                    for idx, j in enumerate(keys):
                        ai = emit_scores(j, idx)
                        if pend is not None:
                            emit_av(*pend)
                        pend = (j, ai, idx == 0, idx == len(keys) - 1)
                    emit_av(*pend)

                    yield
                    for cc in range(4):
                        q2 = cc % 2
                        op("act", lambda e, cc=cc, q2=q2: e.activation(out=sq[q2][:, :n], in_=pso[cc][:, :n],
                                                                      func=AF.Square),
                           R=[B_pso[cc]], W=[B_sq[q2]])
                        op("pe", lambda e, cc=cc, q2=q2: e.matmul(psb[6][:, :n], lhsT=ones_b[:], rhs=sq[q2][:, :n],
                                                                 start=(cc == 0), stop=(cc == 3)),
                           R=[B_sq[q2], B_const], W=[B_ps[6]], signal=True)
                    op("act", lambda e: e.activation(out=rstd[:, :n], in_=psb[6][:, :n], func=AF.Sqrt, bias=EPS,
                                                     scale=1.0 / 512), R=[B_ps[6]], W=[B_rstd])
                    op("dve", lambda e: e.reciprocal(out=rstd[:, :n], in_=rstd[:, :n]), R=[B_rstd], W=[B_rstd])
                    for cc in range(4):
                        q2 = cc % 2
                        op("dve", lambda e, cc=cc, q2=q2: e.tensor_tensor(
                            out=tmp[q2][:, :n], in0=pso[cc][:, :n], in1=rstd[:, :n], op=ALU.mult),
                           R=[B_pso[cc], B_rstd], W=[B_tmp[q2]])
                        op("dve", lambda e, cc=cc, q2=q2: e.tensor_tensor(
                            out=ogT[:, cc, :n], in0=tmp[q2][:, :n], in1=sgb[:, cc, :n], op=ALU.mult),
                           R=[B_tmp[q2], B_sgb[cc]], W=[B_og[cc]])
                    for mch in range(KD):
                        pi_ = 6 + (mch % 2)
                        for cc in range(4):
                            op("pe", lambda e, cc=cc, mch=mch, pi_=pi_: e.matmul(
                                psb[pi_][:, :n], lhsT=wD[:, cc, mch * 128:(mch + 1) * 128], rhs=ogT[:, cc, :n],
                                start=(cc == 0), stop=(cc == 3)),
                               R=[B_wr[sD], B_og[cc]], W=[B_ps[pi_]], signal=(cc == 3))
                        g_ap = modv(l, 2, mch, c)
                        op("dve", lambda e, mch=mch, pi_=pi_, g_ap=g_ap: e.scalar_tensor_tensor(
                            out=xT[:, mch, s:s + n], in0=psb[pi_][:, :n], scalar=g_ap, in1=xT[:, mch, s:s + n],
                            op0=ALU.mult, op1=ALU.add),
                           R=[B_ps[pi_], B_mod, B_x[mch][b]], W=[B_x[mch][b]])

                gens = [block_gen(b) for b in range(5)]
                next(gens[0])
                for b in range(5):
                    next(gens[b])
                    if b + 1 < 5:
                        next(gens[b + 1])
                    for _ in gens[b]:
                        pass
            S.barrier()

    iota_d2 = P.din("iota1", [128, 256])

    def moe_layer(l, with_ctx, n_exp_run=N_EXP):
        nblk = 5 if with_ctx else 4
        ntile = NT if with_ctx else 16
        NS = 288 if with_ctx else 256
        nst = 3 if with_ctx else 2
        st_sz = [128, 128, 32][:nst]
        st_off = [0, 128, 256][:nst]
        with ExitStack() as mes:
            h2tok = P.sb(mes, "h2tok", [128, NT, D], BF16)
            B_h2 = [Buf(f"h2t{t}") for t in range(NT)]
            wr_sb = P.sb(mes, "wr_sb", [128, KD, N_EXP], F32)
            B_wrt = Buf("wrt")
            dma("sp", wr_sb[:], router_d[l].rearrange("(k p) e -> p k e", p=128), W=[B_wrt])
            iota1 = P.sb(mes, "iota1_sb", [128, 256], F32)
            dma("sp", iota1[:], iota_d2, W=[B_wrt])
            aff = P.sb(mes, "aff", [128, NT, N_EXP], F32)
            B_aff = Buf("aff")
            aff_hl = P.sb(mes, "aff_hl", [128, NT, N_EXP, 2], BF16)
            posm_tok = P.sb(mes, "posm_tok", [128, NT, N_EXP], F32)
            B_posm = Buf("posm")

            with ExitStack() as r1:
                tmp = [P.sb(r1, f"mtmp{i}", [128, 512], F32) for i in range(2)]
                B_tmp = [Buf("mtmp0"), Buf("mtmp1")]
                rstd = P.sb(r1, "mrstd", [128, 512], F32)
                B_rstd = Buf("mrstd")
                sq = [P.sb(r1, f"msq{i}", [128, 512], BF16) for i in range(2)]
                B_sq = [Buf("msq0"), Buf("msq1")]
                h2f = P.sb(r1, "h2f", [128, KD, 512], F32)
                B_h2f = [Buf(f"h2f{k}") for k in range(KD)]
                h2b = P.sb(r1, "h2b", [128, KD, 512], BF16)
                B_h2b = [Buf(f"h2b{k}") for k in range(KD)]
                for b in range(nblk):
                    s, n, isc = BLOCKS[b]
                    norm_block(l, 1, b, lambda k, n=n: (h2f[:, k, :n], [B_h2f[k]]),
                               tmp, B_tmp, rstd, B_rstd, sq, B_sq, psn_i=7)
                    for k in range(KD):
                        eng_ = "act" if k % 2 == 0 else "dve"
                        if eng_ == "act":
                            op("act", lambda e, k=k: e.copy(out=h2b[:, k, :n], in_=h2f[:, k, :n]), R=[B_h2f[k]], W=[B_h2b[k]])
                        else:
                            op("dve", lambda e, k=k: e.tensor_copy(out=h2b[:, k, :n], in_=h2f[:, k, :n]),
                               R=[B_h2f[k]], W=[B_h2b[k]])
                    for tt in range(n // 128):
                        t = s // 128 + tt
                        for k in range(KD):
                            op("pe", lambda e, k=k: e.matmul(psb[6][:, 0:N_EXP], lhsT=h2f[:, k, tt * 128:(tt + 1) * 128],
                                                            rhs=wr_sb[:, k, :], start=(k == 0), stop=(k == KD - 1)),
                               R=[B_h2f[k], B_wrt], W=[B_ps[6]], signal=(k == KD - 1))
                        op("act", lambda e: e.copy(out=aff[:, t, :], in_=psb[6][:, 0:N_EXP]), R=[B_ps[6]], W=[B_aff])
                        pi_ = t % 2
                        pbf = psb[pi_][:].bitcast(BF16)
                        for k in range(KD):
                            op("pe", lambda e, k=k: e.transpose(out=pbf[:, k * 128:(k + 1) * 128],
                                                               in_=h2b[:, k, tt * 128:(tt + 1) * 128], identity=ident_b[:]),
                               R=[B_h2b[k], B_const], W=[B_ps[pi_]], signal=(k == KD - 1))
                        op("act", lambda e: e.copy(out=h2tok[:, t, :], in_=pbf), R=[B_ps[pi_]], W=[B_h2[t]])
                mx = P.sb(r1, "smx", [128, NT], F32)
                op("dve", lambda e: e.tensor_reduce(out=mx[:, :ntile], in_=aff[:, :ntile, :], axis=AX.X, op=ALU.max),
                   R=[B_aff], W=[B_rstd])
                op("dve", lambda e: e.tensor_tensor(out=aff[:, :ntile, :], in0=aff[:, :ntile, :],
                                                    in1=mx[:, :ntile].unsqueeze(2).to_broadcast([128, ntile, N_EXP]),
                                                    op=ALU.subtract), R=[B_rstd, B_aff], W=[B_aff])
                op("act", lambda e: e.activation(out=aff[:, :ntile, :], in_=aff[:, :ntile, :], func=AF.Exp),
                   R=[B_aff], W=[B_aff])
                op("dve", lambda e: e.tensor_reduce(out=mx[:, :ntile], in_=aff[:, :ntile, :], axis=AX.X, op=ALU.add),
                   R=[B_aff], W=[B_rstd])
                op("dve", lambda e: e.reciprocal(out=mx[:, :ntile], in_=mx[:, :ntile]), R=[B_rstd], W=[B_rstd])
                op("dve", lambda e: e.tensor_tensor(out=aff[:, :ntile, :], in0=aff[:, :ntile, :],
                                                    in1=mx[:, :ntile].unsqueeze(2).to_broadcast([128, ntile, N_EXP]),
                                                    op=ALU.mult), R=[B_rstd, B_aff], W=[B_aff])
                op("dve", lambda e: e.tensor_copy(out=aff_hl[:, :ntile, :, 0], in_=aff[:, :ntile, :]), R=[B_aff], W=[B_posm])
                op("dve", lambda e: e.tensor_tensor(out=aff_hl[:, :ntile, :, 1], in0=aff[:, :ntile, :],
                                                    in1=aff_hl[:, :ntile, :, 0], op=ALU.subtract),
                   R=[B_aff, B_posm], W=[B_posm])
                S.barrier()
            debug_dump(f"aff{l}", lambda o: [dma("sp", o, aff[:], R=[B_aff])])
            debug_dump(f"h2tok{l}", lambda o: [dma("sp", o, h2tok[:], R=B_h2)])

            with ExitStack() as r2:
                affT = P.sb(r2, "affT", [16, T_ALL], F32)
                mk = P.sb(r2, "mkT", [16, T_ALL], F32)
                cum = P.sb(r2, "cumT", [16, T_ALL], F32)
                sm = P.sb(r2, "bis", [16, 16], F32)
                B_affT, B_mk, B_cum, B_sm = Buf("affT"), Buf("mk"), Buf("cum"), Buf("sm")
                for t in range(ntile):
                    pi_ = t % 2
                    op("pe", lambda e: e.transpose(out=psb[pi_][0:16, 0:128], in_=aff[:, t, :], identity=ident_f[:]),
                       R=[B_aff, B_const], W=[B_ps[pi_]])
                    op("act", lambda e: e.copy(out=affT[:, t * 128:(t + 1) * 128], in_=psb[pi_][0:16, 0:128]),
                       R=[B_ps[pi_]], W=[B_affT])
                sets = [(0, T_LAT, 256)] + ([(T_LAT, T_CTX, 32)] if with_ctx else [])
                one = sm[:, 15:16]
                op("dve", lambda e: e.memset(one, 1.0), W=[B_sm])
                B_smx = [Buf("smA"), Buf("smB")]

                def bisect(si, s0, ns, cap):
                    lo, mid, gs = [sm[:, si * 6 + i: si * 6 + i + 1] for i in range(3)]
                    cnts = [sm[:, si * 6 + 3 + i: si * 6 + 4 + i] for i in range(2)]
                    Bs = B_smx[si]
                    a_ap = affT[:, s0:s0 + ns]
                    op("dve", lambda e: e.memset(lo, 0.0), W=[Bs])
                    op("dve", lambda e: e.memset(mid, 0.5), W=[Bs])
                    NIT = 30
                    for it in range(NIT):
                        step = 0.5 ** (it + 1)
                        cnt = cnts[it % 2]
                        op("dve", lambda e: e.memset(cnt, 0.0), W=[Bs])
                        yield
                        op("dve", lambda e: e.tensor_scalar(out=mk[:, s0:s0 + ns], in0=a_ap, scalar1=mid, scalar2=0.0,
                                                            op0=ALU.is_ge, op1=ALU.add, accum_out=cnt),
                           R=[B_affT, Bs], W=[B_mk, Bs])
                        yield
                        op("dve", lambda e: e.tensor_scalar(out=gs, in0=cnt, scalar1=float(cap), scalar2=step,
                                                            op0=ALU.is_ge, op1=ALU.mult), R=[Bs], W=[Bs])
                        yield
                        if it < NIT - 1:
                            op("dve", lambda e: e.scalar_tensor_tensor(out=mid, in0=gs, scalar=step * 0.5, in1=lo,
                                                                       op0=ALU.add, op1=ALU.add), R=[Bs], W=[Bs])
                            yield
                        op("dve", lambda e: e.tensor_tensor(out=lo, in0=lo, in1=gs, op=ALU.add), R=[Bs], W=[Bs])
                        yield
                    op("dve", lambda e: e.tensor_scalar(out=mk[:, s0:s0 + ns], in0=a_ap, scalar1=lo, scalar2=None,
                                                        op0=ALU.is_ge), R=[B_affT, Bs], W=[B_mk])
                    yield
                    op("dve", lambda e: e.tensor_tensor_scan(out=cum[:, s0:s0 + ns],
                                                             data0=one.to_broadcast([16, ns]), data1=mk[:, s0:s0 + ns],
                                                             initial=0.0, op0=ALU.mult, op1=ALU.add),
                       R=[B_mk, B_sm], W=[B_cum])
                    yield
                    op("dve", lambda e: e.tensor_tensor(out=cum[:, s0:s0 + ns], in0=cum[:, s0:s0 + ns],
                                                        in1=mk[:, s0:s0 + ns], op=ALU.mult), R=[B_mk, B_cum], W=[B_cum])
                    yield

                interleave(*[bisect(si, s0, ns, cap) for si, (s0, ns, cap) in enumerate(sets)])
                for t in range(ntile):
                    pi_ = t % 2
                    op("pe", lambda e: e.transpose(out=psb[pi_][:, 0:16], in_=cum[:, t * 128:(t + 1) * 128],
                                                   identity=ident_f[0:16, 0:16]),
                       R=[B_cum, B_const], W=[B_ps[pi_]])
                    op("act", lambda e: e.copy(out=posm_tok[:, t, :], in_=psb[pi_][:, 0:16]), R=[B_ps[pi_]], W=[B_posm])
                S.barrier()
            debug_dump(f"posm{l}", lambda o: [dma("sp", o, posm_tok[:], R=[B_posm])])
            if P.stop_after == f"route{l}":
                return

            with ExitStack() as xs:
                Pm = P.sb(xs, "Pm", [128, NT, NS], BF16)
                B_Pm = Buf("Pm")
                PTl = P.sb(xs, "PTl", [128, 2, T_LAT], BF16)
                PTc = P.sb(xs, "PTc", [128, T_CTX], BF16)
                B_PT = Buf("PT")

                def PTv(st, a, b2, sz):
                    return PTl[0:sz, st, a:b2] if st < 2 else PTc[0:sz, a - T_LAT:b2 - T_LAT]
                xeT = P.sb(xs, "xeT", [128, KD, NS], BF16)
                B_xe = [Buf(f"xe{k}") for k in range(KD)]
                hmid = P.sb(xs, "hmid", [128, 2, NS], BF16)
                B_hm = [Buf("hm0"), Buf("hm1")]
                sil = P.sb(xs, "sil", [128, 2, NS], BF16)
                B_sil = [Buf("sil0"), Buf("sil1")]
                y_sb = P.sb(xs, "y_sb", [128, nst, D], BF16)
                B_y = [Buf(f"y{i}") for i in range(nst)]
                gsl = P.sb(xs, "gsl", [128, 4], F32)
                B_gsl = Buf("gsl")
                NGU = 5
                NDN = 5 if with_ctx else 6
                wgu = wring + [P.sb(xs, f"wgu{i}", [128, KD, 512], BF16) for i in range(NGU - NSLOT)]
                B_gu = [Buf(f"gu{i}") for i in range(NGU)]
                wdn = [P.sb(xs, f"wdn{i}", [128, 2, D], BF16) for i in range(NDN)]
                B_dn = [Buf(f"dn{i}") for i in range(NDN)]
                op("dve", lambda e: e.memset(Pm[:], 0.0), W=[B_Pm])

                NFB = NFC // 2
                sched = [(e_, fb) for e_ in range(n_exp_run) for fb in range(NFB)]
                issued = {"n": 0}

                def issue_weights(upto):
                    while issued["n"] < min(upto, len(sched)):
                        i = issued["n"]
                        e_, fb = sched[i]
                        if f"wg_{l}_{e_}" not in P.dram:
                            P.din(f"wg_{l}_{e_}", [D, FF])
                            P.din(f"wu_{l}_{e_}", [D, FF])
                            P.din(f"wd_{l}_{e_}", [FF, D])
                        wg_ap, wu_ap, wd_ap = P.dram[f"wg_{l}_{e_}"], P.dram[f"wu_{l}_{e_}"], P.dram[f"wd_{l}_{e_}"]
                        gi, di = i % NGU, i % NDN
                        dma("pool", wgu[gi][:, :, 0:256],
                            wg_ap.rearrange("(k p) f -> p k f", p=128)[:, :, fb * 256:(fb + 1) * 256], W=[B_gu[gi]])
                        dma("pool", wgu[gi][:, :, 256:512],
                            wu_ap.rearrange("(k p) f -> p k f", p=128)[:, :, fb * 256:(fb + 1) * 256], W=[B_gu[gi]])
                        dma("pool", wdn[di][:],
                            wd_ap.rearrange("(c p) d -> p c d", p=128)[:, fb * 2:(fb + 1) * 2, :], W=[B_dn[di]])
                        issued["n"] += 1

                LA = 4 if with_ctx else 5
                issue_weights(LA)
                blk_i = 0
                deferred = {"scatter": None}
                xb = (6, 7) if with_ctx else (4, 5)
                for e_ in range(n_exp_run):
                    op("dve", lambda e: e.tensor_tensor(
                        out=Pm[:, 0:16, 0:256], in0=iota1[:, :].unsqueeze(1).to_broadcast([128, 16, 256]),
                        in1=posm_tok[:, 0:16, e_:e_ + 1].to_broadcast([128, 16, 256]), op=ALU.is_equal),
                       R=[B_posm, B_wrt], W=[B_Pm])
                    if with_ctx:
                        op("dve", lambda e: e.tensor_tensor(
                            out=Pm[:, 16:18, 256:288], in0=iota1[:, 0:32].unsqueeze(1).to_broadcast([128, 2, 32]),
                            in1=posm_tok[:, 16:18, e_:e_ + 1].to_broadcast([128, 2, 32]), op=ALU.is_equal),
                           R=[B_posm, B_wrt], W=[B_Pm])
                    for st in range(nst):
                        tiles = range(16) if st < 2 else range(16, 18)
                        tl = list(tiles)
                        for ti, t in enumerate(tl):
                            op("pe", lambda e: e.matmul(psb[6][0:st_sz[st], st * 2:st * 2 + 2],
                                                        lhsT=Pm[:, t, st_off[st]:st_off[st] + st_sz[st]],
                                                        rhs=aff_hl[:, t, e_, :], start=(ti == 0), stop=(ti == len(tl) - 1)),
                               R=[B_Pm, B_posm], W=[B_ps[6]], signal=(ti == len(tl) - 1))
                    for st in range(nst):
                        op("dve", lambda e: e.tensor_reduce(out=gsl[0:st_sz[st], st:st + 1],
                                                            in_=psb[6][0:st_sz[st], st * 2:st * 2 + 2], axis=AX.X, op=ALU.add),
                           R=[B_ps[6]], W=[B_gsl])
                    def do_PT():
                        gcnt = 0
                        for st in range(nst):
                            tl = list(range(16)) if st < 2 else [16, 17]
                            for g0 in range(0, len(tl), 8):
                                grp = tl[g0:g0 + 8]
                                pi_ = xb[gcnt % 2]
                                gcnt += 1
                                pbf = psb[pi_][:].bitcast(BF16)
                                for gi_, t in enumerate(grp):
                                    op("pe", lambda e: e.transpose(
                                        out=pbf[0:st_sz[st], gi_ * 128:(gi_ + 1) * 128],
                                        in_=Pm[:, t, st_off[st]:st_off[st] + st_sz[st]], identity=ident_b[:]),
                                       R=[B_Pm, B_const], W=[B_ps[pi_]], signal=(gi_ == len(grp) - 1))
                                op("act", lambda e: e.copy(out=PTv(st, grp[0] * 128, (grp[-1] + 1) * 128, st_sz[st]),
                                                           in_=pbf[0:st_sz[st], 0:len(grp) * 128]),
                                   R=[B_ps[pi_]], W=[B_PT])
                    for k in range(KD):
                        pi_ = 6 + (k % 2)
                        for t in range(ntile):
                            op("pe", lambda e: e.matmul(psb[pi_][:, 0:NS], lhsT=h2tok[:, t, k * 128:(k + 1) * 128],
                                                        rhs=Pm[:, t, :], start=(t == 0), stop=(t == ntile - 1)),
                               R=[B_h2[t], B_Pm], W=[B_ps[pi_]], signal=(t == ntile - 1))
                        op("act", lambda e: e.copy(out=xeT[:, k, :], in_=psb[pi_][:, 0:NS]), R=[B_ps[pi_]], W=[B_xe[k]])
                    psY = [[psb[st * 2 + nh] for nh in range(2)] for st in range(nst)]
                    B_psY = [[B_ps[st * 2 + nh] for nh in range(2)] for st in range(nst)]

                    def emit_down(fc, hb, di, first, last):
                        for st in range(nst):
                            for nh in range(2):
                                op("pe", lambda e: e.matmul(
                                    psY[st][nh][0:st_sz[st], :], lhsT=hmid[:, hb, st_off[st]:st_off[st] + st_sz[st]],
                                    rhs=wdn[di][:, fc % 2, nh * 512:(nh + 1) * 512], start=first, stop=last),
                                   R=[B_hm[hb], B_dn[di]], W=[B_psY[st][nh]], signal=(st == nst - 1 and nh == 1))

                    pend = None
                    for fb in range(NFB):
                        i = blk_i
                        blk_i += 1
                        issue_weights(i + LA)
                        if fb == 3:
                            if deferred["scatter"] is not None:
                                deferred["scatter"]()
                                deferred["scatter"] = None
                            do_PT()
                        gi, di = i % NGU, i % NDN
                        for f2 in range(2):
                            fc = fb * 2 + f2
                            hb = fc % 2
                            ia, iu = 6, 7
                            pa, pu = psb[ia], psb[iu]
                            for k in range(KD):
                                op("pe", lambda e: e.matmul(pa[:, 0:NS], lhsT=wgu[gi][:, k, f2 * 128:(f2 + 1) * 128],
                                                            rhs=xeT[:, k, :], start=(k == 0), stop=(k == KD - 1)),
                                   R=[B_gu[gi], B_xe[k]], W=[B_ps[ia]], signal=(k == KD - 1))
                            for k in range(KD):
                                op("pe", lambda e: e.matmul(pu[:, 0:NS],
                                                            lhsT=wgu[gi][:, k, 256 + f2 * 128:256 + (f2 + 1) * 128],
                                                            rhs=xeT[:, k, :], start=(k == 0), stop=(k == KD - 1)),
                                   R=[B_gu[gi], B_xe[k]], W=[B_ps[iu]], signal=(k == KD - 1))
                            op("act", lambda e: e.activation(out=sil[:, hb, :], in_=pa[:, 0:NS], func=AF.Silu),
                               R=[B_ps[ia]], W=[B_sil[hb]])
                            op("dve", lambda e: e.tensor_tensor(out=hmid[:, hb, :], in0=pu[:, 0:NS], in1=sil[:, hb, :],
                                                                op=ALU.mult), R=[B_ps[iu], B_sil[hb]], W=[B_hm[hb]])
                            if pend is not None:
                                emit_down(*pend)
                            pend = (fc, hb, di, fc == 0, fc == NFC - 1)
                    emit_down(*pend)
                    for st in range(nst):
                        for nh in range(2):
                            op("act", lambda e: e.activation(out=y_sb[0:st_sz[st], st, nh * 512:(nh + 1) * 512],
                                                             in_=psY[st][nh][0:st_sz[st], :], func=AF.Copy,
                                                             scale=gsl[0:st_sz[st], st:st + 1]),
                               R=[B_psY[st][nh], B_gsl], W=[B_y[st]])
                    def do_scatter():
                        gi_s = 0
                        for b in range(nblk):
                            s, n, isc = BLOCKS[b]
                            c = 1 if isc else 0
                            sts = [2] if isc else [0, 1]
                            for mch in range(KD):
                                pi_ = xb[gi_s % 2]
                                gi_s += 1
                                for si, st in enumerate(sts):
                                    op("pe", lambda e: e.matmul(psb[pi_][:, :n],
                                                                lhsT=y_sb[0:st_sz[st], st, mch * 128:(mch + 1) * 128],
                                                                rhs=PTv(st, s, s + n, st_sz[st]), start=(si == 0),
                                                                stop=(si == len(sts) - 1)),
                                       R=[B_y[st], B_PT], W=[B_ps[pi_]], signal=(si == len(sts) - 1))
                                g_ap = modv(l, 5, mch, c)
                                op("dve", lambda e: e.scalar_tensor_tensor(
                                    out=xT[:, mch, s:s + n], in0=psb[pi_][:, :n], scalar=g_ap, in1=xT[:, mch, s:s + n],
                                    op0=ALU.mult, op1=ALU.add),
                                   R=[B_ps[pi_], B_mod, B_x[mch][b]], W=[B_x[mch][b]])
                    deferred["scatter"] = do_scatter
                if deferred["scatter"] is not None:
                    deferred["scatter"]()
                S.barrier()

    def hgrn2_layer(l):
        hgin_d = P.din("hg_w_in", [D, 5120])
        hgout_d = P.din("hg_w_out", [D, D])
        hgmisc_d = P.din("hg_misc", [128, 1 + 4 * KD])
        hgmask_d = P.din("hgmask", [128, 256])
        PT_ = 384
        NPART = 6
        NCH = 3
        with ExitStack() as les:
            hT = P.sb(les, "hT1", [128, KD, T_ALL], BF16)
            B_h = [[Buf(f"h1{k}_{b}") for b in range(5)] for k in range(KD)]
            B_hT = Buf("hT1all")
            hgm = P.sb(les, "hgm", [128, 1 + 4 * KD], F32)
            msk = P.sb(les, "hgmask_sb", [128, 256], F32)
            lbs = P.sb(les, "lbs", [128, 2, 2, HG_H], F32)
            B_hc = Buf("hgconst")
            dma("sp", hgm[:], hgmisc_d, W=[B_hc])
            dma("sp", msk[:], hgmask_d, W=[B_hc])
            AA = P.sb(les, "hgAA", [128, 10 * PT_], F32)
            Aset = [[AA[:, (st_ * 5 + i) * PT_:(st_ * 5 + i + 1) * PT_] for i in range(5)] for st_ in range(2)]
            B_Aset = [[Buf(f"hgA{st_}{i}") for i in range(5)] for st_ in range(2)]
            tmp = [AA[:, 0:512], AA[:, 768:1280]]
            B_tmp = [[B_Aset[0][0], B_Aset[0][1]], [B_Aset[0][2], B_Aset[0][3]]]
            rstds = [AA[:, 1536:2048], AA[:, 2304:2816]]
            B_rstds = [[B_Aset[0][4], B_Aset[1][0]], [B_Aset[1][1], B_Aset[1][2]]]
            rstd, B_rstd = rstds[0], B_rstds[0]
            sq = [P.sb(les, f"hsq{i}", [128, 512], BF16) for i in range(2)]
            B_sq = [Buf("hsq0"), Buf("hsq1")]

            class _V:
                def __init__(self, ap):
                    self.ap = ap

                def __getitem__(self, key):
                    return self.ap[key]
            for b in range(5):
                s, n, _ = BLOCKS[b]
                norm_block(l, 0, b, lambda k, b=b, s=s, n=n: (hT[:, k, s:s + n], [B_h[k][b]]),
                           [_V(tmp[0]), _V(tmp[1])], B_tmp, _V(rstd), B_rstd, sq, B_sq, psn_i=7)
            for d_ in range(2):
                op("dve", lambda e: e.tensor_tensor(out=lbs[:, 0, d_, :], in0=hgm[:, 1 + (2 + d_) * 8:1 + (3 + d_) * 8],
                                                    in1=hgm[:, 1 + d_ * 8:1 + (d_ + 1) * 8], op=ALU.subtract),
                   R=[B_hc], W=[B_hc])
            op("act", lambda e: e.activation(out=lbs[:, 0, :, :], in_=lbs[:, 0, :, :], func=AF.Sigmoid), R=[B_hc], W=[B_hc])
            op("dve", lambda e: e.tensor_scalar(out=lbs[:, 1, :, :], in0=lbs[:, 0, :, :], scalar1=-1.0, scalar2=1.0,
                                                op0=ALU.mult, op1=ALU.add), R=[B_hc], W=[B_hc])
            S.barrier()
            debug_dump("hT1", lambda o: [dma("sp", o.rearrange("(k p) t -> p k t", p=128)[:, k, :], hT[:, k, :],
                                             R=[B_hT]) for k in range(KD)])

            qs2 = P.sb(les, "hqs", [128, 2, PT_], BF16)
            B_qs2 = [Buf("hqs0"), Buf("hqs1")]
            arr = [[P.sb(les, f"harr{d_}{i}", [128, T_ALL], BF16) for i in range(3)] for d_ in range(2)]
            B_arr = [[Buf(f"harr{d_}{i}") for i in range(3)] for d_ in range(2)]
            sgT, B_sg = arr[0][2], B_arr[0][2]
            ogT, B_og = arr[1][2], B_arr[1][2]
            B_ogb = [[B_og, Buf(f"ogb{b}")] for b in range(4)]
            vv = P.sb(les, "hvv", [128, NT, 128], BF16)
            B_v = Buf("hvv")
            EF = P.sb(les, "hEF", [128, 2, NT], F32)
            EM = P.sb(les, "hEM", [128, 2, NT], F32)
            t6b = P.sb(les, "ht6", [128, 2, 8], F32)
            B_t6 = [Buf("t6a"), Buf("t6b")]
            B_E = Buf("hE")
            Sst = P.sb(les, "hS", [128, 2, 128], F32)
            B_S = [Buf("hS0"), Buf("hS1")]
            Suse = P.sb(les, "hSuse", [128, 2, 16, 128], BF16)
            B_Su = Buf("hSuse")
            kht = [P.sb(les, f"hkht{i}", [128, 128], BF16) for i in range(2)]
            B_kht = [Buf("kht0"), Buf("kht1")]
            ATs = [P.sb(les, f"hAT{i}", [128, 128], BF16) for i in range(8)]
            B_ATs = [Buf(f"hAT{i}") for i in range(8)]
            one_col = hgm[:, 0:1]
            ones_f = P.sb(les, "hones", [128, 1], F32)
            op("dve", lambda e: e.memset(ones_f[:], 1.0), W=[B_hc])

            win_src = hgin_d.rearrange("(k p) f -> p k f", p=128)
            cnt_ = {"ps": 0, "kht": 0, "at": 0}

            def view3(ap):
                return ap.rearrange("p (c i) -> p c i", i=128)

            for h in range(HG_H):
                sX, sY = wslot(), wslot()
                for gi_, off in enumerate((0, 1024, 2048, 3072)):
                    dma("pool", wring[sX][:, :, gi_ * 128:(gi_ + 1) * 128], win_src[:, :, off + h * 128: off + (h + 1) * 128],
                        W=[B_wr[sX]])
                yflat = wring[sY][:].rearrange("p k f -> p (k f)")
                wG = yflat[:, 0:1024].rearrange("p (k f) -> p k f", k=KD)
                wO = yflat[:, 1024:2048]
                dma("pool", wG, win_src[:, :, 4096 + h * 128:4096 + (h + 1) * 128], W=[B_wr[sY]])
                dma("pool", wO, hgout_d[h * 128:(h + 1) * 128, :], W=[B_wr[sY]])

                def proj(gi_, s, n, pi_):
                    for k in range(KD):
                        op("pe", lambda e: e.matmul(psb[pi_][:, :n], lhsT=wring[sX][:, k, gi_ * 128:(gi_ + 1) * 128],
                                                    rhs=hT[:, k, s:s + n], start=(k == 0), stop=(k == KD - 1)),
                           R=[B_wr[sX], B_hT], W=[B_ps[pi_]], signal=(k == KD - 1))

                for part in range(NPART):
                    p0 = part * PT_
                    c0 = part * NCH
                    qi = part % 2
                    qs = qs2[:, qi, :]
                    B_qs = B_qs2[qi]
                    pi_ = cnt_["ps"] % 2
                    cnt_["ps"] += 1
                    proj(0, p0, PT_, pi_)
                    op("act", lambda e: e.activation(out=qs, in_=psb[pi_][:, :PT_], func=AF.Silu), R=[B_ps[pi_]], W=[B_qs])
                    for tt in range(NCH):
                        t = c0 + tt
                        pi_ = cnt_["ps"] % 2
                        cnt_["ps"] += 1
                        for k in range(KD):
                            op("pe", lambda e: e.matmul(psb[pi_][:, 0:128], lhsT=hT[:, k, t * 128:(t + 1) * 128],
                                                        rhs=wring[sX][:, k, 384:512], start=(k == 0), stop=(k == KD - 1)),
                               R=[B_wr[sX], B_hT], W=[B_ps[pi_]], signal=(k == KD - 1))
                        op("act", lambda e: e.copy(out=vv[:, t, :], in_=psb[pi_][:, 0:128]), R=[B_ps[pi_]], W=[B_v])
                    def chain(d_):
                        si_ = d_
                        A1, A2, A3, A4, A5 = Aset[si_]
                        BA = B_Aset[si_]
                        t6 = t6b[:, si_, :]
                        B_T = B_t6[si_]
                        pi_ = cnt_["ps"] % 2
                        cnt_["ps"] += 1
                        proj(1 + d_, p0, PT_, pi_)
                        oml_ap = lbs[:, 1, d_, h:h + 1]
                        op("act", lambda e: e.activation(out=A2, in_=psb[pi_][:, :PT_], func=AF.Sigmoid),
                           R=[B_ps[pi_]], W=[BA[1]])
                        yield
                        op("act", lambda e: e.activation(out=A1, in_=psb[pi_][:, :PT_], func=AF.Sigmoid, scale=-1.0),
                           R=[B_ps[pi_]], W=[BA[0]])
                        yield
                        op("act", lambda e: e.activation(out=A2, in_=A2, func=AF.Ln, bias=lbs[:, 0, d_, h:h + 1],
                                                         scale=oml_ap), R=[BA[1], B_hc], W=[BA[1]])
                        yield
                        op("dve", lambda e: e.tensor_tensor_scan(out=A3, data0=ones_f[:, 0:1].to_broadcast([128, PT_]),
                                                                 data1=A2, initial=0.0, op0=ALU.mult, op1=ALU.add),
                           R=[BA[1], B_hc], W=[BA[2]])
                        G3, g3 = view3(A3), view3(A2)
                        dst = [arr[d_][i][:, p0:p0 + PT_] for i in range(3)]
                        Bd = B_arr[d_]
                        bc = [128, NCH, 128]
                        if d_ == 0:
                            yield
                            op("dve", lambda e: e.tensor_tensor(out=view3(A4), in0=G3, in1=G3[:, :, 63:64].to_broadcast(bc),
                                                                op=ALU.subtract), R=[BA[2]], W=[BA[3]])
                            yield
                            op("act", lambda e: e.activation(out=A5, in_=A4, func=AF.Exp), R=[BA[3]], W=[BA[4]])
                            yield
                            op("pool", lambda e: e.tensor_tensor(out=dst[0], in0=qs, in1=A5, op=ALU.mult),
                               R=[B_qs, BA[4]], W=[Bd[0]])
                            yield
                            op("dve", lambda e: e.tensor_tensor(out=t6[:, 0:NCH], in0=G3[:, :, 0], in1=g3[:, :, 0], op=ALU.subtract),
                               R=[BA[2], BA[1]], W=[B_T])
                            yield
                            op("dve", lambda e: e.tensor_tensor(out=EF[:, 0, c0:c0 + NCH], in0=G3[:, :, 127], in1=t6[:, 0:NCH],
                                                                op=ALU.subtract), R=[BA[2], B_T], W=[B_E])
                            yield
                            op("dve", lambda e: e.tensor_tensor(out=EM[:, 0, c0:c0 + NCH], in0=G3[:, :, 63], in1=t6[:, 0:NCH],
                                                                op=ALU.subtract), R=[BA[2], B_T], W=[B_E])
                            yield
                            op("act", lambda e: e.activation(out=A2, in_=A4, func=AF.Exp, scale=-1.0), R=[BA[3]], W=[BA[1]])
                            yield
                            op("dve", lambda e: e.scalar_tensor_tensor(out=dst[1], in0=A1, scalar=oml_ap, in1=A2, op0=ALU.mult, op1=ALU.mult),
                               R=[BA[0], BA[1]], W=[Bd[1]])
                            yield
                            op("dve", lambda e: e.tensor_tensor(out=view3(A4), in0=G3, in1=G3[:, :, 127:128].to_broadcast(bc),
                                                                op=ALU.subtract), R=[BA[2]], W=[BA[3]])
                            yield
                            op("act", lambda e: e.activation(out=A5, in_=A4, func=AF.Exp, scale=-1.0), R=[BA[3]], W=[BA[4]])
                            yield
                            op("dve", lambda e: e.scalar_tensor_tensor(out=dst[2], in0=A1, scalar=oml_ap, in1=A5, op0=ALU.mult, op1=ALU.mult),
                               R=[BA[0], BA[4]], W=[Bd[2]])
                        else:
                            yield
                            op("dve", lambda e: e.tensor_copy(out=t6[:, 0:NCH], in_=G3[:, :, 127]), R=[BA[2]], W=[B_T])
                            yield
                            op("pool", lambda e: e.tensor_tensor(out=A2, in0=A3, in1=A2, op=ALU.subtract),
                               R=[BA[2], BA[1]], W=[BA[1]])
                            H3 = view3(A2)
                            yield
                            op("dve", lambda e: e.tensor_tensor(out=view3(A4), in0=H3, in1=H3[:, :, 64:65].to_broadcast(bc),
                                                                op=ALU.subtract), R=[BA[1]], W=[BA[3]])
                            yield
                            op("act", lambda e: e.activation(out=A5, in_=A4, func=AF.Exp, scale=-1.0), R=[BA[3]], W=[BA[4]])
                            yield
                            op("pool", lambda e: e.tensor_tensor(out=dst[0], in0=qs, in1=A5, op=ALU.mult),
                               R=[B_qs, BA[4]], W=[Bd[0]])
                            yield
                            op("act", lambda e: e.activation(out=A3, in_=A4, func=AF.Exp), R=[BA[3]], W=[BA[2]])
                            yield
                            op("dve", lambda e: e.scalar_tensor_tensor(out=dst[1], in0=A1, scalar=oml_ap, in1=A3, op0=ALU.mult, op1=ALU.mult),
                               R=[BA[0], BA[2]], W=[Bd[1]])
                            yield
                            op("dve", lambda e: e.tensor_tensor(out=EF[:, 1, c0:c0 + NCH], in0=t6[:, 0:NCH], in1=H3[:, :, 0],
                                                                op=ALU.subtract), R=[BA[1], B_T], W=[B_E])
                            yield
                            op("dve", lambda e: e.tensor_tensor(out=EM[:, 1, c0:c0 + NCH], in0=t6[:, 0:NCH], in1=H3[:, :, 64],
                                                                op=ALU.subtract), R=[BA[1], B_T], W=[B_E])
                            yield
                            op("dve", lambda e: e.tensor_tensor(out=view3(A4), in0=H3, in1=H3[:, :, 0:1].to_broadcast(bc),
                                                                op=ALU.subtract), R=[BA[1]], W=[BA[3]])
                            yield
                            op("act", lambda e: e.activation(out=A5, in_=A4, func=AF.Exp), R=[BA[3]], W=[BA[4]])
                            yield
                            op("dve", lambda e: e.scalar_tensor_tensor(out=dst[2], in0=A1, scalar=oml_ap, in1=A5, op0=ALU.mult, op1=ALU.mult),
                               R=[BA[0], BA[4]], W=[Bd[2]])
                        yield
                    interleave(chain(0), chain(1))
                op("act", lambda e: e.activation(out=EF[:], in_=EF[:], func=AF.Exp), R=[B_E], W=[B_E])
                op("act", lambda e: e.activation(out=EM[:], in_=EM[:], func=AF.Exp), R=[B_E], W=[B_E])

                orders = [[16, 17] + list(range(16)), [17, 16] + list(range(15, -1, -1))]
                for d_ in range(2):
                    op("dve", lambda e: e.memset(Sst[:, d_, :], 0.0), W=[B_S[d_]])
                items = [(step, d_) for step in range(NT - 1) for d_ in range(2)]

                def emit_T(i):
                    step, d_ = items[i]
                    c = orders[d_][step]
                    ki = i % 2
                    pq = 4 + (i % 2)
                    pbf = psb[pq][:].bitcast(BF16)
                    op("pe", lambda e: e.transpose(out=pbf[:, 0:128], in_=arr[d_][2][:, c * 128:(c + 1) * 128],
                                                   identity=ident_b[:]), R=[B_arr[d_][2], B_const], W=[B_ps[pq]])
                    op("act", lambda e: e.copy(out=kht[ki][:], in_=pbf[:, 0:128]), R=[B_ps[pq]], W=[B_kht[ki]])

                def emit_suse(step, d_):
                    c = orders[d_][step]
                    if c < 16:
                        op("act", lambda e: e.activation(out=Suse[:, d_, c, :], in_=Sst[:, d_, :], func=AF.Copy,
                                                         scale=EM[:, d_, c:c + 1]), R=[B_S[d_], B_E], W=[B_Su])

                emit_T(0)
                for i, (step, d_) in enumerate(items):
                    c = orders[d_][step]
                    if i + 1 < len(items):
                        emit_T(i + 1)
                    emit_suse(step, d_)
                    ki = i % 2
                    pd = 2 + (i % 2)
                    op("pe", lambda e: e.matmul(psb[pd][:, 0:128], lhsT=kht[ki][:], rhs=vv[:, c, :], start=True, stop=True),
                       R=[B_kht[ki], B_v], W=[B_ps[pd]])
                    op("dve", lambda e: e.scalar_tensor_tensor(out=Sst[:, d_, :], in0=Sst[:, d_, :], scalar=EF[:, d_, c:c + 1],
                                                               in1=psb[pd][:, 0:128], op0=ALU.mult, op1=ALU.add),
                       R=[B_S[d_], B_E, B_ps[pd]], W=[B_S[d_]])
                for d_ in range(2):
                    emit_suse(NT - 1, d_)

                for b in range(4):
                    s, n, _ = BLOCKS[b]
                    pi_ = b % 2
                    for k in range(KD):
                        op("pe", lambda e: e.matmul(psb[pi_][:, :n], lhsT=wG[:, k, :], rhs=hT[:, k, s:s + n],
                                                    start=(k == 0), stop=(k == KD - 1)),
                           R=[B_wr[sY], B_hT], W=[B_ps[pi_]], signal=(k == KD - 1))
                    op("act", lambda e: e.activation(out=sgT[:, s:s + n], in_=psb[pi_][:, :n], func=AF.Silu),
                       R=[B_ps[pi_]], W=[B_sg])

                def out_block(b):
                    s, n, _ = BLOCKS[b]
                    bp = b % 2
                    iO = 6 if bp == 0 else 4
                    pO = psb[iO]

                    def scores(cc):
                        c = b * 4 + cc
                        cs = slice(c * 128, (c + 1) * 128)
                        ais = []
                        for d_ in range(2):
                            pa = 2 + (cnt_["at"] % 2)
                            ai = cnt_["at"] % 8
                            cnt_["at"] += 1
                            op("pe", lambda e: e.matmul(psb[pa][:, 0:128], lhsT=arr[d_][1][:, cs], rhs=arr[d_][0][:, cs],
                                                        start=True, stop=True),
                               R=[B_arr[d_][1], B_arr[d_][0]], W=[B_ps[pa]])
                            op("dve", lambda e: e.tensor_tensor(out=ATs[ai][:], in0=psb[pa][:, 0:128],
                                                                in1=msk[:, d_ * 128:(d_ + 1) * 128], op=ALU.mult),
                               R=[B_ps[pa], B_hc], W=[B_ATs[ai]])
                            ais.append(ai)
                        return ais

                    def outs(cc, ais):
                        c = b * 4 + cc
                        cs = slice(c * 128, (c + 1) * 128)
                        oc = pO[:, cc * 128:(cc + 1) * 128]
                        op("pe", lambda e: e.matmul(oc, lhsT=Suse[:, 0, c, :], rhs=arr[0][0][:, cs], start=True, stop=False),
                           R=[B_Su, B_arr[0][0]], W=[B_ps[iO]], signal=False)
                        op("pe", lambda e: e.matmul(oc, lhsT=Suse[:, 1, c, :], rhs=arr[1][0][:, cs], start=False, stop=False),
                           R=[B_Su, B_arr[1][0]], W=[B_ps[iO]], signal=False)
                        op("pe", lambda e: e.matmul(oc, lhsT=vv[:, c, :], rhs=ATs[ais[0]][:], start=False, stop=False),
                           R=[B_v, B_ATs[ais[0]]], W=[B_ps[iO]], signal=False)
                        op("pe", lambda e: e.matmul(oc, lhsT=vv[:, c, :], rhs=ATs[ais[1]][:], start=False, stop=True),
                           R=[B_v, B_ATs[ais[1]]], W=[B_ps[iO]], signal=True)

                    pend = None
                    for cc in range(4):
                        ais = scores(cc)
                        if pend is not None:
                            outs(*pend)
                        pend = (cc, ais)
                    outs(*pend)

                def post_block(b):
                    s, n, _ = BLOCKS[b]
                    bp = b % 2
                    iO, iN = (6, 7) if bp == 0 else (4, 5)
                    pO = psb[iO]
                    rstd, B_rstd = rstds[bp], B_rstds[bp]
                    op("act", lambda e: e.activation(out=sq[bp][:, :n], in_=pO[:, :n], func=AF.Square), R=[B_ps[iO]], W=[B_sq[bp]])
                    op("pe", lambda e: e.matmul(psb[iN][:, :n], lhsT=ones_b[:], rhs=sq[bp][:, :n], start=True, stop=True),
                       R=[B_sq[bp], B_const], W=[B_ps[iN]])
                    op("act", lambda e: e.activation(out=rstd[:, :n], in_=psb[iN][:, :n], func=AF.Sqrt, bias=EPS,
                                                     scale=1.0 / 128), R=[B_ps[iN]], W=[B_rstd])
                    op("dve", lambda e: e.reciprocal(out=rstd[:, :n], in_=rstd[:, :n]), R=[B_rstd], W=[B_rstd])
                    op("dve", lambda e: e.scalar_tensor_tensor(out=tmp[bp][:, :n], in0=pO[:, :n], scalar=hgm[:, 0:1],
                                                               in1=rstd[:, :n], op0=ALU.mult, op1=ALU.mult),
                       R=[B_ps[iO], B_rstd, B_hc], W=[B_tmp[bp]])
                    op("dve", lambda e: e.tensor_tensor(out=ogT[:, s:s + n], in0=tmp[bp][:, :n], in1=sgT[:, s:s + n],
                                                         op=ALU.mult), R=[B_tmp[bp], B_sg], W=[B_ogb[b][1]])

                def proj_block(b):
                    s, n, _ = BLOCKS[b]
                    for mch in range(KD):
                        pi_ = mch % 2
                        op("pe", lambda e: e.matmul(psb[pi_][:, :n], lhsT=wO[:, mch * 128:(mch + 1) * 128],
                                                    rhs=ogT[:, s:s + n], start=True, stop=True),
                           R=[B_wr[sY], B_ogb[b]], W=[B_ps[pi_]])
                        g_ap = modv(l, 2, mch, 0)
                        op("dve", lambda e: e.scalar_tensor_tensor(
                            out=xT[:, mch, s:s + n], in0=psb[pi_][:, :n], scalar=g_ap, in1=xT[:, mch, s:s + n],
                            op0=ALU.mult, op1=ALU.add),
                           R=[B_ps[pi_], B_mod, B_x[mch][b]], W=[B_x[mch][b]])

                out_block(0)
                out_block(1)
                post_block(0)
                out_block(2)
                proj_block(0)
                post_block(1)
                out_block(3)
                proj_block(1)
                post_block(2)
                post_block(3)
                proj_block(2)
                proj_block(3)
            S.barrier()

    if "ret" in P.parts:
        retention_layer(0)
    debug_dump("x_mix0", lambda o: [dma("sp", o.rearrange("(k p) t -> p k t", p=128)[:, k, :], xT[:, k, :],
                                        R=B_x[k]) for k in range(KD)])

    if "moe0" in P.parts:
        moe_layer(0, True, P.n_exp_run)
    debug_dump("x_ffn0", lambda o: [dma("sp", o.rearrange("(k p) t -> p k t", p=128)[:, k, :], xT[:, k, :],
                                        R=B_x[k]) for k in range(KD)])
    if "hg" in P.parts:
        hgrn2_layer(1)
    debug_dump("x_mix1", lambda o: [dma("sp", o.rearrange("(k p) t -> p k t", p=128)[:, k, :], xT[:, k, 0:T_LAT],
                                        R=B_x[k][:4]) for k in range(KD)])
    if "moe1" in P.parts:
        moe_layer(1, False, P.n_exp_run)
    debug_dump("x_ffn1", lambda o: [dma("sp", o.rearrange("(k p) t -> p k t", p=128)[:, k, :], xT[:, k, 0:T_LAT],
                                        R=B_x[k][:4]) for k in range(KD)])
    if "final" in P.parts:
        with ExitStack() as fes:
            ftmp = [P.sb(fes, f"ftmp{i}", [128, 512], F32) for i in range(2)]
            B_ft = [Buf("ft0"), Buf("ft1")]
            frs = P.sb(fes, "frs", [128, 512], F32)
            B_frs = Buf("frs")
            fsq = [P.sb(fes, f"fsq{i}", [128, 512], BF16) for i in range(2)]
            B_fsq = [Buf("fsq0"), Buf("fsq1")]
            fo = [P.sb(fes, f"fo{i}", [128, 512], F32) for i in range(4)]
            B_fo = [Buf(f"fo{i}") for i in range(4)]
            osrc = out_d.rearrange("(k p) t -> p k t", p=128)
            oc_ = {"n": 0}
            for b in range(4):
                s, n, _ = BLOCKS[b]

                def dst(k):
                    i = oc_["n"] % 4
                    oc_["n"] += 1
                    dst.last = i
                    return fo[i][:, :n], [B_fo[i]]
                norm_block(1, 0, b, dst, ftmp, B_ft, frs, B_frs, fsq, B_fsq, psn_i=7, final=True,
                           after=lambda k, b=b, s=s, n=n: dma("sp", osrc[:, k, s:s + n], fo[dst.last][:, :n], R=[B_fo[dst.last]]))
    if P.stop_after is not None:
        osrc = out_d.rearrange("(k p) t -> p k t", p=128)
        for k in range(KD):
            dma("sp", osrc[:, k, :], xT[:, k, 0:T_LAT], R=B_x[k][:4])
    S.barrier()
    es.close()
    return P


def _prep_inputs(inp, b, consts, names=None):
    f = np.float32
    m = {}
    m["xT"] = np.ascontiguousarray(np.concatenate([inp["x"][b].T, inp["ctx"][b].T], axis=1)).astype(f)
    cv = np.stack([inp["c"][b], inp["c_ctx"]], axis=0)
    m["cvec"] = np.ascontiguousarray(cv.reshape(2, KD, 128).transpose(2, 0, 1).reshape(128, 2 * KD))
    m["w_ada"] = inp["w_ada"]
    m["b_ada"] = np.ascontiguousarray(np.tile(inp["b_ada"].reshape(1, 12 * D), (2, 1)))
    nr = np.stack([inp["norm_mix"][0], inp["norm_mix"][1], inp["norm_ffn"][0], inp["norm_ffn"][1],
                   inp["norm_final"]], axis=0)
    m["norms"] = np.ascontiguousarray(nr.reshape(5, KD, 128).transpose(2, 0, 1).reshape(128, 5 * KD))
    m["ret_w_in"] = inp["ret_w_in"][0]
    m["ret_w_out"] = inp["ret_w_out"][0]
    m["hg_w_in"] = inp["hg_w_in"][0]
    m["hg_w_out"] = inp["hg_w_out"][0]
    lb = inp["hg_lower_bounds"].reshape(4, KD, 128).transpose(2, 0, 1).reshape(128, 4 * KD)
    m["hg_misc"] = np.ascontiguousarray(np.concatenate([inp["hg_g_norm"][0].reshape(128, 1), lb], axis=1)).astype(f)
    m["moe_router"] = inp["moe_router"]
    m.update(consts)
    for l in range(2):
        for e in range(N_EXP):
            m[f"wg_{l}_{e}"] = inp["moe_w_gate"][l, e]
            m[f"wu_{l}_{e}"] = inp["moe_w_up"][l, e]
            m[f"wd_{l}_{e}"] = inp["moe_w_down"][l, e]
    if names is not None:
        m = {k: v for k, v in m.items() if k in names}
    return m


def kernel(**inputs):
    inp = {k: np.asarray(v) for k, v in inputs.items()}
    consts = _const_tables()
    P = build_program()
    names = set(P.dram.keys())
    in_maps = [_prep_inputs(inp, b, consts, names) for b in range(8)]
    res = run_bass_kernel_spmd(P.nc, in_maps, core_ids=list(range(8)))
    out = np.stack([np.ascontiguousarray(res.results[b]["outT"].T) for b in range(8)], axis=0)
    return out.astype(np.float32)
```

```python
import math
from contextlib import ExitStack

import numpy as np
import concourse.bass as bass
import concourse.mybir as mybir
from concourse.bass_utils import run_bass_kernel_spmd

F32 = mybir.dt.float32
BF16 = mybir.dt.bfloat16
ALU = mybir.AluOpType
AF = mybir.ActivationFunctionType
AX = mybir.AxisListType

D = 1024
KD = 8
T_LAT = 2048
T_CTX = 256
T_ALL = T_LAT + T_CTX
NT = T_ALL // 128
EPS = 1e-6
N_EXP = 16
FF = 2816
NFC = FF // 128
RET_H = 4
HG_H = 8

BLOCKS = [(0, 512, False), (512, 512, False), (1024, 512, False), (1536, 512, False), (2048, 256, True)]


def interleave(*gens):
    gens = list(gens)
    while gens:
        for g in list(gens):
            try:
                next(g)
            except StopIteration:
                gens.remove(g)


class Buf:
    __slots__ = ("name", "w", "rs")

    def __init__(self, name):
        self.name = name
        self.w = None
        self.rs = {}


class Sched:
    NDMA = 12

    def __init__(self, nc, es):
        self.nc = nc
        self.h = {"pe": nc.tensor, "act": nc.scalar, "dve": nc.vector, "pool": nc.gpsimd, "sp": nc.sync}
        self.sem = {}
        self.cnt = {}
        self.seen = {}
        for e in self.h:
            self.sem[e] = es.enter_context(nc.semaphore("s_" + e))
            self.cnt[e] = 0
            self.seen[e] = {}
        self.dsem = {}
        self.dval = {}
        self.dnext = {}
        for q in ("sp", "pool"):
            self.dsem[q] = [es.enter_context(nc.semaphore(f"d_{q}{i}")) for i in range(self.NDMA)]
            self.dval[q] = [0] * self.NDMA
            self.dnext[q] = 0
        self.ninst = 0

    def _wait(self, eng, tk):
        if tk is None:
            return
        kind = tk[0]
        if kind == "c":
            _, src, n = tk
            if src == eng and eng in ("pe", "sp"):
                return
            if self.seen[eng].get(src, 0) >= n:
                return
            self.h[eng].wait_ge(self.sem[src], n)
            self.seen[eng][src] = n
        else:
            _, q, idx, val = tk
            key = (q, idx)
            if self.seen[eng].get(key, 0) >= val:
                return
            self.h[eng].wait_ge(self.dsem[q][idx], val)
            self.seen[eng][key] = val
        self.ninst += 1

    def _deps(self, eng, R, W):
        for b in R:
            self._wait(eng, b.w)
        for b in W:
            self._wait(eng, b.w)
            for tk in b.rs.values():
                self._wait(eng, tk)

    def _record(self, tk, R, W):
        for b in W:
            b.w = tk
            b.rs = {}
        for b in R:
            if b in W:
                continue
            key = tk[1] if tk[0] == "c" else (tk[1], tk[2])
            b.rs[key] = tk

    @staticmethod
    def _flat(L):
        out = []
        for b in L:
            if isinstance(b, (list, tuple)):
                out.extend(Sched._flat(b))
            else:
                out.append(b)
        return out

    def op(self, eng, fn, R=(), W=(), signal=True):
        R, W = self._flat(R), self._flat(W)
        self._deps(eng, R, W)
        ins = fn(self.h[eng])
        self.ninst += 1
        if signal:
            ins.then_inc(self.sem[eng], 1)
            self.cnt[eng] += 1
            tk = ("c", eng, self.cnt[eng])
            if eng not in ("pe",):
                pass
        else:
            tk = ("c", eng, self.cnt[eng] + 1)
        self._record(tk, R, W)
        return tk

    def dma(self, q, out, in_, R=(), W=()):
        R, W = self._flat(R), self._flat(W)
        self._deps(q, R, W)
        idx = self.dnext[q]
        self.dnext[q] = (idx + 1) % self.NDMA
        prev = self.dval[q][idx]
        if prev > 0:
            self._wait(q, ("d", q, idx, prev))
        val = prev + 16
        self.dval[q][idx] = val
        self.h[q].dma_start(out=out, in_=in_).then_inc(self.dsem[q][idx], 16)
        self.ninst += 1
        tk = ("d", q, idx, val)
        self._record(tk, R, W)
        return tk

    def barrier(self):
        for e in self.h:
            for src in self.h:
                if src != e and self.cnt[src] > 0:
                    self._wait(e, ("c", src, self.cnt[src]))
            for q in ("sp", "pool"):
                for idx in range(self.NDMA):
                    if self.dval[q][idx] > 0:
                        self._wait(e, ("d", q, idx, self.dval[q][idx]))


def _ret_gammas():
    j = np.arange(8, dtype=np.float64)
    g = 1.0 - np.exp2(-5.0 - j / 2)
    return g[0::2], g[1::2]


def _const_tables():
    t = {}
    half = 128
    inv = 10000.0 ** (-np.arange(0, half, 2, dtype=np.float64) / half)
    p = np.arange(128)
    sign = np.where(p < 64, -1.0, 1.0)
    rows = np.arange(T_LAT // 64, dtype=np.float64)
    cols = np.arange(64, dtype=np.float64)
    ang_r = rows[None, :] * inv[p % 64][:, None]
    ang_c = cols[None, :] * inv[p % 64][:, None]
    t["rope"] = np.concatenate(
        [np.cos(ang_r), np.sin(ang_r) * sign[:, None], np.cos(ang_c), np.sin(ang_c) * sign[:, None]], axis=1
    ).astype(np.float32)
    gf, gb = _ret_gammas()
    b = np.arange(128, dtype=np.float64)[:, None]
    a = np.arange(512, dtype=np.float64)[None, :]
    tabs = np.zeros((RET_H, 128, 1920), dtype=np.float64)
    xs = np.arange(896, dtype=np.float64)[None, :] - 384.0
    for h in range(RET_H):
        tabs[h, :, 0:512] = gf[h] ** (a - b)
        tabs[h, :, 512:1024] = gb[h] ** (b + 511 - a)
        dl = xs - b
        tabs[h, :, 1024:1920] = np.where(dl > 0, gf[h] ** np.maximum(dl, 0),
                                         np.where(dl < 0, gb[h] ** np.maximum(-dl, 0), 2.0))
    t["rdec"] = tabs.astype(np.float32)
    t["ident"] = np.eye(128, dtype=np.float32)
    t["hgmask"] = np.concatenate([np.triu(np.ones((128, 128))), np.tril(np.ones((128, 128)))], axis=1).astype(np.float32)
    t["iota1"] = np.tile(np.arange(1, 257, dtype=np.float32)[None, :], (128, 1))
    return t


class Prog:
    def __init__(self, dbg=None, stop_after=None):
        self.dbg = dbg or []
        self.stop_after = stop_after
        self.nc = bass.Bass("TRN2", target_bir_lowering=False)
        self.es = ExitStack()
        self.S = Sched(self.nc, self.es)
        self.dram = {}
        self.dbg_out = {}

    def din(self, name, shape, dt=F32):
        self.dram[name] = self.nc.dram_tensor(name, list(shape), dt, kind="ExternalInput").ap()
        return self.dram[name]

    def dout(self, name, shape, dt=F32):
        self.dram[name] = self.nc.dram_tensor(name, list(shape), dt, kind="ExternalOutput").ap()
        return self.dram[name]

    def sb(self, es, name, shape, dt):
        self._uid = getattr(self, "_uid", 0) + 1
        return es.enter_context(self.nc.sbuf_tensor(f"{name}_u{self._uid}", list(shape), dt))

    def ps(self, es, name, shape, dt=F32):
        return es.enter_context(self.nc.psum_tensor(name, list(shape), dt))


def build_program(dbg=(), stop_after=None, parts=("ret", "moe0", "hg", "moe1", "final"), n_exp_run=N_EXP):
    P = Prog(list(dbg), stop_after)
    P.parts = parts
    P.n_exp_run = n_exp_run
    nc, S, es = P.nc, P.S, P.es
    op, dma = S.op, S.dma

    xT_d = P.din("xT", [D, T_ALL])
    cvec_d = P.din("cvec", [128, 2 * KD])
    wada_d = P.din("w_ada", [2, D, 6 * D])
    bada_d = P.din("b_ada", [2, 12 * D])
    nrm_d = P.din("norms", [128, 5 * KD])
    retin_d = P.din("ret_w_in", [D, 6144])
    retout_d = P.din("ret_w_out", [2048, D])
    router_d = P.din("moe_router", [2, D, N_EXP])
    rope_d = P.din("rope", [128, 192])
    rdec_d = P.din("rdec", [RET_H, 128, 1920])
    ident_d = P.din("ident", [128, 128])
    out_d = P.dout("outT", [D, T_LAT])
    for name, shape, dt_ in P.dbg:
        P.dbg_out[name] = P.dout("dbg_" + name, shape, dt_)

    xT = P.sb(es, "xT_sb", [128, KD, T_ALL], F32)
    B_x = [[Buf(f"x{k}_{b}") for b in range(5)] for k in range(KD)]
    cvec = P.sb(es, "cvec_sb", [128, 2 * KD], F32)
    scc = P.sb(es, "scc", [128, KD, 2], F32)
    nrm = P.sb(es, "nrm", [128, 5 * KD], F32)
    mod = P.sb(es, "mod", [128, 2, 48, 2], F32)
    modA = P.sb(es, "modA", [128, 2, 2, KD, 2], F32)
    ident_f = P.sb(es, "ident_f", [128, 128], F32)
    ident_b = P.sb(es, "ident_b", [128, 128], BF16)
    ones_b = P.sb(es, "ones_b", [128, 128], BF16)
    B_const = Buf("const")
    B_mod = Buf("mod")

    psb = [P.ps(es, f"psb{i}", [128, 512], F32) for i in range(8)]
    B_ps = [Buf(f"ps{i}") for i in range(8)]

    def sl(b):
        s, n, _ = BLOCKS[b]
        return slice(s, s + n)

    xsrc = xT_d.rearrange("(k p) t -> p k t", p=128)
    for k in range(KD):
        dma("sp", xT[:, k, :], xsrc[:, k, :], W=B_x[k])
    dma("sp", cvec[:], cvec_d, W=[B_const])
    dma("sp", nrm[:], nrm_d, W=[B_const])
    dma("sp", ident_f[:], ident_d, W=[B_const])
    op("act", lambda e: e.copy(out=ident_b[:], in_=ident_f[:]), R=[B_const], W=[B_const])
    op("dve", lambda e: e.memset(ones_b[:], 1.0), W=[B_const])
    op("act", lambda e: e.activation(out=scc[:].rearrange("p k c -> p c k"),
                                     in_=cvec[:].rearrange("p (c k) -> p c k", c=2), func=AF.Silu),
       R=[B_const], W=[B_const])

    with ExitStack() as pes:
        NPIECE = 12
        NST = 4
        wa = [P.sb(pes, f"wa{i}", [128, KD, 512], F32) for i in range(NST)]
        B_wa = [Buf(f"wa{i}") for i in range(NST)]
        HALF = 3 * D
        modrow = P.sb(pes, "modrow", [2, HALF], F32)
        B_mr = Buf("modrow")
        bada2 = P.sb(pes, "bada2", [2, HALF], F32)
        B_b2 = Buf("bada2")
        pi = 0
        for l in range(2):
            wsrc = wada_d[l].rearrange("(k p) f -> p k f", p=128)
            for hf in range(2):
                dma("sp", bada2[:], bada_d[:, l * 6 * D + hf * HALF: l * 6 * D + (hf + 1) * HALF], W=[B_b2])
                for pc6 in range(6):
                    pc = hf * 6 + pc6
                    slot = pi % NST
                    dma("sp", wa[slot][:], wsrc[:, :, pc * 512:(pc + 1) * 512], W=[B_wa[slot]])
                    pb = pi % 2
                    for k in range(KD):
                        op("pe", lambda e: e.matmul(psb[pb][0:2, :], lhsT=scc[:, k, :], rhs=wa[slot][:, k, :],
                                                    start=(k == 0), stop=(k == KD - 1)),
                           R=[B_wa[slot], B_const], W=[B_ps[pb]], signal=(k == KD - 1))
                    op("dve", lambda e: e.tensor_tensor(out=modrow[:, pc6 * 512:(pc6 + 1) * 512], in0=psb[pb][0:2, :],
                                                        in1=bada2[:, pc6 * 512:(pc6 + 1) * 512],
                                                        op=ALU.add), R=[B_ps[pb], B_b2], W=[B_mr])
                    pi += 1
                pt_ = 2 + (l * 2 + hf) % 2
                for j in range(24):
                    op("pe", lambda e: e.transpose(out=psb[pt_][:, j * 2:(j + 1) * 2], in_=modrow[:, j * 128:(j + 1) * 128],
                                                   identity=ident_f[0:2, 0:2]),
                       R=[B_mr, B_const], W=[B_ps[pt_]], signal=(j == 23))
                op("dve", lambda e: e.tensor_copy(out=mod[:, l, hf * 24:(hf + 1) * 24, :].rearrange("p j c -> p (j c)"),
                                                  in_=psb[pt_][:, 0:48]), R=[B_ps[pt_]], W=[B_mod])
        for l in range(2):
            for site in range(2):
                sc_j = (1 if site == 0 else 4) * KD
                nw = nrm[:, (site * 2 + l) * KD:(site * 2 + l + 1) * KD]
                op("dve", lambda e, l=l, site=site, sc_j=sc_j, nw=nw: e.scalar_tensor_tensor(
                    out=modA[:, l, site, :, :], in0=mod[:, l, sc_j:sc_j + KD, :], scalar=1.0,
                    in1=nw.unsqueeze(2).to_broadcast([128, KD, 2]), op0=ALU.add, op1=ALU.mult),
                   R=[B_mod, B_const], W=[B_mod])
        S.barrier()

    def modv(l, chunk, k, c):
        return mod[:, l, chunk * KD + k, c:c + 1]

    def norm_block(l, site, b, dst_fn, tmp, B_tmp, rstd, B_rstd, sq, B_sq, psn_i, final=False, after=None):
        s, n, isc = BLOCKS[b]
        c = 1 if isc else 0
        for k in range(KD):
            q = k % 2
            op("act", lambda e, k=k, q=q: e.activation(out=sq[q][:, :n], in_=xT[:, k, s:s + n], func=AF.Square),
               R=[B_x[k][b]], W=[B_sq[q]])
            op("pe", lambda e, k=k, q=q: e.matmul(psb[psn_i][:, :n], lhsT=ones_b[:], rhs=sq[q][:, :n],
                                                 start=(k == 0), stop=(k == KD - 1)),
               R=[B_sq[q], B_const], W=[B_ps[psn_i]], signal=True)
        op("act", lambda e: e.activation(out=rstd[:, :n], in_=psb[psn_i][:, :n], func=AF.Sqrt, bias=EPS,
                                         scale=1.0 / D), R=[B_ps[psn_i]], W=[B_rstd])
        op("dve", lambda e: e.reciprocal(out=rstd[:, :n], in_=rstd[:, :n]), R=[B_rstd], W=[B_rstd])
        for k in range(KD):
            q = k % 2
            out_ap, obufs = dst_fn(k)
            if final:
                a_ap = nrm[:, 4 * KD + k:4 * KD + k + 1]
                op("dve", lambda e, k=k, a_ap=a_ap, out_ap=out_ap: e.scalar_tensor_tensor(
                    out=out_ap, in0=xT[:, k, s:s + n], scalar=a_ap, in1=rstd[:, :n], op0=ALU.mult, op1=ALU.mult),
                   R=[B_x[k][b], B_rstd, B_const], W=obufs)
                if after is not None:
                    after(k)
            else:
                a_ap = modA[:, l, site, k, c:c + 1]
                sh_ap = modv(l, 0 if site == 0 else 3, k, c)
                op("dve", lambda e, k=k, q=q, a_ap=a_ap: e.scalar_tensor_tensor(
                    out=tmp[q][:, :n], in0=xT[:, k, s:s + n], scalar=a_ap, in1=rstd[:, :n],
                    op0=ALU.mult, op1=ALU.mult), R=[B_x[k][b], B_rstd, B_mod], W=[B_tmp[q]])
                op("act", lambda e, q=q, sh_ap=sh_ap, out_ap=out_ap: e.activation(
                    out=out_ap, in_=tmp[q][:, :n], func=AF.Identity, bias=sh_ap, scale=1.0),
                   R=[B_tmp[q], B_mod], W=obufs)

    def debug_dump(name, ap_fn):
        if name in P.dbg_out:
            S.barrier()
            tk = ap_fn(P.dbg_out[name])
            S.barrier()

    NSLOT = 4
    wring = [P.sb(es, f"wring{i}", [128, KD, 512], BF16) for i in range(NSLOT)]
    B_wr = [Buf(f"wr{i}") for i in range(NSLOT)]
    wr_state = {"n": 0}

    def wslot():
        i = wr_state["n"] % NSLOT
        wr_state["n"] += 1
        return i

    def retention_layer(l):
        with ExitStack() as les:
            hT = P.sb(les, "hT", [128, KD, T_ALL], BF16)
            B_h = [[Buf(f"h{k}_{b}") for b in range(5)] for k in range(KD)]
            tmp = [P.sb(les, f"ntmp{i}", [128, 512], F32) for i in range(2)]
            B_tmp = [Buf("ntmp0"), Buf("ntmp1")]
            sg = [P.sb(les, f"sg{i}", [128, 512], F32) for i in range(2)]
            B_sg = [Buf("sg0"), Buf("sg1")]
            rstd = P.sb(les, "rstd", [128, 512], F32)
            B_rstd = Buf("rstd")
            sq = [P.sb(les, f"sq{i}", [128, 512], BF16) for i in range(2)]
            B_sq = [Buf("sq0"), Buf("sq1")]
            rope_sb = P.sb(les, "rope_sb", [128, 192], F32)
            B_rope = Buf("rope")
            dma("sp", rope_sb[:], rope_d, W=[B_rope])
            for b in range(5):
                s, n, _ = BLOCKS[b]
                norm_block(l, 0, b, lambda k, b=b, s=s, n=n: (hT[:, k, s:s + n], [B_h[k][b]]),
                           tmp, B_tmp, rstd, B_rstd, sq, B_sq, psn_i=7)
            debug_dump("hT0", lambda o: [dma("sp", o.rearrange("(k p) t -> p k t", p=128)[:, k, :], hT[:, k, :],
                                             R=B_h[k]) for k in range(KD)])
            if P.stop_after == "hT0":
                return

            qr = P.sb(les, "qr", [128, 2, 2, 512], BF16)
            kr = P.sb(les, "kr", [128, 2, T_ALL], BF16)
            vv = P.sb(les, "vv", [128, NT, 512], BF16)
            B_q = [[Buf(f"q{i}_{m}") for m in range(2)] for i in range(2)]
            B_k = [[Buf(f"k{m}_{t}") for t in range(NT)] for m in range(2)]
            B_v = [Buf(f"v{t}") for t in range(NT)]
            rdec = P.sb(les, "rdec_sb", [128, 1920], F32)
            B_rdec = Buf("rdec")
            Ff = rdec[:, 0:512]
            Fb = rdec[:, 512:1024]

            def Dg(kk, n):
                return rdec[:, 1024 + 384 - 128 * kk: 1024 + 384 - 128 * kk + n]
            rt1, B_rt1 = tmp, B_tmp
            rt2, B_rt2 = sg, B_sg
            ctab, B_ctab = tmp, B_tmp
            AT = [P.sb(les, f"AT{i}", [128, 512], BF16) for i in range(3)]
            B_AT = [Buf(f"AT{i}") for i in range(3)]
            ogT = P.sb(les, "ogT", [128, 4, 512], BF16)
            B_og = [Buf(f"og{c}") for c in range(4)]
            sgb = P.sb(les, "sgb", [128, 4, 512], BF16)
            B_sgb = [Buf(f"sgb{c}") for c in range(4)]
            gf, gb = _ret_gammas()
            rcount = {"rope": 0, "at": 0, "q": 0}

            win_src = retin_d.rearrange("(k p) f -> p k f", p=128)
            wout_src = retout_d.rearrange("(c p) f -> p c f", p=128)

            def proj_rope(sA, qk, m, b, dst_ap, wb):
                s, n, isc = BLOCKS[b]
                pi_ = rcount["rope"] % 2
                rcount["rope"] += 1
                pst = psb[pi_]
                for k in range(KD):
                    op("pe", lambda e, k=k: e.matmul(
                        pst[:, :n], lhsT=wring[sA][:, k, qk * 256 + m * 128: qk * 256 + (m + 1) * 128],
                        rhs=hT[:, k, s:s + n], start=(k == 0), stop=(k == KD - 1)),
                       R=[B_wr[sA], B_h[k][b]], W=[B_ps[pi_]], signal=(k == KD - 1))
                scale = 1.0 if qk == 0 else 1.0 / 16.0
                if isc:
                    op("act", lambda e: e.activation(out=dst_ap, in_=pst[:, :n], func=AF.Copy, scale=scale),
                       R=[B_ps[pi_]], W=wb)
                    return
                g0 = s // 64
                if m == 0:
                    cos_ap = rope_sb[:, g0:g0 + 8].unsqueeze(2).to_broadcast([128, 8, 64])
                    sin_lo = rope_sb[0:64, 32 + g0:32 + g0 + 8].unsqueeze(2).to_broadcast([64, 8, 64])
                    sin_hi = rope_sb[64:128, 32 + g0:32 + g0 + 8].unsqueeze(2).to_broadcast([64, 8, 64])
                else:
                    cos_ap = rope_sb[:, 64:128].unsqueeze(1).to_broadcast([128, 8, 64])
                    sin_lo = rope_sb[0:64, 128:192].unsqueeze(1).to_broadcast([64, 8, 64])
                    sin_hi = rope_sb[64:128, 128:192].unsqueeze(1).to_broadcast([64, 8, 64])
                ti = pi_
                p3 = pst[:].rearrange("p (g t) -> p g t", t=64)
                t1v = rt1[ti][:].rearrange("p (g t) -> p g t", t=64)
                t2v = rt2[ti][:].rearrange("p (g t) -> p g t", t=64)
                op("dve", lambda e: e.scalar_tensor_tensor(
                    out=t1v, in0=p3, scalar=scale, in1=cos_ap, op0=ALU.mult, op1=ALU.mult),
                   R=[B_ps[pi_], B_rope], W=[B_rt1[ti]])
                op("dve", lambda e: e.scalar_tensor_tensor(
                    out=t2v[0:64], in0=p3[64:128], scalar=scale, in1=sin_lo, op0=ALU.mult, op1=ALU.mult),
                   R=[B_ps[pi_], B_rope], W=[B_rt2[ti]])
                op("dve", lambda e: e.scalar_tensor_tensor(
                    out=t2v[64:128], in0=p3[0:64], scalar=scale, in1=sin_hi, op0=ALU.mult, op1=ALU.mult),
                   R=[B_ps[pi_], B_rope, B_rt2[ti]], W=[B_rt2[ti]])
                op("dve", lambda e: e.tensor_tensor(
                    out=dst_ap, in0=rt1[ti][:, :n], in1=rt2[ti][:, :n], op=ALU.add),
                   R=[B_rt1[ti], B_rt2[ti]], W=wb)

            for h in range(RET_H):
                sA, sB, sC, sD = wslot(), wslot(), wslot(), wslot()
                dma("pool", wring[sA][:, :, 0:256], win_src[:, :, h * 256:(h + 1) * 256], W=[B_wr[sA]])
                dma("pool", wring[sA][:, :, 256:512], win_src[:, :, 1024 + h * 256:1024 + (h + 1) * 256], W=[B_wr[sA]])
                dma("pool", wring[sB][:], win_src[:, :, 2048 + h * 512:2048 + (h + 1) * 512], W=[B_wr[sB]])
                dma("pool", wring[sC][:], win_src[:, :, 4096 + h * 512:4096 + (h + 1) * 512], W=[B_wr[sC]])
                wD = wring[sD][:].rearrange("p k f -> p (k f)").rearrange("p (c f) -> p c f", c=4)
                dma("pool", wD, wout_src[:, h * 4:(h + 1) * 4, :], W=[B_wr[sD]])
                dma("sp", rdec[:], rdec_d[h], W=[B_rdec])

                def kproj_gen():
                    for m in range(2):
                        for b in range(5):
                            s, n, isc = BLOCKS[b]
                            proj_rope(sA, 1, m, b, kr[:, m, s:s + n], [B_k[m][t] for t in range(s // 128, (s + n) // 128)])
                            yield

                def vproj_gen():
                    for t in range(NT):
                        b = min(t // 4, 4)
                        pi_ = 6 + (t % 2)
                        for k in range(KD):
                            op("pe", lambda e, k=k, t=t, pi_=pi_: e.matmul(
                                psb[pi_][:, :], lhsT=hT[:, k, t * 128:(t + 1) * 128], rhs=wring[sB][:, k, :],
                                start=(k == 0), stop=(k == KD - 1)),
                               R=[B_wr[sB], B_h[k][b]], W=[B_ps[pi_]], signal=(k == KD - 1))
                        op("act", lambda e, t=t, pi_=pi_: e.copy(out=vv[:, t, :], in_=psb[pi_][:, :]),
                           R=[B_ps[pi_]], W=[B_v[t]])
                        yield
                interleave(kproj_gen(), vproj_gen())
                if h == 0:
                    debug_dump("kr0", lambda o: [dma("sp", o[:, m, :], kr[:, m, :], R=B_k[m]) for m in range(2)])
                    debug_dump("vv0", lambda o: [dma("sp", o, vv[:], R=B_v)])
                    if P.stop_after == "proj0":
                        return

                def block_gen(b):
                    s, n, isc = BLOCKS[b]
                    c = 1 if isc else 0
                    qi = rcount["q"] % 2
                    rcount["q"] += 1
                    for m in range(2):
                        proj_rope(sA, 0, m, b, qr[:, qi, m, :n], [B_q[qi][m]])
                    yield
                    for cc in range(4):
                        pg = cc % 2
                        for k in range(KD):
                            op("pe", lambda e: e.matmul(
                                psb[pg][:, :n], lhsT=wring[sC][:, k, cc * 128:(cc + 1) * 128], rhs=hT[:, k, s:s + n],
                                start=(k == 0), stop=(k == KD - 1)),
                               R=[B_wr[sC], B_h[k][b]], W=[B_ps[pg]], signal=(k == KD - 1))
                        op("act", lambda e: e.activation(out=sgb[:, cc, :n], in_=psb[pg][:, :n], func=AF.Silu),
                           R=[B_ps[pg]], W=[B_sgb[cc]])
                    keys = [16, 17] if isc else list(range(NT))
                    pso = [psb[2 + cc] for cc in range(4)]
                    B_pso = [B_ps[2 + cc] for cc in range(4)]

                    def emit_scores(j, idx):
                        pi_ = 6 + (idx % 2)
                        for m in range(2):
                            op("pe", lambda e, m=m: e.matmul(
                                psb[pi_][:, :n], lhsT=kr[:, m, j * 128:(j + 1) * 128], rhs=qr[:, qi, m, :n],
                                start=(m == 0), stop=(m == 1)),
                               R=[B_k[m][j], B_q[qi][m]], W=[B_ps[pi_]], signal=(m == 1))
                        ai = rcount["at"] % 3
                        rcount["at"] += 1
                        pst = psb[pi_]
                        if isc:
                            tab = Dg(j - 16, n)
                            op("dve", lambda e: e.tensor_tensor(out=AT[ai][:, :n], in0=pst[:, :n], in1=tab, op=ALU.mult),
                               R=[B_ps[pi_], B_rdec], W=[B_AT[ai]])
                        elif j >= 16:
                            jc = j - 16
                            s1 = float(gf[h] ** (s + 256 - 128 * jc))
                            s2 = float(gb[h] ** (T_LAT - s - 511 + 128 * jc))
                            ci = jc
                            op("pool", lambda e: e.tensor_scalar(
                                out=ctab[ci][:], in0=Ff, scalar1=s1, scalar2=None, op0=ALU.mult),
                               R=[B_rdec], W=[B_ctab[ci]])
                            op("dve", lambda e: e.scalar_tensor_tensor(
                                out=ctab[ci][:], in0=Fb, scalar=s2, in1=ctab[ci][:], op0=ALU.mult, op1=ALU.add),
                               R=[B_rdec, B_ctab[ci]], W=[B_ctab[ci]])
                            op("dve", lambda e: e.tensor_tensor(
                                out=AT[ai][:, :n], in0=pst[:, :n], in1=ctab[ci][:, :n], op=ALU.mult),
                               R=[B_ps[pi_], B_ctab[ci]], W=[B_AT[ai]])
                        else:
                            rel = j - 4 * b
                            if 0 <= rel < 4:
                                tab = Dg(rel, 512)
                                op("dve", lambda e: e.tensor_tensor(out=AT[ai][:], in0=pst[:], in1=tab, op=ALU.mult),
                                   R=[B_ps[pi_], B_rdec], W=[B_AT[ai]])
                            elif rel < 0:
                                sc = float(gf[h] ** (s - 128 * j))
                                op("dve", lambda e: e.scalar_tensor_tensor(
                                    out=AT[ai][:], in0=pst[:], scalar=sc, in1=Ff, op0=ALU.mult, op1=ALU.mult),
                                   R=[B_ps[pi_], B_rdec], W=[B_AT[ai]])
                            else:
                                sc = float(gb[h] ** (128 * j - s - 511))
                                op("dve", lambda e: e.scalar_tensor_tensor(
                                    out=AT[ai][:], in0=pst[:], scalar=sc, in1=Fb, op0=ALU.mult, op1=ALU.mult),
                                   R=[B_ps[pi_], B_rdec], W=[B_AT[ai]])
                        return ai

                    def emit_av(j, ai, first, last):
                        for cc in range(4):
                            op("pe", lambda e, cc=cc: e.matmul(
                                pso[cc][:, :n], lhsT=vv[:, j, cc * 128:(cc + 1) * 128], rhs=AT[ai][:, :n],
                                start=first, stop=last),
                               R=[B_v[j], B_AT[ai]], W=[B_pso[cc]], signal=(cc == 3))

                    pend = None
                    for idx, j in enumerate(keys):
                        ai = emit_scores(j, idx)
                        if pend is not None:
                            emit_av(*pend)
                        pend = (j, ai, idx == 0, idx == len(keys) - 1)
                    emit_av(*pend)

                    yield
                    for cc in range(4):
                        q2 = cc % 2
                        op("act", lambda e, cc=cc, q2=q2: e.activation(out=sq[q2][:, :n], in_=pso[cc][:, :n],
                                                                      func=AF.Square),
                           R=[B_pso[cc]], W=[B_sq[q2]])
                        op("pe", lambda e, cc=cc, q2=q2: e.matmul(psb[6][:, :n], lhsT=ones_b[:], rhs=sq[q2][:, :n],
                                                                 start=(cc == 0), stop=(cc == 3)),
                           R=[B_sq[q2], B_const], W=[B_ps[6]], signal=True)
                    op("act", lambda e: e.activation(out=rstd[:, :n], in_=psb[6][:, :n], func=AF.Sqrt, bias=EPS,
                                                     scale=1.0 / 512), R=[B_ps[6]], W=[B_rstd])
                    op("dve", lambda e: e.reciprocal(out=rstd[:, :n], in_=rstd[:, :n]), R=[B_rstd], W=[B_rstd])
                    for cc in range(4):
                        q2 = cc % 2
                        op("dve", lambda e, cc=cc, q2=q2: e.tensor_tensor(
                            out=tmp[q2][:, :n], in0=pso[cc][:, :n], in1=rstd[:, :n], op=ALU.mult),
                           R=[B_pso[cc], B_rstd], W=[B_tmp[q2]])
                        op("dve", lambda e, cc=cc, q2=q2: e.tensor_tensor(
                            out=ogT[:, cc, :n], in0=tmp[q2][:, :n], in1=sgb[:, cc, :n], op=ALU.mult),
                           R=[B_tmp[q2], B_sgb[cc]], W=[B_og[cc]])
                    for mch in range(KD):
                        pi_ = 6 + (mch % 2)
                        for cc in range(4):
                            op("pe", lambda e, cc=cc, mch=mch, pi_=pi_: e.matmul(
                                psb[pi_][:, :n], lhsT=wD[:, cc, mch * 128:(mch + 1) * 128], rhs=ogT[:, cc, :n],
                                start=(cc == 0), stop=(cc == 3)),
                               R=[B_wr[sD], B_og[cc]], W=[B_ps[pi_]], signal=(cc == 3))
                        g_ap = modv(l, 2, mch, c)
                        op("dve", lambda e, mch=mch, pi_=pi_, g_ap=g_ap: e.scalar_tensor_tensor(
                            out=xT[:, mch, s:s + n], in0=psb[pi_][:, :n], scalar=g_ap, in1=xT[:, mch, s:s + n],
                            op0=ALU.mult, op1=ALU.add),
                           R=[B_ps[pi_], B_mod, B_x[mch][b]], W=[B_x[mch][b]])

                gens = [block_gen(b) for b in range(5)]
                next(gens[0])
                for b in range(5):
                    next(gens[b])
                    if b + 1 < 5:
                        next(gens[b + 1])
                    for _ in gens[b]:
                        pass
            S.barrier()

    iota_d2 = P.din("iota1", [128, 256])

    def moe_layer(l, with_ctx, n_exp_run=N_EXP):
        nblk = 5 if with_ctx else 4
        ntile = NT if with_ctx else 16
        NS = 288 if with_ctx else 256
        nst = 3 if with_ctx else 2
        st_sz = [128, 128, 32][:nst]
        st_off = [0, 128, 256][:nst]
        with ExitStack() as mes:
            h2tok = P.sb(mes, "h2tok", [128, NT, D], BF16)
            B_h2 = [Buf(f"h2t{t}") for t in range(NT)]
            wr_sb = P.sb(mes, "wr_sb", [128, KD, N_EXP], F32)
            B_wrt = Buf("wrt")
            dma("sp", wr_sb[:], router_d[l].rearrange("(k p) e -> p k e", p=128), W=[B_wrt])
            iota1 = P.sb(mes, "iota1_sb", [128, 256], F32)
            dma("sp", iota1[:], iota_d2, W=[B_wrt])
            aff = P.sb(mes, "aff", [128, NT, N_EXP], F32)
            B_aff = Buf("aff")
            aff_hl = P.sb(mes, "aff_hl", [128, NT, N_EXP, 2], BF16)
            posm_tok = P.sb(mes, "posm_tok", [128, NT, N_EXP], F32)
            B_posm = Buf("posm")

            with ExitStack() as r1:
                tmp = [P.sb(r1, f"mtmp{i}", [128, 512], F32) for i in range(2)]
                B_tmp = [Buf("mtmp0"), Buf("mtmp1")]
                rstd = P.sb(r1, "mrstd", [128, 512], F32)
                B_rstd = Buf("mrstd")
                sq = [P.sb(r1, f"msq{i}", [128, 512], BF16) for i in range(2)]
                B_sq = [Buf("msq0"), Buf("msq1")]
                h2f = P.sb(r1, "h2f", [128, KD, 512], F32)
                B_h2f = [Buf(f"h2f{k}") for k in range(KD)]
                h2b = P.sb(r1, "h2b", [128, KD, 512], BF16)
                B_h2b = [Buf(f"h2b{k}") for k in range(KD)]
                for b in range(nblk):
                    s, n, isc = BLOCKS[b]
                    norm_block(l, 1, b, lambda k, n=n: (h2f[:, k, :n], [B_h2f[k]]),
                               tmp, B_tmp, rstd, B_rstd, sq, B_sq, psn_i=7)
                    for k in range(KD):
                        eng_ = "act" if k % 2 == 0 else "dve"
                        if eng_ == "act":
                            op("act", lambda e, k=k: e.copy(out=h2b[:, k, :n], in_=h2f[:, k, :n]), R=[B_h2f[k]], W=[B_h2b[k]])
                        else:
                            op("dve", lambda e, k=k: e.tensor_copy(out=h2b[:, k, :n], in_=h2f[:, k, :n]),
                               R=[B_h2f[k]], W=[B_h2b[k]])
                    for tt in range(n // 128):
                        t = s // 128 + tt
                        for k in range(KD):
                            op("pe", lambda e, k=k: e.matmul(psb[6][:, 0:N_EXP], lhsT=h2f[:, k, tt * 128:(tt + 1) * 128],
                                                            rhs=wr_sb[:, k, :], start=(k == 0), stop=(k == KD - 1)),
                               R=[B_h2f[k], B_wrt], W=[B_ps[6]], signal=(k == KD - 1))
                        op("act", lambda e: e.copy(out=aff[:, t, :], in_=psb[6][:, 0:N_EXP]), R=[B_ps[6]], W=[B_aff])
                        pi_ = t % 2
                        pbf = psb[pi_][:].bitcast(BF16)
                        for k in range(KD):
                            op("pe", lambda e, k=k: e.transpose(out=pbf[:, k * 128:(k + 1) * 128],
                                                               in_=h2b[:, k, tt * 128:(tt + 1) * 128], identity=ident_b[:]),
                               R=[B_h2b[k], B_const], W=[B_ps[pi_]], signal=(k == KD - 1))
                        op("act", lambda e: e.copy(out=h2tok[:, t, :], in_=pbf), R=[B_ps[pi_]], W=[B_h2[t]])
                mx = P.sb(r1, "smx", [128, NT], F32)
                op("dve", lambda e: e.tensor_reduce(out=mx[:, :ntile], in_=aff[:, :ntile, :], axis=AX.X, op=ALU.max),
                   R=[B_aff], W=[B_rstd])
                op("dve", lambda e: e.tensor_tensor(out=aff[:, :ntile, :], in0=aff[:, :ntile, :],
                                                    in1=mx[:, :ntile].unsqueeze(2).to_broadcast([128, ntile, N_EXP]),
                                                    op=ALU.subtract), R=[B_rstd, B_aff], W=[B_aff])
                op("act", lambda e: e.activation(out=aff[:, :ntile, :], in_=aff[:, :ntile, :], func=AF.Exp),
                   R=[B_aff], W=[B_aff])
                op("dve", lambda e: e.tensor_reduce(out=mx[:, :ntile], in_=aff[:, :ntile, :], axis=AX.X, op=ALU.add),
                   R=[B_aff], W=[B_rstd])
                op("dve", lambda e: e.reciprocal(out=mx[:, :ntile], in_=mx[:, :ntile]), R=[B_rstd], W=[B_rstd])
                op("dve", lambda e: e.tensor_tensor(out=aff[:, :ntile, :], in0=aff[:, :ntile, :],
                                                    in1=mx[:, :ntile].unsqueeze(2).to_broadcast([128, ntile, N_EXP]),
                                                    op=ALU.mult), R=[B_rstd, B_aff], W=[B_aff])
                op("dve", lambda e: e.tensor_copy(out=aff_hl[:, :ntile, :, 0], in_=aff[:, :ntile, :]), R=[B_aff], W=[B_posm])
                op("dve", lambda e: e.tensor_tensor(out=aff_hl[:, :ntile, :, 1], in0=aff[:, :ntile, :],
                                                    in1=aff_hl[:, :ntile, :, 0], op=ALU.subtract),
                   R=[B_aff, B_posm], W=[B_posm])
                S.barrier()
            debug_dump(f"aff{l}", lambda o: [dma("sp", o, aff[:], R=[B_aff])])
            debug_dump(f"h2tok{l}", lambda o: [dma("sp", o, h2tok[:], R=B_h2)])

            with ExitStack() as r2:
                affT = P.sb(r2, "affT", [16, T_ALL], F32)
                mk = P.sb(r2, "mkT", [16, T_ALL], F32)
                cum = P.sb(r2, "cumT", [16, T_ALL], F32)
                sm = P.sb(r2, "bis", [16, 16], F32)
                B_affT, B_mk, B_cum, B_sm = Buf("affT"), Buf("mk"), Buf("cum"), Buf("sm")
                for t in range(ntile):
                    pi_ = t % 2
                    op("pe", lambda e: e.transpose(out=psb[pi_][0:16, 0:128], in_=aff[:, t, :], identity=ident_f[:]),
                       R=[B_aff, B_const], W=[B_ps[pi_]])
                    op("act", lambda e: e.copy(out=affT[:, t * 128:(t + 1) * 128], in_=psb[pi_][0:16, 0:128]),
                       R=[B_ps[pi_]], W=[B_affT])
                sets = [(0, T_LAT, 256)] + ([(T_LAT, T_CTX, 32)] if with_ctx else [])
                one = sm[:, 15:16]
                op("dve", lambda e: e.memset(one, 1.0), W=[B_sm])
                B_smx = [Buf("smA"), Buf("smB")]

                def bisect(si, s0, ns, cap):
                    lo, mid, gs = [sm[:, si * 6 + i: si * 6 + i + 1] for i in range(3)]
                    cnts = [sm[:, si * 6 + 3 + i: si * 6 + 4 + i] for i in range(2)]
                    Bs = B_smx[si]
                    a_ap = affT[:, s0:s0 + ns]
                    op("dve", lambda e: e.memset(lo, 0.0), W=[Bs])
                    op("dve", lambda e: e.memset(mid, 0.5), W=[Bs])
                    NIT = 30
                    for it in range(NIT):
                        step = 0.5 ** (it + 1)
                        cnt = cnts[it % 2]
                        op("dve", lambda e: e.memset(cnt, 0.0), W=[Bs])
                        yield
                        op("dve", lambda e: e.tensor_scalar(out=mk[:, s0:s0 + ns], in0=a_ap, scalar1=mid, scalar2=0.0,
                                                            op0=ALU.is_ge, op1=ALU.add, accum_out=cnt),
                           R=[B_affT, Bs], W=[B_mk, Bs])
                        yield
                        op("dve", lambda e: e.tensor_scalar(out=gs, in0=cnt, scalar1=float(cap), scalar2=step,
                                                            op0=ALU.is_ge, op1=ALU.mult), R=[Bs], W=[Bs])
                        yield
                        if it < NIT - 1:
                            op("dve", lambda e: e.scalar_tensor_tensor(out=mid, in0=gs, scalar=step * 0.5, in1=lo,
                                                                       op0=ALU.add, op1=ALU.add), R=[Bs], W=[Bs])
                            yield
                        op("dve", lambda e: e.tensor_tensor(out=lo, in0=lo, in1=gs, op=ALU.add), R=[Bs], W=[Bs])
                        yield
                    op("dve", lambda e: e.tensor_scalar(out=mk[:, s0:s0 + ns], in0=a_ap, scalar1=lo, scalar2=None,
                                                        op0=ALU.is_ge), R=[B_affT, Bs], W=[B_mk])
                    yield
                    op("dve", lambda e: e.tensor_tensor_scan(out=cum[:, s0:s0 + ns],
                                                             data0=one.to_broadcast([16, ns]), data1=mk[:, s0:s0 + ns],
                                                             initial=0.0, op0=ALU.mult, op1=ALU.add),
                       R=[B_mk, B_sm], W=[B_cum])
                    yield
                    op("dve", lambda e: e.tensor_tensor(out=cum[:, s0:s0 + ns], in0=cum[:, s0:s0 + ns],
                                                        in1=mk[:, s0:s0 + ns], op=ALU.mult), R=[B_mk, B_cum], W=[B_cum])
                    yield

                interleave(*[bisect(si, s0, ns, cap) for si, (s0, ns, cap) in enumerate(sets)])
                for t in range(ntile):
                    pi_ = t % 2
                    op("pe", lambda e: e.transpose(out=psb[pi_][:, 0:16], in_=cum[:, t * 128:(t + 1) * 128],
                                                   identity=ident_f[0:16, 0:16]),
                       R=[B_cum, B_const], W=[B_ps[pi_]])
                    op("act", lambda e: e.copy(out=posm_tok[:, t, :], in_=psb[pi_][:, 0:16]), R=[B_ps[pi_]], W=[B_posm])
                S.barrier()
            debug_dump(f"posm{l}", lambda o: [dma("sp", o, posm_tok[:], R=[B_posm])])
            if P.stop_after == f"route{l}":
                return

            with ExitStack() as xs:
                Pm = P.sb(xs, "Pm", [128, NT, NS], BF16)
                B_Pm = Buf("Pm")
                PTl = P.sb(xs, "PTl", [128, 2, T_LAT], BF16)
                PTc = P.sb(xs, "PTc", [128, T_CTX], BF16)
                B_PT = Buf("PT")

                def PTv(st, a, b2, sz):
                    return PTl[0:sz, st, a:b2] if st < 2 else PTc[0:sz, a - T_LAT:b2 - T_LAT]
                xeT = P.sb(xs, "xeT", [128, KD, NS], BF16)
                B_xe = [Buf(f"xe{k}") for k in range(KD)]
                hmid = P.sb(xs, "hmid", [128, 2, NS], BF16)
                B_hm = [Buf("hm0"), Buf("hm1")]
                sil = P.sb(xs, "sil", [128, 2, NS], BF16)
                B_sil = [Buf("sil0"), Buf("sil1")]
                y_sb = P.sb(xs, "y_sb", [128, nst, D], BF16)
                B_y = [Buf(f"y{i}") for i in range(nst)]
                gsl = P.sb(xs, "gsl", [128, 4], F32)
                B_gsl = Buf("gsl")
                NGU = 5
                NDN = 5 if with_ctx else 6
                wgu = wring + [P.sb(xs, f"wgu{i}", [128, KD, 512], BF16) for i in range(NGU - NSLOT)]
                B_gu = [Buf(f"gu{i}") for i in range(NGU)]
                wdn = [P.sb(xs, f"wdn{i}", [128, 2, D], BF16) for i in range(NDN)]
                B_dn = [Buf(f"dn{i}") for i in range(NDN)]
                op("dve", lambda e: e.memset(Pm[:], 0.0), W=[B_Pm])

                NFB = NFC // 2
                sched = [(e_, fb) for e_ in range(n_exp_run) for fb in range(NFB)]
                issued = {"n": 0}

                def issue_weights(upto):
                    while issued["n"] < min(upto, len(sched)):
                        i = issued["n"]
                        e_, fb = sched[i]
                        if f"wg_{l}_{e_}" not in P.dram:
                            P.din(f"wg_{l}_{e_}", [D, FF])
                            P.din(f"wu_{l}_{e_}", [D, FF])
                            P.din(f"wd_{l}_{e_}", [FF, D])
                        wg_ap, wu_ap, wd_ap = P.dram[f"wg_{l}_{e_}"], P.dram[f"wu_{l}_{e_}"], P.dram[f"wd_{l}_{e_}"]
                        gi, di = i % NGU, i % NDN
                        dma("pool", wgu[gi][:, :, 0:256],
                            wg_ap.rearrange("(k p) f -> p k f", p=128)[:, :, fb * 256:(fb + 1) * 256], W=[B_gu[gi]])
                        dma("pool", wgu[gi][:, :, 256:512],
                            wu_ap.rearrange("(k p) f -> p k f", p=128)[:, :, fb * 256:(fb + 1) * 256], W=[B_gu[gi]])
                        dma("pool", wdn[di][:],
                            wd_ap.rearrange("(c p) d -> p c d", p=128)[:, fb * 2:(fb + 1) * 2, :], W=[B_dn[di]])
                        issued["n"] += 1

                LA = 4 if with_ctx else 5
                issue_weights(LA)
                blk_i = 0
                deferred = {"scatter": None}
                xb = (6, 7) if with_ctx else (4, 5)
                for e_ in range(n_exp_run):
                    op("dve", lambda e: e.tensor_tensor(
                        out=Pm[:, 0:16, 0:256], in0=iota1[:, :].unsqueeze(1).to_broadcast([128, 16, 256]),
                        in1=posm_tok[:, 0:16, e_:e_ + 1].to_broadcast([128, 16, 256]), op=ALU.is_equal),
                       R=[B_posm, B_wrt], W=[B_Pm])
                    if with_ctx:
                        op("dve", lambda e: e.tensor_tensor(
                            out=Pm[:, 16:18, 256:288], in0=iota1[:, 0:32].unsqueeze(1).to_broadcast([128, 2, 32]),
                            in1=posm_tok[:, 16:18, e_:e_ + 1].to_broadcast([128, 2, 32]), op=ALU.is_equal),
                           R=[B_posm, B_wrt], W=[B_Pm])
                    for st in range(nst):
                        tiles = range(16) if st < 2 else range(16, 18)
                        tl = list(tiles)
                        for ti, t in enumerate(tl):
                            op("pe", lambda e: e.matmul(psb[6][0:st_sz[st], st * 2:st * 2 + 2],
                                                        lhsT=Pm[:, t, st_off[st]:st_off[st] + st_sz[st]],
                                                        rhs=aff_hl[:, t, e_, :], start=(ti == 0), stop=(ti == len(tl) - 1)),
                               R=[B_Pm, B_posm], W=[B_ps[6]], signal=(ti == len(tl) - 1))
                    for st in range(nst):
                        op("dve", lambda e: e.tensor_reduce(out=gsl[0:st_sz[st], st:st + 1],
                                                            in_=psb[6][0:st_sz[st], st * 2:st * 2 + 2], axis=AX.X, op=ALU.add),
                           R=[B_ps[6]], W=[B_gsl])
                    def do_PT():
                        gcnt = 0
                        for st in range(nst):
                            tl = list(range(16)) if st < 2 else [16, 17]
                            for g0 in range(0, len(tl), 8):
                                grp = tl[g0:g0 + 8]
                                pi_ = xb[gcnt % 2]
                                gcnt += 1
                                pbf = psb[pi_][:].bitcast(BF16)
                                for gi_, t in enumerate(grp):
                                    op("pe", lambda e: e.transpose(
                                        out=pbf[0:st_sz[st], gi_ * 128:(gi_ + 1) * 128],
                                        in_=Pm[:, t, st_off[st]:st_off[st] + st_sz[st]], identity=ident_b[:]),
                                       R=[B_Pm, B_const], W=[B_ps[pi_]], signal=(gi_ == len(grp) - 1))
                                op("act", lambda e: e.copy(out=PTv(st, grp[0] * 128, (grp[-1] + 1) * 128, st_sz[st]),
                                                           in_=pbf[0:st_sz[st], 0:len(grp) * 128]),
                                   R=[B_ps[pi_]], W=[B_PT])
                    for k in range(KD):
                        pi_ = 6 + (k % 2)
                        for t in range(ntile):
                            op("pe", lambda e: e.matmul(psb[pi_][:, 0:NS], lhsT=h2tok[:, t, k * 128:(k + 1) * 128],
                                                        rhs=Pm[:, t, :], start=(t == 0), stop=(t == ntile - 1)),
                               R=[B_h2[t], B_Pm], W=[B_ps[pi_]], signal=(t == ntile - 1))
                        op("act", lambda e: e.copy(out=xeT[:, k, :], in_=psb[pi_][:, 0:NS]), R=[B_ps[pi_]], W=[B_xe[k]])
                    psY = [[psb[st * 2 + nh] for nh in range(2)] for st in range(nst)]
                    B_psY = [[B_ps[st * 2 + nh] for nh in range(2)] for st in range(nst)]

                    def emit_down(fc, hb, di, first, last):
                        for st in range(nst):
                            for nh in range(2):
                                op("pe", lambda e: e.matmul(
                                    psY[st][nh][0:st_sz[st], :], lhsT=hmid[:, hb, st_off[st]:st_off[st] + st_sz[st]],
                                    rhs=wdn[di][:, fc % 2, nh * 512:(nh + 1) * 512], start=first, stop=last),
                                   R=[B_hm[hb], B_dn[di]], W=[B_psY[st][nh]], signal=(st == nst - 1 and nh == 1))

                    pend = None
                    for fb in range(NFB):
                        i = blk_i
                        blk_i += 1
                        issue_weights(i + LA)
                        if fb == 3:
                            if deferred["scatter"] is not None:
                                deferred["scatter"]()
                                deferred["scatter"] = None
                            do_PT()
                        gi, di = i % NGU, i % NDN
                        for f2 in range(2):
                            fc = fb * 2 + f2
                            hb = fc % 2
                            ia, iu = 6, 7
                            pa, pu = psb[ia], psb[iu]
                            for k in range(KD):
                                op("pe", lambda e: e.matmul(pa[:, 0:NS], lhsT=wgu[gi][:, k, f2 * 128:(f2 + 1) * 128],
                                                            rhs=xeT[:, k, :], start=(k == 0), stop=(k == KD - 1)),
                                   R=[B_gu[gi], B_xe[k]], W=[B_ps[ia]], signal=(k == KD - 1))
                            for k in range(KD):
                                op("pe", lambda e: e.matmul(pu[:, 0:NS],
                                                            lhsT=wgu[gi][:, k, 256 + f2 * 128:256 + (f2 + 1) * 128],
                                                            rhs=xeT[:, k, :], start=(k == 0), stop=(k == KD - 1)),
                                   R=[B_gu[gi], B_xe[k]], W=[B_ps[iu]], signal=(k == KD - 1))
                            op("act", lambda e: e.activation(out=sil[:, hb, :], in_=pa[:, 0:NS], func=AF.Silu),
                               R=[B_ps[ia]], W=[B_sil[hb]])
                            op("dve", lambda e: e.tensor_tensor(out=hmid[:, hb, :], in0=pu[:, 0:NS], in1=sil[:, hb, :],
                                                                op=ALU.mult), R=[B_ps[iu], B_sil[hb]], W=[B_hm[hb]])
                            if pend is not None:
                                emit_down(*pend)
                            pend = (fc, hb, di, fc == 0, fc == NFC - 1)
                    emit_down(*pend)
                    for st in range(nst):
                        for nh in range(2):
                            op("act", lambda e: e.activation(out=y_sb[0:st_sz[st], st, nh * 512:(nh + 1) * 512],
                                                             in_=psY[st][nh][0:st_sz[st], :], func=AF.Copy,
                                                             scale=gsl[0:st_sz[st], st:st + 1]),
                               R=[B_psY[st][nh], B_gsl], W=[B_y[st]])
                    def do_scatter():
                        gi_s = 0
                        for b in range(nblk):
                            s, n, isc = BLOCKS[b]
                            c = 1 if isc else 0
                            sts = [2] if isc else [0, 1]
                            for mch in range(KD):
                                pi_ = xb[gi_s % 2]
                                gi_s += 1
                                for si, st in enumerate(sts):
                                    op("pe", lambda e: e.matmul(psb[pi_][:, :n],
                                                                lhsT=y_sb[0:st_sz[st], st, mch * 128:(mch + 1) * 128],
                                                                rhs=PTv(st, s, s + n, st_sz[st]), start=(si == 0),
                                                                stop=(si == len(sts) - 1)),
                                       R=[B_y[st], B_PT], W=[B_ps[pi_]], signal=(si == len(sts) - 1))
                                g_ap = modv(l, 5, mch, c)
                                op("dve", lambda e: e.scalar_tensor_tensor(
                                    out=xT[:, mch, s:s + n], in0=psb[pi_][:, :n], scalar=g_ap, in1=xT[:, mch, s:s + n],
                                    op0=ALU.mult, op1=ALU.add),
                                   R=[B_ps[pi_], B_mod, B_x[mch][b]], W=[B_x[mch][b]])
                    deferred["scatter"] = do_scatter
                if deferred["scatter"] is not None:
                    deferred["scatter"]()
                S.barrier()

    def hgrn2_layer(l):
        hgin_d = P.din("hg_w_in", [D, 5120])
        hgout_d = P.din("hg_w_out", [D, D])
        hgmisc_d = P.din("hg_misc", [128, 1 + 4 * KD])
        hgmask_d = P.din("hgmask", [128, 256])
        PT_ = 256
        NPART = 9
        NCH = 2
        with ExitStack() as les:
            hT = P.sb(les, "hT1", [128, KD, T_ALL], BF16)
            B_h = [[Buf(f"h1{k}_{b}") for b in range(5)] for k in range(KD)]
            B_hT = Buf("hT1all")
            hgm = P.sb(les, "hgm", [128, 1 + 4 * KD], F32)
            msk = P.sb(les, "hgmask_sb", [128, 256], F32)
            lbs = P.sb(les, "lbs", [128, 2, 2, HG_H], F32)
            lbs2 = P.sb(les, "lbs2", [128, 2, 2, HG_H], F32)
            gnh = P.sb(les, "gnh", [128, 1], F32)
            B_hc = Buf("hgconst")
            dma("sp", hgm[:], hgmisc_d, W=[B_hc])
            dma("sp", msk[:], hgmask_d, W=[B_hc])
            AA = P.sb(les, "hgAA", [128, 14 * PT_], F32)
            RG = [AA[:, r * PT_:(r + 1) * PT_] for r in range(14)]
            B_R = [Buf(f"hgR{r}") for r in range(14)]

            def Aregs(d_, par):
                idx = [d_ * 2 + par, 4 + d_ * 2 + par, 8 + d_, 10 + d_, 12 + d_]
                return [RG[i] for i in idx], [B_R[i] for i in idx]
            tmp = [AA[:, 0:512], AA[:, 768:1280]]
            B_tmp = [[B_R[0], B_R[1]], [B_R[3], B_R[4]]]
            rstds = [AA[:, 1536:2048], AA[:, 2304:2816]]
            B_rstds = [[B_R[6], B_R[7]], [B_R[9], B_R[10]]]
            rstd, B_rstd = rstds[0], B_rstds[0]
            sq = [P.sb(les, f"hsq{i}", [128, 512], BF16) for i in range(2)]
            B_sq = [Buf("hsq0"), Buf("hsq1")]

            class _V:
                def __init__(self, ap):
                    self.ap = ap

                def __getitem__(self, key):
                    return self.ap[key]
            for b in range(5):
                s, n, _ = BLOCKS[b]
                norm_block(l, 0, b, lambda k, b=b, s=s, n=n: (hT[:, k, s:s + n], [B_h[k][b]]),
                           [_V(tmp[0]), _V(tmp[1])], B_tmp, _V(rstd), B_rstd, sq, B_sq, psn_i=7)
            for d_ in range(2):
                op("dve", lambda e: e.tensor_tensor(out=lbs[:, 0, d_, :], in0=hgm[:, 1 + (2 + d_) * 8:1 + (3 + d_) * 8],
                                                    in1=hgm[:, 1 + d_ * 8:1 + (d_ + 1) * 8], op=ALU.subtract),
                   R=[B_hc], W=[B_hc])
            op("act", lambda e: e.activation(out=lbs[:, 0, :, :], in_=lbs[:, 0, :, :], func=AF.Sigmoid), R=[B_hc], W=[B_hc])
            op("dve", lambda e: e.tensor_scalar(out=lbs[:, 1, :, :], in0=lbs[:, 0, :, :], scalar1=-1.0, scalar2=1.0,
                                                op0=ALU.mult, op1=ALU.add), R=[B_hc], W=[B_hc])
            op("dve", lambda e: e.tensor_scalar(out=lbs2[:, 0, :, :], in0=lbs[:, 1, :, :], scalar1=0.5, scalar2=None,
                                                op0=ALU.mult), R=[B_hc], W=[B_hc])
            op("dve", lambda e: e.tensor_tensor(out=lbs2[:, 1, :, :], in0=lbs2[:, 0, :, :], in1=lbs[:, 0, :, :], op=ALU.add),
               R=[B_hc], W=[B_hc])
            op("dve", lambda e: e.tensor_scalar(out=gnh[:], in0=hgm[:, 0:1], scalar1=0.5, scalar2=None, op0=ALU.mult),
               R=[B_hc], W=[B_hc])
            S.barrier()
            debug_dump("hT1", lambda o: [dma("sp", o.rearrange("(k p) t -> p k t", p=128)[:, k, :], hT[:, k, :],
                                             R=[B_hT]) for k in range(KD)])

            qs2 = P.sb(les, "hqs", [128, 2, PT_], BF16)
            B_qs2 = [Buf("hqs0"), Buf("hqs1")]
            arr = [[P.sb(les, f"harr{d_}{i}", [128, T_ALL], BF16) for i in range(3)] for d_ in range(2)]
            B_arr = [[Buf(f"harr{d_}{i}") for i in range(3)] for d_ in range(2)]
            sgT, B_sg = arr[0][2], B_arr[0][2]
            ogT, B_og = arr[1][2], B_arr[1][2]
            B_ogb = [[B_og, Buf(f"ogb{b}")] for b in range(4)]
            vv = P.sb(les, "hvv", [128, NT, 128], BF16)
            B_v = Buf("hvv")
            EF = P.sb(les, "hEF", [128, 2, NT], F32)
            EM = P.sb(les, "hEM", [128, 2, NT], F32)
            t6b = P.sb(les, "ht6", [128, 2, 8], F32)
            B_t6 = [Buf("t6a"), Buf("t6b")]
            B_E = Buf("hE")
            Sst = P.sb(les, "hS", [128, 2, 128], F32)
            B_S = [Buf("hS0"), Buf("hS1")]
            Suse = P.sb(les, "hSuse", [128, 2, 16, 128], BF16)
            B_Su = Buf("hSuse")
            kht = [P.sb(les, f"hkht{i}", [128, 128], BF16) for i in range(2)]
            B_kht = [Buf("kht0"), Buf("kht1")]
            ATs = [P.sb(les, f"hAT{i}", [128, 128], BF16) for i in range(8)]
            B_ATs = [Buf(f"hAT{i}") for i in range(8)]
            one_col = hgm[:, 0:1]
            ones_f = P.sb(les, "hones", [128, 1], F32)
            op("dve", lambda e: e.memset(ones_f[:], 1.0), W=[B_hc])

            win_src = hgin_d.rearrange("(k p) f -> p k f", p=128)
            cnt_ = {"ps": 0, "kht": 0, "at": 0}

            def view3(ap):
                return ap.rearrange("p (c i) -> p c i", i=128)

            for h in range(HG_H):
                sX, sY = wslot(), wslot()
                for gi_, off in enumerate((0, 1024, 2048, 3072)):
                    dma("pool", wring[sX][:, :, gi_ * 128:(gi_ + 1) * 128], win_src[:, :, off + h * 128: off + (h + 1) * 128],
                        W=[B_wr[sX]])
                yflat = wring[sY][:].rearrange("p k f -> p (k f)")
                wG = yflat[:, 0:1024].rearrange("p (k f) -> p k f", k=KD)
                wO = yflat[:, 1024:2048]
                dma("pool", wG, win_src[:, :, 4096 + h * 128:4096 + (h + 1) * 128], W=[B_wr[sY]])
                dma("pool", wO, hgout_d[h * 128:(h + 1) * 128, :], W=[B_wr[sY]])

                def proj(gi_, s, n, pi_):
                    for k in range(KD):
                        op("pe", lambda e: e.matmul(psb[pi_][:, :n], lhsT=wring[sX][:, k, gi_ * 128:(gi_ + 1) * 128],
                                                    rhs=hT[:, k, s:s + n], start=(k == 0), stop=(k == KD - 1)),
                           R=[B_wr[sX], B_hT], W=[B_ps[pi_]], signal=(k == KD - 1))

                def stageA(part):
                    p0 = part * PT_
                    c0 = part * NCH
                    par = part % 2
                    qs = qs2[:, par, :]
                    B_qs = B_qs2[par]
                    pi_ = cnt_["ps"] % 2
                    cnt_["ps"] += 1
                    proj(0, p0, PT_, pi_)
                    op("act", lambda e: e.activation(out=qs, in_=psb[pi_][:, :PT_], func=AF.Tanh, scale=0.5),
                       R=[B_ps[pi_]], W=[B_qs])
                    yield
                    op("dve", lambda e: e.scalar_tensor_tensor(out=qs, in0=qs, scalar=1.0, in1=psb[pi_][:, :PT_],
                                                               op0=ALU.add, op1=ALU.mult), R=[B_qs, B_ps[pi_]], W=[B_qs])
                    yield
                    for tt in range(NCH):
                        t = c0 + tt
                        pi_ = cnt_["ps"] % 2
                        cnt_["ps"] += 1
                        for k in range(KD):
                            op("pe", lambda e: e.matmul(psb[pi_][:, 0:128], lhsT=hT[:, k, t * 128:(t + 1) * 128],
                                                        rhs=wring[sX][:, k, 384:512], start=(k == 0), stop=(k == KD - 1)),
                               R=[B_wr[sX], B_hT], W=[B_ps[pi_]], signal=(k == KD - 1))
                        op("act", lambda e: e.copy(out=vv[:, t, :], in_=psb[pi_][:, 0:128]), R=[B_ps[pi_]], W=[B_v])
                        yield
                    regs = [Aregs(d_, par) for d_ in range(2)]
                    pis = []
                    for d_ in range(2):
                        pi_ = cnt_["ps"] % 2
                        cnt_["ps"] += 1
                        pis.append(pi_)
                        proj(1 + d_, p0, PT_, pi_)
                    for d_ in range(2):
                        (A1, A2, A3, A4, A5), BA = regs[d_]
                        op("act", lambda e: e.activation(out=A2, in_=psb[pis[d_]][:, :PT_], func=AF.Tanh, scale=0.5),
                           R=[B_ps[pis[d_]]], W=[BA[1]])
                        yield
                    for d_ in range(2):
                        (A1, A2, A3, A4, A5), BA = regs[d_]
                        op("dve", lambda e: e.tensor_scalar(out=A1, in0=A2, scalar1=-0.5, scalar2=0.5, op0=ALU.mult,
                                                            op1=ALU.add), R=[BA[1]], W=[BA[0]])
                        yield
                    for d_ in range(2):
                        (A1, A2, A3, A4, A5), BA = regs[d_]
                        op("act", lambda e: e.activation(out=A2, in_=A2, func=AF.Ln, bias=lbs2[:, 1, d_, h:h + 1],
                                                         scale=lbs2[:, 0, d_, h:h + 1]), R=[BA[1], B_hc], W=[BA[1]])
                        yield

                def stageB_driver(part):
                    p0 = part * PT_
                    c0 = part * NCH
                    par = part % 2
                    qs = qs2[:, par, :]
                    B_qs = B_qs2[par]
                    def chain(d_):
                        si_ = d_
                        (A1, A2, A3, A4, A5), BA = Aregs(d_, par)
                        t6 = t6b[:, si_, :]
                        B_T = B_t6[si_]
                        oml_ap = lbs[:, 1, d_, h:h + 1]
                        op("dve", lambda e: e.tensor_tensor_scan(out=A3, data0=ones_f[:, 0:1].to_broadcast([128, PT_]),
                                                                 data1=A2, initial=0.0, op0=ALU.mult, op1=ALU.add),
                           R=[BA[1], B_hc], W=[BA[2]])
                        G3, g3 = view3(A3), view3(A2)
                        dst = [arr[d_][i][:, p0:p0 + PT_] for i in range(3)]
                        Bd = B_arr[d_]
                        bc = [128, NCH, 128]
                        if d_ == 0:
                            yield
                            op("dve", lambda e: e.tensor_tensor(out=view3(A4), in0=G3, in1=G3[:, :, 63:64].to_broadcast(bc),
                                                                op=ALU.subtract), R=[BA[2]], W=[BA[3]])
                            yield
                            op("act", lambda e: e.activation(out=A5, in_=A4, func=AF.Exp, bias=-0.6931471805599453), R=[BA[3]], W=[BA[4]])
                            yield
                            op("pool", lambda e: e.tensor_tensor(out=dst[0], in0=qs, in1=A5, op=ALU.mult),
                               R=[B_qs, BA[4]], W=[Bd[0]])
                            yield
                            op("dve", lambda e: e.tensor_tensor(out=t6[:, 0:NCH], in0=G3[:, :, 0], in1=g3[:, :, 0], op=ALU.subtract),
                               R=[BA[2], BA[1]], W=[B_T])
                            yield
                            op("dve", lambda e: e.tensor_tensor(out=EF[:, 0, c0:c0 + NCH], in0=G3[:, :, 127], in1=t6[:, 0:NCH],
                                                                op=ALU.subtract), R=[BA[2], B_T], W=[B_E])
                            yield
                            op("dve", lambda e: e.tensor_tensor(out=EM[:, 0, c0:c0 + NCH], in0=G3[:, :, 63], in1=t6[:, 0:NCH],
                                                                op=ALU.subtract), R=[BA[2], B_T], W=[B_E])
                            yield
                            op("act", lambda e: e.activation(out=A2, in_=A4, func=AF.Exp, scale=-1.0), R=[BA[3]], W=[BA[1]])
                            yield
                            op("dve", lambda e: e.scalar_tensor_tensor(out=dst[1], in0=A1, scalar=oml_ap, in1=A2, op0=ALU.mult, op1=ALU.mult),
                               R=[BA[0], BA[1]], W=[Bd[1]])
                            yield
                            op("dve", lambda e: e.tensor_tensor(out=view3(A4), in0=G3, in1=G3[:, :, 127:128].to_broadcast(bc),
                                                                op=ALU.subtract), R=[BA[2]], W=[BA[3]])
                            yield
                            op("act", lambda e: e.activation(out=A5, in_=A4, func=AF.Exp, scale=-1.0), R=[BA[3]], W=[BA[4]])
                            yield
                            op("dve", lambda e: e.scalar_tensor_tensor(out=dst[2], in0=A1, scalar=oml_ap, in1=A5, op0=ALU.mult, op1=ALU.mult),
                               R=[BA[0], BA[4]], W=[Bd[2]])
                        else:
                            yield
                            op("dve", lambda e: e.tensor_copy(out=t6[:, 0:NCH], in_=G3[:, :, 127]), R=[BA[2]], W=[B_T])
                            yield
                            op("pool", lambda e: e.tensor_tensor(out=A2, in0=A3, in1=A2, op=ALU.subtract),
                               R=[BA[2], BA[1]], W=[BA[1]])
                            H3 = view3(A2)
                            yield
                            op("dve", lambda e: e.tensor_tensor(out=view3(A4), in0=H3, in1=H3[:, :, 64:65].to_broadcast(bc),
                                                                op=ALU.subtract), R=[BA[1]], W=[BA[3]])
                            yield
                            op("act", lambda e: e.activation(out=A5, in_=A4, func=AF.Exp, scale=-1.0, bias=-0.6931471805599453), R=[BA[3]], W=[BA[4]])
                            yield
                            op("pool", lambda e: e.tensor_tensor(out=dst[0], in0=qs, in1=A5, op=ALU.mult),
                               R=[B_qs, BA[4]], W=[Bd[0]])
                            yield
                            op("act", lambda e: e.activation(out=A3, in_=A4, func=AF.Exp), R=[BA[3]], W=[BA[2]])
                            yield
                            op("dve", lambda e: e.scalar_tensor_tensor(out=dst[1], in0=A1, scalar=oml_ap, in1=A3, op0=ALU.mult, op1=ALU.mult),
                               R=[BA[0], BA[2]], W=[Bd[1]])
                            yield
                            op("dve", lambda e: e.tensor_tensor(out=EF[:, 1, c0:c0 + NCH], in0=t6[:, 0:NCH], in1=H3[:, :, 0],
                                                                op=ALU.subtract), R=[BA[1], B_T], W=[B_E])
                            yield
                            op("dve", lambda e: e.tensor_tensor(out=EM[:, 1, c0:c0 + NCH], in0=t6[:, 0:NCH], in1=H3[:, :, 64],
                                                                op=ALU.subtract), R=[BA[1], B_T], W=[B_E])
                            yield
                            op("dve", lambda e: e.tensor_tensor(out=view3(A4), in0=H3, in1=H3[:, :, 0:1].to_broadcast(bc),
                                                                op=ALU.subtract), R=[BA[1]], W=[BA[3]])
                            yield
                            op("act", lambda e: e.activation(out=A5, in_=A4, func=AF.Exp), R=[BA[3]], W=[BA[4]])
                            yield
                            op("dve", lambda e: e.scalar_tensor_tensor(out=dst[2], in0=A1, scalar=oml_ap, in1=A5, op0=ALU.mult, op1=ALU.mult),
                               R=[BA[0], BA[4]], W=[Bd[2]])
                        yield
                    return [chain(0), chain(1)]

                for _ in stageA(0):
                    pass
                for part in range(NPART):
                    gens = stageB_driver(part)
                    if part + 1 < NPART:
                        gens.append(stageA(part + 1))
                    interleave(*gens)
                op("act", lambda e: e.activation(out=EF[:], in_=EF[:], func=AF.Exp), R=[B_E], W=[B_E])
                op("act", lambda e: e.activation(out=EM[:], in_=EM[:], func=AF.Exp), R=[B_E], W=[B_E])

                orders = [[16, 17] + list(range(16)), [17, 16] + list(range(15, -1, -1))]
                for d_ in range(2):
                    op("dve", lambda e: e.memset(Sst[:, d_, :], 0.0), W=[B_S[d_]])
                items = [(step, d_) for step in range(NT - 1) for d_ in range(2)]

                def emit_T(i):
                    step, d_ = items[i]
                    c = orders[d_][step]
                    ki = i % 2
                    pq = 4 + (i % 2)
                    pbf = psb[pq][:].bitcast(BF16)
                    op("pe", lambda e: e.transpose(out=pbf[:, 0:128], in_=arr[d_][2][:, c * 128:(c + 1) * 128],
                                                   identity=ident_b[:]), R=[B_arr[d_][2], B_const], W=[B_ps[pq]])
                    op("act", lambda e: e.copy(out=kht[ki][:], in_=pbf[:, 0:128]), R=[B_ps[pq]], W=[B_kht[ki]])

                def emit_suse(step, d_):
                    c = orders[d_][step]
                    if c < 16:
                        op("act", lambda e: e.activation(out=Suse[:, d_, c, :], in_=Sst[:, d_, :], func=AF.Copy,
                                                         scale=EM[:, d_, c:c + 1]), R=[B_S[d_], B_E], W=[B_Su])

                emit_T(0)
                for i, (step, d_) in enumerate(items):
                    c = orders[d_][step]
                    if i + 1 < len(items):
                        emit_T(i + 1)
                    emit_suse(step, d_)
                    ki = i % 2
                    pd = 2 + (i % 2)
                    op("pe", lambda e: e.matmul(psb[pd][:, 0:128], lhsT=kht[ki][:], rhs=vv[:, c, :], start=True, stop=True),
                       R=[B_kht[ki], B_v], W=[B_ps[pd]])
                    op("dve", lambda e: e.scalar_tensor_tensor(out=Sst[:, d_, :], in0=Sst[:, d_, :], scalar=EF[:, d_, c:c + 1],
                                                               in1=psb[pd][:, 0:128], op0=ALU.mult, op1=ALU.add),
                       R=[B_S[d_], B_E, B_ps[pd]], W=[B_S[d_]])
                for d_ in range(2):
                    emit_suse(NT - 1, d_)

                for b in range(4):
                    s, n, _ = BLOCKS[b]
                    pi_ = b % 2
                    for k in range(KD):
                        op("pe", lambda e: e.matmul(psb[pi_][:, :n], lhsT=wG[:, k, :], rhs=hT[:, k, s:s + n],
                                                    start=(k == 0), stop=(k == KD - 1)),
                           R=[B_wr[sY], B_hT], W=[B_ps[pi_]], signal=(k == KD - 1))
                    op("act", lambda e: e.activation(out=sgT[:, s:s + n], in_=psb[pi_][:, :n], func=AF.Tanh, scale=0.5),
                       R=[B_ps[pi_]], W=[B_sg])
                    op("dve", lambda e: e.scalar_tensor_tensor(out=sgT[:, s:s + n], in0=sgT[:, s:s + n], scalar=1.0,
                                                               in1=psb[pi_][:, :n], op0=ALU.add, op1=ALU.mult),
                       R=[B_sg, B_ps[pi_]], W=[B_sg])

                def out_block(b):
                    s, n, _ = BLOCKS[b]
                    bp = b % 2
                    iO = 6 if bp == 0 else 4
                    pO = psb[iO]

                    def scores(cc):
                        c = b * 4 + cc
                        cs = slice(c * 128, (c + 1) * 128)
                        ais = []
                        for d_ in range(2):
                            pa = 2 + (cnt_["at"] % 2)
                            ai = cnt_["at"] % 8
                            cnt_["at"] += 1
                            op("pe", lambda e: e.matmul(psb[pa][:, 0:128], lhsT=arr[d_][1][:, cs], rhs=arr[d_][0][:, cs],
                                                        start=True, stop=True),
                               R=[B_arr[d_][1], B_arr[d_][0]], W=[B_ps[pa]])
                            op("dve", lambda e: e.tensor_tensor(out=ATs[ai][:], in0=psb[pa][:, 0:128],
                                                                in1=msk[:, d_ * 128:(d_ + 1) * 128], op=ALU.mult),
                               R=[B_ps[pa], B_hc], W=[B_ATs[ai]])
                            ais.append(ai)
                        return ais

                    def outs(cc, ais):
                        c = b * 4 + cc
                        cs = slice(c * 128, (c + 1) * 128)
                        oc = pO[:, cc * 128:(cc + 1) * 128]
                        op("pe", lambda e: e.matmul(oc, lhsT=Suse[:, 0, c, :], rhs=arr[0][0][:, cs], start=True, stop=False),
                           R=[B_Su, B_arr[0][0]], W=[B_ps[iO]], signal=False)
                        op("pe", lambda e: e.matmul(oc, lhsT=Suse[:, 1, c, :], rhs=arr[1][0][:, cs], start=False, stop=False),
                           R=[B_Su, B_arr[1][0]], W=[B_ps[iO]], signal=False)
                        op("pe", lambda e: e.matmul(oc, lhsT=vv[:, c, :], rhs=ATs[ais[0]][:], start=False, stop=False),
                           R=[B_v, B_ATs[ais[0]]], W=[B_ps[iO]], signal=False)
                        op("pe", lambda e: e.matmul(oc, lhsT=vv[:, c, :], rhs=ATs[ais[1]][:], start=False, stop=True),
                           R=[B_v, B_ATs[ais[1]]], W=[B_ps[iO]], signal=True)

                    pend = None
                    for cc in range(4):
                        ais = scores(cc)
                        if pend is not None:
                            outs(*pend)
                        pend = (cc, ais)
                    outs(*pend)

                def post_block(b):
                    s, n, _ = BLOCKS[b]
                    bp = b % 2
                    iO, iN = (6, 7) if bp == 0 else (4, 5)
                    pO = psb[iO]
                    rstd, B_rstd = rstds[bp], B_rstds[bp]
                    op("act", lambda e: e.activation(out=sq[bp][:, :n], in_=pO[:, :n], func=AF.Square), R=[B_ps[iO]], W=[B_sq[bp]])
                    op("pe", lambda e: e.matmul(psb[iN][:, :n], lhsT=ones_b[:], rhs=sq[bp][:, :n], start=True, stop=True),
                       R=[B_sq[bp], B_const], W=[B_ps[iN]])
                    op("act", lambda e: e.activation(out=rstd[:, :n], in_=psb[iN][:, :n], func=AF.Sqrt, bias=EPS,
                                                     scale=1.0 / 128), R=[B_ps[iN]], W=[B_rstd])
                    op("dve", lambda e: e.reciprocal(out=rstd[:, :n], in_=rstd[:, :n]), R=[B_rstd], W=[B_rstd])
                    op("dve", lambda e: e.scalar_tensor_tensor(out=tmp[bp][:, :n], in0=pO[:, :n], scalar=gnh[:, 0:1],
                                                               in1=rstd[:, :n], op0=ALU.mult, op1=ALU.mult),
                       R=[B_ps[iO], B_rstd, B_hc], W=[B_tmp[bp]])
                    op("dve", lambda e: e.tensor_tensor(out=ogT[:, s:s + n], in0=tmp[bp][:, :n], in1=sgT[:, s:s + n],
                                                         op=ALU.mult), R=[B_tmp[bp], B_sg], W=[B_ogb[b][1]])

                def proj_block(b):
                    s, n, _ = BLOCKS[b]
                    for mch in range(KD):
                        pi_ = mch % 2
                        op("pe", lambda e: e.matmul(psb[pi_][:, :n], lhsT=wO[:, mch * 128:(mch + 1) * 128],
                                                    rhs=ogT[:, s:s + n], start=True, stop=True),
                           R=[B_wr[sY], B_ogb[b]], W=[B_ps[pi_]])
                        g_ap = modv(l, 2, mch, 0)
                        op("dve", lambda e: e.scalar_tensor_tensor(
                            out=xT[:, mch, s:s + n], in0=psb[pi_][:, :n], scalar=g_ap, in1=xT[:, mch, s:s + n],
                            op0=ALU.mult, op1=ALU.add),
                           R=[B_ps[pi_], B_mod, B_x[mch][b]], W=[B_x[mch][b]])

                out_block(0)
                out_block(1)
                post_block(0)
                out_block(2)
                proj_block(0)
                post_block(1)
                out_block(3)
                proj_block(1)
                post_block(2)
                post_block(3)
                proj_block(2)
                proj_block(3)
            S.barrier()

    if "ret" in P.parts:
        retention_layer(0)
    debug_dump("x_mix0", lambda o: [dma("sp", o.rearrange("(k p) t -> p k t", p=128)[:, k, :], xT[:, k, :],
                                        R=B_x[k]) for k in range(KD)])

    if "moe0" in P.parts:
        moe_layer(0, True, P.n_exp_run)
    debug_dump("x_ffn0", lambda o: [dma("sp", o.rearrange("(k p) t -> p k t", p=128)[:, k, :], xT[:, k, :],
                                        R=B_x[k]) for k in range(KD)])
    if "hg" in P.parts:
        hgrn2_layer(1)
    debug_dump("x_mix1", lambda o: [dma("sp", o.rearrange("(k p) t -> p k t", p=128)[:, k, :], xT[:, k, 0:T_LAT],
                                        R=B_x[k][:4]) for k in range(KD)])
    if "moe1" in P.parts:
        moe_layer(1, False, P.n_exp_run)
    debug_dump("x_ffn1", lambda o: [dma("sp", o.rearrange("(k p) t -> p k t", p=128)[:, k, :], xT[:, k, 0:T_LAT],
                                        R=B_x[k][:4]) for k in range(KD)])
    if "final" in P.parts:
        with ExitStack() as fes:
            ftmp = [P.sb(fes, f"ftmp{i}", [128, 512], F32) for i in range(2)]
            B_ft = [Buf("ft0"), Buf("ft1")]
            frs = P.sb(fes, "frs", [128, 512], F32)
            B_frs = Buf("frs")
            fsq = [P.sb(fes, f"fsq{i}", [128, 512], BF16) for i in range(2)]
            B_fsq = [Buf("fsq0"), Buf("fsq1")]
            fo = [P.sb(fes, f"fo{i}", [128, 512], F32) for i in range(4)]
            B_fo = [Buf(f"fo{i}") for i in range(4)]
            osrc = out_d.rearrange("(k p) t -> p k t", p=128)
            oc_ = {"n": 0}
            for b in range(4):
                s, n, _ = BLOCKS[b]

                def dst(k):
                    i = oc_["n"] % 4
                    oc_["n"] += 1
                    dst.last = i
                    return fo[i][:, :n], [B_fo[i]]
                norm_block(1, 0, b, dst, ftmp, B_ft, frs, B_frs, fsq, B_fsq, psn_i=7, final=True,
                           after=lambda k, b=b, s=s, n=n: dma("sp", osrc[:, k, s:s + n], fo[dst.last][:, :n], R=[B_fo[dst.last]]))
    if P.stop_after is not None:
        osrc = out_d.rearrange("(k p) t -> p k t", p=128)
        for k in range(KD):
            dma("sp", osrc[:, k, :], xT[:, k, 0:T_LAT], R=B_x[k][:4])
    S.barrier()
    es.close()
    return P


def _prep_inputs(inp, b, consts, names=None):
    f = np.float32
    m = {}
    m["xT"] = np.ascontiguousarray(np.concatenate([inp["x"][b].T, inp["ctx"][b].T], axis=1)).astype(f)
    cv = np.stack([inp["c"][b], inp["c_ctx"]], axis=0)
    m["cvec"] = np.ascontiguousarray(cv.reshape(2, KD, 128).transpose(2, 0, 1).reshape(128, 2 * KD))
    m["w_ada"] = inp["w_ada"]
    m["b_ada"] = np.ascontiguousarray(np.tile(inp["b_ada"].reshape(1, 12 * D), (2, 1)))
    nr = np.stack([inp["norm_mix"][0], inp["norm_mix"][1], inp["norm_ffn"][0], inp["norm_ffn"][1],
                   inp["norm_final"]], axis=0)
    m["norms"] = np.ascontiguousarray(nr.reshape(5, KD, 128).transpose(2, 0, 1).reshape(128, 5 * KD))
    m["ret_w_in"] = inp["ret_w_in"][0]
    m["ret_w_out"] = inp["ret_w_out"][0]
    m["hg_w_in"] = inp["hg_w_in"][0]
    m["hg_w_out"] = inp["hg_w_out"][0]
    lb = inp["hg_lower_bounds"].reshape(4, KD, 128).transpose(2, 0, 1).reshape(128, 4 * KD)
    m["hg_misc"] = np.ascontiguousarray(np.concatenate([inp["hg_g_norm"][0].reshape(128, 1), lb], axis=1)).astype(f)
    m["moe_router"] = inp["moe_router"]
    m.update(consts)
    for l in range(2):
        for e in range(N_EXP):
            m[f"wg_{l}_{e}"] = inp["moe_w_gate"][l, e]
            m[f"wu_{l}_{e}"] = inp["moe_w_up"][l, e]
            m[f"wd_{l}_{e}"] = inp["moe_w_down"][l, e]
    if names is not None:
        m = {k: v for k, v in m.items() if k in names}
    return m


def kernel(**inputs):
    inp = {k: np.asarray(v) for k, v in inputs.items()}
    consts = _const_tables()
    P = build_program()
    names = set(P.dram.keys())
    in_maps = [_prep_inputs(inp, b, consts, names) for b in range(8)]
    res = run_bass_kernel_spmd(P.nc, in_maps, core_ids=list(range(8)))
    out = np.stack([np.ascontiguousarray(res.results[b]["outT"].T) for b in range(8)], axis=0)
    return out.astype(np.float32)
```

```python
import math
from contextlib import ExitStack

import numpy as np
import concourse.bass as bass
import concourse.mybir as mybir
from concourse.bass_utils import run_bass_kernel_spmd

F32 = mybir.dt.float32
BF16 = mybir.dt.bfloat16
ALU = mybir.AluOpType
AF = mybir.ActivationFunctionType
AX = mybir.AxisListType

D = 1024
KD = 8
T_LAT = 2048
T_CTX = 256
T_ALL = T_LAT + T_CTX
NT = T_ALL // 128
EPS = 1e-6
N_EXP = 16
FF = 2816
NFC = FF // 128
RET_H = 4
HG_H = 8

BLOCKS = [(0, 512, False), (512, 512, False), (1024, 512, False), (1536, 512, False), (2048, 256, True)]


def interleave(*gens):
    gens = list(gens)
    while gens:
        for g in list(gens):
            try:
                next(g)
            except StopIteration:
                gens.remove(g)


class Buf:
    __slots__ = ("name", "w", "rs")

    def __init__(self, name):
        self.name = name
        self.w = None
        self.rs = {}


class Sched:
    NDMA = 12

    def __init__(self, nc, es):
        self.nc = nc
        self.h = {"pe": nc.tensor, "act": nc.scalar, "dve": nc.vector, "pool": nc.gpsimd, "sp": nc.sync}
        self.sem = {}
        self.cnt = {}
        self.seen = {}
        for e in self.h:
            self.sem[e] = es.enter_context(nc.semaphore("s_" + e))
            self.cnt[e] = 0
            self.seen[e] = {}
        self.dsem = {}
        self.dval = {}
        self.dnext = {}
        for q in ("sp", "pool"):
            self.dsem[q] = [es.enter_context(nc.semaphore(f"d_{q}{i}")) for i in range(self.NDMA)]
            self.dval[q] = [0] * self.NDMA
            self.dnext[q] = 0
        self.ninst = 0

    def _wait(self, eng, tk):
        if tk is None:
            return
        kind = tk[0]
        if kind == "c":
            _, src, n = tk
            if src == eng and eng in ("pe", "sp"):
                return
            if self.seen[eng].get(src, 0) >= n:
                return
            self.h[eng].wait_ge(self.sem[src], n)
            self.seen[eng][src] = n
        else:
            _, q, idx, val = tk
            key = (q, idx)
            if self.seen[eng].get(key, 0) >= val:
                return
            self.h[eng].wait_ge(self.dsem[q][idx], val)
            self.seen[eng][key] = val
        self.ninst += 1

    def _deps(self, eng, R, W):
        for b in R:
            self._wait(eng, b.w)
        for b in W:
            self._wait(eng, b.w)
            for tk in b.rs.values():
                self._wait(eng, tk)

    def _record(self, tk, R, W):
        for b in W:
            b.w = tk
            b.rs = {}
        for b in R:
            if b in W:
                continue
            key = tk[1] if tk[0] == "c" else (tk[1], tk[2])
            b.rs[key] = tk

    @staticmethod
    def _flat(L):
        out = []
        for b in L:
            if isinstance(b, (list, tuple)):
                out.extend(Sched._flat(b))
            else:
                out.append(b)
        return out

    def op(self, eng, fn, R=(), W=(), signal=True):
        R, W = self._flat(R), self._flat(W)
        self._deps(eng, R, W)
        ins = fn(self.h[eng])
        self.ninst += 1
        if signal:
            ins.then_inc(self.sem[eng], 1)
            self.cnt[eng] += 1
            tk = ("c", eng, self.cnt[eng])
            if eng not in ("pe",):
                pass
        else:
            tk = ("c", eng, self.cnt[eng] + 1)
        self._record(tk, R, W)
        return tk

    def dma(self, q, out, in_, R=(), W=()):
        R, W = self._flat(R), self._flat(W)
        self._deps(q, R, W)
        idx = self.dnext[q]
        self.dnext[q] = (idx + 1) % self.NDMA
        prev = self.dval[q][idx]
        if prev > 0:
            self._wait(q, ("d", q, idx, prev))
        val = prev + 16
        self.dval[q][idx] = val
        self.h[q].dma_start(out=out, in_=in_).then_inc(self.dsem[q][idx], 16)
        self.ninst += 1
        tk = ("d", q, idx, val)
        self._record(tk, R, W)
        return tk

    def barrier(self):
        for e in self.h:
            for src in self.h:
                if src != e and self.cnt[src] > 0:
                    self._wait(e, ("c", src, self.cnt[src]))
            for q in ("sp", "pool"):
                for idx in range(self.NDMA):
                    if self.dval[q][idx] > 0:
                        self._wait(e, ("d", q, idx, self.dval[q][idx]))


def _ret_gammas():
    j = np.arange(8, dtype=np.float64)
    g = 1.0 - np.exp2(-5.0 - j / 2)
    return g[0::2], g[1::2]


def _const_tables():
    t = {}
    half = 128
    inv = 10000.0 ** (-np.arange(0, half, 2, dtype=np.float64) / half)
    p = np.arange(128)
    sign = np.where(p < 64, -1.0, 1.0)
    rows = np.arange(T_LAT // 64, dtype=np.float64)
    cols = np.arange(64, dtype=np.float64)
    ang_r = rows[None, :] * inv[p % 64][:, None]
    ang_c = cols[None, :] * inv[p % 64][:, None]
    t["rope"] = np.concatenate(
        [np.cos(ang_r), np.sin(ang_r) * sign[:, None], np.cos(ang_c), np.sin(ang_c) * sign[:, None]], axis=1
    ).astype(np.float32)
    gf, gb = _ret_gammas()
    b = np.arange(128, dtype=np.float64)[:, None]
    a = np.arange(512, dtype=np.float64)[None, :]
    tabs = np.zeros((RET_H, 128, 1920), dtype=np.float64)
    xs = np.arange(896, dtype=np.float64)[None, :] - 384.0
    for h in range(RET_H):
        tabs[h, :, 0:512] = gf[h] ** (a - b)
        tabs[h, :, 512:1024] = gb[h] ** (b + 511 - a)
        dl = xs - b
        tabs[h, :, 1024:1920] = np.where(dl > 0, gf[h] ** np.maximum(dl, 0),
                                         np.where(dl < 0, gb[h] ** np.maximum(-dl, 0), 2.0))
    t["rdec"] = tabs.astype(np.float32)
    t["ident"] = np.eye(128, dtype=np.float32)
    t["hgmask"] = np.concatenate([np.triu(np.ones((128, 128))), np.tril(np.ones((128, 128)))], axis=1).astype(np.float32)
    t["iota1"] = np.tile(np.arange(1, 257, dtype=np.float32)[None, :], (128, 1))
    return t


class Prog:
    def __init__(self, dbg=None, stop_after=None):
        self.dbg = dbg or []
        self.stop_after = stop_after
        self.nc = bass.Bass("TRN2", target_bir_lowering=False)
        self.es = ExitStack()
        self.S = Sched(self.nc, self.es)
        self.dram = {}
        self.dbg_out = {}

    def din(self, name, shape, dt=F32):
        self.dram[name] = self.nc.dram_tensor(name, list(shape), dt, kind="ExternalInput").ap()
        return self.dram[name]

    def dout(self, name, shape, dt=F32):
        self.dram[name] = self.nc.dram_tensor(name, list(shape), dt, kind="ExternalOutput").ap()
        return self.dram[name]

    def sb(self, es, name, shape, dt):
        self._uid = getattr(self, "_uid", 0) + 1
        return es.enter_context(self.nc.sbuf_tensor(f"{name}_u{self._uid}", list(shape), dt))

    def ps(self, es, name, shape, dt=F32):
        return es.enter_context(self.nc.psum_tensor(name, list(shape), dt))


def build_program(dbg=(), stop_after=None, parts=("ret", "moe0", "hg", "moe1", "final"), n_exp_run=N_EXP):
    P = Prog(list(dbg), stop_after)
    P.parts = parts
    P.n_exp_run = n_exp_run
    nc, S, es = P.nc, P.S, P.es
    op, dma = S.op, S.dma

    xT_d = P.din("xT", [D, T_ALL])
    cvec_d = P.din("cvec", [128, 2 * KD])
    wada_d = P.din("w_ada", [2, D, 6 * D])
    bada_d = P.din("b_ada", [2, 12 * D])
    nrm_d = P.din("norms", [128, 5 * KD])
    retin_d = P.din("ret_w_in", [D, 6144])
    retout_d = P.din("ret_w_out", [2048, D])
    router_d = P.din("moe_router", [2, D, N_EXP])
    rope_d = P.din("rope", [128, 192])
    rdec_d = P.din("rdec", [RET_H, 128, 1920])
    ident_d = P.din("ident", [128, 128])
    out_d = P.dout("outT", [D, T_LAT])
    for name, shape, dt_ in P.dbg:
        P.dbg_out[name] = P.dout("dbg_" + name, shape, dt_)

    xT = P.sb(es, "xT_sb", [128, KD, T_ALL], F32)
    B_x = [[Buf(f"x{k}_{b}") for b in range(5)] for k in range(KD)]
    cvec = P.sb(es, "cvec_sb", [128, 2 * KD], F32)
    scc = P.sb(es, "scc", [128, KD, 2], F32)
    nrm = P.sb(es, "nrm", [128, 5 * KD], F32)
    mod = P.sb(es, "mod", [128, 2, 48, 2], F32)
    modA = P.sb(es, "modA", [128, 2, 2, KD, 2], F32)
    ident_f = P.sb(es, "ident_f", [128, 128], F32)
    ident_b = P.sb(es, "ident_b", [128, 128], BF16)
    ones_b = P.sb(es, "ones_b", [128, 128], BF16)
    B_const = Buf("const")
    B_mod = Buf("mod")

    psb = [P.ps(es, f"psb{i}", [128, 512], F32) for i in range(8)]
    B_ps = [Buf(f"ps{i}") for i in range(8)]

    def sl(b):
        s, n, _ = BLOCKS[b]
        return slice(s, s + n)

    xsrc = xT_d.rearrange("(k p) t -> p k t", p=128)
    for k in range(KD):
        dma("sp", xT[:, k, :], xsrc[:, k, :], W=B_x[k])
    dma("sp", cvec[:], cvec_d, W=[B_const])
    dma("sp", nrm[:], nrm_d, W=[B_const])
    dma("sp", ident_f[:], ident_d, W=[B_const])
    op("act", lambda e: e.copy(out=ident_b[:], in_=ident_f[:]), R=[B_const], W=[B_const])
    op("dve", lambda e: e.memset(ones_b[:], 1.0), W=[B_const])
    op("act", lambda e: e.activation(out=scc[:].rearrange("p k c -> p c k"),
                                     in_=cvec[:].rearrange("p (c k) -> p c k", c=2), func=AF.Silu),
       R=[B_const], W=[B_const])

    with ExitStack() as pes:
        NPIECE = 12
        NST = 4
        wa = [P.sb(pes, f"wa{i}", [128, KD, 512], F32) for i in range(NST)]
        B_wa = [Buf(f"wa{i}") for i in range(NST)]
        HALF = 3 * D
        modrow = P.sb(pes, "modrow", [2, HALF], F32)
        B_mr = Buf("modrow")
        bada2 = P.sb(pes, "bada2", [2, HALF], F32)
        B_b2 = Buf("bada2")
        pi = 0
        for l in range(2):
            wsrc = wada_d[l].rearrange("(k p) f -> p k f", p=128)
            for hf in range(2):
                dma("sp", bada2[:], bada_d[:, l * 6 * D + hf * HALF: l * 6 * D + (hf + 1) * HALF], W=[B_b2])
                for pc6 in range(6):
                    pc = hf * 6 + pc6
                    slot = pi % NST
                    dma("sp", wa[slot][:], wsrc[:, :, pc * 512:(pc + 1) * 512], W=[B_wa[slot]])
                    pb = pi % 2
                    for k in range(KD):
                        op("pe", lambda e: e.matmul(psb[pb][0:2, :], lhsT=scc[:, k, :], rhs=wa[slot][:, k, :],
                                                    start=(k == 0), stop=(k == KD - 1)),
                           R=[B_wa[slot], B_const], W=[B_ps[pb]], signal=(k == KD - 1))
                    op("dve", lambda e: e.tensor_tensor(out=modrow[:, pc6 * 512:(pc6 + 1) * 512], in0=psb[pb][0:2, :],
                                                        in1=bada2[:, pc6 * 512:(pc6 + 1) * 512],
                                                        op=ALU.add), R=[B_ps[pb], B_b2], W=[B_mr])
                    pi += 1
                pt_ = 2 + (l * 2 + hf) % 2
                for j in range(24):
                    op("pe", lambda e: e.transpose(out=psb[pt_][:, j * 2:(j + 1) * 2], in_=modrow[:, j * 128:(j + 1) * 128],
                                                   identity=ident_f[0:2, 0:2]),
                       R=[B_mr, B_const], W=[B_ps[pt_]], signal=(j == 23))
                op("dve", lambda e: e.tensor_copy(out=mod[:, l, hf * 24:(hf + 1) * 24, :].rearrange("p j c -> p (j c)"),
                                                  in_=psb[pt_][:, 0:48]), R=[B_ps[pt_]], W=[B_mod])
        for l in range(2):
            for site in range(2):
                sc_j = (1 if site == 0 else 4) * KD
                nw = nrm[:, (site * 2 + l) * KD:(site * 2 + l + 1) * KD]
                op("dve", lambda e, l=l, site=site, sc_j=sc_j, nw=nw: e.scalar_tensor_tensor(
                    out=modA[:, l, site, :, :], in0=mod[:, l, sc_j:sc_j + KD, :], scalar=1.0,
                    in1=nw.unsqueeze(2).to_broadcast([128, KD, 2]), op0=ALU.add, op1=ALU.mult),
                   R=[B_mod, B_const], W=[B_mod])
        S.barrier()

    def modv(l, chunk, k, c):
        return mod[:, l, chunk * KD + k, c:c + 1]

    def norm_block(l, site, b, dst_fn, tmp, B_tmp, rstd, B_rstd, sq, B_sq, psn_i, final=False, after=None):
        s, n, isc = BLOCKS[b]
        c = 1 if isc else 0
        for k in range(KD):
            q = k % 2
            op("act", lambda e, k=k, q=q: e.activation(out=sq[q][:, :n], in_=xT[:, k, s:s + n], func=AF.Square),
               R=[B_x[k][b]], W=[B_sq[q]])
            op("pe", lambda e, k=k, q=q: e.matmul(psb[psn_i][:, :n], lhsT=ones_b[:], rhs=sq[q][:, :n],
                                                 start=(k == 0), stop=(k == KD - 1)),
               R=[B_sq[q], B_const], W=[B_ps[psn_i]], signal=True)
        op("act", lambda e: e.activation(out=rstd[:, :n], in_=psb[psn_i][:, :n], func=AF.Sqrt, bias=EPS,
                                         scale=1.0 / D), R=[B_ps[psn_i]], W=[B_rstd])
        op("dve", lambda e: e.reciprocal(out=rstd[:, :n], in_=rstd[:, :n]), R=[B_rstd], W=[B_rstd])
        for k in range(KD):
            q = k % 2
            out_ap, obufs = dst_fn(k)
            if final:
                a_ap = nrm[:, 4 * KD + k:4 * KD + k + 1]
                op("dve", lambda e, k=k, a_ap=a_ap, out_ap=out_ap: e.scalar_tensor_tensor(
                    out=out_ap, in0=xT[:, k, s:s + n], scalar=a_ap, in1=rstd[:, :n], op0=ALU.mult, op1=ALU.mult),
                   R=[B_x[k][b], B_rstd, B_const], W=obufs)
                if after is not None:
                    after(k)
            else:
                a_ap = modA[:, l, site, k, c:c + 1]
                sh_ap = modv(l, 0 if site == 0 else 3, k, c)
                op("dve", lambda e, k=k, q=q, a_ap=a_ap: e.scalar_tensor_tensor(
                    out=tmp[q][:, :n], in0=xT[:, k, s:s + n], scalar=a_ap, in1=rstd[:, :n],
                    op0=ALU.mult, op1=ALU.mult), R=[B_x[k][b], B_rstd, B_mod], W=[B_tmp[q]])
                op("act", lambda e, q=q, sh_ap=sh_ap, out_ap=out_ap: e.activation(
                    out=out_ap, in_=tmp[q][:, :n], func=AF.Identity, bias=sh_ap, scale=1.0),
                   R=[B_tmp[q], B_mod], W=obufs)

    def debug_dump(name, ap_fn):
        if name in P.dbg_out:
            S.barrier()
            tk = ap_fn(P.dbg_out[name])
            S.barrier()

    NSLOT = 4
    wring = [P.sb(es, f"wring{i}", [128, KD, 512], BF16) for i in range(NSLOT)]
    B_wr = [Buf(f"wr{i}") for i in range(NSLOT)]
    wr_state = {"n": 0}

    def wslot():
        i = wr_state["n"] % NSLOT
        wr_state["n"] += 1
        return i

    def retention_layer(l):
        with ExitStack() as les:
            hT = P.sb(les, "hT", [128, KD, T_ALL], BF16)
            B_h = [[Buf(f"h{k}_{b}") for b in range(5)] for k in range(KD)]
            tmp = [P.sb(les, f"ntmp{i}", [128, 512], F32) for i in range(2)]
            B_tmp = [Buf("ntmp0"), Buf("ntmp1")]
            sg = [P.sb(les, f"sg{i}", [128, 512], F32) for i in range(2)]
            B_sg = [Buf("sg0"), Buf("sg1")]
            rstd = P.sb(les, "rstd", [128, 512], F32)
            B_rstd = Buf("rstd")
            sq = [P.sb(les, f"sq{i}", [128, 512], BF16) for i in range(2)]
            B_sq = [Buf("sq0"), Buf("sq1")]
            rope_sb = P.sb(les, "rope_sb", [128, 192], F32)
            B_rope = Buf("rope")
            dma("sp", rope_sb[:], rope_d, W=[B_rope])
            for b in range(5):
                s, n, _ = BLOCKS[b]
                norm_block(l, 0, b, lambda k, b=b, s=s, n=n: (hT[:, k, s:s + n], [B_h[k][b]]),
                           tmp, B_tmp, rstd, B_rstd, sq, B_sq, psn_i=7)
            debug_dump("hT0", lambda o: [dma("sp", o.rearrange("(k p) t -> p k t", p=128)[:, k, :], hT[:, k, :],
                                             R=B_h[k]) for k in range(KD)])
            if P.stop_after == "hT0":
                return

            qr = P.sb(les, "qr", [128, 2, 2, 512], BF16)
            kr = P.sb(les, "kr", [128, 2, T_ALL], BF16)
            vv = P.sb(les, "vv", [128, NT, 512], BF16)
            B_q = [[Buf(f"q{i}_{m}") for m in range(2)] for i in range(2)]
            B_k = [[Buf(f"k{m}_{t}") for t in range(NT)] for m in range(2)]
            B_v = [Buf(f"v{t}") for t in range(NT)]
            rdec = P.sb(les, "rdec_sb", [128, 1920], F32)
            B_rdec = Buf("rdec")
            Ff = rdec[:, 0:512]
            Fb = rdec[:, 512:1024]

            def Dg(kk, n):
                return rdec[:, 1024 + 384 - 128 * kk: 1024 + 384 - 128 * kk + n]
            rt1, B_rt1 = tmp, B_tmp
            rt2, B_rt2 = sg, B_sg
            ctab, B_ctab = tmp, B_tmp
            AT = [P.sb(les, f"AT{i}", [128, 512], BF16) for i in range(3)]
            B_AT = [Buf(f"AT{i}") for i in range(3)]
            ogT = P.sb(les, "ogT", [128, 4, 512], BF16)
            B_og = [Buf(f"og{c}") for c in range(4)]
            sgb = P.sb(les, "sgb", [128, 4, 512], BF16)
            B_sgb = [Buf(f"sgb{c}") for c in range(4)]
            gf, gb = _ret_gammas()
            rcount = {"rope": 0, "at": 0, "q": 0}

            win_src = retin_d.rearrange("(k p) f -> p k f", p=128)
            wout_src = retout_d.rearrange("(c p) f -> p c f", p=128)

            def proj_rope(sA, qk, m, b, dst_ap, wb):
                s, n, isc = BLOCKS[b]
                pi_ = rcount["rope"] % 2
                rcount["rope"] += 1
                pst = psb[pi_]
                for k in range(KD):
                    op("pe", lambda e, k=k: e.matmul(
                        pst[:, :n], lhsT=wring[sA][:, k, qk * 256 + m * 128: qk * 256 + (m + 1) * 128],
                        rhs=hT[:, k, s:s + n], start=(k == 0), stop=(k == KD - 1)),
                       R=[B_wr[sA], B_h[k][b]], W=[B_ps[pi_]], signal=(k == KD - 1))
                scale = 1.0 if qk == 0 else 1.0 / 16.0
                if isc:
                    op("act", lambda e: e.activation(out=dst_ap, in_=pst[:, :n], func=AF.Copy, scale=scale),
                       R=[B_ps[pi_]], W=wb)
                    return
                g0 = s // 64
                if m == 0:
                    cos_ap = rope_sb[:, g0:g0 + 8].unsqueeze(2).to_broadcast([128, 8, 64])
                    sin_lo = rope_sb[0:64, 32 + g0:32 + g0 + 8].unsqueeze(2).to_broadcast([64, 8, 64])
                    sin_hi = rope_sb[64:128, 32 + g0:32 + g0 + 8].unsqueeze(2).to_broadcast([64, 8, 64])
                else:
                    cos_ap = rope_sb[:, 64:128].unsqueeze(1).to_broadcast([128, 8, 64])
                    sin_lo = rope_sb[0:64, 128:192].unsqueeze(1).to_broadcast([64, 8, 64])
                    sin_hi = rope_sb[64:128, 128:192].unsqueeze(1).to_broadcast([64, 8, 64])
                ti = pi_
                p3 = pst[:].rearrange("p (g t) -> p g t", t=64)
                t1v = rt1[ti][:].rearrange("p (g t) -> p g t", t=64)
                t2v = rt2[ti][:].rearrange("p (g t) -> p g t", t=64)
                op("dve", lambda e: e.scalar_tensor_tensor(
                    out=t1v, in0=p3, scalar=scale, in1=cos_ap, op0=ALU.mult, op1=ALU.mult),
                   R=[B_ps[pi_], B_rope], W=[B_rt1[ti]])
                op("dve", lambda e: e.scalar_tensor_tensor(
                    out=t2v[0:64], in0=p3[64:128], scalar=scale, in1=sin_lo, op0=ALU.mult, op1=ALU.mult),
                   R=[B_ps[pi_], B_rope], W=[B_rt2[ti]])
                op("dve", lambda e: e.scalar_tensor_tensor(
                    out=t2v[64:128], in0=p3[0:64], scalar=scale, in1=sin_hi, op0=ALU.mult, op1=ALU.mult),
                   R=[B_ps[pi_], B_rope, B_rt2[ti]], W=[B_rt2[ti]])
                op("dve", lambda e: e.tensor_tensor(
                    out=dst_ap, in0=rt1[ti][:, :n], in1=rt2[ti][:, :n], op=ALU.add),
                   R=[B_rt1[ti], B_rt2[ti]], W=wb)

            for h in range(RET_H):
                sA, sB, sC, sD = wslot(), wslot(), wslot(), wslot()
                dma("pool", wring[sA][:, :, 0:256], win_src[:, :, h * 256:(h + 1) * 256], W=[B_wr[sA]])
                dma("pool", wring[sA][:, :, 256:512], win_src[:, :, 1024 + h * 256:1024 + (h + 1) * 256], W=[B_wr[sA]])
                dma("pool", wring[sB][:], win_src[:, :, 2048 + h * 512:2048 + (h + 1) * 512], W=[B_wr[sB]])
                dma("pool", wring[sC][:], win_src[:, :, 4096 + h * 512:4096 + (h + 1) * 512], W=[B_wr[sC]])
                wD = wring[sD][:].rearrange("p k f -> p (k f)").rearrange("p (c f) -> p c f", c=4)
                dma("pool", wD, wout_src[:, h * 4:(h + 1) * 4, :], W=[B_wr[sD]])
                dma("sp", rdec[:], rdec_d[h], W=[B_rdec])

                def kproj_gen():
                    for m in range(2):
                        for b in range(5):
                            s, n, isc = BLOCKS[b]
                            proj_rope(sA, 1, m, b, kr[:, m, s:s + n], [B_k[m][t] for t in range(s // 128, (s + n) // 128)])
                            yield

                def vproj_gen():
                    for t in range(NT):
                        b = min(t // 4, 4)
                        pi_ = 6 + (t % 2)
                        for k in range(KD):
                            op("pe", lambda e, k=k, t=t, pi_=pi_: e.matmul(
                                psb[pi_][:, :], lhsT=hT[:, k, t * 128:(t + 1) * 128], rhs=wring[sB][:, k, :],
                                start=(k == 0), stop=(k == KD - 1)),
                               R=[B_wr[sB], B_h[k][b]], W=[B_ps[pi_]], signal=(k == KD - 1))
                        op("act", lambda e, t=t, pi_=pi_: e.copy(out=vv[:, t, :], in_=psb[pi_][:, :]),
                           R=[B_ps[pi_]], W=[B_v[t]])
                        yield
                interleave(kproj_gen(), vproj_gen())
                if h == 0:
                    debug_dump("kr0", lambda o: [dma("sp", o[:, m, :], kr[:, m, :], R=B_k[m]) for m in range(2)])
                    debug_dump("vv0", lambda o: [dma("sp", o, vv[:], R=B_v)])
                    if P.stop_after == "proj0":
                        return

                def block_gen(b):
                    s, n, isc = BLOCKS[b]
                    c = 1 if isc else 0
                    qi = rcount["q"] % 2
                    rcount["q"] += 1
                    for m in range(2):
                        proj_rope(sA, 0, m, b, qr[:, qi, m, :n], [B_q[qi][m]])
                    yield
                    for cc in range(4):
                        pg = cc % 2
                        for k in range(KD):
                            op("pe", lambda e: e.matmul(
                                psb[pg][:, :n], lhsT=wring[sC][:, k, cc * 128:(cc + 1) * 128], rhs=hT[:, k, s:s + n],
                                start=(k == 0), stop=(k == KD - 1)),
                               R=[B_wr[sC], B_h[k][b]], W=[B_ps[pg]], signal=(k == KD - 1))
                        op("act", lambda e: e.activation(out=sgb[:, cc, :n], in_=psb[pg][:, :n], func=AF.Silu),
                           R=[B_ps[pg]], W=[B_sgb[cc]])
                    keys = [16, 17] if isc else list(range(NT))
                    pso = [psb[2 + cc] for cc in range(4)]
                    B_pso = [B_ps[2 + cc] for cc in range(4)]

                    def emit_scores(j, idx):
                        pi_ = 6 + (idx % 2)
                        for m in range(2):
                            op("pe", lambda e, m=m: e.matmul(
                                psb[pi_][:, :n], lhsT=kr[:, m, j * 128:(j + 1) * 128], rhs=qr[:, qi, m, :n],
                                start=(m == 0), stop=(m == 1)),
                               R=[B_k[m][j], B_q[qi][m]], W=[B_ps[pi_]], signal=(m == 1))
                        ai = rcount["at"] % 3
                        rcount["at"] += 1
                        pst = psb[pi_]
                        if isc:
                            tab = Dg(j - 16, n)
                            op("dve", lambda e: e.tensor_tensor(out=AT[ai][:, :n], in0=pst[:, :n], in1=tab, op=ALU.mult),
                               R=[B_ps[pi_], B_rdec], W=[B_AT[ai]])
                        elif j >= 16:
                            jc = j - 16
                            s1 = float(gf[h] ** (s + 256 - 128 * jc))
                            s2 = float(gb[h] ** (T_LAT - s - 511 + 128 * jc))
                            ci = jc
                            op("pool", lambda e: e.tensor_scalar(
                                out=ctab[ci][:], in0=Ff, scalar1=s1, scalar2=None, op0=ALU.mult),
                               R=[B_rdec], W=[B_ctab[ci]])
                            op("dve", lambda e: e.scalar_tensor_tensor(
                                out=ctab[ci][:], in0=Fb, scalar=s2, in1=ctab[ci][:], op0=ALU.mult, op1=ALU.add),
                               R=[B_rdec, B_ctab[ci]], W=[B_ctab[ci]])
                            op("dve", lambda e: e.tensor_tensor(
                                out=AT[ai][:, :n], in0=pst[:, :n], in1=ctab[ci][:, :n], op=ALU.mult),
                               R=[B_ps[pi_], B_ctab[ci]], W=[B_AT[ai]])
                        else:
                            rel = j - 4 * b
                            if 0 <= rel < 4:
                                tab = Dg(rel, 512)
                                op("dve", lambda e: e.tensor_tensor(out=AT[ai][:], in0=pst[:], in1=tab, op=ALU.mult),
                                   R=[B_ps[pi_], B_rdec], W=[B_AT[ai]])
                            elif rel < 0:
                                sc = float(gf[h] ** (s - 128 * j))
                                op("dve", lambda e: e.scalar_tensor_tensor(
                                    out=AT[ai][:], in0=pst[:], scalar=sc, in1=Ff, op0=ALU.mult, op1=ALU.mult),
                                   R=[B_ps[pi_], B_rdec], W=[B_AT[ai]])
                            else:
                                sc = float(gb[h] ** (128 * j - s - 511))
                                op("dve", lambda e: e.scalar_tensor_tensor(
                                    out=AT[ai][:], in0=pst[:], scalar=sc, in1=Fb, op0=ALU.mult, op1=ALU.mult),
                                   R=[B_ps[pi_], B_rdec], W=[B_AT[ai]])
                        return ai

                    def emit_av(j, ai, first, last):
                        for cc in range(4):
                            op("pe", lambda e, cc=cc: e.matmul(
                                pso[cc][:, :n], lhsT=vv[:, j, cc * 128:(cc + 1) * 128], rhs=AT[ai][:, :n],
                                start=first, stop=last),
                               R=[B_v[j], B_AT[ai]], W=[B_pso[cc]], signal=(cc == 3))

                    pend = None
                    for idx, j in enumerate(keys):
                        ai = emit_scores(j, idx)
                        if pend is not None:
                            emit_av(*pend)
                        pend = (j, ai, idx == 0, idx == len(keys) - 1)
                    emit_av(*pend)

                    yield
                    for cc in range(4):
                        q2 = cc % 2
                        op("act", lambda e, cc=cc, q2=q2: e.activation(out=sq[q2][:, :n], in_=pso[cc][:, :n],
                                                                      func=AF.Square),
                           R=[B_pso[cc]], W=[B_sq[q2]])
                        op("pe", lambda e, cc=cc, q2=q2: e.matmul(psb[6][:, :n], lhsT=ones_b[:], rhs=sq[q2][:, :n],
                                                                 start=(cc == 0), stop=(cc == 3)),
                           R=[B_sq[q2], B_const], W=[B_ps[6]], signal=True)
                    op("act", lambda e: e.activation(out=rstd[:, :n], in_=psb[6][:, :n], func=AF.Sqrt, bias=EPS,
                                                     scale=1.0 / 512), R=[B_ps[6]], W=[B_rstd])
                    op("dve", lambda e: e.reciprocal(out=rstd[:, :n], in_=rstd[:, :n]), R=[B_rstd], W=[B_rstd])
                    for cc in range(4):
                        q2 = cc % 2
                        op("dve", lambda e, cc=cc, q2=q2: e.tensor_tensor(
                            out=tmp[q2][:, :n], in0=pso[cc][:, :n], in1=rstd[:, :n], op=ALU.mult),
                           R=[B_pso[cc], B_rstd], W=[B_tmp[q2]])
                        op("dve", lambda e, cc=cc, q2=q2: e.tensor_tensor(
                            out=ogT[:, cc, :n], in0=tmp[q2][:, :n], in1=sgb[:, cc, :n], op=ALU.mult),
                           R=[B_tmp[q2], B_sgb[cc]], W=[B_og[cc]])
                    for mch in range(KD):
                        pi_ = 6 + (mch % 2)
                        for cc in range(4):
                            op("pe", lambda e, cc=cc, mch=mch, pi_=pi_: e.matmul(
                                psb[pi_][:, :n], lhsT=wD[:, cc, mch * 128:(mch + 1) * 128], rhs=ogT[:, cc, :n],
                                start=(cc == 0), stop=(cc == 3)),
                               R=[B_wr[sD], B_og[cc]], W=[B_ps[pi_]], signal=(cc == 3))
                        g_ap = modv(l, 2, mch, c)
                        op("dve", lambda e, mch=mch, pi_=pi_, g_ap=g_ap: e.scalar_tensor_tensor(
                            out=xT[:, mch, s:s + n], in0=psb[pi_][:, :n], scalar=g_ap, in1=xT[:, mch, s:s + n],
                            op0=ALU.mult, op1=ALU.add),
                           R=[B_ps[pi_], B_mod, B_x[mch][b]], W=[B_x[mch][b]])

                gens = [block_gen(b) for b in range(5)]
                next(gens[0])
                for b in range(5):
                    next(gens[b])
                    if b + 1 < 5:
                        next(gens[b + 1])
                    for _ in gens[b]:
                        pass
            S.barrier()

    iota_d2 = P.din("iota1", [128, 256])

    def moe_layer(l, with_ctx, n_exp_run=N_EXP):
        nblk = 5 if with_ctx else 4
        ntile = NT if with_ctx else 16
        NS = 288 if with_ctx else 256
        nst = 3 if with_ctx else 2
        st_sz = [128, 128, 32][:nst]
        st_off = [0, 128, 256][:nst]
        with ExitStack() as mes:
            h2tok = P.sb(mes, "h2tok", [128, NT, D], BF16)
            B_h2 = [Buf(f"h2t{t}") for t in range(NT)]
            wr_sb = P.sb(mes, "wr_sb", [128, KD, N_EXP], F32)
            B_wrt = Buf("wrt")
            dma("sp", wr_sb[:], router_d[l].rearrange("(k p) e -> p k e", p=128), W=[B_wrt])
            iota1 = P.sb(mes, "iota1_sb", [128, 256], F32)
            dma("sp", iota1[:], iota_d2, W=[B_wrt])
            aff = P.sb(mes, "aff", [128, NT, N_EXP], F32)
            B_aff = Buf("aff")
            aff_hl = P.sb(mes, "aff_hl", [128, NT, N_EXP, 2], BF16)
            posm_tok = P.sb(mes, "posm_tok", [128, NT, N_EXP], F32)
            B_posm = Buf("posm")

            with ExitStack() as r1:
                tmp = [P.sb(r1, f"mtmp{i}", [128, 512], F32) for i in range(2)]
                B_tmp = [Buf("mtmp0"), Buf("mtmp1")]
                rstd = P.sb(r1, "mrstd", [128, 512], F32)
                B_rstd = Buf("mrstd")
                sq = [P.sb(r1, f"msq{i}", [128, 512], BF16) for i in range(2)]
                B_sq = [Buf("msq0"), Buf("msq1")]
                h2f = P.sb(r1, "h2f", [128, KD, 512], F32)
                B_h2f = [Buf(f"h2f{k}") for k in range(KD)]
                h2b = P.sb(r1, "h2b", [128, KD, 512], BF16)
                B_h2b = [Buf(f"h2b{k}") for k in range(KD)]
                for b in range(nblk):
                    s, n, isc = BLOCKS[b]
                    norm_block(l, 1, b, lambda k, n=n: (h2f[:, k, :n], [B_h2f[k]]),
                               tmp, B_tmp, rstd, B_rstd, sq, B_sq, psn_i=7)
                    for k in range(KD):
                        eng_ = "act" if k % 2 == 0 else "dve"
                        if eng_ == "act":
                            op("act", lambda e, k=k: e.copy(out=h2b[:, k, :n], in_=h2f[:, k, :n]), R=[B_h2f[k]], W=[B_h2b[k]])
                        else:
                            op("dve", lambda e, k=k: e.tensor_copy(out=h2b[:, k, :n], in_=h2f[:, k, :n]),
                               R=[B_h2f[k]], W=[B_h2b[k]])
                    for tt in range(n // 128):
                        t = s // 128 + tt
                        for k in range(KD):
                            op("pe", lambda e, k=k: e.matmul(psb[6][:, 0:N_EXP], lhsT=h2f[:, k, tt * 128:(tt + 1) * 128],
                                                            rhs=wr_sb[:, k, :], start=(k == 0), stop=(k == KD - 1)),
                               R=[B_h2f[k], B_wrt], W=[B_ps[6]], signal=(k == KD - 1))
                        op("act", lambda e: e.copy(out=aff[:, t, :], in_=psb[6][:, 0:N_EXP]), R=[B_ps[6]], W=[B_aff])
                        pi_ = t % 2
                        pbf = psb[pi_][:].bitcast(BF16)
                        for k in range(KD):
                            op("pe", lambda e, k=k: e.transpose(out=pbf[:, k * 128:(k + 1) * 128],
                                                               in_=h2b[:, k, tt * 128:(tt + 1) * 128], identity=ident_b[:]),
                               R=[B_h2b[k], B_const], W=[B_ps[pi_]], signal=(k == KD - 1))
                        op("act", lambda e: e.copy(out=h2tok[:, t, :], in_=pbf), R=[B_ps[pi_]], W=[B_h2[t]])
                mx = P.sb(r1, "smx", [128, NT], F32)
                op("dve", lambda e: e.tensor_reduce(out=mx[:, :ntile], in_=aff[:, :ntile, :], axis=AX.X, op=ALU.max),
                   R=[B_aff], W=[B_rstd])
                op("dve", lambda e: e.tensor_tensor(out=aff[:, :ntile, :], in0=aff[:, :ntile, :],
                                                    in1=mx[:, :ntile].unsqueeze(2).to_broadcast([128, ntile, N_EXP]),
                                                    op=ALU.subtract), R=[B_rstd, B_aff], W=[B_aff])
                op("act", lambda e: e.activation(out=aff[:, :ntile, :], in_=aff[:, :ntile, :], func=AF.Exp),
                   R=[B_aff], W=[B_aff])
                op("dve", lambda e: e.tensor_reduce(out=mx[:, :ntile], in_=aff[:, :ntile, :], axis=AX.X, op=ALU.add),
                   R=[B_aff], W=[B_rstd])
                op("dve", lambda e: e.reciprocal(out=mx[:, :ntile], in_=mx[:, :ntile]), R=[B_rstd], W=[B_rstd])
                op("dve", lambda e: e.tensor_tensor(out=aff[:, :ntile, :], in0=aff[:, :ntile, :],
                                                    in1=mx[:, :ntile].unsqueeze(2).to_broadcast([128, ntile, N_EXP]),
                                                    op=ALU.mult), R=[B_rstd, B_aff], W=[B_aff])
                op("dve", lambda e: e.tensor_copy(out=aff_hl[:, :ntile, :, 0], in_=aff[:, :ntile, :]), R=[B_aff], W=[B_posm])
                op("dve", lambda e: e.tensor_tensor(out=aff_hl[:, :ntile, :, 1], in0=aff[:, :ntile, :],
                                                    in1=aff_hl[:, :ntile, :, 0], op=ALU.subtract),
                   R=[B_aff, B_posm], W=[B_posm])
                S.barrier()
            debug_dump(f"aff{l}", lambda o: [dma("sp", o, aff[:], R=[B_aff])])
            debug_dump(f"h2tok{l}", lambda o: [dma("sp", o, h2tok[:], R=B_h2)])

            with ExitStack() as r2:
                affT = P.sb(r2, "affT", [16, T_ALL], F32)
                mk = P.sb(r2, "mkT", [16, T_ALL], F32)
                cum = P.sb(r2, "cumT", [16, T_ALL], F32)
                sm = P.sb(r2, "bis", [16, 16], F32)
                B_affT, B_mk, B_cum, B_sm = Buf("affT"), Buf("mk"), Buf("cum"), Buf("sm")
                for t in range(ntile):
                    pi_ = t % 2
                    op("pe", lambda e: e.transpose(out=psb[pi_][0:16, 0:128], in_=aff[:, t, :], identity=ident_f[:]),
                       R=[B_aff, B_const], W=[B_ps[pi_]])
                    op("act", lambda e: e.copy(out=affT[:, t * 128:(t + 1) * 128], in_=psb[pi_][0:16, 0:128]),
                       R=[B_ps[pi_]], W=[B_affT])
                sets = [(0, T_LAT, 256)] + ([(T_LAT, T_CTX, 32)] if with_ctx else [])
                one = sm[:, 15:16]
                op("dve", lambda e: e.memset(one, 1.0), W=[B_sm])
                B_smx = [Buf("smA"), Buf("smB")]

                def bisect(si, s0, ns, cap):
                    lo, mid, gs = [sm[:, si * 6 + i: si * 6 + i + 1] for i in range(3)]
                    cnts = [sm[:, si * 6 + 3 + i: si * 6 + 4 + i] for i in range(2)]
                    Bs = B_smx[si]
                    a_ap = affT[:, s0:s0 + ns]
                    op("dve", lambda e: e.memset(lo, 0.0), W=[Bs])
                    op("dve", lambda e: e.memset(mid, 0.5), W=[Bs])
                    NIT = 30
                    for it in range(NIT):
                        step = 0.5 ** (it + 1)
                        cnt = cnts[it % 2]
                        op("dve", lambda e: e.memset(cnt, 0.0), W=[Bs])
                        yield
                        op("dve", lambda e: e.tensor_scalar(out=mk[:, s0:s0 + ns], in0=a_ap, scalar1=mid, scalar2=0.0,
                                                            op0=ALU.is_ge, op1=ALU.add, accum_out=cnt),
                           R=[B_affT, Bs], W=[B_mk, Bs])
                        yield
                        op("dve", lambda e: e.tensor_scalar(out=gs, in0=cnt, scalar1=float(cap), scalar2=step,
                                                            op0=ALU.is_ge, op1=ALU.mult), R=[Bs], W=[Bs])
                        yield
                        if it < NIT - 1:
                            op("dve", lambda e: e.scalar_tensor_tensor(out=mid, in0=gs, scalar=step * 0.5, in1=lo,
                                                                       op0=ALU.add, op1=ALU.add), R=[Bs], W=[Bs])
                            yield
                        op("dve", lambda e: e.tensor_tensor(out=lo, in0=lo, in1=gs, op=ALU.add), R=[Bs], W=[Bs])
                        yield
                    op("dve", lambda e: e.tensor_scalar(out=mk[:, s0:s0 + ns], in0=a_ap, scalar1=lo, scalar2=None,
                                                        op0=ALU.is_ge), R=[B_affT, Bs], W=[B_mk])
                    yield
                    op("dve", lambda e: e.tensor_tensor_scan(out=cum[:, s0:s0 + ns],
                                                             data0=one.to_broadcast([16, ns]), data1=mk[:, s0:s0 + ns],
                                                             initial=0.0, op0=ALU.mult, op1=ALU.add),
                       R=[B_mk, B_sm], W=[B_cum])
                    yield
                    op("dve", lambda e: e.tensor_tensor(out=cum[:, s0:s0 + ns], in0=cum[:, s0:s0 + ns],
                                                        in1=mk[:, s0:s0 + ns], op=ALU.mult), R=[B_mk, B_cum], W=[B_cum])
                    yield

                interleave(*[bisect(si, s0, ns, cap) for si, (s0, ns, cap) in enumerate(sets)])
                for t in range(ntile):
                    pi_ = t % 2
                    op("pe", lambda e: e.transpose(out=psb[pi_][:, 0:16], in_=cum[:, t * 128:(t + 1) * 128],
                                                   identity=ident_f[0:16, 0:16]),
                       R=[B_cum, B_const], W=[B_ps[pi_]])
                    op("act", lambda e: e.copy(out=posm_tok[:, t, :], in_=psb[pi_][:, 0:16]), R=[B_ps[pi_]], W=[B_posm])
                S.barrier()
            debug_dump(f"posm{l}", lambda o: [dma("sp", o, posm_tok[:], R=[B_posm])])
            if P.stop_after == f"route{l}":
                return

            with ExitStack() as xs:
                Pm = P.sb(xs, "Pm", [128, NT, NS], BF16)
                B_Pm = Buf("Pm")
                PTl = P.sb(xs, "PTl", [128, 2, T_LAT], BF16)
                PTc = P.sb(xs, "PTc", [128, T_CTX], BF16)
                B_PT = Buf("PT")

                def PTv(st, a, b2, sz):
                    return PTl[0:sz, st, a:b2] if st < 2 else PTc[0:sz, a - T_LAT:b2 - T_LAT]
                xeT = P.sb(xs, "xeT", [128, KD, NS], BF16)
                B_xe = [Buf(f"xe{k}") for k in range(KD)]
                hmid = P.sb(xs, "hmid", [128, 2, NS], BF16)
                B_hm = [Buf("hm0"), Buf("hm1")]
                sil = P.sb(xs, "sil", [128, 2, NS], BF16)
                B_sil = [Buf("sil0"), Buf("sil1")]
                y_sb = P.sb(xs, "y_sb", [128, nst, D], BF16)
                B_y = [Buf(f"y{i}") for i in range(nst)]
                gsl = P.sb(xs, "gsl", [128, 4], F32)
                B_gsl = Buf("gsl")
                NGU = 5
                NDN = 5 if with_ctx else 6
                wgu = wring + [P.sb(xs, f"wgu{i}", [128, KD, 512], BF16) for i in range(NGU - NSLOT)]
                B_gu = [Buf(f"gu{i}") for i in range(NGU)]
                wdn = [P.sb(xs, f"wdn{i}", [128, 2, D], BF16) for i in range(NDN)]
                B_dn = [Buf(f"dn{i}") for i in range(NDN)]
                op("dve", lambda e: e.memset(Pm[:], 0.0), W=[B_Pm])

                NFB = NFC // 2
                sched = [(e_, fb) for e_ in range(n_exp_run) for fb in range(NFB)]
                issued = {"n": 0}

                def issue_weights(upto):
                    while issued["n"] < min(upto, len(sched)):
                        i = issued["n"]
                        e_, fb = sched[i]
                        if f"wgu_{l}_{e_}" not in P.dram:
                            P.din(f"wgu_{l}_{e_}", [NFC // 2, 128, KD, 512])
                            P.din(f"wdt_{l}_{e_}", [NFC // 2, 128, 2, D])
                        wgu_ap, wd_ap = P.dram[f"wgu_{l}_{e_}"], P.dram[f"wdt_{l}_{e_}"]
                        gi, di = i % NGU, i % NDN
                        dma("pool", wgu[gi][:], wgu_ap[fb], W=[B_gu[gi]])
                        dma("pool", wdn[di][:], wd_ap[fb], W=[B_dn[di]])
                        issued["n"] += 1

                LA = 4 if with_ctx else 5
                issue_weights(LA)
                blk_i = 0
                deferred = {"scatter": None}
                xb = (6, 7) if with_ctx else (4, 5)
                for e_ in range(n_exp_run):
                    op("dve", lambda e: e.tensor_tensor(
                        out=Pm[:, 0:16, 0:256], in0=iota1[:, :].unsqueeze(1).to_broadcast([128, 16, 256]),
                        in1=posm_tok[:, 0:16, e_:e_ + 1].to_broadcast([128, 16, 256]), op=ALU.is_equal),
                       R=[B_posm, B_wrt], W=[B_Pm])
                    if with_ctx:
                        op("dve", lambda e: e.tensor_tensor(
                            out=Pm[:, 16:18, 256:288], in0=iota1[:, 0:32].unsqueeze(1).to_broadcast([128, 2, 32]),
                            in1=posm_tok[:, 16:18, e_:e_ + 1].to_broadcast([128, 2, 32]), op=ALU.is_equal),
                           R=[B_posm, B_wrt], W=[B_Pm])
                    for st in range(nst):
                        tiles = range(16) if st < 2 else range(16, 18)
                        tl = list(tiles)
                        for ti, t in enumerate(tl):
                            op("pe", lambda e: e.matmul(psb[6][0:st_sz[st], st * 2:st * 2 + 2],
                                                        lhsT=Pm[:, t, st_off[st]:st_off[st] + st_sz[st]],
                                                        rhs=aff_hl[:, t, e_, :], start=(ti == 0), stop=(ti == len(tl) - 1)),
                               R=[B_Pm, B_posm], W=[B_ps[6]], signal=(ti == len(tl) - 1))
                    for st in range(nst):
                        op("dve", lambda e: e.tensor_reduce(out=gsl[0:st_sz[st], st:st + 1],
                                                            in_=psb[6][0:st_sz[st], st * 2:st * 2 + 2], axis=AX.X, op=ALU.add),
                           R=[B_ps[6]], W=[B_gsl])
                    def do_PT():
                        gcnt = 0
                        for st in range(nst):
                            tl = list(range(16)) if st < 2 else [16, 17]
                            for g0 in range(0, len(tl), 8):
                                grp = tl[g0:g0 + 8]
                                pi_ = xb[gcnt % 2]
                                gcnt += 1
                                pbf = psb[pi_][:].bitcast(BF16)
                                for gi_, t in enumerate(grp):
                                    op("pe", lambda e: e.transpose(
                                        out=pbf[0:st_sz[st], gi_ * 128:(gi_ + 1) * 128],
                                        in_=Pm[:, t, st_off[st]:st_off[st] + st_sz[st]], identity=ident_b[:]),
                                       R=[B_Pm, B_const], W=[B_ps[pi_]], signal=(gi_ == len(grp) - 1))
                                op("act", lambda e: e.copy(out=PTv(st, grp[0] * 128, (grp[-1] + 1) * 128, st_sz[st]),
                                                           in_=pbf[0:st_sz[st], 0:len(grp) * 128]),
                                   R=[B_ps[pi_]], W=[B_PT])
                    for k in range(KD):
                        pi_ = 6 + (k % 2)
                        for t in range(ntile):
                            op("pe", lambda e: e.matmul(psb[pi_][:, 0:NS], lhsT=h2tok[:, t, k * 128:(k + 1) * 128],
                                                        rhs=Pm[:, t, :], start=(t == 0), stop=(t == ntile - 1)),
                               R=[B_h2[t], B_Pm], W=[B_ps[pi_]], signal=(t == ntile - 1))
                        op("act", lambda e: e.copy(out=xeT[:, k, :], in_=psb[pi_][:, 0:NS]), R=[B_ps[pi_]], W=[B_xe[k]])
                    psY = [[psb[st * 2 + nh] for nh in range(2)] for st in range(nst)]
                    B_psY = [[B_ps[st * 2 + nh] for nh in range(2)] for st in range(nst)]

                    def emit_down(fc, hb, di, first, last):
                        for st in range(nst):
                            for nh in range(2):
                                op("pe", lambda e: e.matmul(
                                    psY[st][nh][0:st_sz[st], :], lhsT=hmid[:, hb, st_off[st]:st_off[st] + st_sz[st]],
                                    rhs=wdn[di][:, fc % 2, nh * 512:(nh + 1) * 512], start=first, stop=last),
                                   R=[B_hm[hb], B_dn[di]], W=[B_psY[st][nh]], signal=(st == nst - 1 and nh == 1))

                    pend = None
                    for fb in range(NFB):
                        i = blk_i
                        blk_i += 1
                        issue_weights(i + LA)
                        if fb == 3:
                            if deferred["scatter"] is not None:
                                deferred["scatter"]()
                                deferred["scatter"] = None
                            do_PT()
                        gi, di = i % NGU, i % NDN
                        for f2 in range(2):
                            fc = fb * 2 + f2
                            hb = fc % 2
                            ia, iu = 6, 7
                            pa, pu = psb[ia], psb[iu]
                            for k in range(KD):
                                op("pe", lambda e: e.matmul(pa[:, 0:NS], lhsT=wgu[gi][:, k, f2 * 128:(f2 + 1) * 128],
                                                            rhs=xeT[:, k, :], start=(k == 0), stop=(k == KD - 1)),
                                   R=[B_gu[gi], B_xe[k]], W=[B_ps[ia]], signal=(k == KD - 1))
                            for k in range(KD):
                                op("pe", lambda e: e.matmul(pu[:, 0:NS],
                                                            lhsT=wgu[gi][:, k, 256 + f2 * 128:256 + (f2 + 1) * 128],
                                                            rhs=xeT[:, k, :], start=(k == 0), stop=(k == KD - 1)),
                                   R=[B_gu[gi], B_xe[k]], W=[B_ps[iu]], signal=(k == KD - 1))
                            op("act", lambda e: e.activation(out=sil[:, hb, :], in_=pa[:, 0:NS], func=AF.Silu),
                               R=[B_ps[ia]], W=[B_sil[hb]])
                            op("dve", lambda e: e.tensor_tensor(out=hmid[:, hb, :], in0=pu[:, 0:NS], in1=sil[:, hb, :],
                                                                op=ALU.mult), R=[B_ps[iu], B_sil[hb]], W=[B_hm[hb]])
                            if pend is not None:
                                emit_down(*pend)
                            pend = (fc, hb, di, fc == 0, fc == NFC - 1)
                    emit_down(*pend)
                    for st in range(nst):
                        for nh in range(2):
                            op("act", lambda e: e.activation(out=y_sb[0:st_sz[st], st, nh * 512:(nh + 1) * 512],
                                                             in_=psY[st][nh][0:st_sz[st], :], func=AF.Copy,
                                                             scale=gsl[0:st_sz[st], st:st + 1]),
                               R=[B_psY[st][nh], B_gsl], W=[B_y[st]])
                    def do_scatter():
                        gi_s = 0
                        for b in range(nblk):
                            s, n, isc = BLOCKS[b]
                            c = 1 if isc else 0
                            sts = [2] if isc else [0, 1]
                            for mch in range(KD):
                                pi_ = xb[gi_s % 2]
                                gi_s += 1
                                for si, st in enumerate(sts):
                                    op("pe", lambda e: e.matmul(psb[pi_][:, :n],
                                                                lhsT=y_sb[0:st_sz[st], st, mch * 128:(mch + 1) * 128],
                                                                rhs=PTv(st, s, s + n, st_sz[st]), start=(si == 0),
                                                                stop=(si == len(sts) - 1)),
                                       R=[B_y[st], B_PT], W=[B_ps[pi_]], signal=(si == len(sts) - 1))
                                g_ap = modv(l, 5, mch, c)
                                op("dve", lambda e: e.scalar_tensor_tensor(
                                    out=xT[:, mch, s:s + n], in0=psb[pi_][:, :n], scalar=g_ap, in1=xT[:, mch, s:s + n],
                                    op0=ALU.mult, op1=ALU.add),
                                   R=[B_ps[pi_], B_mod, B_x[mch][b]], W=[B_x[mch][b]])
                    deferred["scatter"] = do_scatter
                if deferred["scatter"] is not None:
                    deferred["scatter"]()
                S.barrier()

    def hgrn2_layer(l):
        hgin_d = P.din("hg_w_in", [D, 5120])
        hgout_d = P.din("hg_w_out", [D, D])
        hgmisc_d = P.din("hg_misc", [128, 1 + 4 * KD])
        hgmask_d = P.din("hgmask", [128, 256])
        PT_ = 256
        NPART = 9
        NCH = 2
        with ExitStack() as les:
            hT = P.sb(les, "hT1", [128, KD, T_ALL], BF16)
            B_h = [[Buf(f"h1{k}_{b}") for b in range(5)] for k in range(KD)]
            B_hT = Buf("hT1all")
            hgm = P.sb(les, "hgm", [128, 1 + 4 * KD], F32)
            msk = P.sb(les, "hgmask_sb", [128, 256], F32)
            lbs = P.sb(les, "lbs", [128, 2, 2, HG_H], F32)
            lbs2 = P.sb(les, "lbs2", [128, 2, 2, HG_H], F32)
            gnh = P.sb(les, "gnh", [128, 1], F32)
            B_hc = Buf("hgconst")
            dma("sp", hgm[:], hgmisc_d, W=[B_hc])
            dma("sp", msk[:], hgmask_d, W=[B_hc])
            AA = P.sb(les, "hgAA", [128, 14 * PT_], F32)
            RG = [AA[:, r * PT_:(r + 1) * PT_] for r in range(14)]
            B_R = [Buf(f"hgR{r}") for r in range(14)]

            def Aregs(d_, par):
                idx = [d_ * 2 + par, 4 + d_ * 2 + par, 8 + d_, 10 + d_, 12 + d_]
                return [RG[i] for i in idx], [B_R[i] for i in idx]
            tmp = [AA[:, 0:512], AA[:, 768:1280]]
            B_tmp = [[B_R[0], B_R[1]], [B_R[3], B_R[4]]]
            rstds = [AA[:, 1536:2048], AA[:, 2304:2816]]
            B_rstds = [[B_R[6], B_R[7]], [B_R[9], B_R[10]]]
            rstd, B_rstd = rstds[0], B_rstds[0]
            sq = [P.sb(les, f"hsq{i}", [128, 512], BF16) for i in range(2)]
            B_sq = [Buf("hsq0"), Buf("hsq1")]

            class _V:
                def __init__(self, ap):
                    self.ap = ap

                def __getitem__(self, key):
                    return self.ap[key]
            for b in range(5):
                s, n, _ = BLOCKS[b]
                norm_block(l, 0, b, lambda k, b=b, s=s, n=n: (hT[:, k, s:s + n], [B_h[k][b]]),
                           [_V(tmp[0]), _V(tmp[1])], B_tmp, _V(rstd), B_rstd, sq, B_sq, psn_i=7)
            for d_ in range(2):
                op("dve", lambda e: e.tensor_tensor(out=lbs[:, 0, d_, :], in0=hgm[:, 1 + (2 + d_) * 8:1 + (3 + d_) * 8],
                                                    in1=hgm[:, 1 + d_ * 8:1 + (d_ + 1) * 8], op=ALU.subtract),
                   R=[B_hc], W=[B_hc])
            op("act", lambda e: e.activation(out=lbs[:, 0, :, :], in_=lbs[:, 0, :, :], func=AF.Sigmoid), R=[B_hc], W=[B_hc])
            op("dve", lambda e: e.tensor_scalar(out=lbs[:, 1, :, :], in0=lbs[:, 0, :, :], scalar1=-1.0, scalar2=1.0,
                                                op0=ALU.mult, op1=ALU.add), R=[B_hc], W=[B_hc])
            op("dve", lambda e: e.tensor_scalar(out=lbs2[:, 0, :, :], in0=lbs[:, 1, :, :], scalar1=0.5, scalar2=None,
                                                op0=ALU.mult), R=[B_hc], W=[B_hc])
            op("dve", lambda e: e.tensor_tensor(out=lbs2[:, 1, :, :], in0=lbs2[:, 0, :, :], in1=lbs[:, 0, :, :], op=ALU.add),
               R=[B_hc], W=[B_hc])
            op("dve", lambda e: e.tensor_scalar(out=gnh[:], in0=hgm[:, 0:1], scalar1=0.5, scalar2=None, op0=ALU.mult),
               R=[B_hc], W=[B_hc])
            S.barrier()
            debug_dump("hT1", lambda o: [dma("sp", o.rearrange("(k p) t -> p k t", p=128)[:, k, :], hT[:, k, :],
                                             R=[B_hT]) for k in range(KD)])

            qs2 = P.sb(les, "hqs", [128, 2, PT_], BF16)
            B_qs2 = [Buf("hqs0"), Buf("hqs1")]
            arr = [[P.sb(les, f"harr{d_}{i}", [128, T_ALL], BF16) for i in range(3)] for d_ in range(2)]
            B_arr = [[Buf(f"harr{d_}{i}") for i in range(3)] for d_ in range(2)]
            sgT, B_sg = arr[0][2], B_arr[0][2]
            ogT, B_og = arr[1][2], B_arr[1][2]
            B_ogb = [[B_og, Buf(f"ogb{b}")] for b in range(4)]
            vv = P.sb(les, "hvv", [128, NT, 128], BF16)
            B_v = Buf("hvv")
            EF = P.sb(les, "hEF", [128, 2, NT], F32)
            EM = P.sb(les, "hEM", [128, 2, NT], F32)
            t6b = P.sb(les, "ht6", [128, 2, 8], F32)
            B_t6 = [Buf("t6a"), Buf("t6b")]
            B_E = Buf("hE")
            Sst = P.sb(les, "hS", [128, 2, 128], F32)
            B_S = [Buf("hS0"), Buf("hS1")]
            Suse = P.sb(les, "hSuse", [128, 2, 16, 128], BF16)
            B_Su = Buf("hSuse")
            kht = [P.sb(les, f"hkht{i}", [128, 128], BF16) for i in range(2)]
            B_kht = [Buf("kht0"), Buf("kht1")]
            ATs = [P.sb(les, f"hAT{i}", [128, 128], BF16) for i in range(8)]
            B_ATs = [Buf(f"hAT{i}") for i in range(8)]
            one_col = hgm[:, 0:1]
            ones_f = P.sb(les, "hones", [128, 1], F32)
            op("dve", lambda e: e.memset(ones_f[:], 1.0), W=[B_hc])

            win_src = hgin_d.rearrange("(k p) f -> p k f", p=128)
            cnt_ = {"ps": 0, "kht": 0, "at": 0}

            def view3(ap):
                return ap.rearrange("p (c i) -> p c i", i=128)

            for h in range(HG_H):
                sX, sY = wslot(), wslot()
                for gi_, off in enumerate((0, 1024, 2048, 3072)):
                    dma("pool", wring[sX][:, :, gi_ * 128:(gi_ + 1) * 128], win_src[:, :, off + h * 128: off + (h + 1) * 128],
                        W=[B_wr[sX]])
                yflat = wring[sY][:].rearrange("p k f -> p (k f)")
                wG = yflat[:, 0:1024].rearrange("p (k f) -> p k f", k=KD)
                wO = yflat[:, 1024:2048]
                dma("pool", wG, win_src[:, :, 4096 + h * 128:4096 + (h + 1) * 128], W=[B_wr[sY]])
                dma("pool", wO, hgout_d[h * 128:(h + 1) * 128, :], W=[B_wr[sY]])

                def proj(gi_, s, n, pi_):
                    for k in range(KD):
                        op("pe", lambda e: e.matmul(psb[pi_][:, :n], lhsT=wring[sX][:, k, gi_ * 128:(gi_ + 1) * 128],
                                                    rhs=hT[:, k, s:s + n], start=(k == 0), stop=(k == KD - 1)),
                           R=[B_wr[sX], B_hT], W=[B_ps[pi_]], signal=(k == KD - 1))

                def stageA(part):
                    p0 = part * PT_
                    c0 = part * NCH
                    par = part % 2
                    qs = qs2[:, par, :]
                    B_qs = B_qs2[par]
                    pi_ = cnt_["ps"] % 2
                    cnt_["ps"] += 1
                    proj(0, p0, PT_, pi_)
                    op("act", lambda e: e.activation(out=qs, in_=psb[pi_][:, :PT_], func=AF.Tanh, scale=0.5),
                       R=[B_ps[pi_]], W=[B_qs])
                    yield
                    op("dve", lambda e: e.scalar_tensor_tensor(out=qs, in0=qs, scalar=1.0, in1=psb[pi_][:, :PT_],
                                                               op0=ALU.add, op1=ALU.mult), R=[B_qs, B_ps[pi_]], W=[B_qs])
                    yield
                    for tt in range(NCH):
                        t = c0 + tt
                        pi_ = cnt_["ps"] % 2
                        cnt_["ps"] += 1
                        for k in range(KD):
                            op("pe", lambda e: e.matmul(psb[pi_][:, 0:128], lhsT=hT[:, k, t * 128:(t + 1) * 128],
                                                        rhs=wring[sX][:, k, 384:512], start=(k == 0), stop=(k == KD - 1)),
                               R=[B_wr[sX], B_hT], W=[B_ps[pi_]], signal=(k == KD - 1))
                        op("act", lambda e: e.copy(out=vv[:, t, :], in_=psb[pi_][:, 0:128]), R=[B_ps[pi_]], W=[B_v])
                        yield
                    regs = [Aregs(d_, par) for d_ in range(2)]
                    pis = []
                    for d_ in range(2):
                        pi_ = cnt_["ps"] % 2
                        cnt_["ps"] += 1
                        pis.append(pi_)
                        proj(1 + d_, p0, PT_, pi_)
                    for d_ in range(2):
                        (A1, A2, A3, A4, A5), BA = regs[d_]
                        op("act", lambda e: e.activation(out=A2, in_=psb[pis[d_]][:, :PT_], func=AF.Tanh, scale=0.5),
                           R=[B_ps[pis[d_]]], W=[BA[1]])
                        yield
                    for d_ in range(2):
                        (A1, A2, A3, A4, A5), BA = regs[d_]
                        op("dve", lambda e: e.tensor_scalar(out=A1, in0=A2, scalar1=-0.5, scalar2=0.5, op0=ALU.mult,
                                                            op1=ALU.add), R=[BA[1]], W=[BA[0]])
                        yield
                    for d_ in range(2):
                        (A1, A2, A3, A4, A5), BA = regs[d_]
                        op("act", lambda e: e.activation(out=A2, in_=A2, func=AF.Ln, bias=lbs2[:, 1, d_, h:h + 1],
                                                         scale=lbs2[:, 0, d_, h:h + 1]), R=[BA[1], B_hc], W=[BA[1]])
                        yield

                def stageB_driver(part):
                    p0 = part * PT_
                    c0 = part * NCH
                    par = part % 2
                    qs = qs2[:, par, :]
                    B_qs = B_qs2[par]
                    def chain(d_):
                        si_ = d_
                        (A1, A2, A3, A4, A5), BA = Aregs(d_, par)
                        t6 = t6b[:, si_, :]
                        B_T = B_t6[si_]
                        oml_ap = lbs[:, 1, d_, h:h + 1]
                        op("dve", lambda e: e.tensor_tensor_scan(out=A3, data0=ones_f[:, 0:1].to_broadcast([128, PT_]),
                                                                 data1=A2, initial=0.0, op0=ALU.mult, op1=ALU.add),
                           R=[BA[1], B_hc], W=[BA[2]])
                        G3, g3 = view3(A3), view3(A2)
                        dst = [arr[d_][i][:, p0:p0 + PT_] for i in range(3)]
                        Bd = B_arr[d_]
                        bc = [128, NCH, 128]
                        if d_ == 0:
                            yield
                            op("dve", lambda e: e.tensor_tensor(out=view3(A4), in0=G3, in1=G3[:, :, 63:64].to_broadcast(bc),
                                                                op=ALU.subtract), R=[BA[2]], W=[BA[3]])
                            yield
                            op("act", lambda e: e.activation(out=A5, in_=A4, func=AF.Exp, bias=-0.6931471805599453), R=[BA[3]], W=[BA[4]])
                            yield
                            op("pool", lambda e: e.tensor_tensor(out=dst[0], in0=qs, in1=A5, op=ALU.mult),
                               R=[B_qs, BA[4]], W=[Bd[0]])
                            yield
                            op("dve", lambda e: e.tensor_tensor(out=t6[:, 0:NCH], in0=G3[:, :, 0], in1=g3[:, :, 0], op=ALU.subtract),
                               R=[BA[2], BA[1]], W=[B_T])
                            yield
                            op("dve", lambda e: e.tensor_tensor(out=EF[:, 0, c0:c0 + NCH], in0=G3[:, :, 127], in1=t6[:, 0:NCH],
                                                                op=ALU.subtract), R=[BA[2], B_T], W=[B_E])
                            yield
                            op("dve", lambda e: e.tensor_tensor(out=EM[:, 0, c0:c0 + NCH], in0=G3[:, :, 63], in1=t6[:, 0:NCH],
                                                                op=ALU.subtract), R=[BA[2], B_T], W=[B_E])
                            yield
                            op("act", lambda e: e.activation(out=A2, in_=A4, func=AF.Exp, scale=-1.0), R=[BA[3]], W=[BA[1]])
                            yield
                            op("dve", lambda e: e.scalar_tensor_tensor(out=dst[1], in0=A1, scalar=oml_ap, in1=A2, op0=ALU.mult, op1=ALU.mult),
                               R=[BA[0], BA[1]], W=[Bd[1]])
                            yield
                            op("dve", lambda e: e.tensor_tensor(out=view3(A4), in0=G3, in1=G3[:, :, 127:128].to_broadcast(bc),
                                                                op=ALU.subtract), R=[BA[2]], W=[BA[3]])
                            yield
                            op("act", lambda e: e.activation(out=A5, in_=A4, func=AF.Exp, scale=-1.0), R=[BA[3]], W=[BA[4]])
                            yield
                            op("dve", lambda e: e.scalar_tensor_tensor(out=dst[2], in0=A1, scalar=oml_ap, in1=A5, op0=ALU.mult, op1=ALU.mult),
                               R=[BA[0], BA[4]], W=[Bd[2]])
                        else:
                            yield
                            op("dve", lambda e: e.tensor_copy(out=t6[:, 0:NCH], in_=G3[:, :, 127]), R=[BA[2]], W=[B_T])
                            yield
                            op("pool", lambda e: e.tensor_tensor(out=A2, in0=A3, in1=A2, op=ALU.subtract),
                               R=[BA[2], BA[1]], W=[BA[1]])
                            H3 = view3(A2)
                            yield
                            op("dve", lambda e: e.tensor_tensor(out=view3(A4), in0=H3, in1=H3[:, :, 64:65].to_broadcast(bc),
                                                                op=ALU.subtract), R=[BA[1]], W=[BA[3]])
                            yield
                            op("act", lambda e: e.activation(out=A5, in_=A4, func=AF.Exp, scale=-1.0, bias=-0.6931471805599453), R=[BA[3]], W=[BA[4]])
                            yield
                            op("pool", lambda e: e.tensor_tensor(out=dst[0], in0=qs, in1=A5, op=ALU.mult),
                               R=[B_qs, BA[4]], W=[Bd[0]])
                            yield
                            op("act", lambda e: e.activation(out=A3, in_=A4, func=AF.Exp), R=[BA[3]], W=[BA[2]])
                            yield
                            op("dve", lambda e: e.scalar_tensor_tensor(out=dst[1], in0=A1, scalar=oml_ap, in1=A3, op0=ALU.mult, op1=ALU.mult),
                               R=[BA[0], BA[2]], W=[Bd[1]])
                            yield
                            op("dve", lambda e: e.tensor_tensor(out=EF[:, 1, c0:c0 + NCH], in0=t6[:, 0:NCH], in1=H3[:, :, 0],
                                                                op=ALU.subtract), R=[BA[1], B_T], W=[B_E])
                            yield
                            op("dve", lambda e: e.tensor_tensor(out=EM[:, 1, c0:c0 + NCH], in0=t6[:, 0:NCH], in1=H3[:, :, 64],
                                                                op=ALU.subtract), R=[BA[1], B_T], W=[B_E])
                            yield
                            op("dve", lambda e: e.tensor_tensor(out=view3(A4), in0=H3, in1=H3[:, :, 0:1].to_broadcast(bc),
                                                                op=ALU.subtract), R=[BA[1]], W=[BA[3]])
                            yield
                            op("act", lambda e: e.activation(out=A5, in_=A4, func=AF.Exp), R=[BA[3]], W=[BA[4]])
                            yield
                            op("dve", lambda e: e.scalar_tensor_tensor(out=dst[2], in0=A1, scalar=oml_ap, in1=A5, op0=ALU.mult, op1=ALU.mult),
                               R=[BA[0], BA[4]], W=[Bd[2]])
                        yield
                    return [chain(0), chain(1)]

                for _ in stageA(0):
                    pass
                for part in range(NPART):
                    gens = stageB_driver(part)
                    if part + 1 < NPART:
                        gens.append(stageA(part + 1))
                    interleave(*gens)
                op("act", lambda e: e.activation(out=EF[:], in_=EF[:], func=AF.Exp), R=[B_E], W=[B_E])
                op("act", lambda e: e.activation(out=EM[:], in_=EM[:], func=AF.Exp), R=[B_E], W=[B_E])

                orders = [[16, 17] + list(range(16)), [17, 16] + list(range(15, -1, -1))]
                for d_ in range(2):
                    op("dve", lambda e: e.memset(Sst[:, d_, :], 0.0), W=[B_S[d_]])
                items = [(step, d_) for step in range(NT - 1) for d_ in range(2)]

                def emit_T(i):
                    step, d_ = items[i]
                    c = orders[d_][step]
                    ki = i % 2
                    pq = 4 + (i % 2)
                    pbf = psb[pq][:].bitcast(BF16)
                    op("pe", lambda e: e.transpose(out=pbf[:, 0:128], in_=arr[d_][2][:, c * 128:(c + 1) * 128],
                                                   identity=ident_b[:]), R=[B_arr[d_][2], B_const], W=[B_ps[pq]])
                    op("act", lambda e: e.copy(out=kht[ki][:], in_=pbf[:, 0:128]), R=[B_ps[pq]], W=[B_kht[ki]])

                def emit_suse(step, d_):
                    c = orders[d_][step]
                    if c < 16:
                        op("act", lambda e: e.activation(out=Suse[:, d_, c, :], in_=Sst[:, d_, :], func=AF.Copy,
                                                         scale=EM[:, d_, c:c + 1]), R=[B_S[d_], B_E], W=[B_Su])

                emit_T(0)
                for i, (step, d_) in enumerate(items):
                    c = orders[d_][step]
                    if i + 1 < len(items):
                        emit_T(i + 1)
                    emit_suse(step, d_)
                    ki = i % 2
                    pd = 2 + (i % 2)
                    op("pe", lambda e: e.matmul(psb[pd][:, 0:128], lhsT=kht[ki][:], rhs=vv[:, c, :], start=True, stop=True),
                       R=[B_kht[ki], B_v], W=[B_ps[pd]])
                    op("dve", lambda e: e.scalar_tensor_tensor(out=Sst[:, d_, :], in0=Sst[:, d_, :], scalar=EF[:, d_, c:c + 1],
                                                               in1=psb[pd][:, 0:128], op0=ALU.mult, op1=ALU.add),
                       R=[B_S[d_], B_E, B_ps[pd]], W=[B_S[d_]])
                for d_ in range(2):
                    emit_suse(NT - 1, d_)

                for b in range(4):
                    s, n, _ = BLOCKS[b]
                    pi_ = b % 2
                    for k in range(KD):
                        op("pe", lambda e: e.matmul(psb[pi_][:, :n], lhsT=wG[:, k, :], rhs=hT[:, k, s:s + n],
                                                    start=(k == 0), stop=(k == KD - 1)),
                           R=[B_wr[sY], B_hT], W=[B_ps[pi_]], signal=(k == KD - 1))
                    op("act", lambda e: e.activation(out=sgT[:, s:s + n], in_=psb[pi_][:, :n], func=AF.Tanh, scale=0.5),
                       R=[B_ps[pi_]], W=[B_sg])
                    op("dve", lambda e: e.scalar_tensor_tensor(out=sgT[:, s:s + n], in0=sgT[:, s:s + n], scalar=1.0,
                                                               in1=psb[pi_][:, :n], op0=ALU.add, op1=ALU.mult),
                       R=[B_sg, B_ps[pi_]], W=[B_sg])

                def out_block(b):
                    s, n, _ = BLOCKS[b]
                    bp = b % 2
                    iO = 6 if bp == 0 else 4
                    pO = psb[iO]

                    def scores(cc):
                        c = b * 4 + cc
                        cs = slice(c * 128, (c + 1) * 128)
                        ais = []
                        for d_ in range(2):
                            pa = 2 + (cnt_["at"] % 2)
                            ai = cnt_["at"] % 8
                            cnt_["at"] += 1
                            op("pe", lambda e: e.matmul(psb[pa][:, 0:128], lhsT=arr[d_][1][:, cs], rhs=arr[d_][0][:, cs],
                                                        start=True, stop=True),
                               R=[B_arr[d_][1], B_arr[d_][0]], W=[B_ps[pa]])
                            op("dve", lambda e: e.tensor_tensor(out=ATs[ai][:], in0=psb[pa][:, 0:128],
                                                                in1=msk[:, d_ * 128:(d_ + 1) * 128], op=ALU.mult),
                               R=[B_ps[pa], B_hc], W=[B_ATs[ai]])
                            ais.append(ai)
                        return ais

                    def outs(cc, ais):
                        c = b * 4 + cc
                        cs = slice(c * 128, (c + 1) * 128)
                        oc = pO[:, cc * 128:(cc + 1) * 128]
                        op("pe", lambda e: e.matmul(oc, lhsT=Suse[:, 0, c, :], rhs=arr[0][0][:, cs], start=True, stop=False),
                           R=[B_Su, B_arr[0][0]], W=[B_ps[iO]], signal=False)
                        op("pe", lambda e: e.matmul(oc, lhsT=Suse[:, 1, c, :], rhs=arr[1][0][:, cs], start=False, stop=False),
                           R=[B_Su, B_arr[1][0]], W=[B_ps[iO]], signal=False)
                        op("pe", lambda e: e.matmul(oc, lhsT=vv[:, c, :], rhs=ATs[ais[0]][:], start=False, stop=False),
                           R=[B_v, B_ATs[ais[0]]], W=[B_ps[iO]], signal=False)
                        op("pe", lambda e: e.matmul(oc, lhsT=vv[:, c, :], rhs=ATs[ais[1]][:], start=False, stop=True),
                           R=[B_v, B_ATs[ais[1]]], W=[B_ps[iO]], signal=True)

                    pend = None
                    for cc in range(4):
                        ais = scores(cc)
                        if pend is not None:
                            outs(*pend)
                        pend = (cc, ais)
                    outs(*pend)

                def post_block(b):
                    s, n, _ = BLOCKS[b]
                    bp = b % 2
                    iO, iN = (6, 7) if bp == 0 else (4, 5)
                    pO = psb[iO]
                    rstd, B_rstd = rstds[bp], B_rstds[bp]
                    op("act", lambda e: e.activation(out=sq[bp][:, :n], in_=pO[:, :n], func=AF.Square), R=[B_ps[iO]], W=[B_sq[bp]])
                    op("pe", lambda e: e.matmul(psb[iN][:, :n], lhsT=ones_b[:], rhs=sq[bp][:, :n], start=True, stop=True),
                       R=[B_sq[bp], B_const], W=[B_ps[iN]])
                    op("act", lambda e: e.activation(out=rstd[:, :n], in_=psb[iN][:, :n], func=AF.Sqrt, bias=EPS,
                                                     scale=1.0 / 128), R=[B_ps[iN]], W=[B_rstd])
                    op("dve", lambda e: e.reciprocal(out=rstd[:, :n], in_=rstd[:, :n]), R=[B_rstd], W=[B_rstd])
                    op("dve", lambda e: e.scalar_tensor_tensor(out=tmp[bp][:, :n], in0=pO[:, :n], scalar=gnh[:, 0:1],
                                                               in1=rstd[:, :n], op0=ALU.mult, op1=ALU.mult),
                       R=[B_ps[iO], B_rstd, B_hc], W=[B_tmp[bp]])
                    op("dve", lambda e: e.tensor_tensor(out=ogT[:, s:s + n], in0=tmp[bp][:, :n], in1=sgT[:, s:s + n],
                                                         op=ALU.mult), R=[B_tmp[bp], B_sg], W=[B_ogb[b][1]])

                def proj_block(b):
                    s, n, _ = BLOCKS[b]
                    for mch in range(KD):
                        pi_ = mch % 2
                        op("pe", lambda e: e.matmul(psb[pi_][:, :n], lhsT=wO[:, mch * 128:(mch + 1) * 128],
                                                    rhs=ogT[:, s:s + n], start=True, stop=True),
                           R=[B_wr[sY], B_ogb[b]], W=[B_ps[pi_]])
                        g_ap = modv(l, 2, mch, 0)
                        op("dve", lambda e: e.scalar_tensor_tensor(
                            out=xT[:, mch, s:s + n], in0=psb[pi_][:, :n], scalar=g_ap, in1=xT[:, mch, s:s + n],
                            op0=ALU.mult, op1=ALU.add),
                           R=[B_ps[pi_], B_mod, B_x[mch][b]], W=[B_x[mch][b]])

                out_block(0)
                out_block(1)
                post_block(0)
                out_block(2)
                proj_block(0)
                post_block(1)
                out_block(3)
                proj_block(1)
                post_block(2)
                post_block(3)
                proj_block(2)
                proj_block(3)
            S.barrier()

    if "ret" in P.parts:
        retention_layer(0)
    debug_dump("x_mix0", lambda o: [dma("sp", o.rearrange("(k p) t -> p k t", p=128)[:, k, :], xT[:, k, :],
                                        R=B_x[k]) for k in range(KD)])

    if "moe0" in P.parts:
        moe_layer(0, True, P.n_exp_run)
    debug_dump("x_ffn0", lambda o: [dma("sp", o.rearrange("(k p) t -> p k t", p=128)[:, k, :], xT[:, k, :],
                                        R=B_x[k]) for k in range(KD)])
    if "hg" in P.parts:
        hgrn2_layer(1)
    debug_dump("x_mix1", lambda o: [dma("sp", o.rearrange("(k p) t -> p k t", p=128)[:, k, :], xT[:, k, 0:T_LAT],
                                        R=B_x[k][:4]) for k in range(KD)])
    if "moe1" in P.parts:
        moe_layer(1, False, P.n_exp_run)
    debug_dump("x_ffn1", lambda o: [dma("sp", o.rearrange("(k p) t -> p k t", p=128)[:, k, :], xT[:, k, 0:T_LAT],
                                        R=B_x[k][:4]) for k in range(KD)])
    if "final" in P.parts:
        with ExitStack() as fes:
            ftmp = [P.sb(fes, f"ftmp{i}", [128, 512], F32) for i in range(2)]
            B_ft = [Buf("ft0"), Buf("ft1")]
            frs = P.sb(fes, "frs", [128, 512], F32)
            B_frs = Buf("frs")
            fsq = [P.sb(fes, f"fsq{i}", [128, 512], BF16) for i in range(2)]
            B_fsq = [Buf("fsq0"), Buf("fsq1")]
            fo = [P.sb(fes, f"fo{i}", [128, 512], F32) for i in range(4)]
            B_fo = [Buf(f"fo{i}") for i in range(4)]
            osrc = out_d.rearrange("(k p) t -> p k t", p=128)
            oc_ = {"n": 0}
            for b in range(4):
                s, n, _ = BLOCKS[b]

                def dst(k):
                    i = oc_["n"] % 4
                    oc_["n"] += 1
                    dst.last = i
                    return fo[i][:, :n], [B_fo[i]]
                norm_block(1, 0, b, dst, ftmp, B_ft, frs, B_frs, fsq, B_fsq, psn_i=7, final=True,
                           after=lambda k, b=b, s=s, n=n: dma("sp", osrc[:, k, s:s + n], fo[dst.last][:, :n], R=[B_fo[dst.last]]))
    if P.stop_after is not None:
        osrc = out_d.rearrange("(k p) t -> p k t", p=128)
        for k in range(KD):
            dma("sp", osrc[:, k, :], xT[:, k, 0:T_LAT], R=B_x[k][:4])
    S.barrier()
    es.close()
    return P


def _prep_inputs(inp, b, consts, names=None):
    f = np.float32
    m = {}
    m["xT"] = np.ascontiguousarray(np.concatenate([inp["x"][b].T, inp["ctx"][b].T], axis=1)).astype(f)
    cv = np.stack([inp["c"][b], inp["c_ctx"]], axis=0)
    m["cvec"] = np.ascontiguousarray(cv.reshape(2, KD, 128).transpose(2, 0, 1).reshape(128, 2 * KD))
    m["w_ada"] = inp["w_ada"]
    m["b_ada"] = np.ascontiguousarray(np.tile(inp["b_ada"].reshape(1, 12 * D), (2, 1)))
    nr = np.stack([inp["norm_mix"][0], inp["norm_mix"][1], inp["norm_ffn"][0], inp["norm_ffn"][1],
                   inp["norm_final"]], axis=0)
    m["norms"] = np.ascontiguousarray(nr.reshape(5, KD, 128).transpose(2, 0, 1).reshape(128, 5 * KD))
    m["ret_w_in"] = inp["ret_w_in"][0]
    m["ret_w_out"] = inp["ret_w_out"][0]
    m["hg_w_in"] = inp["hg_w_in"][0]
    m["hg_w_out"] = inp["hg_w_out"][0]
    lb = inp["hg_lower_bounds"].reshape(4, KD, 128).transpose(2, 0, 1).reshape(128, 4 * KD)
    m["hg_misc"] = np.ascontiguousarray(np.concatenate([inp["hg_g_norm"][0].reshape(128, 1), lb], axis=1)).astype(f)
    m["moe_router"] = inp["moe_router"]
    m.update(consts)
    for l in range(2):
        for e in range(N_EXP):
            if names is not None and f"wgu_{l}_{e}" not in names:
                continue
            nfb = NFC // 2
            g = inp["moe_w_gate"][l, e].reshape(KD, 128, nfb, 256).transpose(2, 1, 0, 3)
            u = inp["moe_w_up"][l, e].reshape(KD, 128, nfb, 256).transpose(2, 1, 0, 3)
            m[f"wgu_{l}_{e}"] = np.ascontiguousarray(np.concatenate([g, u], axis=3))
            m[f"wdt_{l}_{e}"] = np.ascontiguousarray(
                inp["moe_w_down"][l, e].reshape(nfb, 2, 128, D).transpose(0, 2, 1, 3))
    if names is not None:
        m = {k: v for k, v in m.items() if k in names}
    return m


def kernel(**inputs):
    inp = {k: np.asarray(v) for k, v in inputs.items()}
    consts = _const_tables()
    P = build_program()
    names = set(P.dram.keys())
    shared = _prep_inputs(inp, 0, consts, names)
    in_maps = []
    for b in range(8):
        mb = dict(shared)
        mb["xT"] = np.ascontiguousarray(np.concatenate([inp["x"][b].T, inp["ctx"][b].T], axis=1)).astype(np.float32)
        cv = np.stack([inp["c"][b], inp["c_ctx"]], axis=0)
        mb["cvec"] = np.ascontiguousarray(cv.reshape(2, KD, 128).transpose(2, 0, 1).reshape(128, 2 * KD))
        in_maps.append(mb)
    res = run_bass_kernel_spmd(P.nc, in_maps, core_ids=list(range(8)))
    out = np.stack([np.ascontiguousarray(res.results[b]["outT"].T) for b in range(8)], axis=0)
    return out.astype(np.float32)
```

```python
import math
from contextlib import ExitStack

import numpy as np
import concourse.bass as bass
import concourse.mybir as mybir
from concourse.bass_utils import run_bass_kernel_spmd

F32 = mybir.dt.float32
BF16 = mybir.dt.bfloat16
ALU = mybir.AluOpType
AF = mybir.ActivationFunctionType
AX = mybir.AxisListType

D = 1024
KD = 8
T_LAT = 2048
T_CTX = 256
T_ALL = T_LAT + T_CTX
NT = T_ALL // 128
EPS = 1e-6
N_EXP = 16
FF = 2816
NFC = FF // 128
RET_H = 4
HG_H = 8

BLOCKS = [(0, 512, False), (512, 512, False), (1024, 512, False), (1536, 512, False), (2048, 256, True)]


def interleave(*gens):
    gens = list(gens)
    while gens:
        for g in list(gens):
            try:
                next(g)
            except StopIteration:
                gens.remove(g)


class Buf:
    __slots__ = ("name", "w", "rs")

    def __init__(self, name):
        self.name = name
        self.w = None
        self.rs = {}


class Sched:
    NDMA = 12

    def __init__(self, nc, es):
        self.nc = nc
        self.h = {"pe": nc.tensor, "act": nc.scalar, "dve": nc.vector, "pool": nc.gpsimd, "sp": nc.sync}
        self.sem = {}
        self.cnt = {}
        self.seen = {}
        for e in self.h:
            self.sem[e] = es.enter_context(nc.semaphore("s_" + e))
            self.cnt[e] = 0
            self.seen[e] = {}
        self.dsem = {}
        self.dval = {}
        self.dnext = {}
        for q in ("sp", "pool"):
            self.dsem[q] = [es.enter_context(nc.semaphore(f"d_{q}{i}")) for i in range(self.NDMA)]
            self.dval[q] = [0] * self.NDMA
            self.dnext[q] = 0
        self.ninst = 0

    def _wait(self, eng, tk):
        if tk is None:
            return
        kind = tk[0]
        if kind == "c":
            _, src, n = tk
            if src == eng and eng in ("pe", "sp"):
                return
            if self.seen[eng].get(src, 0) >= n:
                return
            self.h[eng].wait_ge(self.sem[src], n)
            self.seen[eng][src] = n
        else:
            _, q, idx, val = tk
            key = (q, idx)
            if self.seen[eng].get(key, 0) >= val:
                return
            self.h[eng].wait_ge(self.dsem[q][idx], val)
            self.seen[eng][key] = val
        self.ninst += 1

    def _deps(self, eng, R, W):
        for b in R:
            self._wait(eng, b.w)
        for b in W:
            self._wait(eng, b.w)
            for tk in b.rs.values():
                self._wait(eng, tk)

    def _record(self, tk, R, W):
        for b in W:
            b.w = tk
            b.rs = {}
        for b in R:
            if b in W:
                continue
            key = tk[1] if tk[0] == "c" else (tk[1], tk[2])
            b.rs[key] = tk

    @staticmethod
    def _flat(L):
        out = []
        for b in L:
            if isinstance(b, (list, tuple)):
                out.extend(Sched._flat(b))
            else:
                out.append(b)
        return out

    def op(self, eng, fn, R=(), W=(), signal=True):
        R, W = self._flat(R), self._flat(W)
        self._deps(eng, R, W)
        ins = fn(self.h[eng])
        self.ninst += 1
        if signal:
            ins.then_inc(self.sem[eng], 1)
            self.cnt[eng] += 1
            tk = ("c", eng, self.cnt[eng])
            if eng not in ("pe",):
                pass
        else:
            tk = ("c", eng, self.cnt[eng] + 1)
        self._record(tk, R, W)
        return tk

    def dma(self, q, out, in_, R=(), W=()):
        R, W = self._flat(R), self._flat(W)
        self._deps(q, R, W)
        idx = self.dnext[q]
        self.dnext[q] = (idx + 1) % self.NDMA
        prev = self.dval[q][idx]
        if prev > 0:
            self._wait(q, ("d", q, idx, prev))
        val = prev + 16
        self.dval[q][idx] = val
        self.h[q].dma_start(out=out, in_=in_).then_inc(self.dsem[q][idx], 16)
        self.ninst += 1
        tk = ("d", q, idx, val)
        self._record(tk, R, W)
        return tk

    def barrier(self):
        for e in self.h:
            for src in self.h:
                if src != e and self.cnt[src] > 0:
                    self._wait(e, ("c", src, self.cnt[src]))
            for q in ("sp", "pool"):
                for idx in range(self.NDMA):
                    if self.dval[q][idx] > 0:
                        self._wait(e, ("d", q, idx, self.dval[q][idx]))


def _ret_gammas():
    j = np.arange(8, dtype=np.float64)
    g = 1.0 - np.exp2(-5.0 - j / 2)
    return g[0::2], g[1::2]


def _const_tables():
    t = {}
    half = 128
    inv = 10000.0 ** (-np.arange(0, half, 2, dtype=np.float64) / half)
    p = np.arange(128)
    sign = np.where(p < 64, -1.0, 1.0)
    rows = np.arange(T_LAT // 64, dtype=np.float64)
    cols = np.arange(64, dtype=np.float64)
    ang_r = rows[None, :] * inv[p % 64][:, None]
    ang_c = cols[None, :] * inv[p % 64][:, None]
    t["rope"] = np.concatenate(
        [np.cos(ang_r), np.sin(ang_r) * sign[:, None], np.cos(ang_c), np.sin(ang_c) * sign[:, None]], axis=1
    ).astype(np.float32)
    gf, gb = _ret_gammas()
    b = np.arange(128, dtype=np.float64)[:, None]
    a = np.arange(512, dtype=np.float64)[None, :]
    tabs = np.zeros((RET_H, 128, 1920), dtype=np.float64)
    xs = np.arange(896, dtype=np.float64)[None, :] - 384.0
    for h in range(RET_H):
        tabs[h, :, 0:512] = gf[h] ** (a - b)
        tabs[h, :, 512:1024] = gb[h] ** (b + 511 - a)
        dl = xs - b
        tabs[h, :, 1024:1920] = np.where(dl > 0, gf[h] ** np.maximum(dl, 0),
                                         np.where(dl < 0, gb[h] ** np.maximum(-dl, 0), 2.0))
    t["rdec"] = tabs.astype(np.float32)
    t["ident"] = np.eye(128, dtype=np.float32)
    t["hgmask"] = np.concatenate([np.triu(np.ones((128, 128))), np.tril(np.ones((128, 128)))], axis=1).astype(np.float32)
    t["iota1"] = np.tile(np.arange(1, 257, dtype=np.float32)[None, :], (128, 1))
    return t


class Prog:
    def __init__(self, dbg=None, stop_after=None):
        self.dbg = dbg or []
        self.stop_after = stop_after
        self.nc = bass.Bass("TRN2", target_bir_lowering=False)
        self.es = ExitStack()
        self.S = Sched(self.nc, self.es)
        self.dram = {}
        self.dbg_out = {}

    def din(self, name, shape, dt=F32):
        self.dram[name] = self.nc.dram_tensor(name, list(shape), dt, kind="ExternalInput").ap()
        return self.dram[name]

    def dout(self, name, shape, dt=F32):
        self.dram[name] = self.nc.dram_tensor(name, list(shape), dt, kind="ExternalOutput").ap()
        return self.dram[name]

    def sb(self, es, name, shape, dt):
        self._uid = getattr(self, "_uid", 0) + 1
        return es.enter_context(self.nc.sbuf_tensor(f"{name}_u{self._uid}", list(shape), dt))

    def ps(self, es, name, shape, dt=F32):
        return es.enter_context(self.nc.psum_tensor(name, list(shape), dt))


def build_program(dbg=(), stop_after=None, parts=("ret", "moe0", "hg", "moe1", "final"), n_exp_run=N_EXP):
    P = Prog(list(dbg), stop_after)
    P.parts = parts
    P.n_exp_run = n_exp_run
    nc, S, es = P.nc, P.S, P.es
    op, dma = S.op, S.dma

    xT_d = P.din("xT", [D, T_ALL])
    cvec_d = P.din("cvec", [128, 2 * KD])
    wada_d = P.din("w_ada", [2, 12, 128, KD, 512])
    bada_d = P.din("b_ada", [2, 12 * D])
    nrm_d = P.din("norms", [128, 5 * KD])
    retin_d = P.din("ret_w_in", [D, 6144])
    retout_d = P.din("ret_w_out", [2048, D])
    router_d = P.din("moe_router", [2, D, N_EXP])
    rope_d = P.din("rope", [128, 192])
    rdec_d = P.din("rdec", [RET_H, 128, 1920])
    ident_d = P.din("ident", [128, 128])
    out_d = P.dout("outT", [D, T_LAT])
    for name, shape, dt_ in P.dbg:
        P.dbg_out[name] = P.dout("dbg_" + name, shape, dt_)

    xT = P.sb(es, "xT_sb", [128, KD, T_ALL], F32)
    B_x = [[Buf(f"x{k}_{b}") for b in range(5)] for k in range(KD)]
    cvec = P.sb(es, "cvec_sb", [128, 2 * KD], F32)
    scc = P.sb(es, "scc", [128, KD, 2], F32)
    nrm = P.sb(es, "nrm", [128, 5 * KD], F32)
    mod = P.sb(es, "mod", [128, 2, 48, 2], F32)
    modA = P.sb(es, "modA", [128, 2, 2, KD, 2], F32)
    ident_f = P.sb(es, "ident_f", [128, 128], F32)
    ident_b = P.sb(es, "ident_b", [128, 128], BF16)
    ones_b = P.sb(es, "ones_b", [128, 128], BF16)
    B_const = Buf("const")
    B_mod = Buf("mod")

    psb = [P.ps(es, f"psb{i}", [128, 512], F32) for i in range(8)]
    B_ps = [Buf(f"ps{i}") for i in range(8)]

    def sl(b):
        s, n, _ = BLOCKS[b]
        return slice(s, s + n)

    xsrc = xT_d.rearrange("(k p) t -> p k t", p=128)
    for k in range(KD):
        dma("sp", xT[:, k, :], xsrc[:, k, :], W=B_x[k])
    dma("sp", cvec[:], cvec_d, W=[B_const])
    dma("sp", nrm[:], nrm_d, W=[B_const])
    dma("sp", ident_f[:], ident_d, W=[B_const])
    op("act", lambda e: e.copy(out=ident_b[:], in_=ident_f[:]), R=[B_const], W=[B_const])
    op("dve", lambda e: e.memset(ones_b[:], 1.0), W=[B_const])
    op("act", lambda e: e.activation(out=scc[:].rearrange("p k c -> p c k"),
                                     in_=cvec[:].rearrange("p (c k) -> p c k", c=2), func=AF.Silu),
       R=[B_const], W=[B_const])

    with ExitStack() as pes:
        NPIECE = 12
        NST = 4
        wa = [P.sb(pes, f"wa{i}", [128, KD, 512], F32) for i in range(NST)]
        B_wa = [Buf(f"wa{i}") for i in range(NST)]
        HALF = 3 * D
        modrow = P.sb(pes, "modrow", [2, HALF], F32)
        B_mr = Buf("modrow")
        bada2 = P.sb(pes, "bada2", [2, HALF], F32)
        B_b2 = Buf("bada2")
        pi = 0
        for l in range(2):
            for hf in range(2):
                dma("sp", bada2[:], bada_d[:, l * 6 * D + hf * HALF: l * 6 * D + (hf + 1) * HALF], W=[B_b2])
                for pc6 in range(6):
                    pc = hf * 6 + pc6
                    slot = pi % NST
                    dma("sp", wa[slot][:], wada_d[l, pc], W=[B_wa[slot]])
                    pb = pi % 2
                    for k in range(KD):
                        op("pe", lambda e: e.matmul(psb[pb][0:2, :], lhsT=scc[:, k, :], rhs=wa[slot][:, k, :],
                                                    start=(k == 0), stop=(k == KD - 1)),
                           R=[B_wa[slot], B_const], W=[B_ps[pb]], signal=(k == KD - 1))
                    op("dve", lambda e: e.tensor_tensor(out=modrow[:, pc6 * 512:(pc6 + 1) * 512], in0=psb[pb][0:2, :],
                                                        in1=bada2[:, pc6 * 512:(pc6 + 1) * 512],
                                                        op=ALU.add), R=[B_ps[pb], B_b2], W=[B_mr])
                    pi += 1
                pt_ = 2 + (l * 2 + hf) % 2
                for j in range(24):
                    op("pe", lambda e: e.transpose(out=psb[pt_][:, j * 2:(j + 1) * 2], in_=modrow[:, j * 128:(j + 1) * 128],
                                                   identity=ident_f[0:2, 0:2]),
                       R=[B_mr, B_const], W=[B_ps[pt_]], signal=(j == 23))
                op("dve", lambda e: e.tensor_copy(out=mod[:, l, hf * 24:(hf + 1) * 24, :].rearrange("p j c -> p (j c)"),
                                                  in_=psb[pt_][:, 0:48]), R=[B_ps[pt_]], W=[B_mod])
        for l in range(2):
            for site in range(2):
                sc_j = (1 if site == 0 else 4) * KD
                nw = nrm[:, (site * 2 + l) * KD:(site * 2 + l + 1) * KD]
                op("dve", lambda e, l=l, site=site, sc_j=sc_j, nw=nw: e.scalar_tensor_tensor(
                    out=modA[:, l, site, :, :], in0=mod[:, l, sc_j:sc_j + KD, :], scalar=1.0,
                    in1=nw.unsqueeze(2).to_broadcast([128, KD, 2]), op0=ALU.add, op1=ALU.mult),
                   R=[B_mod, B_const], W=[B_mod])
        S.barrier()

    def modv(l, chunk, k, c):
        return mod[:, l, chunk * KD + k, c:c + 1]

    def norm_block(l, site, b, dst_fn, tmp, B_tmp, rstd, B_rstd, sq, B_sq, psn_i, final=False, after=None):
        s, n, isc = BLOCKS[b]
        c = 1 if isc else 0
        for k in range(KD):
            q = k % 2
            op("act", lambda e, k=k, q=q: e.activation(out=sq[q][:, :n], in_=xT[:, k, s:s + n], func=AF.Square),
               R=[B_x[k][b]], W=[B_sq[q]])
            op("pe", lambda e, k=k, q=q: e.matmul(psb[psn_i][:, :n], lhsT=ones_b[:], rhs=sq[q][:, :n],
                                                 start=(k == 0), stop=(k == KD - 1)),
               R=[B_sq[q], B_const], W=[B_ps[psn_i]], signal=True)
        op("act", lambda e: e.activation(out=rstd[:, :n], in_=psb[psn_i][:, :n], func=AF.Sqrt, bias=EPS,
                                         scale=1.0 / D), R=[B_ps[psn_i]], W=[B_rstd])
        op("dve", lambda e: e.reciprocal(out=rstd[:, :n], in_=rstd[:, :n]), R=[B_rstd], W=[B_rstd])
        for k in range(KD):
            q = k % 2
            out_ap, obufs = dst_fn(k)
            if final:
                a_ap = nrm[:, 4 * KD + k:4 * KD + k + 1]
                op("dve", lambda e, k=k, a_ap=a_ap, out_ap=out_ap: e.scalar_tensor_tensor(
                    out=out_ap, in0=xT[:, k, s:s + n], scalar=a_ap, in1=rstd[:, :n], op0=ALU.mult, op1=ALU.mult),
                   R=[B_x[k][b], B_rstd, B_const], W=obufs)
                if after is not None:
                    after(k)
            else:
                a_ap = modA[:, l, site, k, c:c + 1]
                sh_ap = modv(l, 0 if site == 0 else 3, k, c)
                op("dve", lambda e, k=k, q=q, a_ap=a_ap: e.scalar_tensor_tensor(
                    out=tmp[q][:, :n], in0=xT[:, k, s:s + n], scalar=a_ap, in1=rstd[:, :n],
                    op0=ALU.mult, op1=ALU.mult), R=[B_x[k][b], B_rstd, B_mod], W=[B_tmp[q]])
                op("act", lambda e, q=q, sh_ap=sh_ap, out_ap=out_ap: e.activation(
                    out=out_ap, in_=tmp[q][:, :n], func=AF.Identity, bias=sh_ap, scale=1.0),
                   R=[B_tmp[q], B_mod], W=obufs)

    def debug_dump(name, ap_fn):
        if name in P.dbg_out:
            S.barrier()
            tk = ap_fn(P.dbg_out[name])
            S.barrier()

    NSLOT = 4
    wring = [P.sb(es, f"wring{i}", [128, KD, 512], BF16) for i in range(NSLOT)]
    B_wr = [Buf(f"wr{i}") for i in range(NSLOT)]
    wr_state = {"n": 0}

    def wslot():
        i = wr_state["n"] % NSLOT
        wr_state["n"] += 1
        return i

    def retention_layer(l):
        with ExitStack() as les:
            hT = P.sb(les, "hT", [128, KD, T_ALL], BF16)
            B_h = [[Buf(f"h{k}_{b}") for b in range(5)] for k in range(KD)]
            tmp = [P.sb(les, f"ntmp{i}", [128, 512], F32) for i in range(2)]
            B_tmp = [Buf("ntmp0"), Buf("ntmp1")]
            sg = [P.sb(les, f"sg{i}", [128, 512], F32) for i in range(2)]
            B_sg = [Buf("sg0"), Buf("sg1")]
            rstd = P.sb(les, "rstd", [128, 512], F32)
            B_rstd = Buf("rstd")
            sq = [P.sb(les, f"sq{i}", [128, 512], BF16) for i in range(2)]
            B_sq = [Buf("sq0"), Buf("sq1")]
            rope_sb = P.sb(les, "rope_sb", [128, 192], F32)
            B_rope = Buf("rope")
            dma("sp", rope_sb[:], rope_d, W=[B_rope])
            for b in range(5):
                s, n, _ = BLOCKS[b]
                norm_block(l, 0, b, lambda k, b=b, s=s, n=n: (hT[:, k, s:s + n], [B_h[k][b]]),
                           tmp, B_tmp, rstd, B_rstd, sq, B_sq, psn_i=7)
            debug_dump("hT0", lambda o: [dma("sp", o.rearrange("(k p) t -> p k t", p=128)[:, k, :], hT[:, k, :],
                                             R=B_h[k]) for k in range(KD)])
            if P.stop_after == "hT0":
                return

            qr = P.sb(les, "qr", [128, 2, 2, 512], BF16)
            kr = P.sb(les, "kr", [128, 2, T_ALL], BF16)
            vv = P.sb(les, "vv", [128, NT, 512], BF16)
            B_q = [[Buf(f"q{i}_{m}") for m in range(2)] for i in range(2)]
            B_k = [[Buf(f"k{m}_{t}") for t in range(NT)] for m in range(2)]
            B_v = [Buf(f"v{t}") for t in range(NT)]
            rdec = P.sb(les, "rdec_sb", [128, 1920], F32)
            B_rdec = Buf("rdec")
            Ff = rdec[:, 0:512]
            Fb = rdec[:, 512:1024]

            def Dg(kk, n):
                return rdec[:, 1024 + 384 - 128 * kk: 1024 + 384 - 128 * kk + n]
            rt1, B_rt1 = tmp, B_tmp
            rt2, B_rt2 = sg, B_sg
            ctab, B_ctab = tmp, B_tmp
            AT = [P.sb(les, f"AT{i}", [128, 512], BF16) for i in range(3)]
            B_AT = [Buf(f"AT{i}") for i in range(3)]
            ogT = P.sb(les, "ogT", [128, 4, 512], BF16)
            B_og = [Buf(f"og{c}") for c in range(4)]
            sgb = P.sb(les, "sgb", [128, 4, 512], BF16)
            B_sgb = [Buf(f"sgb{c}") for c in range(4)]
            gf, gb = _ret_gammas()
            rcount = {"rope": 0, "at": 0, "q": 0}

            win_src = retin_d.rearrange("(k p) f -> p k f", p=128)
            wout_src = retout_d.rearrange("(c p) f -> p c f", p=128)

            def proj_rope(sA, qk, m, b, dst_ap, wb):
                s, n, isc = BLOCKS[b]
                pi_ = rcount["rope"] % 2
                rcount["rope"] += 1
                pst = psb[pi_]
                for k in range(KD):
                    op("pe", lambda e, k=k: e.matmul(
                        pst[:, :n], lhsT=wring[sA][:, k, qk * 256 + m * 128: qk * 256 + (m + 1) * 128],
                        rhs=hT[:, k, s:s + n], start=(k == 0), stop=(k == KD - 1)),
                       R=[B_wr[sA], B_h[k][b]], W=[B_ps[pi_]], signal=(k == KD - 1))
                scale = 1.0 if qk == 0 else 1.0 / 16.0
                if isc:
                    op("act", lambda e: e.activation(out=dst_ap, in_=pst[:, :n], func=AF.Copy, scale=scale),
                       R=[B_ps[pi_]], W=wb)
                    return
                g0 = s // 64
                if m == 0:
                    cos_ap = rope_sb[:, g0:g0 + 8].unsqueeze(2).to_broadcast([128, 8, 64])
                    sin_lo = rope_sb[0:64, 32 + g0:32 + g0 + 8].unsqueeze(2).to_broadcast([64, 8, 64])
                    sin_hi = rope_sb[64:128, 32 + g0:32 + g0 + 8].unsqueeze(2).to_broadcast([64, 8, 64])
                else:
                    cos_ap = rope_sb[:, 64:128].unsqueeze(1).to_broadcast([128, 8, 64])
                    sin_lo = rope_sb[0:64, 128:192].unsqueeze(1).to_broadcast([64, 8, 64])
                    sin_hi = rope_sb[64:128, 128:192].unsqueeze(1).to_broadcast([64, 8, 64])
                ti = pi_
                p3 = pst[:].rearrange("p (g t) -> p g t", t=64)
                t1v = rt1[ti][:].rearrange("p (g t) -> p g t", t=64)
                t2v = rt2[ti][:].rearrange("p (g t) -> p g t", t=64)
                op("dve", lambda e: e.scalar_tensor_tensor(
                    out=t1v, in0=p3, scalar=scale, in1=cos_ap, op0=ALU.mult, op1=ALU.mult),
                   R=[B_ps[pi_], B_rope], W=[B_rt1[ti]])
                op("dve", lambda e: e.scalar_tensor_tensor(
                    out=t2v[0:64], in0=p3[64:128], scalar=scale, in1=sin_lo, op0=ALU.mult, op1=ALU.mult),
                   R=[B_ps[pi_], B_rope], W=[B_rt2[ti]])
                op("dve", lambda e: e.scalar_tensor_tensor(
                    out=t2v[64:128], in0=p3[0:64], scalar=scale, in1=sin_hi, op0=ALU.mult, op1=ALU.mult),
                   R=[B_ps[pi_], B_rope, B_rt2[ti]], W=[B_rt2[ti]])
                op("dve", lambda e: e.tensor_tensor(
                    out=dst_ap, in0=rt1[ti][:, :n], in1=rt2[ti][:, :n], op=ALU.add),
                   R=[B_rt1[ti], B_rt2[ti]], W=wb)

            for h in range(RET_H):
                sA, sB, sC, sD = wslot(), wslot(), wslot(), wslot()
                dma("pool", wring[sA][:, :, 0:256], win_src[:, :, h * 256:(h + 1) * 256], W=[B_wr[sA]])
                dma("pool", wring[sA][:, :, 256:512], win_src[:, :, 1024 + h * 256:1024 + (h + 1) * 256], W=[B_wr[sA]])
                dma("pool", wring[sB][:], win_src[:, :, 2048 + h * 512:2048 + (h + 1) * 512], W=[B_wr[sB]])
                dma("pool", wring[sC][:], win_src[:, :, 4096 + h * 512:4096 + (h + 1) * 512], W=[B_wr[sC]])
                wD = wring[sD][:].rearrange("p k f -> p (k f)").rearrange("p (c f) -> p c f", c=4)
                dma("pool", wD, wout_src[:, h * 4:(h + 1) * 4, :], W=[B_wr[sD]])
                dma("sp", rdec[:], rdec_d[h], W=[B_rdec])

                def kproj_gen():
                    for m in range(2):
                        for b in range(5):
                            s, n, isc = BLOCKS[b]
                            proj_rope(sA, 1, m, b, kr[:, m, s:s + n], [B_k[m][t] for t in range(s // 128, (s + n) // 128)])
                            yield

                def vproj_gen():
                    for t in range(NT):
                        b = min(t // 4, 4)
                        pi_ = 6 + (t % 2)
                        for k in range(KD):
                            op("pe", lambda e, k=k, t=t, pi_=pi_: e.matmul(
                                psb[pi_][:, :], lhsT=hT[:, k, t * 128:(t + 1) * 128], rhs=wring[sB][:, k, :],
                                start=(k == 0), stop=(k == KD - 1)),
                               R=[B_wr[sB], B_h[k][b]], W=[B_ps[pi_]], signal=(k == KD - 1))
                        op("act", lambda e, t=t, pi_=pi_: e.copy(out=vv[:, t, :], in_=psb[pi_][:, :]),
                           R=[B_ps[pi_]], W=[B_v[t]])
                        yield
                interleave(kproj_gen(), vproj_gen())
                if h == 0:
                    debug_dump("kr0", lambda o: [dma("sp", o[:, m, :], kr[:, m, :], R=B_k[m]) for m in range(2)])
                    debug_dump("vv0", lambda o: [dma("sp", o, vv[:], R=B_v)])
                    if P.stop_after == "proj0":
                        return

                def block_gen(b):
                    s, n, isc = BLOCKS[b]
                    c = 1 if isc else 0
                    qi = rcount["q"] % 2
                    rcount["q"] += 1
                    for m in range(2):
                        proj_rope(sA, 0, m, b, qr[:, qi, m, :n], [B_q[qi][m]])
                    yield
                    for cc in range(4):
                        pg = cc % 2
                        for k in range(KD):
                            op("pe", lambda e: e.matmul(
                                psb[pg][:, :n], lhsT=wring[sC][:, k, cc * 128:(cc + 1) * 128], rhs=hT[:, k, s:s + n],
                                start=(k == 0), stop=(k == KD - 1)),
                               R=[B_wr[sC], B_h[k][b]], W=[B_ps[pg]], signal=(k == KD - 1))
                        op("act", lambda e: e.activation(out=sgb[:, cc, :n], in_=psb[pg][:, :n], func=AF.Silu),
                           R=[B_ps[pg]], W=[B_sgb[cc]])
                    keys = [16, 17] if isc else list(range(NT))
                    pso = [psb[2 + cc] for cc in range(4)]
                    B_pso = [B_ps[2 + cc] for cc in range(4)]

                    def emit_scores(j, idx):
                        pi_ = 6 + (idx % 2)
                        for m in range(2):
                            op("pe", lambda e, m=m: e.matmul(
                                psb[pi_][:, :n], lhsT=kr[:, m, j * 128:(j + 1) * 128], rhs=qr[:, qi, m, :n],
                                start=(m == 0), stop=(m == 1)),
                               R=[B_k[m][j], B_q[qi][m]], W=[B_ps[pi_]], signal=(m == 1))
                        ai = rcount["at"] % 3
                        rcount["at"] += 1
                        pst = psb[pi_]
                        if isc:
                            tab = Dg(j - 16, n)
                            op("dve", lambda e: e.tensor_tensor(out=AT[ai][:, :n], in0=pst[:, :n], in1=tab, op=ALU.mult),
                               R=[B_ps[pi_], B_rdec], W=[B_AT[ai]])
                        elif j >= 16:
                            jc = j - 16
                            s1 = float(gf[h] ** (s + 256 - 128 * jc))
                            s2 = float(gb[h] ** (T_LAT - s - 511 + 128 * jc))
                            ci = jc
                            op("pool", lambda e: e.tensor_scalar(
                                out=ctab[ci][:], in0=Ff, scalar1=s1, scalar2=None, op0=ALU.mult),
                               R=[B_rdec], W=[B_ctab[ci]])
                            op("dve", lambda e: e.scalar_tensor_tensor(
                                out=ctab[ci][:], in0=Fb, scalar=s2, in1=ctab[ci][:], op0=ALU.mult, op1=ALU.add),
                               R=[B_rdec, B_ctab[ci]], W=[B_ctab[ci]])
                            op("dve", lambda e: e.tensor_tensor(
                                out=AT[ai][:, :n], in0=pst[:, :n], in1=ctab[ci][:, :n], op=ALU.mult),
                               R=[B_ps[pi_], B_ctab[ci]], W=[B_AT[ai]])
                        else:
                            rel = j - 4 * b
                            if 0 <= rel < 4:
                                tab = Dg(rel, 512)
                                op("dve", lambda e: e.tensor_tensor(out=AT[ai][:], in0=pst[:], in1=tab, op=ALU.mult),
                                   R=[B_ps[pi_], B_rdec], W=[B_AT[ai]])
                            elif rel < 0:
                                sc = float(gf[h] ** (s - 128 * j))
                                op("dve", lambda e: e.scalar_tensor_tensor(
                                    out=AT[ai][:], in0=pst[:], scalar=sc, in1=Ff, op0=ALU.mult, op1=ALU.mult),
                                   R=[B_ps[pi_], B_rdec], W=[B_AT[ai]])
                            else:
                                sc = float(gb[h] ** (128 * j - s - 511))
                                op("dve", lambda e: e.scalar_tensor_tensor(
                                    out=AT[ai][:], in0=pst[:], scalar=sc, in1=Fb, op0=ALU.mult, op1=ALU.mult),
                                   R=[B_ps[pi_], B_rdec], W=[B_AT[ai]])
                        return ai

                    def emit_av(j, ai, first, last):
                        for cc in range(4):
                            op("pe", lambda e, cc=cc: e.matmul(
                                pso[cc][:, :n], lhsT=vv[:, j, cc * 128:(cc + 1) * 128], rhs=AT[ai][:, :n],
                                start=first, stop=last),
                               R=[B_v[j], B_AT[ai]], W=[B_pso[cc]], signal=(cc == 3))

                    pend = None
                    for idx, j in enumerate(keys):
                        ai = emit_scores(j, idx)
                        if pend is not None:
                            emit_av(*pend)
                        pend = (j, ai, idx == 0, idx == len(keys) - 1)
                    emit_av(*pend)

                    yield
                    for cc in range(4):
                        q2 = cc % 2
                        op("act", lambda e, cc=cc, q2=q2: e.activation(out=sq[q2][:, :n], in_=pso[cc][:, :n],
                                                                      func=AF.Square),
                           R=[B_pso[cc]], W=[B_sq[q2]])
                        op("pe", lambda e, cc=cc, q2=q2: e.matmul(psb[6][:, :n], lhsT=ones_b[:], rhs=sq[q2][:, :n],
                                                                 start=(cc == 0), stop=(cc == 3)),
                           R=[B_sq[q2], B_const], W=[B_ps[6]], signal=True)
                    op("act", lambda e: e.activation(out=rstd[:, :n], in_=psb[6][:, :n], func=AF.Sqrt, bias=EPS,
                                                     scale=1.0 / 512), R=[B_ps[6]], W=[B_rstd])
                    op("dve", lambda e: e.reciprocal(out=rstd[:, :n], in_=rstd[:, :n]), R=[B_rstd], W=[B_rstd])
                    for cc in range(4):
                        q2 = cc % 2
                        op("dve", lambda e, cc=cc, q2=q2: e.tensor_tensor(
                            out=tmp[q2][:, :n], in0=pso[cc][:, :n], in1=rstd[:, :n], op=ALU.mult),
                           R=[B_pso[cc], B_rstd], W=[B_tmp[q2]])
                        op("dve", lambda e, cc=cc, q2=q2: e.tensor_tensor(
                            out=ogT[:, cc, :n], in0=tmp[q2][:, :n], in1=sgb[:, cc, :n], op=ALU.mult),
                           R=[B_tmp[q2], B_sgb[cc]], W=[B_og[cc]])
                    for mch in range(KD):
                        pi_ = 6 + (mch % 2)
                        for cc in range(4):
                            op("pe", lambda e, cc=cc, mch=mch, pi_=pi_: e.matmul(
                                psb[pi_][:, :n], lhsT=wD[:, cc, mch * 128:(mch + 1) * 128], rhs=ogT[:, cc, :n],
                                start=(cc == 0), stop=(cc == 3)),
                               R=[B_wr[sD], B_og[cc]], W=[B_ps[pi_]], signal=(cc == 3))
                        g_ap = modv(l, 2, mch, c)
                        op("dve", lambda e, mch=mch, pi_=pi_, g_ap=g_ap: e.scalar_tensor_tensor(
                            out=xT[:, mch, s:s + n], in0=psb[pi_][:, :n], scalar=g_ap, in1=xT[:, mch, s:s + n],
                            op0=ALU.mult, op1=ALU.add),
                           R=[B_ps[pi_], B_mod, B_x[mch][b]], W=[B_x[mch][b]])

                gens = [block_gen(b) for b in range(5)]
                next(gens[0])
                for b in range(5):
                    next(gens[b])
                    if b + 1 < 5:
                        next(gens[b + 1])
                    for _ in gens[b]:
                        pass
            S.barrier()

    iota_d2 = P.din("iota1", [128, 256])

    def moe_layer(l, with_ctx, n_exp_run=N_EXP):
        nblk = 5 if with_ctx else 4
        ntile = NT if with_ctx else 16
        NS = 288 if with_ctx else 256
        nst = 3 if with_ctx else 2
        st_sz = [128, 128, 32][:nst]
        st_off = [0, 128, 256][:nst]
        with ExitStack() as mes:
            h2tok = P.sb(mes, "h2tok", [128, NT, D], BF16)
            B_h2 = [Buf(f"h2t{t}") for t in range(NT)]
            wr_sb = P.sb(mes, "wr_sb", [128, KD, N_EXP], F32)
            B_wrt = Buf("wrt")
            dma("sp", wr_sb[:], router_d[l].rearrange("(k p) e -> p k e", p=128), W=[B_wrt])
            iota1 = P.sb(mes, "iota1_sb", [128, 256], F32)
            dma("sp", iota1[:], iota_d2, W=[B_wrt])
            aff = P.sb(mes, "aff", [128, NT, N_EXP], F32)
            B_aff = Buf("aff")
            aff_hl = P.sb(mes, "aff_hl", [128, NT, N_EXP, 2], BF16)
            posm_tok = P.sb(mes, "posm_tok", [128, NT, N_EXP], F32)
            B_posm = Buf("posm")

            with ExitStack() as r1:
                tmp = [P.sb(r1, f"mtmp{i}", [128, 512], F32) for i in range(2)]
                B_tmp = [Buf("mtmp0"), Buf("mtmp1")]
                rstd = P.sb(r1, "mrstd", [128, 512], F32)
                B_rstd = Buf("mrstd")
                sq = [P.sb(r1, f"msq{i}", [128, 512], BF16) for i in range(2)]
                B_sq = [Buf("msq0"), Buf("msq1")]
                h2f = P.sb(r1, "h2f", [128, KD, 512], F32)
                B_h2f = [Buf(f"h2f{k}") for k in range(KD)]
                h2b = P.sb(r1, "h2b", [128, KD, 512], BF16)
                B_h2b = [Buf(f"h2b{k}") for k in range(KD)]
                for b in range(nblk):
                    s, n, isc = BLOCKS[b]
                    norm_block(l, 1, b, lambda k, n=n: (h2f[:, k, :n], [B_h2f[k]]),
                               tmp, B_tmp, rstd, B_rstd, sq, B_sq, psn_i=7)
                    for k in range(KD):
                        eng_ = "act" if k % 2 == 0 else "dve"
                        if eng_ == "act":
                            op("act", lambda e, k=k: e.copy(out=h2b[:, k, :n], in_=h2f[:, k, :n]), R=[B_h2f[k]], W=[B_h2b[k]])
                        else:
                            op("dve", lambda e, k=k: e.tensor_copy(out=h2b[:, k, :n], in_=h2f[:, k, :n]),
                               R=[B_h2f[k]], W=[B_h2b[k]])
                    for tt in range(n // 128):
                        t = s // 128 + tt
                        for k in range(KD):
                            op("pe", lambda e, k=k: e.matmul(psb[6][:, 0:N_EXP], lhsT=h2f[:, k, tt * 128:(tt + 1) * 128],
                                                            rhs=wr_sb[:, k, :], start=(k == 0), stop=(k == KD - 1)),
                               R=[B_h2f[k], B_wrt], W=[B_ps[6]], signal=(k == KD - 1))
                        op("act", lambda e: e.copy(out=aff[:, t, :], in_=psb[6][:, 0:N_EXP]), R=[B_ps[6]], W=[B_aff])
                        pi_ = t % 2
                        pbf = psb[pi_][:].bitcast(BF16)
                        for k in range(KD):
                            op("pe", lambda e, k=k: e.transpose(out=pbf[:, k * 128:(k + 1) * 128],
                                                               in_=h2b[:, k, tt * 128:(tt + 1) * 128], identity=ident_b[:]),
                               R=[B_h2b[k], B_const], W=[B_ps[pi_]], signal=(k == KD - 1))
                        op("act", lambda e: e.copy(out=h2tok[:, t, :], in_=pbf), R=[B_ps[pi_]], W=[B_h2[t]])
                mx = P.sb(r1, "smx", [128, NT], F32)
                op("dve", lambda e: e.tensor_reduce(out=mx[:, :ntile], in_=aff[:, :ntile, :], axis=AX.X, op=ALU.max),
                   R=[B_aff], W=[B_rstd])
                op("dve", lambda e: e.tensor_tensor(out=aff[:, :ntile, :], in0=aff[:, :ntile, :],
                                                    in1=mx[:, :ntile].unsqueeze(2).to_broadcast([128, ntile, N_EXP]),
                                                    op=ALU.subtract), R=[B_rstd, B_aff], W=[B_aff])
                op("act", lambda e: e.activation(out=aff[:, :ntile, :], in_=aff[:, :ntile, :], func=AF.Exp),
                   R=[B_aff], W=[B_aff])
                op("dve", lambda e: e.tensor_reduce(out=mx[:, :ntile], in_=aff[:, :ntile, :], axis=AX.X, op=ALU.add),
                   R=[B_aff], W=[B_rstd])
                op("dve", lambda e: e.reciprocal(out=mx[:, :ntile], in_=mx[:, :ntile]), R=[B_rstd], W=[B_rstd])
                op("dve", lambda e: e.tensor_tensor(out=aff[:, :ntile, :], in0=aff[:, :ntile, :],
                                                    in1=mx[:, :ntile].unsqueeze(2).to_broadcast([128, ntile, N_EXP]),
                                                    op=ALU.mult), R=[B_rstd, B_aff], W=[B_aff])
                op("dve", lambda e: e.tensor_copy(out=aff_hl[:, :ntile, :, 0], in_=aff[:, :ntile, :]), R=[B_aff], W=[B_posm])
                op("dve", lambda e: e.tensor_tensor(out=aff_hl[:, :ntile, :, 1], in0=aff[:, :ntile, :],
                                                    in1=aff_hl[:, :ntile, :, 0], op=ALU.subtract),
                   R=[B_aff, B_posm], W=[B_posm])
                S.barrier()
            debug_dump(f"aff{l}", lambda o: [dma("sp", o, aff[:], R=[B_aff])])
            debug_dump(f"h2tok{l}", lambda o: [dma("sp", o, h2tok[:], R=B_h2)])

            with ExitStack() as r2:
                affT = P.sb(r2, "affT", [16, T_ALL], F32)
                mk = P.sb(r2, "mkT", [16, T_ALL], F32)
                cum = P.sb(r2, "cumT", [16, T_ALL], F32)
                sm = P.sb(r2, "bis", [16, 16], F32)
                B_affT, B_mk, B_cum, B_sm = Buf("affT"), Buf("mk"), Buf("cum"), Buf("sm")
                for t in range(ntile):
                    pi_ = t % 2
                    op("pe", lambda e: e.transpose(out=psb[pi_][0:16, 0:128], in_=aff[:, t, :], identity=ident_f[:]),
                       R=[B_aff, B_const], W=[B_ps[pi_]])
                    op("act", lambda e: e.copy(out=affT[:, t * 128:(t + 1) * 128], in_=psb[pi_][0:16, 0:128]),
                       R=[B_ps[pi_]], W=[B_affT])
                sets = [(0, T_LAT, 256)] + ([(T_LAT, T_CTX, 32)] if with_ctx else [])
                one = sm[:, 15:16]
                op("dve", lambda e: e.memset(one, 1.0), W=[B_sm])
                B_smx = [Buf("smA"), Buf("smB")]

                def bisect(si, s0, ns, cap):
                    lo, mid, gs = [sm[:, si * 6 + i: si * 6 + i + 1] for i in range(3)]
                    cnts = [sm[:, si * 6 + 3 + i: si * 6 + 4 + i] for i in range(2)]
                    Bs = B_smx[si]
                    a_ap = affT[:, s0:s0 + ns]
                    op("dve", lambda e: e.memset(lo, 0.0), W=[Bs])
                    op("dve", lambda e: e.memset(mid, 0.5), W=[Bs])
                    NIT = 30
                    for it in range(NIT):
                        step = 0.5 ** (it + 1)
                        cnt = cnts[it % 2]
                        op("dve", lambda e: e.memset(cnt, 0.0), W=[Bs])
                        yield
                        op("dve", lambda e: e.tensor_scalar(out=mk[:, s0:s0 + ns], in0=a_ap, scalar1=mid, scalar2=0.0,
                                                            op0=ALU.is_ge, op1=ALU.add, accum_out=cnt),
                           R=[B_affT, Bs], W=[B_mk, Bs])
                        yield
                        op("dve", lambda e: e.tensor_scalar(out=gs, in0=cnt, scalar1=float(cap), scalar2=step,
                                                            op0=ALU.is_ge, op1=ALU.mult), R=[Bs], W=[Bs])
                        yield
                        if it < NIT - 1:
                            op("dve", lambda e: e.scalar_tensor_tensor(out=mid, in0=gs, scalar=step * 0.5, in1=lo,
                                                                       op0=ALU.add, op1=ALU.add), R=[Bs], W=[Bs])
                            yield
                        op("dve", lambda e: e.tensor_tensor(out=lo, in0=lo, in1=gs, op=ALU.add), R=[Bs], W=[Bs])
                        yield
                    op("dve", lambda e: e.tensor_scalar(out=mk[:, s0:s0 + ns], in0=a_ap, scalar1=lo, scalar2=None,
                                                        op0=ALU.is_ge), R=[B_affT, Bs], W=[B_mk])
                    yield
                    op("dve", lambda e: e.tensor_tensor_scan(out=cum[:, s0:s0 + ns],
                                                             data0=one.to_broadcast([16, ns]), data1=mk[:, s0:s0 + ns],
                                                             initial=0.0, op0=ALU.mult, op1=ALU.add),
                       R=[B_mk, B_sm], W=[B_cum])
                    yield
                    op("dve", lambda e: e.tensor_tensor(out=cum[:, s0:s0 + ns], in0=cum[:, s0:s0 + ns],
                                                        in1=mk[:, s0:s0 + ns], op=ALU.mult), R=[B_mk, B_cum], W=[B_cum])
                    yield

                interleave(*[bisect(si, s0, ns, cap) for si, (s0, ns, cap) in enumerate(sets)])
                for t in range(ntile):
                    pi_ = t % 2
                    op("pe", lambda e: e.transpose(out=psb[pi_][:, 0:16], in_=cum[:, t * 128:(t + 1) * 128],
                                                   identity=ident_f[0:16, 0:16]),
                       R=[B_cum, B_const], W=[B_ps[pi_]])
                    op("act", lambda e: e.copy(out=posm_tok[:, t, :], in_=psb[pi_][:, 0:16]), R=[B_ps[pi_]], W=[B_posm])
                S.barrier()
            debug_dump(f"posm{l}", lambda o: [dma("sp", o, posm_tok[:], R=[B_posm])])
            if P.stop_after == f"route{l}":
                return

            with ExitStack() as xs:
                Pm = P.sb(xs, "Pm", [128, NT, NS], BF16)
                B_Pm = Buf("Pm")
                PTl = P.sb(xs, "PTl", [128, 2, T_LAT], BF16)
                PTc = P.sb(xs, "PTc", [128, T_CTX], BF16)
                B_PT = Buf("PT")

                def PTv(st, a, b2, sz):
                    return PTl[0:sz, st, a:b2] if st < 2 else PTc[0:sz, a - T_LAT:b2 - T_LAT]
                xeT = P.sb(xs, "xeT", [128, KD, NS], BF16)
                B_xe = [Buf(f"xe{k}") for k in range(KD)]
                hmid = P.sb(xs, "hmid", [128, 2, NS], BF16)
                B_hm = [Buf("hm0"), Buf("hm1")]
                sil = P.sb(xs, "sil", [128, 2, NS], BF16)
                B_sil = [Buf("sil0"), Buf("sil1")]
                y_sb = P.sb(xs, "y_sb", [128, nst, D], BF16)
                B_y = [Buf(f"y{i}") for i in range(nst)]
                gsl = P.sb(xs, "gsl", [128, 4], F32)
                B_gsl = Buf("gsl")
                NGU = 5
                NDN = 5 if with_ctx else 6
                wgu = wring + [P.sb(xs, f"wgu{i}", [128, KD, 512], BF16) for i in range(NGU - NSLOT)]
                B_gu = [Buf(f"gu{i}") for i in range(NGU)]
                wdn = [P.sb(xs, f"wdn{i}", [128, 2, D], BF16) for i in range(NDN)]
                B_dn = [Buf(f"dn{i}") for i in range(NDN)]
                op("dve", lambda e: e.memset(Pm[:], 0.0), W=[B_Pm])

                NFB = NFC // 2
                sched = [(e_, fb) for e_ in range(n_exp_run) for fb in range(NFB)]
                issued = {"n": 0}

                def issue_weights(upto):
                    while issued["n"] < min(upto, len(sched)):
                        i = issued["n"]
                        e_, fb = sched[i]
                        if f"wgu_{l}_{e_}" not in P.dram:
                            P.din(f"wgu_{l}_{e_}", [NFC // 2, 128, KD, 512])
                            P.din(f"wdt_{l}_{e_}", [NFC // 2, 128, 2, D])
                        wgu_ap, wd_ap = P.dram[f"wgu_{l}_{e_}"], P.dram[f"wdt_{l}_{e_}"]
                        gi, di = i % NGU, i % NDN
                        dma("pool", wgu[gi][:], wgu_ap[fb], W=[B_gu[gi]])
                        dma("pool", wdn[di][:], wd_ap[fb], W=[B_dn[di]])
                        issued["n"] += 1

                LA = 4 if with_ctx else 5
                issue_weights(LA)
                blk_i = 0
                deferred = {"scatter": None}
                xb = (6, 7) if with_ctx else (4, 5)
                for e_ in range(n_exp_run):
                    op("dve", lambda e: e.tensor_tensor(
                        out=Pm[:, 0:16, 0:256], in0=iota1[:, :].unsqueeze(1).to_broadcast([128, 16, 256]),
                        in1=posm_tok[:, 0:16, e_:e_ + 1].to_broadcast([128, 16, 256]), op=ALU.is_equal),
                       R=[B_posm, B_wrt], W=[B_Pm])
                    if with_ctx:
                        op("dve", lambda e: e.tensor_tensor(
                            out=Pm[:, 16:18, 256:288], in0=iota1[:, 0:32].unsqueeze(1).to_broadcast([128, 2, 32]),
                            in1=posm_tok[:, 16:18, e_:e_ + 1].to_broadcast([128, 2, 32]), op=ALU.is_equal),
                           R=[B_posm, B_wrt], W=[B_Pm])
                    for st in range(nst):
                        tiles = range(16) if st < 2 else range(16, 18)
                        tl = list(tiles)
                        for ti, t in enumerate(tl):
                            op("pe", lambda e: e.matmul(psb[6][0:st_sz[st], st * 2:st * 2 + 2],
                                                        lhsT=Pm[:, t, st_off[st]:st_off[st] + st_sz[st]],
                                                        rhs=aff_hl[:, t, e_, :], start=(ti == 0), stop=(ti == len(tl) - 1)),
                               R=[B_Pm, B_posm], W=[B_ps[6]], signal=(ti == len(tl) - 1))
                    for st in range(nst):
                        op("dve", lambda e: e.tensor_reduce(out=gsl[0:st_sz[st], st:st + 1],
                                                            in_=psb[6][0:st_sz[st], st * 2:st * 2 + 2], axis=AX.X, op=ALU.add),
                           R=[B_ps[6]], W=[B_gsl])
                    def do_PT():
                        gcnt = 0
                        for st in range(nst):
                            tl = list(range(16)) if st < 2 else [16, 17]
                            for g0 in range(0, len(tl), 8):
                                grp = tl[g0:g0 + 8]
                                pi_ = xb[gcnt % 2]
                                gcnt += 1
                                pbf = psb[pi_][:].bitcast(BF16)
                                for gi_, t in enumerate(grp):
                                    op("pe", lambda e: e.transpose(
                                        out=pbf[0:st_sz[st], gi_ * 128:(gi_ + 1) * 128],
                                        in_=Pm[:, t, st_off[st]:st_off[st] + st_sz[st]], identity=ident_b[:]),
                                       R=[B_Pm, B_const], W=[B_ps[pi_]], signal=(gi_ == len(grp) - 1))
                                op("act", lambda e: e.copy(out=PTv(st, grp[0] * 128, (grp[-1] + 1) * 128, st_sz[st]),
                                                           in_=pbf[0:st_sz[st], 0:len(grp) * 128]),
                                   R=[B_ps[pi_]], W=[B_PT])
                    for k in range(KD):
                        pi_ = 6 + (k % 2)
                        for t in range(ntile):
                            op("pe", lambda e: e.matmul(psb[pi_][:, 0:NS], lhsT=h2tok[:, t, k * 128:(k + 1) * 128],
                                                        rhs=Pm[:, t, :], start=(t == 0), stop=(t == ntile - 1)),
                               R=[B_h2[t], B_Pm], W=[B_ps[pi_]], signal=(t == ntile - 1))
                        op("act", lambda e: e.copy(out=xeT[:, k, :], in_=psb[pi_][:, 0:NS]), R=[B_ps[pi_]], W=[B_xe[k]])
                    psY = [[psb[st * 2 + nh] for nh in range(2)] for st in range(nst)]
                    B_psY = [[B_ps[st * 2 + nh] for nh in range(2)] for st in range(nst)]

                    def emit_down(fc, hb, di, first, last):
                        for st in range(nst):
                            for nh in range(2):
                                op("pe", lambda e: e.matmul(
                                    psY[st][nh][0:st_sz[st], :], lhsT=hmid[:, hb, st_off[st]:st_off[st] + st_sz[st]],
                                    rhs=wdn[di][:, fc % 2, nh * 512:(nh + 1) * 512], start=first, stop=last),
                                   R=[B_hm[hb], B_dn[di]], W=[B_psY[st][nh]], signal=(st == nst - 1 and nh == 1))

                    pend = None
                    for fb in range(NFB):
                        i = blk_i
                        blk_i += 1
                        issue_weights(i + LA)
                        if fb == 3:
                            if deferred["scatter"] is not None:
                                deferred["scatter"]()
                                deferred["scatter"] = None
                            do_PT()
                        gi, di = i % NGU, i % NDN
                        for f2 in range(2):
                            fc = fb * 2 + f2
                            hb = fc % 2
                            ia, iu = 6, 7
                            pa, pu = psb[ia], psb[iu]
                            for k in range(KD):
                                op("pe", lambda e: e.matmul(pa[:, 0:NS], lhsT=wgu[gi][:, k, f2 * 128:(f2 + 1) * 128],
                                                            rhs=xeT[:, k, :], start=(k == 0), stop=(k == KD - 1)),
                                   R=[B_gu[gi], B_xe[k]], W=[B_ps[ia]], signal=(k == KD - 1))
                            for k in range(KD):
                                op("pe", lambda e: e.matmul(pu[:, 0:NS],
                                                            lhsT=wgu[gi][:, k, 256 + f2 * 128:256 + (f2 + 1) * 128],
                                                            rhs=xeT[:, k, :], start=(k == 0), stop=(k == KD - 1)),
                                   R=[B_gu[gi], B_xe[k]], W=[B_ps[iu]], signal=(k == KD - 1))
                            op("act", lambda e: e.activation(out=sil[:, hb, :], in_=pa[:, 0:NS], func=AF.Silu),
                               R=[B_ps[ia]], W=[B_sil[hb]])
                            op("dve", lambda e: e.tensor_tensor(out=hmid[:, hb, :], in0=pu[:, 0:NS], in1=sil[:, hb, :],
                                                                op=ALU.mult), R=[B_ps[iu], B_sil[hb]], W=[B_hm[hb]])
                            if pend is not None:
                                emit_down(*pend)
                            pend = (fc, hb, di, fc == 0, fc == NFC - 1)
                    emit_down(*pend)
                    for st in range(nst):
                        for nh in range(2):
                            op("act", lambda e: e.activation(out=y_sb[0:st_sz[st], st, nh * 512:(nh + 1) * 512],
                                                             in_=psY[st][nh][0:st_sz[st], :], func=AF.Copy,
                                                             scale=gsl[0:st_sz[st], st:st + 1]),
                               R=[B_psY[st][nh], B_gsl], W=[B_y[st]])
                    def do_scatter():
                        gi_s = 0
                        for b in range(nblk):
                            s, n, isc = BLOCKS[b]
                            c = 1 if isc else 0
                            sts = [2] if isc else [0, 1]
                            for mch in range(KD):
                                pi_ = xb[gi_s % 2]
                                gi_s += 1
                                for si, st in enumerate(sts):
                                    op("pe", lambda e: e.matmul(psb[pi_][:, :n],
                                                                lhsT=y_sb[0:st_sz[st], st, mch * 128:(mch + 1) * 128],
                                                                rhs=PTv(st, s, s + n, st_sz[st]), start=(si == 0),
                                                                stop=(si == len(sts) - 1)),
                                       R=[B_y[st], B_PT], W=[B_ps[pi_]], signal=(si == len(sts) - 1))
                                g_ap = modv(l, 5, mch, c)
                                op("dve", lambda e: e.scalar_tensor_tensor(
                                    out=xT[:, mch, s:s + n], in0=psb[pi_][:, :n], scalar=g_ap, in1=xT[:, mch, s:s + n],
                                    op0=ALU.mult, op1=ALU.add),
                                   R=[B_ps[pi_], B_mod, B_x[mch][b]], W=[B_x[mch][b]])
                    deferred["scatter"] = do_scatter
                if deferred["scatter"] is not None:
                    deferred["scatter"]()
                S.barrier()

    def hgrn2_layer(l):
        hgin_d = P.din("hg_w_in", [D, 5120])
        hgout_d = P.din("hg_w_out", [D, D])
        hgmisc_d = P.din("hg_misc", [128, 1 + 4 * KD])
        hgmask_d = P.din("hgmask", [128, 256])
        PT_ = 256
        NPART = 9
        NCH = 2
        with ExitStack() as les:
            hT = P.sb(les, "hT1", [128, KD, T_ALL], BF16)
            B_h = [[Buf(f"h1{k}_{b}") for b in range(5)] for k in range(KD)]
            B_hT = Buf("hT1all")
            hgm = P.sb(les, "hgm", [128, 1 + 4 * KD], F32)
            msk = P.sb(les, "hgmask_sb", [128, 256], F32)
            lbs = P.sb(les, "lbs", [128, 2, 2, HG_H], F32)
            lbs2 = P.sb(les, "lbs2", [128, 2, 2, HG_H], F32)
            gnh = P.sb(les, "gnh", [128, 1], F32)
            B_hc = Buf("hgconst")
            dma("sp", hgm[:], hgmisc_d, W=[B_hc])
            dma("sp", msk[:], hgmask_d, W=[B_hc])
            AA = P.sb(les, "hgAA", [128, 14 * PT_], F32)
            RG = [AA[:, r * PT_:(r + 1) * PT_] for r in range(14)]
            B_R = [Buf(f"hgR{r}") for r in range(14)]

            def Aregs(d_, par):
                idx = [d_ * 2 + par, 4 + d_ * 2 + par, 8 + d_, 10 + d_, 12 + d_]
                return [RG[i] for i in idx], [B_R[i] for i in idx]
            tmp = [AA[:, 0:512], AA[:, 768:1280]]
            B_tmp = [[B_R[0], B_R[1]], [B_R[3], B_R[4]]]
            rstds = [AA[:, 1536:2048], AA[:, 2304:2816]]
            B_rstds = [[B_R[6], B_R[7]], [B_R[9], B_R[10]]]
            rstd, B_rstd = rstds[0], B_rstds[0]
            sq = [P.sb(les, f"hsq{i}", [128, 512], BF16) for i in range(2)]
            B_sq = [Buf("hsq0"), Buf("hsq1")]

            class _V:
                def __init__(self, ap):
                    self.ap = ap

                def __getitem__(self, key):
                    return self.ap[key]
            for b in range(5):
                s, n, _ = BLOCKS[b]
                norm_block(l, 0, b, lambda k, b=b, s=s, n=n: (hT[:, k, s:s + n], [B_h[k][b]]),
                           [_V(tmp[0]), _V(tmp[1])], B_tmp, _V(rstd), B_rstd, sq, B_sq, psn_i=7)
            for d_ in range(2):
                op("dve", lambda e: e.tensor_tensor(out=lbs[:, 0, d_, :], in0=hgm[:, 1 + (2 + d_) * 8:1 + (3 + d_) * 8],
                                                    in1=hgm[:, 1 + d_ * 8:1 + (d_ + 1) * 8], op=ALU.subtract),
                   R=[B_hc], W=[B_hc])
            op("act", lambda e: e.activation(out=lbs[:, 0, :, :], in_=lbs[:, 0, :, :], func=AF.Sigmoid), R=[B_hc], W=[B_hc])
            op("dve", lambda e: e.tensor_scalar(out=lbs[:, 1, :, :], in0=lbs[:, 0, :, :], scalar1=-1.0, scalar2=1.0,
                                                op0=ALU.mult, op1=ALU.add), R=[B_hc], W=[B_hc])
            op("dve", lambda e: e.tensor_scalar(out=lbs2[:, 0, :, :], in0=lbs[:, 1, :, :], scalar1=0.5, scalar2=None,
                                                op0=ALU.mult), R=[B_hc], W=[B_hc])
            op("dve", lambda e: e.tensor_tensor(out=lbs2[:, 1, :, :], in0=lbs2[:, 0, :, :], in1=lbs[:, 0, :, :], op=ALU.add),
               R=[B_hc], W=[B_hc])
            op("dve", lambda e: e.tensor_scalar(out=gnh[:], in0=hgm[:, 0:1], scalar1=0.5, scalar2=None, op0=ALU.mult),
               R=[B_hc], W=[B_hc])
            S.barrier()
            debug_dump("hT1", lambda o: [dma("sp", o.rearrange("(k p) t -> p k t", p=128)[:, k, :], hT[:, k, :],
                                             R=[B_hT]) for k in range(KD)])

            qs2 = P.sb(les, "hqs", [128, 2, PT_], BF16)
            B_qs2 = [Buf("hqs0"), Buf("hqs1")]
            arr = [[P.sb(les, f"harr{d_}{i}", [128, T_ALL], BF16) for i in range(3)] for d_ in range(2)]
            B_arr = [[Buf(f"harr{d_}{i}") for i in range(3)] for d_ in range(2)]
            sgT, B_sg = arr[0][2], B_arr[0][2]
            ogT, B_og = arr[1][2], B_arr[1][2]
            B_ogb = [[B_og, Buf(f"ogb{b}")] for b in range(4)]
            vv = P.sb(les, "hvv", [128, NT, 128], BF16)
            B_v = Buf("hvv")
            EF = P.sb(les, "hEF", [128, 2, NT], F32)
            EM = P.sb(les, "hEM", [128, 2, NT], F32)
            t6b = P.sb(les, "ht6", [128, 2, 8], F32)
            B_t6 = [Buf("t6a"), Buf("t6b")]
            B_E = Buf("hE")
            Sst = P.sb(les, "hS", [128, 2, 128], F32)
            B_S = [Buf("hS0"), Buf("hS1")]
            Suse = P.sb(les, "hSuse", [128, 2, 16, 128], BF16)
            B_Su = Buf("hSuse")
            kht = [P.sb(les, f"hkht{i}", [128, 128], BF16) for i in range(2)]
            B_kht = [Buf("kht0"), Buf("kht1")]
            ATs = [P.sb(les, f"hAT{i}", [128, 128], BF16) for i in range(8)]
            B_ATs = [Buf(f"hAT{i}") for i in range(8)]
            one_col = hgm[:, 0:1]
            ones_f = P.sb(les, "hones", [128, 1], F32)
            op("dve", lambda e: e.memset(ones_f[:], 1.0), W=[B_hc])
            eps_col = P.sb(les, "heps", [128, 1], F32)
            op("dve", lambda e: e.memset(eps_col[:], EPS), W=[B_hc])

            win_src = hgin_d.rearrange("(k p) f -> p k f", p=128)
            cnt_ = {"ps": 0, "kht": 0, "at": 0}

            def view3(ap):
                return ap.rearrange("p (c i) -> p c i", i=128)

            for h in range(HG_H):
                sX, sY = wslot(), wslot()
                for gi_, off in enumerate((0, 1024, 2048, 3072)):
                    dma("pool", wring[sX][:, :, gi_ * 128:(gi_ + 1) * 128], win_src[:, :, off + h * 128: off + (h + 1) * 128],
                        W=[B_wr[sX]])
                yflat = wring[sY][:].rearrange("p k f -> p (k f)")
                wG = yflat[:, 0:1024].rearrange("p (k f) -> p k f", k=KD)
                wO = yflat[:, 1024:2048]
                dma("pool", wG, win_src[:, :, 4096 + h * 128:4096 + (h + 1) * 128], W=[B_wr[sY]])
                dma("pool", wO, hgout_d[h * 128:(h + 1) * 128, :], W=[B_wr[sY]])

                def proj(gi_, s, n, pi_):
                    for k in range(KD):
                        op("pe", lambda e: e.matmul(psb[pi_][:, :n], lhsT=wring[sX][:, k, gi_ * 128:(gi_ + 1) * 128],
                                                    rhs=hT[:, k, s:s + n], start=(k == 0), stop=(k == KD - 1)),
                           R=[B_wr[sX], B_hT], W=[B_ps[pi_]], signal=(k == KD - 1))

                def stageA(part):
                    p0 = part * PT_
                    c0 = part * NCH
                    par = part % 2
                    qs = qs2[:, par, :]
                    B_qs = B_qs2[par]
                    pi_ = cnt_["ps"] % 2
                    cnt_["ps"] += 1
                    proj(0, p0, PT_, pi_)
                    op("act", lambda e: e.activation(out=qs, in_=psb[pi_][:, :PT_], func=AF.Tanh, scale=0.5),
                       R=[B_ps[pi_]], W=[B_qs])
                    yield
                    op("dve", lambda e: e.scalar_tensor_tensor(out=qs, in0=qs, scalar=1.0, in1=psb[pi_][:, :PT_],
                                                               op0=ALU.add, op1=ALU.mult), R=[B_qs, B_ps[pi_]], W=[B_qs])
                    yield
                    for tt in range(NCH):
                        t = c0 + tt
                        pi_ = cnt_["ps"] % 2
                        cnt_["ps"] += 1
                        for k in range(KD):
                            op("pe", lambda e: e.matmul(psb[pi_][:, 0:128], lhsT=hT[:, k, t * 128:(t + 1) * 128],
                                                        rhs=wring[sX][:, k, 384:512], start=(k == 0), stop=(k == KD - 1)),
                               R=[B_wr[sX], B_hT], W=[B_ps[pi_]], signal=(k == KD - 1))
                        op("act", lambda e: e.copy(out=vv[:, t, :], in_=psb[pi_][:, 0:128]), R=[B_ps[pi_]], W=[B_v])
                        yield
                    regs = [Aregs(d_, par) for d_ in range(2)]
                    pis = []
                    for d_ in range(2):
                        pi_ = cnt_["ps"] % 2
                        cnt_["ps"] += 1
                        pis.append(pi_)
                        proj(1 + d_, p0, PT_, pi_)
                    for d_ in range(2):
                        (A1, A2, A3, A4, A5), BA = regs[d_]
                        op("act", lambda e: e.activation(out=A2, in_=psb[pis[d_]][:, :PT_], func=AF.Tanh, scale=0.5),
                           R=[B_ps[pis[d_]]], W=[BA[1]])
                        yield
                    for d_ in range(2):
                        (A1, A2, A3, A4, A5), BA = regs[d_]
                        op("dve", lambda e: e.tensor_scalar(out=A1, in0=A2, scalar1=-0.5, scalar2=0.5, op0=ALU.mult,
                                                            op1=ALU.add), R=[BA[1]], W=[BA[0]])
                        yield
                    for d_ in range(2):
                        (A1, A2, A3, A4, A5), BA = regs[d_]
                        op("act", lambda e: e.activation(out=A2, in_=A2, func=AF.Ln, bias=lbs2[:, 1, d_, h:h + 1],
                                                         scale=lbs2[:, 0, d_, h:h + 1]), R=[BA[1], B_hc], W=[BA[1]])
                        yield

                def stageB_driver(part):
                    p0 = part * PT_
                    c0 = part * NCH
                    par = part % 2
                    qs = qs2[:, par, :]
                    B_qs = B_qs2[par]
                    def chain(d_):
                        si_ = d_
                        (A1, A2, A3, A4, A5), BA = Aregs(d_, par)
                        t6 = t6b[:, si_, :]
                        B_T = B_t6[si_]
                        oml_ap = lbs[:, 1, d_, h:h + 1]
                        op("dve", lambda e: e.tensor_tensor_scan(out=A3, data0=ones_f[:, 0:1].to_broadcast([128, PT_]),
                                                                 data1=A2, initial=0.0, op0=ALU.mult, op1=ALU.add),
                           R=[BA[1], B_hc], W=[BA[2]])
                        G3, g3 = view3(A3), view3(A2)
                        dst = [arr[d_][i][:, p0:p0 + PT_] for i in range(3)]
                        Bd = B_arr[d_]
                        bc = [128, NCH, 128]
                        if d_ == 0:
                            yield
                            op("dve", lambda e: e.tensor_tensor(out=view3(A4), in0=G3, in1=G3[:, :, 63:64].to_broadcast(bc),
                                                                op=ALU.subtract), R=[BA[2]], W=[BA[3]])
                            yield
                            op("act", lambda e: e.activation(out=A5, in_=A4, func=AF.Exp, bias=-0.6931471805599453), R=[BA[3]], W=[BA[4]])
                            yield
                            op("pool", lambda e: e.tensor_tensor(out=dst[0], in0=qs, in1=A5, op=ALU.mult),
                               R=[B_qs, BA[4]], W=[Bd[0]])
                            yield
                            op("dve", lambda e: e.tensor_tensor(out=t6[:, 0:NCH], in0=G3[:, :, 0], in1=g3[:, :, 0], op=ALU.subtract),
                               R=[BA[2], BA[1]], W=[B_T])
                            yield
                            op("dve", lambda e: e.tensor_tensor(out=EF[:, 0, c0:c0 + NCH], in0=G3[:, :, 127], in1=t6[:, 0:NCH],
                                                                op=ALU.subtract), R=[BA[2], B_T], W=[B_E])
                            yield
                            op("dve", lambda e: e.tensor_tensor(out=EM[:, 0, c0:c0 + NCH], in0=G3[:, :, 63], in1=t6[:, 0:NCH],
                                                                op=ALU.subtract), R=[BA[2], B_T], W=[B_E])
                            yield
                            op("act", lambda e: e.activation(out=A2, in_=A4, func=AF.Exp, scale=-1.0), R=[BA[3]], W=[BA[1]])
                            yield
                            op("dve", lambda e: e.scalar_tensor_tensor(out=dst[1], in0=A1, scalar=oml_ap, in1=A2, op0=ALU.mult, op1=ALU.mult),
                               R=[BA[0], BA[1]], W=[Bd[1]])
                            yield
                            op("dve", lambda e: e.tensor_tensor(out=view3(A4), in0=G3, in1=G3[:, :, 127:128].to_broadcast(bc),
                                                                op=ALU.subtract), R=[BA[2]], W=[BA[3]])
                            yield
                            op("act", lambda e: e.activation(out=A5, in_=A4, func=AF.Exp, scale=-1.0), R=[BA[3]], W=[BA[4]])
                            yield
                            op("dve", lambda e: e.scalar_tensor_tensor(out=dst[2], in0=A1, scalar=oml_ap, in1=A5, op0=ALU.mult, op1=ALU.mult),
                               R=[BA[0], BA[4]], W=[Bd[2]])
                        else:
                            yield
                            op("dve", lambda e: e.tensor_copy(out=t6[:, 0:NCH], in_=G3[:, :, 127]), R=[BA[2]], W=[B_T])
                            yield
                            op("pool", lambda e: e.tensor_tensor(out=A2, in0=A3, in1=A2, op=ALU.subtract),
                               R=[BA[2], BA[1]], W=[BA[1]])
                            H3 = view3(A2)
                            yield
                            op("dve", lambda e: e.tensor_tensor(out=view3(A4), in0=H3, in1=H3[:, :, 64:65].to_broadcast(bc),
                                                                op=ALU.subtract), R=[BA[1]], W=[BA[3]])
                            yield
                            op("act", lambda e: e.activation(out=A5, in_=A4, func=AF.Exp, scale=-1.0, bias=-0.6931471805599453), R=[BA[3]], W=[BA[4]])
                            yield
                            op("pool", lambda e: e.tensor_tensor(out=dst[0], in0=qs, in1=A5, op=ALU.mult),
                               R=[B_qs, BA[4]], W=[Bd[0]])
                            yield
                            op("act", lambda e: e.activation(out=A3, in_=A4, func=AF.Exp), R=[BA[3]], W=[BA[2]])
                            yield
                            op("dve", lambda e: e.scalar_tensor_tensor(out=dst[1], in0=A1, scalar=oml_ap, in1=A3, op0=ALU.mult, op1=ALU.mult),
                               R=[BA[0], BA[2]], W=[Bd[1]])
                            yield
                            op("dve", lambda e: e.tensor_tensor(out=EF[:, 1, c0:c0 + NCH], in0=t6[:, 0:NCH], in1=H3[:, :, 0],
                                                                op=ALU.subtract), R=[BA[1], B_T], W=[B_E])
                            yield
                            op("dve", lambda e: e.tensor_tensor(out=EM[:, 1, c0:c0 + NCH], in0=t6[:, 0:NCH], in1=H3[:, :, 64],
                                                                op=ALU.subtract), R=[BA[1], B_T], W=[B_E])
                            yield
                            op("dve", lambda e: e.tensor_tensor(out=view3(A4), in0=H3, in1=H3[:, :, 0:1].to_broadcast(bc),
                                                                op=ALU.subtract), R=[BA[1]], W=[BA[3]])
                            yield
                            op("act", lambda e: e.activation(out=A5, in_=A4, func=AF.Exp), R=[BA[3]], W=[BA[4]])
                            yield
                            op("dve", lambda e: e.scalar_tensor_tensor(out=dst[2], in0=A1, scalar=oml_ap, in1=A5, op0=ALU.mult, op1=ALU.mult),
                               R=[BA[0], BA[4]], W=[Bd[2]])
                        yield
                    return [chain(0), chain(1)]

                for _ in stageA(0):
                    pass
                for part in range(NPART):
                    gens = stageB_driver(part)
                    if part + 1 < NPART:
                        gens.append(stageA(part + 1))
                    interleave(*gens)
                op("act", lambda e: e.activation(out=EF[:], in_=EF[:], func=AF.Exp), R=[B_E], W=[B_E])
                op("act", lambda e: e.activation(out=EM[:], in_=EM[:], func=AF.Exp), R=[B_E], W=[B_E])

                orders = [[16, 17] + list(range(16)), [17, 16] + list(range(15, -1, -1))]
                for d_ in range(2):
                    op("dve", lambda e: e.memset(Sst[:, d_, :], 0.0), W=[B_S[d_]])
                items = [(step, d_) for step in range(NT - 1) for d_ in range(2)]

                def emit_T(i):
                    step, d_ = items[i]
                    c = orders[d_][step]
                    ki = i % 2
                    pq = 4 + (i % 2)
                    pbf = psb[pq][:].bitcast(BF16)
                    op("pe", lambda e: e.transpose(out=pbf[:, 0:128], in_=arr[d_][2][:, c * 128:(c + 1) * 128],
                                                   identity=ident_b[:]), R=[B_arr[d_][2], B_const], W=[B_ps[pq]])
                    op("act", lambda e: e.copy(out=kht[ki][:], in_=pbf[:, 0:128]), R=[B_ps[pq]], W=[B_kht[ki]])

                def emit_suse(step, d_):
                    c = orders[d_][step]
                    if c < 16:
                        op("act", lambda e: e.activation(out=Suse[:, d_, c, :], in_=Sst[:, d_, :], func=AF.Copy,
                                                         scale=EM[:, d_, c:c + 1]), R=[B_S[d_], B_E], W=[B_Su])

                emit_T(0)
                for i, (step, d_) in enumerate(items):
                    c = orders[d_][step]
                    if i + 1 < len(items):
                        emit_T(i + 1)
                    emit_suse(step, d_)
                    ki = i % 2
                    pd = 2 + (i % 2)
                    op("pe", lambda e: e.matmul(psb[pd][:, 0:128], lhsT=kht[ki][:], rhs=vv[:, c, :], start=True, stop=True),
                       R=[B_kht[ki], B_v], W=[B_ps[pd]])
                    op("dve", lambda e: e.scalar_tensor_tensor(out=Sst[:, d_, :], in0=Sst[:, d_, :], scalar=EF[:, d_, c:c + 1],
                                                               in1=psb[pd][:, 0:128], op0=ALU.mult, op1=ALU.add),
                       R=[B_S[d_], B_E, B_ps[pd]], W=[B_S[d_]])
                for d_ in range(2):
                    emit_suse(NT - 1, d_)

                for b in range(4):
                    s, n, _ = BLOCKS[b]
                    pi_ = b % 2
                    for k in range(KD):
                        op("pe", lambda e: e.matmul(psb[pi_][:, :n], lhsT=wG[:, k, :], rhs=hT[:, k, s:s + n],
                                                    start=(k == 0), stop=(k == KD - 1)),
                           R=[B_wr[sY], B_hT], W=[B_ps[pi_]], signal=(k == KD - 1))
                    op("act", lambda e: e.activation(out=sgT[:, s:s + n], in_=psb[pi_][:, :n], func=AF.Tanh, scale=0.5),
                       R=[B_ps[pi_]], W=[B_sg])
                    op("dve", lambda e: e.scalar_tensor_tensor(out=sgT[:, s:s + n], in0=sgT[:, s:s + n], scalar=1.0,
                                                               in1=psb[pi_][:, :n], op0=ALU.add, op1=ALU.mult),
                       R=[B_sg, B_ps[pi_]], W=[B_sg])

                def out_block(b):
                    s, n, _ = BLOCKS[b]
                    bp = b % 2
                    iO = 6 if bp == 0 else 4
                    pO = psb[iO]

                    def scores(cc):
                        c = b * 4 + cc
                        cs = slice(c * 128, (c + 1) * 128)
                        ais = []
                        for d_ in range(2):
                            pa = 2 + (cnt_["at"] % 2)
                            ai = cnt_["at"] % 8
                            cnt_["at"] += 1
                            op("pe", lambda e: e.matmul(psb[pa][:, 0:128], lhsT=arr[d_][1][:, cs], rhs=arr[d_][0][:, cs],
                                                        start=True, stop=True),
                               R=[B_arr[d_][1], B_arr[d_][0]], W=[B_ps[pa]])
                            op("dve", lambda e: e.tensor_tensor(out=ATs[ai][:], in0=psb[pa][:, 0:128],
                                                                in1=msk[:, d_ * 128:(d_ + 1) * 128], op=ALU.mult),
                               R=[B_ps[pa], B_hc], W=[B_ATs[ai]])
                            ais.append(ai)
                        return ais

                    def outs(cc, ais):
                        c = b * 4 + cc
                        cs = slice(c * 128, (c + 1) * 128)
                        oc = pO[:, cc * 128:(cc + 1) * 128]
                        op("pe", lambda e: e.matmul(oc, lhsT=Suse[:, 0, c, :], rhs=arr[0][0][:, cs], start=True, stop=False),
                           R=[B_Su, B_arr[0][0]], W=[B_ps[iO]], signal=False)
                        op("pe", lambda e: e.matmul(oc, lhsT=Suse[:, 1, c, :], rhs=arr[1][0][:, cs], start=False, stop=False),
                           R=[B_Su, B_arr[1][0]], W=[B_ps[iO]], signal=False)
                        op("pe", lambda e: e.matmul(oc, lhsT=vv[:, c, :], rhs=ATs[ais[0]][:], start=False, stop=False),
                           R=[B_v, B_ATs[ais[0]]], W=[B_ps[iO]], signal=False)
                        op("pe", lambda e: e.matmul(oc, lhsT=vv[:, c, :], rhs=ATs[ais[1]][:], start=False, stop=True),
                           R=[B_v, B_ATs[ais[1]]], W=[B_ps[iO]], signal=True)

                    pend = None
                    for cc in range(4):
                        ais = scores(cc)
                        if pend is not None:
                            outs(*pend)
                        pend = (cc, ais)
                    outs(*pend)

                def post_block(b):
                    s, n, _ = BLOCKS[b]
                    bp = b % 2
                    iO, iN = (6, 7) if bp == 0 else (4, 5)
                    pO = psb[iO]
                    rstd, B_rstd = rstds[bp], B_rstds[bp]
                    op("act", lambda e: e.activation(out=sq[bp][:, :n], in_=pO[:, :n], func=AF.Square), R=[B_ps[iO]], W=[B_sq[bp]])
                    op("pe", lambda e: e.matmul(psb[iN][:, :n], lhsT=ones_b[:], rhs=sq[bp][:, :n], start=True, stop=True),
                       R=[B_sq[bp], B_const], W=[B_ps[iN]])
                    op("act", lambda e: e.activation(out=rstd[:, :n], in_=psb[iN][:, :n], func=AF.Ln, bias=eps_col[:, 0:1],
                                                     scale=1.0 / 128), R=[B_ps[iN], B_hc], W=[B_rstd])
                    op("act", lambda e: e.activation(out=rstd[:, :n], in_=rstd[:, :n], func=AF.Exp, scale=-0.5),
                       R=[B_rstd], W=[B_rstd])
                    op("dve", lambda e: e.scalar_tensor_tensor(out=tmp[bp][:, :n], in0=pO[:, :n], scalar=gnh[:, 0:1],
                                                               in1=rstd[:, :n], op0=ALU.mult, op1=ALU.mult),
                       R=[B_ps[iO], B_rstd, B_hc], W=[B_tmp[bp]])
                    op("dve", lambda e: e.tensor_tensor(out=ogT[:, s:s + n], in0=tmp[bp][:, :n], in1=sgT[:, s:s + n],
                                                         op=ALU.mult), R=[B_tmp[bp], B_sg], W=[B_ogb[b][1]])

                def proj_block(b):
                    s, n, _ = BLOCKS[b]
                    for mch in range(KD):
                        pi_ = mch % 2
                        op("pe", lambda e: e.matmul(psb[pi_][:, :n], lhsT=wO[:, mch * 128:(mch + 1) * 128],
                                                    rhs=ogT[:, s:s + n], start=True, stop=True),
                           R=[B_wr[sY], B_ogb[b]], W=[B_ps[pi_]])
                        g_ap = modv(l, 2, mch, 0)
                        op("dve", lambda e: e.scalar_tensor_tensor(
                            out=xT[:, mch, s:s + n], in0=psb[pi_][:, :n], scalar=g_ap, in1=xT[:, mch, s:s + n],
                            op0=ALU.mult, op1=ALU.add),
                           R=[B_ps[pi_], B_mod, B_x[mch][b]], W=[B_x[mch][b]])

                out_block(0)
                out_block(1)
                post_block(0)
                out_block(2)
                proj_block(0)
                post_block(1)
                out_block(3)
                proj_block(1)
                post_block(2)
                post_block(3)
                proj_block(2)
                proj_block(3)
            S.barrier()

    if "ret" in P.parts:
        retention_layer(0)
    debug_dump("x_mix0", lambda o: [dma("sp", o.rearrange("(k p) t -> p k t", p=128)[:, k, :], xT[:, k, :],
                                        R=B_x[k]) for k in range(KD)])

    if "moe0" in P.parts:
        moe_layer(0, True, P.n_exp_run)
    debug_dump("x_ffn0", lambda o: [dma("sp", o.rearrange("(k p) t -> p k t", p=128)[:, k, :], xT[:, k, :],
                                        R=B_x[k]) for k in range(KD)])
    if "hg" in P.parts:
        hgrn2_layer(1)
    debug_dump("x_mix1", lambda o: [dma("sp", o.rearrange("(k p) t -> p k t", p=128)[:, k, :], xT[:, k, 0:T_LAT],
                                        R=B_x[k][:4]) for k in range(KD)])
    if "moe1" in P.parts:
        moe_layer(1, False, P.n_exp_run)
    debug_dump("x_ffn1", lambda o: [dma("sp", o.rearrange("(k p) t -> p k t", p=128)[:, k, :], xT[:, k, 0:T_LAT],
                                        R=B_x[k][:4]) for k in range(KD)])
    if "final" in P.parts:
        with ExitStack() as fes:
            ftmp = [P.sb(fes, f"ftmp{i}", [128, 512], F32) for i in range(2)]
            B_ft = [Buf("ft0"), Buf("ft1")]
            frs = P.sb(fes, "frs", [128, 512], F32)
            B_frs = Buf("frs")
            fsq = [P.sb(fes, f"fsq{i}", [128, 512], BF16) for i in range(2)]
            B_fsq = [Buf("fsq0"), Buf("fsq1")]
            fo = [P.sb(fes, f"fo{i}", [128, 512], F32) for i in range(4)]
            B_fo = [Buf(f"fo{i}") for i in range(4)]
            osrc = out_d.rearrange("(k p) t -> p k t", p=128)
            oc_ = {"n": 0}
            for b in range(4):
                s, n, _ = BLOCKS[b]

                def dst(k):
                    i = oc_["n"] % 4
                    oc_["n"] += 1
                    dst.last = i
                    return fo[i][:, :n], [B_fo[i]]
                norm_block(1, 0, b, dst, ftmp, B_ft, frs, B_frs, fsq, B_fsq, psn_i=7, final=True,
                           after=lambda k, b=b, s=s, n=n: dma("sp", osrc[:, k, s:s + n], fo[dst.last][:, :n], R=[B_fo[dst.last]]))
    if P.stop_after is not None:
        osrc = out_d.rearrange("(k p) t -> p k t", p=128)
        for k in range(KD):
            dma("sp", osrc[:, k, :], xT[:, k, 0:T_LAT], R=B_x[k][:4])
    S.barrier()
    es.close()
    return P


def _prep_inputs(inp, b, consts, names=None):
    f = np.float32
    m = {}
    m["xT"] = np.ascontiguousarray(np.concatenate([inp["x"][b].T, inp["ctx"][b].T], axis=1)).astype(f)
    cv = np.stack([inp["c"][b], inp["c_ctx"]], axis=0)
    m["cvec"] = np.ascontiguousarray(cv.reshape(2, KD, 128).transpose(2, 0, 1).reshape(128, 2 * KD))
    m["w_ada"] = np.ascontiguousarray(inp["w_ada"].reshape(2, KD, 128, 12, 512).transpose(0, 3, 2, 1, 4))
    m["b_ada"] = np.ascontiguousarray(np.tile(inp["b_ada"].reshape(1, 12 * D), (2, 1)))
    nr = np.stack([inp["norm_mix"][0], inp["norm_mix"][1], inp["norm_ffn"][0], inp["norm_ffn"][1],
                   inp["norm_final"]], axis=0)
    m["norms"] = np.ascontiguousarray(nr.reshape(5, KD, 128).transpose(2, 0, 1).reshape(128, 5 * KD))
    m["ret_w_in"] = inp["ret_w_in"][0]
    m["ret_w_out"] = inp["ret_w_out"][0]
    m["hg_w_in"] = inp["hg_w_in"][0]
    m["hg_w_out"] = inp["hg_w_out"][0]
    lb = inp["hg_lower_bounds"].reshape(4, KD, 128).transpose(2, 0, 1).reshape(128, 4 * KD)
    m["hg_misc"] = np.ascontiguousarray(np.concatenate([inp["hg_g_norm"][0].reshape(128, 1), lb], axis=1)).astype(f)
    m["moe_router"] = inp["moe_router"]
    m.update(consts)
    for l in range(2):
        for e in range(N_EXP):
            if names is not None and f"wgu_{l}_{e}" not in names:
                continue
            nfb = NFC // 2
            g = inp["moe_w_gate"][l, e].reshape(KD, 128, nfb, 256).transpose(2, 1, 0, 3)
            u = inp["moe_w_up"][l, e].reshape(KD, 128, nfb, 256).transpose(2, 1, 0, 3)
            m[f"wgu_{l}_{e}"] = np.ascontiguousarray(np.concatenate([g, u], axis=3))
            m[f"wdt_{l}_{e}"] = np.ascontiguousarray(
                inp["moe_w_down"][l, e].reshape(nfb, 2, 128, D).transpose(0, 2, 1, 3))
    if names is not None:
        m = {k: v for k, v in m.items() if k in names}
    return m


def kernel(**inputs):
    inp = {k: np.asarray(v) for k, v in inputs.items()}
    consts = _const_tables()
    P = build_program()
    names = set(P.dram.keys())
    shared = _prep_inputs(inp, 0, consts, names)
    in_maps = []
    for b in range(8):
        mb = dict(shared)
        mb["xT"] = np.ascontiguousarray(np.concatenate([inp["x"][b].T, inp["ctx"][b].T], axis=1)).astype(np.float32)
        cv = np.stack([inp["c"][b], inp["c_ctx"]], axis=0)
        mb["cvec"] = np.ascontiguousarray(cv.reshape(2, KD, 128).transpose(2, 0, 1).reshape(128, 2 * KD))
        in_maps.append(mb)
    res = run_bass_kernel_spmd(P.nc, in_maps, core_ids=list(range(8)))
    out = np.stack([np.ascontiguousarray(res.results[b]["outT"].T) for b in range(8)], axis=0)
    return out.astype(np.float32)
```

```python
import math
from contextlib import ExitStack

import numpy as np
import concourse.bass as bass
import concourse.mybir as mybir
from concourse.bass_utils import run_bass_kernel_spmd

F32 = mybir.dt.float32
BF16 = mybir.dt.bfloat16
ALU = mybir.AluOpType
AF = mybir.ActivationFunctionType
AX = mybir.AxisListType

D = 1024
KD = 8
T_LAT = 2048
T_CTX = 256
T_ALL = T_LAT + T_CTX
NT = T_ALL // 128
EPS = 1e-6
N_EXP = 16
FF = 2816
NFC = FF // 128
RET_H = 4
HG_H = 8

BLOCKS = [(0, 512, False), (512, 512, False), (1024, 512, False), (1536, 512, False), (2048, 256, True)]


def interleave(*gens):
    gens = list(gens)
    while gens:
        for g in list(gens):
            try:
                next(g)
            except StopIteration:
                gens.remove(g)


class Buf:
    __slots__ = ("name", "w", "rs")

    def __init__(self, name):
        self.name = name
        self.w = None
        self.rs = {}


class Sched:
    NDMA = 12

    def __init__(self, nc, es):
        self.nc = nc
        self.h = {"pe": nc.tensor, "act": nc.scalar, "dve": nc.vector, "pool": nc.gpsimd, "sp": nc.sync}
        self.sem = {}
        self.cnt = {}
        self.seen = {}
        for e in self.h:
            self.sem[e] = es.enter_context(nc.semaphore("s_" + e))
            self.cnt[e] = 0
            self.seen[e] = {}
        self.dsem = {}
        self.dval = {}
        self.dnext = {}
        for q in ("sp", "pool"):
            self.dsem[q] = [es.enter_context(nc.semaphore(f"d_{q}{i}")) for i in range(self.NDMA)]
            self.dval[q] = [0] * self.NDMA
            self.dnext[q] = 0
        self.ninst = 0

    def _wait(self, eng, tk):
        if tk is None:
            return
        kind = tk[0]
        if kind == "c":
            _, src, n = tk
            if src == eng and eng in ("pe", "sp"):
                return
            if self.seen[eng].get(src, 0) >= n:
                return
            self.h[eng].wait_ge(self.sem[src], n)
            self.seen[eng][src] = n
        else:
            _, q, idx, val = tk
            key = (q, idx)
            if self.seen[eng].get(key, 0) >= val:
                return
            self.h[eng].wait_ge(self.dsem[q][idx], val)
            self.seen[eng][key] = val
        self.ninst += 1

    def _deps(self, eng, R, W):
        for b in R:
            self._wait(eng, b.w)
        for b in W:
            self._wait(eng, b.w)
            for tk in b.rs.values():
                self._wait(eng, tk)

    def _record(self, tk, R, W):
        for b in W:
            b.w = tk
            b.rs = {}
        for b in R:
            if b in W:
                continue
            key = tk[1] if tk[0] == "c" else (tk[1], tk[2])
            b.rs[key] = tk

    @staticmethod
    def _flat(L):
        out = []
        for b in L:
            if isinstance(b, (list, tuple)):
                out.extend(Sched._flat(b))
            else:
                out.append(b)
        return out

    def op(self, eng, fn, R=(), W=(), signal=True):
        R, W = self._flat(R), self._flat(W)
        self._deps(eng, R, W)
        ins = fn(self.h[eng])
        self.ninst += 1
        if signal:
            ins.then_inc(self.sem[eng], 1)
            self.cnt[eng] += 1
            tk = ("c", eng, self.cnt[eng])
            if eng not in ("pe",):
                pass
        else:
            tk = ("c", eng, self.cnt[eng] + 1)
        self._record(tk, R, W)
        return tk

    def dma(self, q, out, in_, R=(), W=()):
        R, W = self._flat(R), self._flat(W)
        self._deps(q, R, W)
        idx = self.dnext[q]
        self.dnext[q] = (idx + 1) % self.NDMA
        prev = self.dval[q][idx]
        if prev > 0:
            self._wait(q, ("d", q, idx, prev))
        val = prev + 16
        self.dval[q][idx] = val
        self.h[q].dma_start(out=out, in_=in_).then_inc(self.dsem[q][idx], 16)
        self.ninst += 1
        tk = ("d", q, idx, val)
        self._record(tk, R, W)
        return tk

    def barrier(self):
        for e in self.h:
            for src in self.h:
                if src != e and self.cnt[src] > 0:
                    self._wait(e, ("c", src, self.cnt[src]))
            for q in ("sp", "pool"):
                for idx in range(self.NDMA):
                    if self.dval[q][idx] > 0:
                        self._wait(e, ("d", q, idx, self.dval[q][idx]))


def _ret_gammas():
    j = np.arange(8, dtype=np.float64)
    g = 1.0 - np.exp2(-5.0 - j / 2)
    return g[0::2], g[1::2]


def _const_tables():
    t = {}
    half = 128
    inv = 10000.0 ** (-np.arange(0, half, 2, dtype=np.float64) / half)
    p = np.arange(128)
    sign = np.where(p < 64, -1.0, 1.0)
    rows = np.arange(T_LAT // 64, dtype=np.float64)
    cols = np.arange(64, dtype=np.float64)
    ang_r = rows[None, :] * inv[p % 64][:, None]
    ang_c = cols[None, :] * inv[p % 64][:, None]
    t["rope"] = np.concatenate(
        [np.cos(ang_r), np.sin(ang_r) * sign[:, None], np.cos(ang_c), np.sin(ang_c) * sign[:, None]], axis=1
    ).astype(np.float32)
    gf, gb = _ret_gammas()
    b = np.arange(128, dtype=np.float64)[:, None]
    a = np.arange(512, dtype=np.float64)[None, :]
    tabs = np.zeros((RET_H, 128, 1920), dtype=np.float64)
    xs = np.arange(896, dtype=np.float64)[None, :] - 384.0
    for h in range(RET_H):
        tabs[h, :, 0:512] = gf[h] ** (a - b)
        tabs[h, :, 512:1024] = gb[h] ** (b + 511 - a)
        dl = xs - b
        tabs[h, :, 1024:1920] = np.where(dl > 0, gf[h] ** np.maximum(dl, 0),
                                         np.where(dl < 0, gb[h] ** np.maximum(-dl, 0), 2.0))
    t["rdec"] = tabs.astype(np.float32)
    t["ident"] = np.eye(128, dtype=np.float32)
    t["hgmask"] = np.concatenate([np.triu(np.ones((128, 128))), np.tril(np.ones((128, 128)))], axis=1).astype(np.float32)
    t["iota1"] = np.tile(np.arange(1, 257, dtype=np.float32)[None, :], (128, 1))
    return t


class Prog:
    def __init__(self, dbg=None, stop_after=None):
        self.dbg = dbg or []
        self.stop_after = stop_after
        self.nc = bass.Bass("TRN2", target_bir_lowering=False)
        self.es = ExitStack()
        self.S = Sched(self.nc, self.es)
        self.dram = {}
        self.dbg_out = {}

    def din(self, name, shape, dt=F32):
        self.dram[name] = self.nc.dram_tensor(name, list(shape), dt, kind="ExternalInput").ap()
        return self.dram[name]

    def dout(self, name, shape, dt=F32):
        self.dram[name] = self.nc.dram_tensor(name, list(shape), dt, kind="ExternalOutput").ap()
        return self.dram[name]

    def sb(self, es, name, shape, dt):
        self._uid = getattr(self, "_uid", 0) + 1
        return es.enter_context(self.nc.sbuf_tensor(f"{name}_u{self._uid}", list(shape), dt))

    def ps(self, es, name, shape, dt=F32):
        return es.enter_context(self.nc.psum_tensor(name, list(shape), dt))


def build_program(dbg=(), stop_after=None, parts=("ret", "moe0", "hg", "moe1", "final"), n_exp_run=N_EXP):
    P = Prog(list(dbg), stop_after)
    P.parts = parts
    P.n_exp_run = n_exp_run
    nc, S, es = P.nc, P.S, P.es
    op, dma = S.op, S.dma

    xT_d = P.din("xT", [D, T_ALL])
    cvec_d = P.din("cvec", [128, 2 * KD])
    wada_d = P.din("w_ada", [2, 12, 128, KD, 512])
    bada_d = P.din("b_ada", [2, 12 * D])
    nrm_d = P.din("norms", [128, 5 * KD])
    retin_d = P.din("ret_w_in", [D, 6144])
    retout_d = P.din("ret_w_out", [2048, D])
    router_d = P.din("moe_router", [2, D, N_EXP])
    rope_d = P.din("rope", [128, 192])
    rdec_d = P.din("rdec", [RET_H, 128, 1920])
    ident_d = P.din("ident", [128, 128])
    out_d = P.dout("outT", [D, T_LAT])
    for name, shape, dt_ in P.dbg:
        P.dbg_out[name] = P.dout("dbg_" + name, shape, dt_)

    xT = P.sb(es, "xT_sb", [128, KD, T_ALL], F32)
    B_x = [[Buf(f"x{k}_{b}") for b in range(5)] for k in range(KD)]
    cvec = P.sb(es, "cvec_sb", [128, 2 * KD], F32)
    scc = P.sb(es, "scc", [128, KD, 2], F32)
    nrm = P.sb(es, "nrm", [128, 5 * KD], F32)
    mod = P.sb(es, "mod", [128, 2, 48, 2], F32)
    modA = P.sb(es, "modA", [128, 2, 2, KD, 2], F32)
    ident_f = P.sb(es, "ident_f", [128, 128], F32)
    ident_b = P.sb(es, "ident_b", [128, 128], BF16)
    ones_b = P.sb(es, "ones_b", [128, 128], BF16)
    B_const = Buf("const")
    B_mod = Buf("mod")

    psb = [P.ps(es, f"psb{i}", [128, 512], F32) for i in range(8)]
    B_ps = [Buf(f"ps{i}") for i in range(8)]

    def sl(b):
        s, n, _ = BLOCKS[b]
        return slice(s, s + n)

    xsrc = xT_d.rearrange("(k p) t -> p k t", p=128)
    for k in range(KD):
        dma("sp", xT[:, k, :], xsrc[:, k, :], W=B_x[k])
    dma("sp", cvec[:], cvec_d, W=[B_const])
    dma("sp", nrm[:], nrm_d, W=[B_const])
    dma("sp", ident_f[:], ident_d, W=[B_const])
    op("act", lambda e: e.copy(out=ident_b[:], in_=ident_f[:]), R=[B_const], W=[B_const])
    op("dve", lambda e: e.memset(ones_b[:], 1.0), W=[B_const])
    op("act", lambda e: e.activation(out=scc[:].rearrange("p k c -> p c k"),
                                     in_=cvec[:].rearrange("p (c k) -> p c k", c=2), func=AF.Silu),
       R=[B_const], W=[B_const])

    with ExitStack() as pes:
        NPIECE = 12
        NST = 4
        wa = [P.sb(pes, f"wa{i}", [128, KD, 512], F32) for i in range(NST)]
        B_wa = [Buf(f"wa{i}") for i in range(NST)]
        HALF = 3 * D
        modrow = P.sb(pes, "modrow", [2, HALF], F32)
        B_mr = Buf("modrow")
        bada2 = P.sb(pes, "bada2", [2, HALF], F32)
        B_b2 = Buf("bada2")
        pi = 0
        for l in range(2):
            for hf in range(2):
                dma("sp", bada2[:], bada_d[:, l * 6 * D + hf * HALF: l * 6 * D + (hf + 1) * HALF], W=[B_b2])
                for pc6 in range(6):
                    pc = hf * 6 + pc6
                    slot = pi % NST
                    dma("sp", wa[slot][:], wada_d[l, pc], W=[B_wa[slot]])
                    pb = pi % 2
                    for k in range(KD):
                        op("pe", lambda e: e.matmul(psb[pb][0:2, :], lhsT=scc[:, k, :], rhs=wa[slot][:, k, :],
                                                    start=(k == 0), stop=(k == KD - 1)),
                           R=[B_wa[slot], B_const], W=[B_ps[pb]], signal=(k == KD - 1))
                    op("dve", lambda e: e.tensor_tensor(out=modrow[:, pc6 * 512:(pc6 + 1) * 512], in0=psb[pb][0:2, :],
                                                        in1=bada2[:, pc6 * 512:(pc6 + 1) * 512],
                                                        op=ALU.add), R=[B_ps[pb], B_b2], W=[B_mr])
                    pi += 1
                pt_ = 2 + (l * 2 + hf) % 2
                for j in range(24):
                    op("pe", lambda e: e.transpose(out=psb[pt_][:, j * 2:(j + 1) * 2], in_=modrow[:, j * 128:(j + 1) * 128],
                                                   identity=ident_f[0:2, 0:2]),
                       R=[B_mr, B_const], W=[B_ps[pt_]], signal=(j == 23))
                op("dve", lambda e: e.tensor_copy(out=mod[:, l, hf * 24:(hf + 1) * 24, :].rearrange("p j c -> p (j c)"),
                                                  in_=psb[pt_][:, 0:48]), R=[B_ps[pt_]], W=[B_mod])
        for l in range(2):
            for site in range(2):
                sc_j = (1 if site == 0 else 4) * KD
                nw = nrm[:, (site * 2 + l) * KD:(site * 2 + l + 1) * KD]
                op("dve", lambda e, l=l, site=site, sc_j=sc_j, nw=nw: e.scalar_tensor_tensor(
                    out=modA[:, l, site, :, :], in0=mod[:, l, sc_j:sc_j + KD, :], scalar=1.0,
                    in1=nw.unsqueeze(2).to_broadcast([128, KD, 2]), op0=ALU.add, op1=ALU.mult),
                   R=[B_mod, B_const], W=[B_mod])
        S.barrier()

    def modv(l, chunk, k, c):
        return mod[:, l, chunk * KD + k, c:c + 1]

    def norm_block(l, site, b, dst_fn, tmp, B_tmp, rstd, B_rstd, sq, B_sq, psn_i, final=False, after=None):
        s, n, isc = BLOCKS[b]
        c = 1 if isc else 0
        for k in range(KD):
            q = k % 2
            op("act", lambda e, k=k, q=q: e.activation(out=sq[q][:, :n], in_=xT[:, k, s:s + n], func=AF.Square),
               R=[B_x[k][b]], W=[B_sq[q]])
            op("pe", lambda e, k=k, q=q: e.matmul(psb[psn_i][:, :n], lhsT=ones_b[:], rhs=sq[q][:, :n],
                                                 start=(k == 0), stop=(k == KD - 1)),
               R=[B_sq[q], B_const], W=[B_ps[psn_i]], signal=True)
        op("act", lambda e: e.activation(out=rstd[:, :n], in_=psb[psn_i][:, :n], func=AF.Sqrt, bias=EPS,
                                         scale=1.0 / D), R=[B_ps[psn_i]], W=[B_rstd])
        op("dve", lambda e: e.reciprocal(out=rstd[:, :n], in_=rstd[:, :n]), R=[B_rstd], W=[B_rstd])
        for k in range(KD):
            q = k % 2
            out_ap, obufs = dst_fn(k)
            if final:
                a_ap = nrm[:, 4 * KD + k:4 * KD + k + 1]
                op("dve", lambda e, k=k, a_ap=a_ap, out_ap=out_ap: e.scalar_tensor_tensor(
                    out=out_ap, in0=xT[:, k, s:s + n], scalar=a_ap, in1=rstd[:, :n], op0=ALU.mult, op1=ALU.mult),
                   R=[B_x[k][b], B_rstd, B_const], W=obufs)
                if after is not None:
                    after(k)
            else:
                a_ap = modA[:, l, site, k, c:c + 1]
                sh_ap = modv(l, 0 if site == 0 else 3, k, c)
                op("dve", lambda e, k=k, q=q, a_ap=a_ap: e.scalar_tensor_tensor(
                    out=tmp[q][:, :n], in0=xT[:, k, s:s + n], scalar=a_ap, in1=rstd[:, :n],
                    op0=ALU.mult, op1=ALU.mult), R=[B_x[k][b], B_rstd, B_mod], W=[B_tmp[q]])
                op("act", lambda e, q=q, sh_ap=sh_ap, out_ap=out_ap: e.activation(
                    out=out_ap, in_=tmp[q][:, :n], func=AF.Identity, bias=sh_ap, scale=1.0),
                   R=[B_tmp[q], B_mod], W=obufs)

    def debug_dump(name, ap_fn):
        if name in P.dbg_out:
            S.barrier()
            tk = ap_fn(P.dbg_out[name])
            S.barrier()

    NSLOT = 4
    wring = [P.sb(es, f"wring{i}", [128, KD, 512], BF16) for i in range(NSLOT)]
    B_wr = [Buf(f"wr{i}") for i in range(NSLOT)]
    wr_state = {"n": 0}

    def wslot():
        i = wr_state["n"] % NSLOT
        wr_state["n"] += 1
        return i

    def retention_layer(l):
        with ExitStack() as les:
            hT = P.sb(les, "hT", [128, KD, T_ALL], BF16)
            B_h = [[Buf(f"h{k}_{b}") for b in range(5)] for k in range(KD)]
            tmp = [P.sb(les, f"ntmp{i}", [128, 512], F32) for i in range(2)]
            B_tmp = [Buf("ntmp0"), Buf("ntmp1")]
            sg = [P.sb(les, f"sg{i}", [128, 512], F32) for i in range(2)]
            B_sg = [Buf("sg0"), Buf("sg1")]
            rstd = P.sb(les, "rstd", [128, 512], F32)
            B_rstd = Buf("rstd")
            sq = [P.sb(les, f"sq{i}", [128, 512], BF16) for i in range(2)]
            B_sq = [Buf("sq0"), Buf("sq1")]
            rope_sb = P.sb(les, "rope_sb", [128, 192], F32)
            B_rope = Buf("rope")
            dma("sp", rope_sb[:], rope_d, W=[B_rope])
            for b in range(5):
                s, n, _ = BLOCKS[b]
                norm_block(l, 0, b, lambda k, b=b, s=s, n=n: (hT[:, k, s:s + n], [B_h[k][b]]),
                           tmp, B_tmp, rstd, B_rstd, sq, B_sq, psn_i=7)
            debug_dump("hT0", lambda o: [dma("sp", o.rearrange("(k p) t -> p k t", p=128)[:, k, :], hT[:, k, :],
                                             R=B_h[k]) for k in range(KD)])
            if P.stop_after == "hT0":
                return

            qr = P.sb(les, "qr", [128, 2, 2, 512], BF16)
            kr = P.sb(les, "kr", [128, 2, T_ALL], BF16)
            vv = P.sb(les, "vv", [128, NT, 512], BF16)
            B_q = [[Buf(f"q{i}_{m}") for m in range(2)] for i in range(2)]
            B_k = [[Buf(f"k{m}_{t}") for t in range(NT)] for m in range(2)]
            B_v = [Buf(f"v{t}") for t in range(NT)]
            rdec = P.sb(les, "rdec_sb", [128, 1920], F32)
            B_rdec = Buf("rdec")
            Ff = rdec[:, 0:512]
            Fb = rdec[:, 512:1024]

            def Dg(kk, n):
                return rdec[:, 1024 + 384 - 128 * kk: 1024 + 384 - 128 * kk + n]
            rt1, B_rt1 = tmp, B_tmp
            rt2, B_rt2 = sg, B_sg
            ctab, B_ctab = tmp, B_tmp
            AT = [P.sb(les, f"AT{i}", [128, 512], BF16) for i in range(3)]
            B_AT = [Buf(f"AT{i}") for i in range(3)]
            ogT = P.sb(les, "ogT", [128, 4, 512], BF16)
            B_og = [Buf(f"og{c}") for c in range(4)]
            sgb = P.sb(les, "sgb", [128, 4, 512], BF16)
            B_sgb = [Buf(f"sgb{c}") for c in range(4)]
            gf, gb = _ret_gammas()
            rcount = {"rope": 0, "at": 0, "q": 0}

            win_src = retin_d.rearrange("(k p) f -> p k f", p=128)
            wout_src = retout_d.rearrange("(c p) f -> p c f", p=128)

            def proj_rope(sA, qk, m, b, dst_ap, wb):
                s, n, isc = BLOCKS[b]
                pi_ = rcount["rope"] % 2
                rcount["rope"] += 1
                pst = psb[pi_]
                for k in range(KD):
                    op("pe", lambda e, k=k: e.matmul(
                        pst[:, :n], lhsT=wring[sA][:, k, qk * 256 + m * 128: qk * 256 + (m + 1) * 128],
                        rhs=hT[:, k, s:s + n], start=(k == 0), stop=(k == KD - 1)),
                       R=[B_wr[sA], B_h[k][b]], W=[B_ps[pi_]], signal=(k == KD - 1))
                scale = 1.0 if qk == 0 else 1.0 / 16.0
                if isc:
                    op("act", lambda e: e.activation(out=dst_ap, in_=pst[:, :n], func=AF.Copy, scale=scale),
                       R=[B_ps[pi_]], W=wb)
                    return
                g0 = s // 64
                if m == 0:
                    cos_ap = rope_sb[:, g0:g0 + 8].unsqueeze(2).to_broadcast([128, 8, 64])
                    sin_lo = rope_sb[0:64, 32 + g0:32 + g0 + 8].unsqueeze(2).to_broadcast([64, 8, 64])
                    sin_hi = rope_sb[64:128, 32 + g0:32 + g0 + 8].unsqueeze(2).to_broadcast([64, 8, 64])
                else:
                    cos_ap = rope_sb[:, 64:128].unsqueeze(1).to_broadcast([128, 8, 64])
                    sin_lo = rope_sb[0:64, 128:192].unsqueeze(1).to_broadcast([64, 8, 64])
                    sin_hi = rope_sb[64:128, 128:192].unsqueeze(1).to_broadcast([64, 8, 64])
                ti = pi_
                p3 = pst[:].rearrange("p (g t) -> p g t", t=64)
                t1v = rt1[ti][:].rearrange("p (g t) -> p g t", t=64)
                t2v = rt2[ti][:].rearrange("p (g t) -> p g t", t=64)
                op("dve", lambda e: e.scalar_tensor_tensor(
                    out=t1v, in0=p3, scalar=scale, in1=cos_ap, op0=ALU.mult, op1=ALU.mult),
                   R=[B_ps[pi_], B_rope], W=[B_rt1[ti]])
                op("dve", lambda e: e.scalar_tensor_tensor(
                    out=t2v[0:64], in0=p3[64:128], scalar=scale, in1=sin_lo, op0=ALU.mult, op1=ALU.mult),
                   R=[B_ps[pi_], B_rope], W=[B_rt2[ti]])
                op("dve", lambda e: e.scalar_tensor_tensor(
                    out=t2v[64:128], in0=p3[0:64], scalar=scale, in1=sin_hi, op0=ALU.mult, op1=ALU.mult),
                   R=[B_ps[pi_], B_rope, B_rt2[ti]], W=[B_rt2[ti]])
                op("dve", lambda e: e.tensor_tensor(
                    out=dst_ap, in0=rt1[ti][:, :n], in1=rt2[ti][:, :n], op=ALU.add),
                   R=[B_rt1[ti], B_rt2[ti]], W=wb)

            for h in range(RET_H):
                sA, sB, sC, sD = wslot(), wslot(), wslot(), wslot()
                dma("pool", wring[sA][:, :, 0:256], win_src[:, :, h * 256:(h + 1) * 256], W=[B_wr[sA]])
                dma("pool", wring[sA][:, :, 256:512], win_src[:, :, 1024 + h * 256:1024 + (h + 1) * 256], W=[B_wr[sA]])
                dma("pool", wring[sB][:], win_src[:, :, 2048 + h * 512:2048 + (h + 1) * 512], W=[B_wr[sB]])
                dma("pool", wring[sC][:], win_src[:, :, 4096 + h * 512:4096 + (h + 1) * 512], W=[B_wr[sC]])
                wD = wring[sD][:].rearrange("p k f -> p (k f)").rearrange("p (c f) -> p c f", c=4)
                dma("pool", wD, wout_src[:, h * 4:(h + 1) * 4, :], W=[B_wr[sD]])
                dma("sp", rdec[:], rdec_d[h], W=[B_rdec])

                def kproj_gen():
                    for m in range(2):
                        for b in range(5):
                            s, n, isc = BLOCKS[b]
                            proj_rope(sA, 1, m, b, kr[:, m, s:s + n], [B_k[m][t] for t in range(s // 128, (s + n) // 128)])
                            yield

                def vproj_gen():
                    for t in range(NT):
                        b = min(t // 4, 4)
                        pi_ = 6 + (t % 2)
                        for k in range(KD):
                            op("pe", lambda e, k=k, t=t, pi_=pi_: e.matmul(
                                psb[pi_][:, :], lhsT=hT[:, k, t * 128:(t + 1) * 128], rhs=wring[sB][:, k, :],
                                start=(k == 0), stop=(k == KD - 1)),
                               R=[B_wr[sB], B_h[k][b]], W=[B_ps[pi_]], signal=(k == KD - 1))
                        op("act", lambda e, t=t, pi_=pi_: e.copy(out=vv[:, t, :], in_=psb[pi_][:, :]),
                           R=[B_ps[pi_]], W=[B_v[t]])
                        yield
                interleave(kproj_gen(), vproj_gen())
                if h == 0:
                    debug_dump("kr0", lambda o: [dma("sp", o[:, m, :], kr[:, m, :], R=B_k[m]) for m in range(2)])
                    debug_dump("vv0", lambda o: [dma("sp", o, vv[:], R=B_v)])
                    if P.stop_after == "proj0":
                        return

                def block_gen(b):
                    s, n, isc = BLOCKS[b]
                    c = 1 if isc else 0
                    qi = rcount["q"] % 2
                    rcount["q"] += 1
                    for m in range(2):
                        proj_rope(sA, 0, m, b, qr[:, qi, m, :n], [B_q[qi][m]])
                    yield
                    for cc in range(4):
                        pg = cc % 2
                        for k in range(KD):
                            op("pe", lambda e: e.matmul(
                                psb[pg][:, :n], lhsT=wring[sC][:, k, cc * 128:(cc + 1) * 128], rhs=hT[:, k, s:s + n],
                                start=(k == 0), stop=(k == KD - 1)),
                               R=[B_wr[sC], B_h[k][b]], W=[B_ps[pg]], signal=(k == KD - 1))
                        op("act", lambda e: e.activation(out=sgb[:, cc, :n], in_=psb[pg][:, :n], func=AF.Silu),
                           R=[B_ps[pg]], W=[B_sgb[cc]])
                    keys = [16, 17] if isc else list(range(NT))
                    pso = [psb[2 + cc] for cc in range(4)]
                    B_pso = [B_ps[2 + cc] for cc in range(4)]

                    def emit_scores(j, idx):
                        pi_ = 6 + (idx % 2)
                        for m in range(2):
                            op("pe", lambda e, m=m: e.matmul(
                                psb[pi_][:, :n], lhsT=kr[:, m, j * 128:(j + 1) * 128], rhs=qr[:, qi, m, :n],
                                start=(m == 0), stop=(m == 1)),
                               R=[B_k[m][j], B_q[qi][m]], W=[B_ps[pi_]], signal=(m == 1))
                        ai = rcount["at"] % 3
                        rcount["at"] += 1
                        pst = psb[pi_]
                        if isc:
                            tab = Dg(j - 16, n)
                            op("dve", lambda e: e.tensor_tensor(out=AT[ai][:, :n], in0=pst[:, :n], in1=tab, op=ALU.mult),
                               R=[B_ps[pi_], B_rdec], W=[B_AT[ai]])
                        elif j >= 16:
                            jc = j - 16
                            s1 = float(gf[h] ** (s + 256 - 128 * jc))
                            s2 = float(gb[h] ** (T_LAT - s - 511 + 128 * jc))
                            ci = jc
                            op("pool", lambda e: e.tensor_scalar(
                                out=ctab[ci][:], in0=Ff, scalar1=s1, scalar2=None, op0=ALU.mult),
                               R=[B_rdec], W=[B_ctab[ci]])
                            op("dve", lambda e: e.scalar_tensor_tensor(
                                out=ctab[ci][:], in0=Fb, scalar=s2, in1=ctab[ci][:], op0=ALU.mult, op1=ALU.add),
                               R=[B_rdec, B_ctab[ci]], W=[B_ctab[ci]])
                            op("dve", lambda e: e.tensor_tensor(
                                out=AT[ai][:, :n], in0=pst[:, :n], in1=ctab[ci][:, :n], op=ALU.mult),
                               R=[B_ps[pi_], B_ctab[ci]], W=[B_AT[ai]])
                        else:
                            rel = j - 4 * b
                            if 0 <= rel < 4:
                                tab = Dg(rel, 512)
                                op("dve", lambda e: e.tensor_tensor(out=AT[ai][:], in0=pst[:], in1=tab, op=ALU.mult),
                                   R=[B_ps[pi_], B_rdec], W=[B_AT[ai]])
                            elif rel < 0:
                                sc = float(gf[h] ** (s - 128 * j))
                                op("dve", lambda e: e.scalar_tensor_tensor(
                                    out=AT[ai][:], in0=pst[:], scalar=sc, in1=Ff, op0=ALU.mult, op1=ALU.mult),
                                   R=[B_ps[pi_], B_rdec], W=[B_AT[ai]])
                            else:
                                sc = float(gb[h] ** (128 * j - s - 511))
                                op("dve", lambda e: e.scalar_tensor_tensor(
                                    out=AT[ai][:], in0=pst[:], scalar=sc, in1=Fb, op0=ALU.mult, op1=ALU.mult),
                                   R=[B_ps[pi_], B_rdec], W=[B_AT[ai]])
                        return ai

                    def emit_av(j, ai, first, last):
                        for cc in range(4):
                            op("pe", lambda e, cc=cc: e.matmul(
                                pso[cc][:, :n], lhsT=vv[:, j, cc * 128:(cc + 1) * 128], rhs=AT[ai][:, :n],
                                start=first, stop=last),
                               R=[B_v[j], B_AT[ai]], W=[B_pso[cc]], signal=(cc == 3))

                    pend = None
                    for idx, j in enumerate(keys):
                        ai = emit_scores(j, idx)
                        if pend is not None:
                            emit_av(*pend)
                        pend = (j, ai, idx == 0, idx == len(keys) - 1)
                    emit_av(*pend)

                    yield
                    for cc in range(4):
                        q2 = cc % 2
                        op("act", lambda e, cc=cc, q2=q2: e.activation(out=sq[q2][:, :n], in_=pso[cc][:, :n],
                                                                      func=AF.Square),
                           R=[B_pso[cc]], W=[B_sq[q2]])
                        op("pe", lambda e, cc=cc, q2=q2: e.matmul(psb[6][:, :n], lhsT=ones_b[:], rhs=sq[q2][:, :n],
                                                                 start=(cc == 0), stop=(cc == 3)),
                           R=[B_sq[q2], B_const], W=[B_ps[6]], signal=True)
                    op("act", lambda e: e.activation(out=rstd[:, :n], in_=psb[6][:, :n], func=AF.Sqrt, bias=EPS,
                                                     scale=1.0 / 512), R=[B_ps[6]], W=[B_rstd])
                    op("dve", lambda e: e.reciprocal(out=rstd[:, :n], in_=rstd[:, :n]), R=[B_rstd], W=[B_rstd])
                    for cc in range(4):
                        q2 = cc % 2
                        op("dve", lambda e, cc=cc, q2=q2: e.tensor_tensor(
                            out=tmp[q2][:, :n], in0=pso[cc][:, :n], in1=rstd[:, :n], op=ALU.mult),
                           R=[B_pso[cc], B_rstd], W=[B_tmp[q2]])
                        op("dve", lambda e, cc=cc, q2=q2: e.tensor_tensor(
                            out=ogT[:, cc, :n], in0=tmp[q2][:, :n], in1=sgb[:, cc, :n], op=ALU.mult),
                           R=[B_tmp[q2], B_sgb[cc]], W=[B_og[cc]])
                    for mch in range(KD):
                        pi_ = 6 + (mch % 2)
                        for cc in range(4):
                            op("pe", lambda e, cc=cc, mch=mch, pi_=pi_: e.matmul(
                                psb[pi_][:, :n], lhsT=wD[:, cc, mch * 128:(mch + 1) * 128], rhs=ogT[:, cc, :n],
                                start=(cc == 0), stop=(cc == 3)),
                               R=[B_wr[sD], B_og[cc]], W=[B_ps[pi_]], signal=(cc == 3))
                        g_ap = modv(l, 2, mch, c)
                        op("dve", lambda e, mch=mch, pi_=pi_, g_ap=g_ap: e.scalar_tensor_tensor(
                            out=xT[:, mch, s:s + n], in0=psb[pi_][:, :n], scalar=g_ap, in1=xT[:, mch, s:s + n],
                            op0=ALU.mult, op1=ALU.add),
                           R=[B_ps[pi_], B_mod, B_x[mch][b]], W=[B_x[mch][b]])

                gens = [block_gen(b) for b in range(5)]
                next(gens[0])
                for b in range(5):
                    next(gens[b])
                    if b + 1 < 5:
                        next(gens[b + 1])
                    for _ in gens[b]:
                        pass
            S.barrier()

    iota_d2 = P.din("iota1", [128, 256])

    def moe_layer(l, with_ctx, n_exp_run=N_EXP):
        nblk = 5 if with_ctx else 4
        ntile = NT if with_ctx else 16
        NS = 288 if with_ctx else 256
        nst = 3 if with_ctx else 2
        st_sz = [128, 128, 32][:nst]
        st_off = [0, 128, 256][:nst]
        with ExitStack() as mes:
            h2tok = P.sb(mes, "h2tok", [128, NT, D], BF16)
            B_h2 = [Buf(f"h2t{t}") for t in range(NT)]
            wr_sb = P.sb(mes, "wr_sb", [128, KD, N_EXP], F32)
            B_wrt = Buf("wrt")
            dma("sp", wr_sb[:], router_d[l].rearrange("(k p) e -> p k e", p=128), W=[B_wrt])
            iota1 = P.sb(mes, "iota1_sb", [128, 256], F32)
            dma("sp", iota1[:], iota_d2, W=[B_wrt])
            aff = P.sb(mes, "aff", [128, NT, N_EXP], F32)
            B_aff = Buf("aff")
            aff_hl = P.sb(mes, "aff_hl", [128, NT, N_EXP, 2], BF16)
            posm_tok = P.sb(mes, "posm_tok", [128, NT, N_EXP], F32)
            B_posm = Buf("posm")

            with ExitStack() as r1:
                tmp = [P.sb(r1, f"mtmp{i}", [128, 512], F32) for i in range(2)]
                B_tmp = [Buf("mtmp0"), Buf("mtmp1")]
                rstd = P.sb(r1, "mrstd", [128, 512], F32)
                B_rstd = Buf("mrstd")
                sq = [P.sb(r1, f"msq{i}", [128, 512], BF16) for i in range(2)]
                B_sq = [Buf("msq0"), Buf("msq1")]
                h2f = P.sb(r1, "h2f", [128, KD, 512], F32)
                B_h2f = [Buf(f"h2f{k}") for k in range(KD)]
                h2b = P.sb(r1, "h2b", [128, KD, 512], BF16)
                B_h2b = [Buf(f"h2b{k}") for k in range(KD)]
                for b in range(nblk):
                    s, n, isc = BLOCKS[b]
                    norm_block(l, 1, b, lambda k, n=n: (h2f[:, k, :n], [B_h2f[k]]),
                               tmp, B_tmp, rstd, B_rstd, sq, B_sq, psn_i=7)
                    for k in range(KD):
                        eng_ = "act" if k % 2 == 0 else "dve"
                        if eng_ == "act":
                            op("act", lambda e, k=k: e.copy(out=h2b[:, k, :n], in_=h2f[:, k, :n]), R=[B_h2f[k]], W=[B_h2b[k]])
                        else:
                            op("dve", lambda e, k=k: e.tensor_copy(out=h2b[:, k, :n], in_=h2f[:, k, :n]),
                               R=[B_h2f[k]], W=[B_h2b[k]])
                    for tt in range(n // 128):
                        t = s // 128 + tt
                        for k in range(KD):
                            op("pe", lambda e, k=k: e.matmul(psb[6][:, 0:N_EXP], lhsT=h2f[:, k, tt * 128:(tt + 1) * 128],
                                                            rhs=wr_sb[:, k, :], start=(k == 0), stop=(k == KD - 1)),
                               R=[B_h2f[k], B_wrt], W=[B_ps[6]], signal=(k == KD - 1))
                        op("act", lambda e: e.copy(out=aff[:, t, :], in_=psb[6][:, 0:N_EXP]), R=[B_ps[6]], W=[B_aff])
                        pi_ = t % 2
                        pbf = psb[pi_][:].bitcast(BF16)
                        for k in range(KD):
                            op("pe", lambda e, k=k: e.transpose(out=pbf[:, k * 128:(k + 1) * 128],
                                                               in_=h2b[:, k, tt * 128:(tt + 1) * 128], identity=ident_b[:]),
                               R=[B_h2b[k], B_const], W=[B_ps[pi_]], signal=(k == KD - 1))
                        op("act", lambda e: e.copy(out=h2tok[:, t, :], in_=pbf), R=[B_ps[pi_]], W=[B_h2[t]])
                mx = P.sb(r1, "smx", [128, NT], F32)
                op("dve", lambda e: e.tensor_reduce(out=mx[:, :ntile], in_=aff[:, :ntile, :], axis=AX.X, op=ALU.max),
                   R=[B_aff], W=[B_rstd])
                op("dve", lambda e: e.tensor_tensor(out=aff[:, :ntile, :], in0=aff[:, :ntile, :],
                                                    in1=mx[:, :ntile].unsqueeze(2).to_broadcast([128, ntile, N_EXP]),
                                                    op=ALU.subtract), R=[B_rstd, B_aff], W=[B_aff])
                op("act", lambda e: e.activation(out=aff[:, :ntile, :], in_=aff[:, :ntile, :], func=AF.Exp),
                   R=[B_aff], W=[B_aff])
                op("dve", lambda e: e.tensor_reduce(out=mx[:, :ntile], in_=aff[:, :ntile, :], axis=AX.X, op=ALU.add),
                   R=[B_aff], W=[B_rstd])
                op("dve", lambda e: e.reciprocal(out=mx[:, :ntile], in_=mx[:, :ntile]), R=[B_rstd], W=[B_rstd])
                op("dve", lambda e: e.tensor_tensor(out=aff[:, :ntile, :], in0=aff[:, :ntile, :],
                                                    in1=mx[:, :ntile].unsqueeze(2).to_broadcast([128, ntile, N_EXP]),
                                                    op=ALU.mult), R=[B_rstd, B_aff], W=[B_aff])
                op("dve", lambda e: e.tensor_copy(out=aff_hl[:, :ntile, :, 0], in_=aff[:, :ntile, :]), R=[B_aff], W=[B_posm])
                op("dve", lambda e: e.tensor_tensor(out=aff_hl[:, :ntile, :, 1], in0=aff[:, :ntile, :],
                                                    in1=aff_hl[:, :ntile, :, 0], op=ALU.subtract),
                   R=[B_aff, B_posm], W=[B_posm])
                S.barrier()
            debug_dump(f"aff{l}", lambda o: [dma("sp", o, aff[:], R=[B_aff])])
            debug_dump(f"h2tok{l}", lambda o: [dma("sp", o, h2tok[:], R=B_h2)])

            with ExitStack() as r2:
                affT = P.sb(r2, "affT", [16, T_ALL], F32)
                mk = P.sb(r2, "mkT", [16, T_ALL], F32)
                cum = P.sb(r2, "cumT", [16, T_ALL], F32)
                sm = P.sb(r2, "bis", [16, 16], F32)
                B_affT, B_mk, B_cum, B_sm = Buf("affT"), Buf("mk"), Buf("cum"), Buf("sm")
                for t in range(ntile):
                    pi_ = t % 2
                    op("pe", lambda e: e.transpose(out=psb[pi_][0:16, 0:128], in_=aff[:, t, :], identity=ident_f[:]),
                       R=[B_aff, B_const], W=[B_ps[pi_]])
                    op("act", lambda e: e.copy(out=affT[:, t * 128:(t + 1) * 128], in_=psb[pi_][0:16, 0:128]),
                       R=[B_ps[pi_]], W=[B_affT])
                sets = [(0, T_LAT, 256)] + ([(T_LAT, T_CTX, 32)] if with_ctx else [])
                one = sm[:, 15:16]
                op("dve", lambda e: e.memset(one, 1.0), W=[B_sm])
                B_smx = [Buf("smA"), Buf("smB")]

                def bisect(si, s0, ns, cap):
                    lo, mid, gs = [sm[:, si * 6 + i: si * 6 + i + 1] for i in range(3)]
                    cnts = [sm[:, si * 6 + 3 + i: si * 6 + 4 + i] for i in range(2)]
                    Bs = B_smx[si]
                    a_ap = affT[:, s0:s0 + ns]
                    op("dve", lambda e: e.memset(lo, 0.0), W=[Bs])
                    op("dve", lambda e: e.memset(mid, 0.5), W=[Bs])
                    NIT = 30
                    for it in range(NIT):
                        step = 0.5 ** (it + 1)
                        cnt = cnts[it % 2]
                        op("dve", lambda e: e.tensor_scalar(out=mk[:, s0:s0 + ns], in0=a_ap, scalar1=mid, scalar2=0.0,
                                                            op0=ALU.is_ge, op1=ALU.add, accum_out=cnt),
                           R=[B_affT, Bs], W=[B_mk, Bs])
                        yield
                        op("dve", lambda e: e.tensor_scalar(out=gs, in0=cnt, scalar1=float(cap), scalar2=step,
                                                            op0=ALU.is_ge, op1=ALU.mult), R=[Bs], W=[Bs])
                        yield
                        if it < NIT - 1:
                            op("dve", lambda e: e.scalar_tensor_tensor(out=mid, in0=gs, scalar=step * 0.5, in1=lo,
                                                                       op0=ALU.add, op1=ALU.add), R=[Bs], W=[Bs])
                            yield
                        op("dve", lambda e: e.tensor_tensor(out=lo, in0=lo, in1=gs, op=ALU.add), R=[Bs], W=[Bs])
                        yield
                    op("dve", lambda e: e.tensor_scalar(out=mk[:, s0:s0 + ns], in0=a_ap, scalar1=lo, scalar2=None,
                                                        op0=ALU.is_ge), R=[B_affT, Bs], W=[B_mk])
                    yield
                    op("dve", lambda e: e.tensor_tensor_scan(out=cum[:, s0:s0 + ns],
                                                             data0=one.to_broadcast([16, ns]), data1=mk[:, s0:s0 + ns],
                                                             initial=0.0, op0=ALU.mult, op1=ALU.add),
                       R=[B_mk, B_sm], W=[B_cum])
                    yield
                    op("dve", lambda e: e.tensor_tensor(out=cum[:, s0:s0 + ns], in0=cum[:, s0:s0 + ns],
                                                        in1=mk[:, s0:s0 + ns], op=ALU.mult), R=[B_mk, B_cum], W=[B_cum])
                    yield

                interleave(*[bisect(si, s0, ns, cap) for si, (s0, ns, cap) in enumerate(sets)])
                for t in range(ntile):
                    pi_ = t % 2
                    op("pe", lambda e: e.transpose(out=psb[pi_][:, 0:16], in_=cum[:, t * 128:(t + 1) * 128],
                                                   identity=ident_f[0:16, 0:16]),
                       R=[B_cum, B_const], W=[B_ps[pi_]])
                    op("act", lambda e: e.copy(out=posm_tok[:, t, :], in_=psb[pi_][:, 0:16]), R=[B_ps[pi_]], W=[B_posm])
                S.barrier()
            debug_dump(f"posm{l}", lambda o: [dma("sp", o, posm_tok[:], R=[B_posm])])
            if P.stop_after == f"route{l}":
                return

            with ExitStack() as xs:
                Pm = P.sb(xs, "Pm", [128, NT, NS], BF16)
                B_Pm = Buf("Pm")
                PTl = P.sb(xs, "PTl", [128, 2, T_LAT], BF16)
                PTc = P.sb(xs, "PTc", [128, T_CTX], BF16)
                B_PT = Buf("PT")

                def PTv(st, a, b2, sz):
                    return PTl[0:sz, st, a:b2] if st < 2 else PTc[0:sz, a - T_LAT:b2 - T_LAT]
                xeT = P.sb(xs, "xeT", [128, KD, NS], BF16)
                B_xe = [Buf(f"xe{k}") for k in range(KD)]
                hmid = P.sb(xs, "hmid", [128, 2, NS], BF16)
                B_hm = [Buf("hm0"), Buf("hm1")]
                sil = P.sb(xs, "sil", [128, 2, NS], BF16)
                B_sil = [Buf("sil0"), Buf("sil1")]
                y_sb = P.sb(xs, "y_sb", [128, nst, D], BF16)
                B_y = [Buf(f"y{i}") for i in range(nst)]
                gsl = P.sb(xs, "gsl", [128, 4], F32)
                B_gsl = Buf("gsl")
                NGU = 5
                NDN = 5 if with_ctx else 6
                wgu = wring + [P.sb(xs, f"wgu{i}", [128, KD, 512], BF16) for i in range(NGU - NSLOT)]
                B_gu = [Buf(f"gu{i}") for i in range(NGU)]
                wdn = [P.sb(xs, f"wdn{i}", [128, 2, D], BF16) for i in range(NDN)]
                B_dn = [Buf(f"dn{i}") for i in range(NDN)]
                op("dve", lambda e: e.memset(Pm[:], 0.0), W=[B_Pm])

                NFB = NFC // 2
                sched = [(e_, fb) for e_ in range(n_exp_run) for fb in range(NFB)]
                issued = {"n": 0}

                def issue_weights(upto):
                    while issued["n"] < min(upto, len(sched)):
                        i = issued["n"]
                        e_, fb = sched[i]
                        if f"wgu_{l}_{e_}" not in P.dram:
                            P.din(f"wgu_{l}_{e_}", [NFC // 2, 128, KD, 512])
                            P.din(f"wdt_{l}_{e_}", [NFC // 2, 128, 2, D])
                        wgu_ap, wd_ap = P.dram[f"wgu_{l}_{e_}"], P.dram[f"wdt_{l}_{e_}"]
                        gi, di = i % NGU, i % NDN
                        dma("pool", wgu[gi][:], wgu_ap[fb], W=[B_gu[gi]])
                        dma("pool", wdn[di][:], wd_ap[fb], W=[B_dn[di]])
                        issued["n"] += 1

                LA = 4 if with_ctx else 5
                issue_weights(LA)
                blk_i = 0
                deferred = {"scatter": None}
                xb = (6, 7) if with_ctx else (4, 5)
                for e_ in range(n_exp_run):
                    op("dve", lambda e: e.tensor_tensor(
                        out=Pm[:, 0:16, 0:256], in0=iota1[:, :].unsqueeze(1).to_broadcast([128, 16, 256]),
                        in1=posm_tok[:, 0:16, e_:e_ + 1].to_broadcast([128, 16, 256]), op=ALU.is_equal),
                       R=[B_posm, B_wrt], W=[B_Pm])
                    if with_ctx:
                        op("dve", lambda e: e.tensor_tensor(
                            out=Pm[:, 16:18, 256:288], in0=iota1[:, 0:32].unsqueeze(1).to_broadcast([128, 2, 32]),
                            in1=posm_tok[:, 16:18, e_:e_ + 1].to_broadcast([128, 2, 32]), op=ALU.is_equal),
                           R=[B_posm, B_wrt], W=[B_Pm])
                    for st in range(nst):
                        tiles = range(16) if st < 2 else range(16, 18)
                        tl = list(tiles)
                        for ti, t in enumerate(tl):
                            op("pe", lambda e: e.matmul(psb[6][0:st_sz[st], st * 2:st * 2 + 2],
                                                        lhsT=Pm[:, t, st_off[st]:st_off[st] + st_sz[st]],
                                                        rhs=aff_hl[:, t, e_, :], start=(ti == 0), stop=(ti == len(tl) - 1)),
                               R=[B_Pm, B_posm], W=[B_ps[6]], signal=(ti == len(tl) - 1))
                    for st in range(nst):
                        op("dve", lambda e: e.tensor_reduce(out=gsl[0:st_sz[st], st:st + 1],
                                                            in_=psb[6][0:st_sz[st], st * 2:st * 2 + 2], axis=AX.X, op=ALU.add),
                           R=[B_ps[6]], W=[B_gsl])
                    def do_PT():
                        gcnt = 0
                        for st in range(nst):
                            tl = list(range(16)) if st < 2 else [16, 17]
                            for g0 in range(0, len(tl), 8):
                                grp = tl[g0:g0 + 8]
                                pi_ = xb[gcnt % 2]
                                gcnt += 1
                                pbf = psb[pi_][:].bitcast(BF16)
                                for gi_, t in enumerate(grp):
                                    op("pe", lambda e: e.transpose(
                                        out=pbf[0:st_sz[st], gi_ * 128:(gi_ + 1) * 128],
                                        in_=Pm[:, t, st_off[st]:st_off[st] + st_sz[st]], identity=ident_b[:]),
                                       R=[B_Pm, B_const], W=[B_ps[pi_]], signal=(gi_ == len(grp) - 1))
                                op("act", lambda e: e.copy(out=PTv(st, grp[0] * 128, (grp[-1] + 1) * 128, st_sz[st]),
                                                           in_=pbf[0:st_sz[st], 0:len(grp) * 128]),
                                   R=[B_ps[pi_]], W=[B_PT])
                    for k in range(KD):
                        pi_ = 6 + (k % 2)
                        for t in range(ntile):
                            op("pe", lambda e: e.matmul(psb[pi_][:, 0:NS], lhsT=h2tok[:, t, k * 128:(k + 1) * 128],
                                                        rhs=Pm[:, t, :], start=(t == 0), stop=(t == ntile - 1)),
                               R=[B_h2[t], B_Pm], W=[B_ps[pi_]], signal=(t == ntile - 1))
                        op("act", lambda e: e.copy(out=xeT[:, k, :], in_=psb[pi_][:, 0:NS]), R=[B_ps[pi_]], W=[B_xe[k]])
                    psY = [[psb[st * 2 + nh] for nh in range(2)] for st in range(nst)]
                    B_psY = [[B_ps[st * 2 + nh] for nh in range(2)] for st in range(nst)]

                    def emit_down(fc, hb, di, first, last):
                        for st in range(nst):
                            for nh in range(2):
                                op("pe", lambda e: e.matmul(
                                    psY[st][nh][0:st_sz[st], :], lhsT=hmid[:, hb, st_off[st]:st_off[st] + st_sz[st]],
                                    rhs=wdn[di][:, fc % 2, nh * 512:(nh + 1) * 512], start=first, stop=last),
                                   R=[B_hm[hb], B_dn[di]], W=[B_psY[st][nh]], signal=(st == nst - 1 and nh == 1))

                    pend = None
                    for fb in range(NFB):
                        i = blk_i
                        blk_i += 1
                        issue_weights(i + LA)
                        if fb == 3:
                            if deferred["scatter"] is not None:
                                deferred["scatter"]()
                                deferred["scatter"] = None
                            do_PT()
                        gi, di = i % NGU, i % NDN
                        for f2 in range(2):
                            fc = fb * 2 + f2
                            hb = fc % 2
                            ia, iu = 6, 7
                            pa, pu = psb[ia], psb[iu]
                            for k in range(KD):
                                op("pe", lambda e: e.matmul(pa[:, 0:NS], lhsT=wgu[gi][:, k, f2 * 128:(f2 + 1) * 128],
                                                            rhs=xeT[:, k, :], start=(k == 0), stop=(k == KD - 1)),
                                   R=[B_gu[gi], B_xe[k]], W=[B_ps[ia]], signal=(k == KD - 1))
                            for k in range(KD):
                                op("pe", lambda e: e.matmul(pu[:, 0:NS],
                                                            lhsT=wgu[gi][:, k, 256 + f2 * 128:256 + (f2 + 1) * 128],
                                                            rhs=xeT[:, k, :], start=(k == 0), stop=(k == KD - 1)),
                                   R=[B_gu[gi], B_xe[k]], W=[B_ps[iu]], signal=(k == KD - 1))
                            op("act", lambda e: e.activation(out=sil[:, hb, :], in_=pa[:, 0:NS], func=AF.Silu),
                               R=[B_ps[ia]], W=[B_sil[hb]])
                            op("dve", lambda e: e.tensor_tensor(out=hmid[:, hb, :], in0=pu[:, 0:NS], in1=sil[:, hb, :],
                                                                op=ALU.mult), R=[B_ps[iu], B_sil[hb]], W=[B_hm[hb]])
                            if pend is not None:
                                emit_down(*pend)
                            pend = (fc, hb, di, fc == 0, fc == NFC - 1)
                    emit_down(*pend)
                    for st in range(nst):
                        for nh in range(2):
                            op("act", lambda e: e.activation(out=y_sb[0:st_sz[st], st, nh * 512:(nh + 1) * 512],
                                                             in_=psY[st][nh][0:st_sz[st], :], func=AF.Copy,
                                                             scale=gsl[0:st_sz[st], st:st + 1]),
                               R=[B_psY[st][nh], B_gsl], W=[B_y[st]])
                    def do_scatter():
                        gi_s = 0
                        for b in range(nblk):
                            s, n, isc = BLOCKS[b]
                            c = 1 if isc else 0
                            sts = [2] if isc else [0, 1]
                            for mch in range(KD):
                                pi_ = xb[gi_s % 2]
                                gi_s += 1
                                for si, st in enumerate(sts):
                                    op("pe", lambda e: e.matmul(psb[pi_][:, :n],
                                                                lhsT=y_sb[0:st_sz[st], st, mch * 128:(mch + 1) * 128],
                                                                rhs=PTv(st, s, s + n, st_sz[st]), start=(si == 0),
                                                                stop=(si == len(sts) - 1)),
                                       R=[B_y[st], B_PT], W=[B_ps[pi_]], signal=(si == len(sts) - 1))
                                g_ap = modv(l, 5, mch, c)
                                op("dve", lambda e: e.scalar_tensor_tensor(
                                    out=xT[:, mch, s:s + n], in0=psb[pi_][:, :n], scalar=g_ap, in1=xT[:, mch, s:s + n],
                                    op0=ALU.mult, op1=ALU.add),
                                   R=[B_ps[pi_], B_mod, B_x[mch][b]], W=[B_x[mch][b]])
                    deferred["scatter"] = do_scatter
                if deferred["scatter"] is not None:
                    deferred["scatter"]()
                S.barrier()

    def hgrn2_layer(l):
        hgin_d = P.din("hg_w_in", [D, 5120])
        hgout_d = P.din("hg_w_out", [D, D])
        hgmisc_d = P.din("hg_misc", [128, 1 + 4 * KD])
        hgmask_d = P.din("hgmask", [128, 256])
        PT_ = 256
        NPART = 9
        NCH = 2
        with ExitStack() as les:
            hT = P.sb(les, "hT1", [128, KD, T_ALL], BF16)
            B_h = [[Buf(f"h1{k}_{b}") for b in range(5)] for k in range(KD)]
            B_hT = Buf("hT1all")
            hgm = P.sb(les, "hgm", [128, 1 + 4 * KD], F32)
            msk = P.sb(les, "hgmask_sb", [128, 256], F32)
            lbs = P.sb(les, "lbs", [128, 2, 2, HG_H], F32)
            lbs2 = P.sb(les, "lbs2", [128, 2, 2, HG_H], F32)
            gnh = P.sb(les, "gnh", [128, 1], F32)
            B_hc = Buf("hgconst")
            dma("sp", hgm[:], hgmisc_d, W=[B_hc])
            dma("sp", msk[:], hgmask_d, W=[B_hc])
            AA = P.sb(les, "hgAA", [128, 14 * PT_], F32)
            RG = [AA[:, r * PT_:(r + 1) * PT_] for r in range(14)]
            B_R = [Buf(f"hgR{r}") for r in range(14)]

            def Aregs(d_, par):
                idx = [d_ * 2 + par, 4 + d_ * 2 + par, 8 + d_, 10 + d_, 12 + d_]
                return [RG[i] for i in idx], [B_R[i] for i in idx]
            tmp = [AA[:, 0:512], AA[:, 768:1280]]
            B_tmp = [[B_R[0], B_R[1]], [B_R[3], B_R[4]]]
            rstds = [AA[:, 1536:2048], AA[:, 2304:2816]]
            B_rstds = [[B_R[6], B_R[7]], [B_R[9], B_R[10]]]
            rstd, B_rstd = rstds[0], B_rstds[0]
            sq = [P.sb(les, f"hsq{i}", [128, 512], BF16) for i in range(2)]
            B_sq = [Buf("hsq0"), Buf("hsq1")]

            class _V:
                def __init__(self, ap):
                    self.ap = ap

                def __getitem__(self, key):
                    return self.ap[key]
            for b in range(5):
                s, n, _ = BLOCKS[b]
                norm_block(l, 0, b, lambda k, b=b, s=s, n=n: (hT[:, k, s:s + n], [B_h[k][b]]),
                           [_V(tmp[0]), _V(tmp[1])], B_tmp, _V(rstd), B_rstd, sq, B_sq, psn_i=7)
            for d_ in range(2):
                op("dve", lambda e: e.tensor_tensor(out=lbs[:, 0, d_, :], in0=hgm[:, 1 + (2 + d_) * 8:1 + (3 + d_) * 8],
                                                    in1=hgm[:, 1 + d_ * 8:1 + (d_ + 1) * 8], op=ALU.subtract),
                   R=[B_hc], W=[B_hc])
            op("act", lambda e: e.activation(out=lbs[:, 0, :, :], in_=lbs[:, 0, :, :], func=AF.Sigmoid), R=[B_hc], W=[B_hc])
            op("dve", lambda e: e.tensor_scalar(out=lbs[:, 1, :, :], in0=lbs[:, 0, :, :], scalar1=-1.0, scalar2=1.0,
                                                op0=ALU.mult, op1=ALU.add), R=[B_hc], W=[B_hc])
            op("dve", lambda e: e.tensor_scalar(out=lbs2[:, 0, :, :], in0=lbs[:, 1, :, :], scalar1=0.5, scalar2=None,
                                                op0=ALU.mult), R=[B_hc], W=[B_hc])
            op("dve", lambda e: e.tensor_tensor(out=lbs2[:, 1, :, :], in0=lbs2[:, 0, :, :], in1=lbs[:, 0, :, :], op=ALU.add),
               R=[B_hc], W=[B_hc])
            op("dve", lambda e: e.tensor_scalar(out=gnh[:], in0=hgm[:, 0:1], scalar1=0.5, scalar2=None, op0=ALU.mult),
               R=[B_hc], W=[B_hc])
            S.barrier()
            debug_dump("hT1", lambda o: [dma("sp", o.rearrange("(k p) t -> p k t", p=128)[:, k, :], hT[:, k, :],
                                             R=[B_hT]) for k in range(KD)])

            qs2 = P.sb(les, "hqs", [128, 2, PT_], BF16)
            B_qs2 = [Buf("hqs0"), Buf("hqs1")]
            arr = [[P.sb(les, f"harr{d_}{i}", [128, T_ALL], BF16) for i in range(3)] for d_ in range(2)]
            B_arr = [[Buf(f"harr{d_}{i}") for i in range(3)] for d_ in range(2)]
            sgT, B_sg = arr[0][2], B_arr[0][2]
            ogT, B_og = arr[1][2], B_arr[1][2]
            B_ogb = [[B_og, Buf(f"ogb{b}")] for b in range(4)]
            vv = P.sb(les, "hvv", [128, NT, 128], BF16)
            B_v = Buf("hvv")
            EF = P.sb(les, "hEF", [128, 2, NT], F32)
            EM = P.sb(les, "hEM", [128, 2, NT], F32)
            t6b = P.sb(les, "ht6", [128, 2, 8], F32)
            B_t6 = [Buf("t6a"), Buf("t6b")]
            B_E = Buf("hE")
            Sst = P.sb(les, "hS", [128, 2, 128], F32)
            B_S = [Buf("hS0"), Buf("hS1")]
            Suse = P.sb(les, "hSuse", [128, 2, 16, 128], BF16)
            B_Su = Buf("hSuse")
            kht = [P.sb(les, f"hkht{i}", [128, 128], BF16) for i in range(2)]
            B_kht = [Buf("kht0"), Buf("kht1")]
            ATs = [P.sb(les, f"hAT{i}", [128, 128], BF16) for i in range(8)]
            B_ATs = [Buf(f"hAT{i}") for i in range(8)]
            one_col = hgm[:, 0:1]
            ones_f = P.sb(les, "hones", [128, 1], F32)
            op("dve", lambda e: e.memset(ones_f[:], 1.0), W=[B_hc])
            eps_col = P.sb(les, "heps", [128, 1], F32)
            op("dve", lambda e: e.memset(eps_col[:], EPS), W=[B_hc])

            win_src = hgin_d.rearrange("(k p) f -> p k f", p=128)
            cnt_ = {"ps": 0, "kht": 0, "at": 0}

            def view3(ap):
                return ap.rearrange("p (c i) -> p c i", i=128)

            for h in range(HG_H):
                sX, sY = wslot(), wslot()
                for gi_, off in enumerate((0, 1024, 2048, 3072)):
                    dma("pool", wring[sX][:, :, gi_ * 128:(gi_ + 1) * 128], win_src[:, :, off + h * 128: off + (h + 1) * 128],
                        W=[B_wr[sX]])
                yflat = wring[sY][:].rearrange("p k f -> p (k f)")
                wG = yflat[:, 0:1024].rearrange("p (k f) -> p k f", k=KD)
                wO = yflat[:, 1024:2048]
                dma("pool", wG, win_src[:, :, 4096 + h * 128:4096 + (h + 1) * 128], W=[B_wr[sY]])
                dma("pool", wO, hgout_d[h * 128:(h + 1) * 128, :], W=[B_wr[sY]])

                def proj(gi_, s, n, pi_):
                    for k in range(KD):
                        op("pe", lambda e: e.matmul(psb[pi_][:, :n], lhsT=wring[sX][:, k, gi_ * 128:(gi_ + 1) * 128],
                                                    rhs=hT[:, k, s:s + n], start=(k == 0), stop=(k == KD - 1)),
                           R=[B_wr[sX], B_hT], W=[B_ps[pi_]], signal=(k == KD - 1))

                def stageA(part):
                    p0 = part * PT_
                    c0 = part * NCH
                    par = part % 2
                    qs = qs2[:, par, :]
                    B_qs = B_qs2[par]
                    pi_ = cnt_["ps"] % 2
                    cnt_["ps"] += 1
                    proj(0, p0, PT_, pi_)
                    op("act", lambda e: e.activation(out=qs, in_=psb[pi_][:, :PT_], func=AF.Tanh, scale=0.5),
                       R=[B_ps[pi_]], W=[B_qs])
                    yield
                    op("dve", lambda e: e.scalar_tensor_tensor(out=qs, in0=qs, scalar=1.0, in1=psb[pi_][:, :PT_],
                                                               op0=ALU.add, op1=ALU.mult), R=[B_qs, B_ps[pi_]], W=[B_qs])
                    yield
                    for tt in range(NCH):
                        t = c0 + tt
                        pi_ = cnt_["ps"] % 2
                        cnt_["ps"] += 1
                        for k in range(KD):
                            op("pe", lambda e: e.matmul(psb[pi_][:, 0:128], lhsT=hT[:, k, t * 128:(t + 1) * 128],
                                                        rhs=wring[sX][:, k, 384:512], start=(k == 0), stop=(k == KD - 1)),
                               R=[B_wr[sX], B_hT], W=[B_ps[pi_]], signal=(k == KD - 1))
                        op("dve", lambda e: e.tensor_copy(out=vv[:, t, :], in_=psb[pi_][:, 0:128]), R=[B_ps[pi_]], W=[B_v])
                        yield
                    regs = [Aregs(d_, par) for d_ in range(2)]
                    pis = []
                    for d_ in range(2):
                        pi_ = cnt_["ps"] % 2
                        cnt_["ps"] += 1
                        pis.append(pi_)
                        proj(1 + d_, p0, PT_, pi_)
                    for d_ in range(2):
                        (A1, A2, A3, A4, A5), BA = regs[d_]
                        op("act", lambda e: e.activation(out=A2, in_=psb[pis[d_]][:, :PT_], func=AF.Tanh, scale=0.5),
                           R=[B_ps[pis[d_]]], W=[BA[1]])
                        yield
                    for d_ in range(2):
                        (A1, A2, A3, A4, A5), BA = regs[d_]
                        op("dve", lambda e: e.tensor_scalar(out=A1, in0=A2, scalar1=-0.5, scalar2=0.5, op0=ALU.mult,
                                                            op1=ALU.add), R=[BA[1]], W=[BA[0]])
                        yield
                    for d_ in range(2):
                        (A1, A2, A3, A4, A5), BA = regs[d_]
                        op("act", lambda e: e.activation(out=A2, in_=A2, func=AF.Ln, bias=lbs2[:, 1, d_, h:h + 1],
                                                         scale=lbs2[:, 0, d_, h:h + 1]), R=[BA[1], B_hc], W=[BA[1]])
                        yield

                def stageB_driver(part):
                    p0 = part * PT_
                    c0 = part * NCH
                    par = part % 2
                    qs = qs2[:, par, :]
                    B_qs = B_qs2[par]
                    def chain(d_):
                        si_ = d_
                        (A1, A2, A3, A4, A5), BA = Aregs(d_, par)
                        t6 = t6b[:, si_, :]
                        B_T = B_t6[si_]
                        oml_ap = lbs[:, 1, d_, h:h + 1]
                        op("dve", lambda e: e.tensor_tensor_scan(out=A3, data0=ones_f[:, 0:1].to_broadcast([128, PT_]),
                                                                 data1=A2, initial=0.0, op0=ALU.mult, op1=ALU.add),
                           R=[BA[1], B_hc], W=[BA[2]])
                        G3, g3 = view3(A3), view3(A2)
                        dst = [arr[d_][i][:, p0:p0 + PT_] for i in range(3)]
                        Bd = B_arr[d_]
                        bc = [128, NCH, 128]
                        if d_ == 0:
                            yield
                            op("dve", lambda e: e.tensor_tensor(out=view3(A4), in0=G3, in1=G3[:, :, 63:64].to_broadcast(bc),
                                                                op=ALU.subtract), R=[BA[2]], W=[BA[3]])
                            yield
                            op("act", lambda e: e.activation(out=A5, in_=A4, func=AF.Exp, bias=-0.6931471805599453), R=[BA[3]], W=[BA[4]])
                            yield
                            op("pool", lambda e: e.tensor_tensor(out=dst[0], in0=qs, in1=A5, op=ALU.mult),
                               R=[B_qs, BA[4]], W=[Bd[0]])
                            yield
                            op("dve", lambda e: e.tensor_tensor(out=t6[:, 0:NCH], in0=G3[:, :, 0], in1=g3[:, :, 0], op=ALU.subtract),
                               R=[BA[2], BA[1]], W=[B_T])
                            yield
                            op("dve", lambda e: e.tensor_tensor(out=EF[:, 0, c0:c0 + NCH], in0=G3[:, :, 127], in1=t6[:, 0:NCH],
                                                                op=ALU.subtract), R=[BA[2], B_T], W=[B_E])
                            yield
                            op("dve", lambda e: e.tensor_tensor(out=EM[:, 0, c0:c0 + NCH], in0=G3[:, :, 63], in1=t6[:, 0:NCH],
                                                                op=ALU.subtract), R=[BA[2], B_T], W=[B_E])
                            yield
                            op("act", lambda e: e.activation(out=A2, in_=A4, func=AF.Exp, scale=-1.0), R=[BA[3]], W=[BA[1]])
                            yield
                            op("dve", lambda e: e.scalar_tensor_tensor(out=dst[1], in0=A1, scalar=oml_ap, in1=A2, op0=ALU.mult, op1=ALU.mult),
                               R=[BA[0], BA[1]], W=[Bd[1]])
                            yield
                            op("dve", lambda e: e.tensor_tensor(out=view3(A4), in0=G3, in1=G3[:, :, 127:128].to_broadcast(bc),
                                                                op=ALU.subtract), R=[BA[2]], W=[BA[3]])
                            yield
                            op("act", lambda e: e.activation(out=A5, in_=A4, func=AF.Exp, scale=-1.0), R=[BA[3]], W=[BA[4]])
                            yield
                            op("dve", lambda e: e.scalar_tensor_tensor(out=dst[2], in0=A1, scalar=oml_ap, in1=A5, op0=ALU.mult, op1=ALU.mult),
                               R=[BA[0], BA[4]], W=[Bd[2]])
                        else:
                            yield
                            op("dve", lambda e: e.tensor_copy(out=t6[:, 0:NCH], in_=G3[:, :, 127]), R=[BA[2]], W=[B_T])
                            yield
                            op("pool", lambda e: e.tensor_tensor(out=A2, in0=A3, in1=A2, op=ALU.subtract),
                               R=[BA[2], BA[1]], W=[BA[1]])
                            H3 = view3(A2)
                            yield
                            op("dve", lambda e: e.tensor_tensor(out=view3(A4), in0=H3, in1=H3[:, :, 64:65].to_broadcast(bc),
                                                                op=ALU.subtract), R=[BA[1]], W=[BA[3]])
                            yield
                            op("act", lambda e: e.activation(out=A5, in_=A4, func=AF.Exp, scale=-1.0, bias=-0.6931471805599453), R=[BA[3]], W=[BA[4]])
                            yield
                            op("pool", lambda e: e.tensor_tensor(out=dst[0], in0=qs, in1=A5, op=ALU.mult),
                               R=[B_qs, BA[4]], W=[Bd[0]])
                            yield
                            op("act", lambda e: e.activation(out=A3, in_=A4, func=AF.Exp), R=[BA[3]], W=[BA[2]])
                            yield
                            op("dve", lambda e: e.scalar_tensor_tensor(out=dst[1], in0=A1, scalar=oml_ap, in1=A3, op0=ALU.mult, op1=ALU.mult),
                               R=[BA[0], BA[2]], W=[Bd[1]])
                            yield
                            op("dve", lambda e: e.tensor_tensor(out=EF[:, 1, c0:c0 + NCH], in0=t6[:, 0:NCH], in1=H3[:, :, 0],
                                                                op=ALU.subtract), R=[BA[1], B_T], W=[B_E])
                            yield
                            op("dve", lambda e: e.tensor_tensor(out=EM[:, 1, c0:c0 + NCH], in0=t6[:, 0:NCH], in1=H3[:, :, 64],
                                                                op=ALU.subtract), R=[BA[1], B_T], W=[B_E])
                            yield
                            op("dve", lambda e: e.tensor_tensor(out=view3(A4), in0=H3, in1=H3[:, :, 0:1].to_broadcast(bc),
                                                                op=ALU.subtract), R=[BA[1]], W=[BA[3]])
                            yield
                            op("act", lambda e: e.activation(out=A5, in_=A4, func=AF.Exp), R=[BA[3]], W=[BA[4]])
                            yield
                            op("dve", lambda e: e.scalar_tensor_tensor(out=dst[2], in0=A1, scalar=oml_ap, in1=A5, op0=ALU.mult, op1=ALU.mult),
                               R=[BA[0], BA[4]], W=[Bd[2]])
                        yield
                    return [chain(0), chain(1)]

                for _ in stageA(0):
                    pass
                for part in range(NPART):
                    gens = stageB_driver(part)
                    if part + 1 < NPART:
                        gens.append(stageA(part + 1))
                    interleave(*gens)
                op("act", lambda e: e.activation(out=EF[:], in_=EF[:], func=AF.Exp), R=[B_E], W=[B_E])
                op("act", lambda e: e.activation(out=EM[:], in_=EM[:], func=AF.Exp), R=[B_E], W=[B_E])

                orders = [[16, 17] + list(range(16)), [17, 16] + list(range(15, -1, -1))]
                for d_ in range(2):
                    op("dve", lambda e: e.memset(Sst[:, d_, :], 0.0), W=[B_S[d_]])
                items = [(step, d_) for step in range(NT - 1) for d_ in range(2)]

                def emit_T(i):
                    step, d_ = items[i]
                    c = orders[d_][step]
                    ki = i % 2
                    pq = 4 + (i % 2)
                    pbf = psb[pq][:].bitcast(BF16)
                    op("pe", lambda e: e.transpose(out=pbf[:, 0:128], in_=arr[d_][2][:, c * 128:(c + 1) * 128],
                                                   identity=ident_b[:]), R=[B_arr[d_][2], B_const], W=[B_ps[pq]])
                    op("act", lambda e: e.copy(out=kht[ki][:], in_=pbf[:, 0:128]), R=[B_ps[pq]], W=[B_kht[ki]])

                def emit_suse(step, d_):
                    c = orders[d_][step]
                    if c < 16:
                        op("act", lambda e: e.activation(out=Suse[:, d_, c, :], in_=Sst[:, d_, :], func=AF.Copy,
                                                         scale=EM[:, d_, c:c + 1]), R=[B_S[d_], B_E], W=[B_Su])

                emit_T(0)
                for i, (step, d_) in enumerate(items):
                    c = orders[d_][step]
                    if i + 1 < len(items):
                        emit_T(i + 1)
                    emit_suse(step, d_)
                    ki = i % 2
                    pd = 2 + (i % 2)
                    op("pe", lambda e: e.matmul(psb[pd][:, 0:128], lhsT=kht[ki][:], rhs=vv[:, c, :], start=True, stop=True),
                       R=[B_kht[ki], B_v], W=[B_ps[pd]])
                    op("dve", lambda e: e.scalar_tensor_tensor(out=Sst[:, d_, :], in0=Sst[:, d_, :], scalar=EF[:, d_, c:c + 1],
                                                               in1=psb[pd][:, 0:128], op0=ALU.mult, op1=ALU.add),
                       R=[B_S[d_], B_E, B_ps[pd]], W=[B_S[d_]])
                for d_ in range(2):
                    emit_suse(NT - 1, d_)

                for b in range(4):
                    s, n, _ = BLOCKS[b]
                    pi_ = b % 2
                    for k in range(KD):
                        op("pe", lambda e: e.matmul(psb[pi_][:, :n], lhsT=wG[:, k, :], rhs=hT[:, k, s:s + n],
                                                    start=(k == 0), stop=(k == KD - 1)),
                           R=[B_wr[sY], B_hT], W=[B_ps[pi_]], signal=(k == KD - 1))
                    op("act", lambda e: e.activation(out=sgT[:, s:s + n], in_=psb[pi_][:, :n], func=AF.Tanh, scale=0.5),
                       R=[B_ps[pi_]], W=[B_sg])
                    op("dve", lambda e: e.scalar_tensor_tensor(out=sgT[:, s:s + n], in0=sgT[:, s:s + n], scalar=1.0,
                                                               in1=psb[pi_][:, :n], op0=ALU.add, op1=ALU.mult),
                       R=[B_sg, B_ps[pi_]], W=[B_sg])

                def out_block(b):
                    s, n, _ = BLOCKS[b]
                    bp = b % 2
                    iO = 6 if bp == 0 else 4
                    pO = psb[iO]

                    def scores(cc):
                        c = b * 4 + cc
                        cs = slice(c * 128, (c + 1) * 128)
                        ais = []
                        for d_ in range(2):
                            pa = 2 + (cnt_["at"] % 2)
                            ai = cnt_["at"] % 8
                            cnt_["at"] += 1
                            op("pe", lambda e: e.matmul(psb[pa][:, 0:128], lhsT=arr[d_][1][:, cs], rhs=arr[d_][0][:, cs],
                                                        start=True, stop=True),
                               R=[B_arr[d_][1], B_arr[d_][0]], W=[B_ps[pa]])
                            op("dve", lambda e: e.tensor_tensor(out=ATs[ai][:], in0=psb[pa][:, 0:128],
                                                                in1=msk[:, d_ * 128:(d_ + 1) * 128], op=ALU.mult),
                               R=[B_ps[pa], B_hc], W=[B_ATs[ai]])
                            ais.append(ai)
                        return ais

                    def outs(cc, ais):
                        c = b * 4 + cc
                        cs = slice(c * 128, (c + 1) * 128)
                        oc = pO[:, cc * 128:(cc + 1) * 128]
                        op("pe", lambda e: e.matmul(oc, lhsT=Suse[:, 0, c, :], rhs=arr[0][0][:, cs], start=True, stop=False),
                           R=[B_Su, B_arr[0][0]], W=[B_ps[iO]], signal=False)
                        op("pe", lambda e: e.matmul(oc, lhsT=Suse[:, 1, c, :], rhs=arr[1][0][:, cs], start=False, stop=False),
                           R=[B_Su, B_arr[1][0]], W=[B_ps[iO]], signal=False)
                        op("pe", lambda e: e.matmul(oc, lhsT=vv[:, c, :], rhs=ATs[ais[0]][:], start=False, stop=False),
                           R=[B_v, B_ATs[ais[0]]], W=[B_ps[iO]], signal=False)
                        op("pe", lambda e: e.matmul(oc, lhsT=vv[:, c, :], rhs=ATs[ais[1]][:], start=False, stop=True),
                           R=[B_v, B_ATs[ais[1]]], W=[B_ps[iO]], signal=True)

                    pend = None
                    for cc in range(4):
                        ais = scores(cc)
                        if pend is not None:
                            outs(*pend)
                        pend = (cc, ais)
                    outs(*pend)

                def post_block(b):
                    s, n, _ = BLOCKS[b]
                    bp = b % 2
                    iO, iN = (6, 7) if bp == 0 else (4, 5)
                    pO = psb[iO]
                    rstd, B_rstd = rstds[bp], B_rstds[bp]
                    op("act", lambda e: e.activation(out=sq[bp][:, :n], in_=pO[:, :n], func=AF.Square), R=[B_ps[iO]], W=[B_sq[bp]])
                    op("pe", lambda e: e.matmul(psb[iN][:, :n], lhsT=ones_b[:], rhs=sq[bp][:, :n], start=True, stop=True),
                       R=[B_sq[bp], B_const], W=[B_ps[iN]])
                    op("act", lambda e: e.activation(out=rstd[:, :n], in_=psb[iN][:, :n], func=AF.Ln, bias=eps_col[:, 0:1],
                                                     scale=1.0 / 128), R=[B_ps[iN], B_hc], W=[B_rstd])
                    op("act", lambda e: e.activation(out=rstd[:, :n], in_=rstd[:, :n], func=AF.Exp, scale=-0.5),
                       R=[B_rstd], W=[B_rstd])
                    op("dve", lambda e: e.scalar_tensor_tensor(out=tmp[bp][:, :n], in0=pO[:, :n], scalar=gnh[:, 0:1],
                                                               in1=rstd[:, :n], op0=ALU.mult, op1=ALU.mult),
                       R=[B_ps[iO], B_rstd, B_hc], W=[B_tmp[bp]])
                    op("dve", lambda e: e.tensor_tensor(out=ogT[:, s:s + n], in0=tmp[bp][:, :n], in1=sgT[:, s:s + n],
                                                         op=ALU.mult), R=[B_tmp[bp], B_sg], W=[B_ogb[b][1]])

                def proj_block(b):
                    s, n, _ = BLOCKS[b]
                    for mch in range(KD):
                        pi_ = mch % 2
                        op("pe", lambda e: e.matmul(psb[pi_][:, :n], lhsT=wO[:, mch * 128:(mch + 1) * 128],
                                                    rhs=ogT[:, s:s + n], start=True, stop=True),
                           R=[B_wr[sY], B_ogb[b]], W=[B_ps[pi_]])
                        g_ap = modv(l, 2, mch, 0)
                        op("dve", lambda e: e.scalar_tensor_tensor(
                            out=xT[:, mch, s:s + n], in0=psb[pi_][:, :n], scalar=g_ap, in1=xT[:, mch, s:s + n],
                            op0=ALU.mult, op1=ALU.add),
                           R=[B_ps[pi_], B_mod, B_x[mch][b]], W=[B_x[mch][b]])

                out_block(0)
                out_block(1)
                post_block(0)
                out_block(2)
                proj_block(0)
                post_block(1)
                out_block(3)
                proj_block(1)
                post_block(2)
                post_block(3)
                proj_block(2)
                proj_block(3)
            S.barrier()

    if "ret" in P.parts:
        retention_layer(0)
    debug_dump("x_mix0", lambda o: [dma("sp", o.rearrange("(k p) t -> p k t", p=128)[:, k, :], xT[:, k, :],
                                        R=B_x[k]) for k in range(KD)])

    if "moe0" in P.parts:
        moe_layer(0, True, P.n_exp_run)
    debug_dump("x_ffn0", lambda o: [dma("sp", o.rearrange("(k p) t -> p k t", p=128)[:, k, :], xT[:, k, :],
                                        R=B_x[k]) for k in range(KD)])
    if "hg" in P.parts:
        hgrn2_layer(1)
    debug_dump("x_mix1", lambda o: [dma("sp", o.rearrange("(k p) t -> p k t", p=128)[:, k, :], xT[:, k, 0:T_LAT],
                                        R=B_x[k][:4]) for k in range(KD)])
    if "moe1" in P.parts:
        moe_layer(1, False, P.n_exp_run)
    debug_dump("x_ffn1", lambda o: [dma("sp", o.rearrange("(k p) t -> p k t", p=128)[:, k, :], xT[:, k, 0:T_LAT],
                                        R=B_x[k][:4]) for k in range(KD)])
    if "final" in P.parts:
        with ExitStack() as fes:
            ftmp = [P.sb(fes, f"ftmp{i}", [128, 512], F32) for i in range(2)]
            B_ft = [Buf("ft0"), Buf("ft1")]
            frs = P.sb(fes, "frs", [128, 512], F32)
            B_frs = Buf("frs")
            fsq = [P.sb(fes, f"fsq{i}", [128, 512], BF16) for i in range(2)]
            B_fsq = [Buf("fsq0"), Buf("fsq1")]
            fo = [P.sb(fes, f"fo{i}", [128, 512], F32) for i in range(4)]
            B_fo = [Buf(f"fo{i}") for i in range(4)]
            osrc = out_d.rearrange("(k p) t -> p k t", p=128)
            oc_ = {"n": 0}
            for b in range(4):
                s, n, _ = BLOCKS[b]

                def dst(k):
                    i = oc_["n"] % 4
                    oc_["n"] += 1
                    dst.last = i
                    return fo[i][:, :n], [B_fo[i]]
                norm_block(1, 0, b, dst, ftmp, B_ft, frs, B_frs, fsq, B_fsq, psn_i=7, final=True,
                           after=lambda k, b=b, s=s, n=n: dma("sp", osrc[:, k, s:s + n], fo[dst.last][:, :n], R=[B_fo[dst.last]]))
    if P.stop_after is not None:
        osrc = out_d.rearrange("(k p) t -> p k t", p=128)
        for k in range(KD):
            dma("sp", osrc[:, k, :], xT[:, k, 0:T_LAT], R=B_x[k][:4])
    S.barrier()
    es.close()
    return P


def _prep_inputs(inp, b, consts, names=None):
    f = np.float32
    m = {}
    m["xT"] = np.ascontiguousarray(np.concatenate([inp["x"][b].T, inp["ctx"][b].T], axis=1)).astype(f)
    cv = np.stack([inp["c"][b], inp["c_ctx"]], axis=0)
    m["cvec"] = np.ascontiguousarray(cv.reshape(2, KD, 128).transpose(2, 0, 1).reshape(128, 2 * KD))
    m["w_ada"] = np.ascontiguousarray(inp["w_ada"].reshape(2, KD, 128, 12, 512).transpose(0, 3, 2, 1, 4))
    m["b_ada"] = np.ascontiguousarray(np.tile(inp["b_ada"].reshape(1, 12 * D), (2, 1)))
    nr = np.stack([inp["norm_mix"][0], inp["norm_mix"][1], inp["norm_ffn"][0], inp["norm_ffn"][1],
                   inp["norm_final"]], axis=0)
    m["norms"] = np.ascontiguousarray(nr.reshape(5, KD, 128).transpose(2, 0, 1).reshape(128, 5 * KD))
    m["ret_w_in"] = inp["ret_w_in"][0]
    m["ret_w_out"] = inp["ret_w_out"][0]
    m["hg_w_in"] = inp["hg_w_in"][0]
    m["hg_w_out"] = inp["hg_w_out"][0]
    lb = inp["hg_lower_bounds"].reshape(4, KD, 128).transpose(2, 0, 1).reshape(128, 4 * KD)
    m["hg_misc"] = np.ascontiguousarray(np.concatenate([inp["hg_g_norm"][0].reshape(128, 1), lb], axis=1)).astype(f)
    m["moe_router"] = inp["moe_router"]
    m.update(consts)
    for l in range(2):
        for e in range(N_EXP):
            if names is not None and f"wgu_{l}_{e}" not in names:
                continue
            nfb = NFC // 2
            g = inp["moe_w_gate"][l, e].reshape(KD, 128, nfb, 256).transpose(2, 1, 0, 3)
            u = inp["moe_w_up"][l, e].reshape(KD, 128, nfb, 256).transpose(2, 1, 0, 3)
            m[f"wgu_{l}_{e}"] = np.ascontiguousarray(np.concatenate([g, u], axis=3))
            m[f"wdt_{l}_{e}"] = np.ascontiguousarray(
                inp["moe_w_down"][l, e].reshape(nfb, 2, 128, D).transpose(0, 2, 1, 3))
    if names is not None:
        m = {k: v for k, v in m.items() if k in names}
    return m


def kernel(**inputs):
    inp = {k: np.asarray(v) for k, v in inputs.items()}
    consts = _const_tables()
    P = build_program()
    names = set(P.dram.keys())
    shared = _prep_inputs(inp, 0, consts, names)
    in_maps = []
    for b in range(8):
        mb = dict(shared)
        mb["xT"] = np.ascontiguousarray(np.concatenate([inp["x"][b].T, inp["ctx"][b].T], axis=1)).astype(np.float32)
        cv = np.stack([inp["c"][b], inp["c_ctx"]], axis=0)
        mb["cvec"] = np.ascontiguousarray(cv.reshape(2, KD, 128).transpose(2, 0, 1).reshape(128, 2 * KD))
        in_maps.append(mb)
    res = run_bass_kernel_spmd(P.nc, in_maps, core_ids=list(range(8)))
    out = np.stack([np.ascontiguousarray(res.results[b]["outT"].T) for b in range(8)], axis=0)
    return out.astype(np.float32)
```

```python
import math
from contextlib import ExitStack

import numpy as np
import concourse.bass as bass
import concourse.mybir as mybir
from concourse.bass_utils import run_bass_kernel_spmd

F32 = mybir.dt.float32
BF16 = mybir.dt.bfloat16
ALU = mybir.AluOpType
AF = mybir.ActivationFunctionType
AX = mybir.AxisListType

D = 1024
KD = 8
T_LAT = 2048
T_CTX = 256
T_ALL = T_LAT + T_CTX
NT = T_ALL // 128
EPS = 1e-6
N_EXP = 16
FF = 2816
NFC = FF // 128
RET_H = 4
HG_H = 8

BLOCKS = [(0, 512, False), (512, 512, False), (1024, 512, False), (1536, 512, False), (2048, 256, True)]


def interleave(*gens):
    gens = list(gens)
    while gens:
        for g in list(gens):
            try:
                next(g)
            except StopIteration:
                gens.remove(g)


class Buf:
    __slots__ = ("name", "w", "rs")

    def __init__(self, name):
        self.name = name
        self.w = None
        self.rs = {}


class Sched:
    NDMA = 12

    def __init__(self, nc, es):
        self.nc = nc
        self.h = {"pe": nc.tensor, "act": nc.scalar, "dve": nc.vector, "pool": nc.gpsimd, "sp": nc.sync}
        self.sem = {}
        self.cnt = {}
        self.seen = {}
        for e in self.h:
            self.sem[e] = es.enter_context(nc.semaphore("s_" + e))
            self.cnt[e] = 0
            self.seen[e] = {}
        self.dsem = {}
        self.dval = {}
        self.dnext = {}
        for q in ("sp", "pool"):
            self.dsem[q] = [es.enter_context(nc.semaphore(f"d_{q}{i}")) for i in range(self.NDMA)]
            self.dval[q] = [0] * self.NDMA
            self.dnext[q] = 0
        self.ninst = 0

    def _wait(self, eng, tk):
        if tk is None:
            return
        kind = tk[0]
        if kind == "c":
            _, src, n = tk
            if src == eng and eng in ("pe", "sp"):
                return
            if self.seen[eng].get(src, 0) >= n:
                return
            self.h[eng].wait_ge(self.sem[src], n)
            self.seen[eng][src] = n
        else:
            _, q, idx, val = tk
            key = (q, idx)
            if self.seen[eng].get(key, 0) >= val:
                return
            self.h[eng].wait_ge(self.dsem[q][idx], val)
            self.seen[eng][key] = val
        self.ninst += 1

    def _deps(self, eng, R, W):
        for b in R:
            self._wait(eng, b.w)
        for b in W:
            self._wait(eng, b.w)
            for tk in b.rs.values():
                self._wait(eng, tk)

    def _record(self, tk, R, W):
        for b in W:
            b.w = tk
            b.rs = {}
        for b in R:
            if b in W:
                continue
            key = tk[1] if tk[0] == "c" else (tk[1], tk[2])
            b.rs[key] = tk

    @staticmethod
    def _flat(L):
        out = []
        for b in L:
            if isinstance(b, (list, tuple)):
                out.extend(Sched._flat(b))
            else:
                out.append(b)
        return out

    def op(self, eng, fn, R=(), W=(), signal=True):
        R, W = self._flat(R), self._flat(W)
        self._deps(eng, R, W)
        ins = fn(self.h[eng])
        self.ninst += 1
        if signal:
            ins.then_inc(self.sem[eng], 1)
            self.cnt[eng] += 1
            tk = ("c", eng, self.cnt[eng])
            if eng not in ("pe",):
                pass
        else:
            tk = ("c", eng, self.cnt[eng] + 1)
        self._record(tk, R, W)
        return tk

    def dma(self, q, out, in_, R=(), W=()):
        R, W = self._flat(R), self._flat(W)
        self._deps(q, R, W)
        idx = self.dnext[q]
        self.dnext[q] = (idx + 1) % self.NDMA
        prev = self.dval[q][idx]
        if prev > 0:
            self._wait(q, ("d", q, idx, prev))
        val = prev + 16
        self.dval[q][idx] = val
        self.h[q].dma_start(out=out, in_=in_).then_inc(self.dsem[q][idx], 16)
        self.ninst += 1
        tk = ("d", q, idx, val)
        self._record(tk, R, W)
        return tk

    def barrier(self):
        for e in self.h:
            for src in self.h:
                if src != e and self.cnt[src] > 0:
                    self._wait(e, ("c", src, self.cnt[src]))
            for q in ("sp", "pool"):
                for idx in range(self.NDMA):
                    if self.dval[q][idx] > 0:
                        self._wait(e, ("d", q, idx, self.dval[q][idx]))


def _ret_gammas():
    j = np.arange(8, dtype=np.float64)
    g = 1.0 - np.exp2(-5.0 - j / 2)
    return g[0::2], g[1::2]


def _const_tables():
    t = {}
    half = 128
    inv = 10000.0 ** (-np.arange(0, half, 2, dtype=np.float64) / half)
    p = np.arange(128)
    sign = np.where(p < 64, -1.0, 1.0)
    rows = np.arange(T_LAT // 64, dtype=np.float64)
    cols = np.arange(64, dtype=np.float64)
    ang_r = rows[None, :] * inv[p % 64][:, None]
    ang_c = cols[None, :] * inv[p % 64][:, None]
    t["rope"] = np.concatenate(
        [np.cos(ang_r), np.sin(ang_r) * sign[:, None], np.cos(ang_c), np.sin(ang_c) * sign[:, None]], axis=1
    ).astype(np.float32)
    gf, gb = _ret_gammas()
    b = np.arange(128, dtype=np.float64)[:, None]
    a = np.arange(512, dtype=np.float64)[None, :]
    tabs = np.zeros((RET_H, 128, 1920), dtype=np.float64)
    xs = np.arange(896, dtype=np.float64)[None, :] - 384.0
    for h in range(RET_H):
        tabs[h, :, 0:512] = gf[h] ** (a - b)
        tabs[h, :, 512:1024] = gb[h] ** (b + 511 - a)
        dl = xs - b
        tabs[h, :, 1024:1920] = np.where(dl > 0, gf[h] ** np.maximum(dl, 0),
                                         np.where(dl < 0, gb[h] ** np.maximum(-dl, 0), 2.0))
    t["rdec"] = tabs.astype(np.float32)
    t["ident"] = np.eye(128, dtype=np.float32)
    t["hgmask"] = np.concatenate([np.triu(np.ones((128, 128))), np.tril(np.ones((128, 128)))], axis=1).astype(np.float32)
    t["iota1"] = np.tile(np.arange(1, 257, dtype=np.float32)[None, :], (128, 1))
    return t


class Prog:
    def __init__(self, dbg=None, stop_after=None):
        self.dbg = dbg or []
        self.stop_after = stop_after
        self.nc = bass.Bass("TRN2", target_bir_lowering=False)
        self.es = ExitStack()
        self.S = Sched(self.nc, self.es)
        self.dram = {}
        self.dbg_out = {}

    def din(self, name, shape, dt=F32):
        self.dram[name] = self.nc.dram_tensor(name, list(shape), dt, kind="ExternalInput").ap()
        return self.dram[name]

    def dout(self, name, shape, dt=F32):
        self.dram[name] = self.nc.dram_tensor(name, list(shape), dt, kind="ExternalOutput").ap()
        return self.dram[name]

    def sb(self, es, name, shape, dt):
        self._uid = getattr(self, "_uid", 0) + 1
        return es.enter_context(self.nc.sbuf_tensor(f"{name}_u{self._uid}", list(shape), dt))

    def ps(self, es, name, shape, dt=F32):
        return es.enter_context(self.nc.psum_tensor(name, list(shape), dt))


def build_program(dbg=(), stop_after=None, parts=("ret", "moe0", "hg", "moe1", "final"), n_exp_run=N_EXP):
    P = Prog(list(dbg), stop_after)
    P.parts = parts
    P.n_exp_run = n_exp_run
    nc, S, es = P.nc, P.S, P.es
    op, dma = S.op, S.dma

    xT_d = P.din("xT", [D, T_ALL])
    cvec_d = P.din("cvec", [128, 2 * KD])
    wada_d = P.din("w_ada", [2, 12, 128, KD, 512])
    bada_d = P.din("b_ada", [2, 12 * D])
    nrm_d = P.din("norms", [128, 5 * KD])
    retin_d = P.din("ret_w_in", [D, 6144])
    retout_d = P.din("ret_w_out", [2048, D])
    router_d = P.din("moe_router", [2, D, N_EXP])
    rope_d = P.din("rope", [128, 192])
    rdec_d = P.din("rdec", [RET_H, 128, 1920])
    ident_d = P.din("ident", [128, 128])
    out_d = P.dout("outT", [D, T_LAT])
    for name, shape, dt_ in P.dbg:
        P.dbg_out[name] = P.dout("dbg_" + name, shape, dt_)

    xT = P.sb(es, "xT_sb", [128, KD, T_ALL], F32)
    B_x = [[Buf(f"x{k}_{b}") for b in range(5)] for k in range(KD)]
    cvec = P.sb(es, "cvec_sb", [128, 2 * KD], F32)
    scc = P.sb(es, "scc", [128, KD, 2], F32)
    nrm = P.sb(es, "nrm", [128, 5 * KD], F32)
    mod = P.sb(es, "mod", [128, 2, 48, 2], F32)
    modA = P.sb(es, "modA", [128, 2, 2, KD, 2], F32)
    ident_f = P.sb(es, "ident_f", [128, 128], F32)
    ident_b = P.sb(es, "ident_b", [128, 128], BF16)
    ones_b = P.sb(es, "ones_b", [128, 128], BF16)
    B_const = Buf("const")
    B_mod = Buf("mod")

    psb = [P.ps(es, f"psb{i}", [128, 512], F32) for i in range(8)]
    B_ps = [Buf(f"ps{i}") for i in range(8)]

    def sl(b):
        s, n, _ = BLOCKS[b]
        return slice(s, s + n)

    xsrc = xT_d.rearrange("(k p) t -> p k t", p=128)
    for k in range(KD):
        dma("sp", xT[:, k, :], xsrc[:, k, :], W=B_x[k])
    dma("sp", cvec[:], cvec_d, W=[B_const])
    dma("sp", nrm[:], nrm_d, W=[B_const])
    dma("sp", ident_f[:], ident_d, W=[B_const])
    op("act", lambda e: e.copy(out=ident_b[:], in_=ident_f[:]), R=[B_const], W=[B_const])
    op("dve", lambda e: e.memset(ones_b[:], 1.0), W=[B_const])
    op("act", lambda e: e.activation(out=scc[:].rearrange("p k c -> p c k"),
                                     in_=cvec[:].rearrange("p (c k) -> p c k", c=2), func=AF.Silu),
       R=[B_const], W=[B_const])

    with ExitStack() as pes:
        NPIECE = 12
        NST = 4
        wa = [P.sb(pes, f"wa{i}", [128, KD, 512], F32) for i in range(NST)]
        B_wa = [Buf(f"wa{i}") for i in range(NST)]
        HALF = 3 * D
        modrow = P.sb(pes, "modrow", [2, HALF], F32)
        B_mr = Buf("modrow")
        bada2 = P.sb(pes, "bada2", [2, HALF], F32)
        B_b2 = Buf("bada2")
        pi = 0
        for l in range(2):
            for hf in range(2):
                dma("sp", bada2[:], bada_d[:, l * 6 * D + hf * HALF: l * 6 * D + (hf + 1) * HALF], W=[B_b2])
                for pc6 in range(6):
                    pc = hf * 6 + pc6
                    slot = pi % NST
                    dma("sp", wa[slot][:], wada_d[l, pc], W=[B_wa[slot]])
                    pb = pi % 2
                    for k in range(KD):
                        op("pe", lambda e: e.matmul(psb[pb][0:2, :], lhsT=scc[:, k, :], rhs=wa[slot][:, k, :],
                                                    start=(k == 0), stop=(k == KD - 1)),
                           R=[B_wa[slot], B_const], W=[B_ps[pb]], signal=(k == KD - 1))
                    op("dve", lambda e: e.tensor_tensor(out=modrow[:, pc6 * 512:(pc6 + 1) * 512], in0=psb[pb][0:2, :],
                                                        in1=bada2[:, pc6 * 512:(pc6 + 1) * 512],
                                                        op=ALU.add), R=[B_ps[pb], B_b2], W=[B_mr])
                    pi += 1
                pt_ = 2 + (l * 2 + hf) % 2
                for j in range(24):
                    op("pe", lambda e: e.transpose(out=psb[pt_][:, j * 2:(j + 1) * 2], in_=modrow[:, j * 128:(j + 1) * 128],
                                                   identity=ident_f[0:2, 0:2]),
                       R=[B_mr, B_const], W=[B_ps[pt_]], signal=(j == 23))
                op("dve", lambda e: e.tensor_copy(out=mod[:, l, hf * 24:(hf + 1) * 24, :].rearrange("p j c -> p (j c)"),
                                                  in_=psb[pt_][:, 0:48]), R=[B_ps[pt_]], W=[B_mod])
        for l in range(2):
            for site in range(2):
                sc_j = (1 if site == 0 else 4) * KD
                nw = nrm[:, (site * 2 + l) * KD:(site * 2 + l + 1) * KD]
                op("dve", lambda e, l=l, site=site, sc_j=sc_j, nw=nw: e.scalar_tensor_tensor(
                    out=modA[:, l, site, :, :], in0=mod[:, l, sc_j:sc_j + KD, :], scalar=1.0,
                    in1=nw.unsqueeze(2).to_broadcast([128, KD, 2]), op0=ALU.add, op1=ALU.mult),
                   R=[B_mod, B_const], W=[B_mod])
        S.barrier()

    def modv(l, chunk, k, c):
        return mod[:, l, chunk * KD + k, c:c + 1]

    def norm_block(l, site, b, dst_fn, tmp, B_tmp, rstd, B_rstd, sq, B_sq, psn_i, final=False, after=None):
        s, n, isc = BLOCKS[b]
        c = 1 if isc else 0
        for k in range(KD):
            q = k % 2
            op("act", lambda e, k=k, q=q: e.activation(out=sq[q][:, :n], in_=xT[:, k, s:s + n], func=AF.Square),
               R=[B_x[k][b]], W=[B_sq[q]])
            op("pe", lambda e, k=k, q=q: e.matmul(psb[psn_i][:, :n], lhsT=ones_b[:], rhs=sq[q][:, :n],
                                                 start=(k == 0), stop=(k == KD - 1)),
               R=[B_sq[q], B_const], W=[B_ps[psn_i]], signal=True)
        op("act", lambda e: e.activation(out=rstd[:, :n], in_=psb[psn_i][:, :n], func=AF.Sqrt, bias=EPS,
                                         scale=1.0 / D), R=[B_ps[psn_i]], W=[B_rstd])
        op("dve", lambda e: e.reciprocal(out=rstd[:, :n], in_=rstd[:, :n]), R=[B_rstd], W=[B_rstd])
        for k in range(KD):
            q = k % 2
            out_ap, obufs = dst_fn(k)
            if final:
                a_ap = nrm[:, 4 * KD + k:4 * KD + k + 1]
                op("dve", lambda e, k=k, a_ap=a_ap, out_ap=out_ap: e.scalar_tensor_tensor(
                    out=out_ap, in0=xT[:, k, s:s + n], scalar=a_ap, in1=rstd[:, :n], op0=ALU.mult, op1=ALU.mult),
                   R=[B_x[k][b], B_rstd, B_const], W=obufs)
                if after is not None:
                    after(k)
            else:
                a_ap = modA[:, l, site, k, c:c + 1]
                sh_ap = modv(l, 0 if site == 0 else 3, k, c)
                op("dve", lambda e, k=k, q=q, a_ap=a_ap: e.scalar_tensor_tensor(
                    out=tmp[q][:, :n], in0=xT[:, k, s:s + n], scalar=a_ap, in1=rstd[:, :n],
                    op0=ALU.mult, op1=ALU.mult), R=[B_x[k][b], B_rstd, B_mod], W=[B_tmp[q]])
                op("act", lambda e, q=q, sh_ap=sh_ap, out_ap=out_ap: e.activation(
                    out=out_ap, in_=tmp[q][:, :n], func=AF.Identity, bias=sh_ap, scale=1.0),
                   R=[B_tmp[q], B_mod], W=obufs)

    def debug_dump(name, ap_fn):
        if name in P.dbg_out:
            S.barrier()
            tk = ap_fn(P.dbg_out[name])
            S.barrier()

    NSLOT = 4
    wring = [P.sb(es, f"wring{i}", [128, KD, 512], BF16) for i in range(NSLOT)]
    B_wr = [Buf(f"wr{i}") for i in range(NSLOT)]
    wr_state = {"n": 0}

    def wslot():
        i = wr_state["n"] % NSLOT
        wr_state["n"] += 1
        return i

    def retention_layer(l):
        with ExitStack() as les:
            hT = P.sb(les, "hT", [128, KD, T_ALL], BF16)
            B_h = [[Buf(f"h{k}_{b}") for b in range(5)] for k in range(KD)]
            tmp = [P.sb(les, f"ntmp{i}", [128, 512], F32) for i in range(2)]
            B_tmp = [Buf("ntmp0"), Buf("ntmp1")]
            sg = [P.sb(les, f"sg{i}", [128, 512], F32) for i in range(2)]
            B_sg = [Buf("sg0"), Buf("sg1")]
            rstd = P.sb(les, "rstd", [128, 512], F32)
            B_rstd = Buf("rstd")
            sq = [P.sb(les, f"sq{i}", [128, 512], BF16) for i in range(2)]
            B_sq = [Buf("sq0"), Buf("sq1")]
            rope_sb = P.sb(les, "rope_sb", [128, 192], F32)
            B_rope = Buf("rope")
            dma("sp", rope_sb[:], rope_d, W=[B_rope])
            for b in range(5):
                s, n, _ = BLOCKS[b]
                norm_block(l, 0, b, lambda k, b=b, s=s, n=n: (hT[:, k, s:s + n], [B_h[k][b]]),
                           tmp, B_tmp, rstd, B_rstd, sq, B_sq, psn_i=7)
            debug_dump("hT0", lambda o: [dma("sp", o.rearrange("(k p) t -> p k t", p=128)[:, k, :], hT[:, k, :],
                                             R=B_h[k]) for k in range(KD)])
            if P.stop_after == "hT0":
                return

            qr = P.sb(les, "qr", [128, 2, 2, 512], BF16)
            kr = P.sb(les, "kr", [128, 2, T_ALL], BF16)
            vv = P.sb(les, "vv", [128, NT, 512], BF16)
            B_q = [[Buf(f"q{i}_{m}") for m in range(2)] for i in range(2)]
            B_k = [[Buf(f"k{m}_{t}") for t in range(NT)] for m in range(2)]
            B_v = [Buf(f"v{t}") for t in range(NT)]
            rdec = P.sb(les, "rdec_sb", [128, 1920], F32)
            B_rdec = Buf("rdec")
            Ff = rdec[:, 0:512]
            Fb = rdec[:, 512:1024]

            def Dg(kk, n):
                return rdec[:, 1024 + 384 - 128 * kk: 1024 + 384 - 128 * kk + n]
            rt1, B_rt1 = tmp, B_tmp
            rt2, B_rt2 = sg, B_sg
            ctab, B_ctab = tmp, B_tmp
            AT = [P.sb(les, f"AT{i}", [128, 512], BF16) for i in range(3)]
            B_AT = [Buf(f"AT{i}") for i in range(3)]
            ogT = P.sb(les, "ogT", [128, 4, 512], BF16)
            B_og = [Buf(f"og{c}") for c in range(4)]
            sgb = P.sb(les, "sgb", [128, 4, 512], BF16)
            B_sgb = [Buf(f"sgb{c}") for c in range(4)]
            gf, gb = _ret_gammas()
            rcount = {"rope": 0, "at": 0, "q": 0}

            win_src = retin_d.rearrange("(k p) f -> p k f", p=128)
            wout_src = retout_d.rearrange("(c p) f -> p c f", p=128)

            def proj_rope(sA, qk, m, b, dst_ap, wb):
                s, n, isc = BLOCKS[b]
                pi_ = rcount["rope"] % 2
                rcount["rope"] += 1
                pst = psb[pi_]
                for k in range(KD):
                    op("pe", lambda e, k=k: e.matmul(
                        pst[:, :n], lhsT=wring[sA][:, k, qk * 256 + m * 128: qk * 256 + (m + 1) * 128],
                        rhs=hT[:, k, s:s + n], start=(k == 0), stop=(k == KD - 1)),
                       R=[B_wr[sA], B_h[k][b]], W=[B_ps[pi_]], signal=(k == KD - 1))
                scale = 1.0 if qk == 0 else 1.0 / 16.0
                if isc:
                    op("act", lambda e: e.activation(out=dst_ap, in_=pst[:, :n], func=AF.Copy, scale=scale),
                       R=[B_ps[pi_]], W=wb)
                    return
                g0 = s // 64
                if m == 0:
                    cos_ap = rope_sb[:, g0:g0 + 8].unsqueeze(2).to_broadcast([128, 8, 64])
                    sin_lo = rope_sb[0:64, 32 + g0:32 + g0 + 8].unsqueeze(2).to_broadcast([64, 8, 64])
                    sin_hi = rope_sb[64:128, 32 + g0:32 + g0 + 8].unsqueeze(2).to_broadcast([64, 8, 64])
                else:
                    cos_ap = rope_sb[:, 64:128].unsqueeze(1).to_broadcast([128, 8, 64])
                    sin_lo = rope_sb[0:64, 128:192].unsqueeze(1).to_broadcast([64, 8, 64])
                    sin_hi = rope_sb[64:128, 128:192].unsqueeze(1).to_broadcast([64, 8, 64])
                ti = pi_
                p3 = pst[:].rearrange("p (g t) -> p g t", t=64)
                t1v = rt1[ti][:].rearrange("p (g t) -> p g t", t=64)
                t2v = rt2[ti][:].rearrange("p (g t) -> p g t", t=64)
                op("dve", lambda e: e.scalar_tensor_tensor(
                    out=t1v, in0=p3, scalar=scale, in1=cos_ap, op0=ALU.mult, op1=ALU.mult),
                   R=[B_ps[pi_], B_rope], W=[B_rt1[ti]])
                op("dve", lambda e: e.scalar_tensor_tensor(
                    out=t2v[0:64], in0=p3[64:128], scalar=scale, in1=sin_lo, op0=ALU.mult, op1=ALU.mult),
                   R=[B_ps[pi_], B_rope], W=[B_rt2[ti]])
                op("dve", lambda e: e.scalar_tensor_tensor(
                    out=t2v[64:128], in0=p3[0:64], scalar=scale, in1=sin_hi, op0=ALU.mult, op1=ALU.mult),
                   R=[B_ps[pi_], B_rope, B_rt2[ti]], W=[B_rt2[ti]])
                op("dve", lambda e: e.tensor_tensor(
                    out=dst_ap, in0=rt1[ti][:, :n], in1=rt2[ti][:, :n], op=ALU.add),
                   R=[B_rt1[ti], B_rt2[ti]], W=wb)

            for h in range(RET_H):
                sA, sB, sC, sD = wslot(), wslot(), wslot(), wslot()
                dma("pool", wring[sA][:, :, 0:256], win_src[:, :, h * 256:(h + 1) * 256], W=[B_wr[sA]])
                dma("pool", wring[sA][:, :, 256:512], win_src[:, :, 1024 + h * 256:1024 + (h + 1) * 256], W=[B_wr[sA]])
                dma("pool", wring[sB][:], win_src[:, :, 2048 + h * 512:2048 + (h + 1) * 512], W=[B_wr[sB]])
                dma("pool", wring[sC][:], win_src[:, :, 4096 + h * 512:4096 + (h + 1) * 512], W=[B_wr[sC]])
                wD = wring[sD][:].rearrange("p k f -> p (k f)").rearrange("p (c f) -> p c f", c=4)
                dma("pool", wD, wout_src[:, h * 4:(h + 1) * 4, :], W=[B_wr[sD]])
                dma("sp", rdec[:], rdec_d[h], W=[B_rdec])

                def kproj_gen():
                    for m in range(2):
                        for b in range(5):
                            s, n, isc = BLOCKS[b]
                            proj_rope(sA, 1, m, b, kr[:, m, s:s + n], [B_k[m][t] for t in range(s // 128, (s + n) // 128)])
                            yield

                def vproj_gen():
                    for t in range(NT):
                        b = min(t // 4, 4)
                        pi_ = 6 + (t % 2)
                        for k in range(KD):
                            op("pe", lambda e, k=k, t=t, pi_=pi_: e.matmul(
                                psb[pi_][:, :], lhsT=hT[:, k, t * 128:(t + 1) * 128], rhs=wring[sB][:, k, :],
                                start=(k == 0), stop=(k == KD - 1)),
                               R=[B_wr[sB], B_h[k][b]], W=[B_ps[pi_]], signal=(k == KD - 1))
                        op("act", lambda e, t=t, pi_=pi_: e.copy(out=vv[:, t, :], in_=psb[pi_][:, :]),
                           R=[B_ps[pi_]], W=[B_v[t]])
                        yield
                interleave(kproj_gen(), vproj_gen())
                if h == 0:
                    debug_dump("kr0", lambda o: [dma("sp", o[:, m, :], kr[:, m, :], R=B_k[m]) for m in range(2)])
                    debug_dump("vv0", lambda o: [dma("sp", o, vv[:], R=B_v)])
                    if P.stop_after == "proj0":
                        return

                def block_gen(b):
                    s, n, isc = BLOCKS[b]
                    c = 1 if isc else 0
                    qi = rcount["q"] % 2
                    rcount["q"] += 1
                    for m in range(2):
                        proj_rope(sA, 0, m, b, qr[:, qi, m, :n], [B_q[qi][m]])
                    yield
                    for cc in range(4):
                        pg = cc % 2
                        for k in range(KD):
                            op("pe", lambda e: e.matmul(
                                psb[pg][:, :n], lhsT=wring[sC][:, k, cc * 128:(cc + 1) * 128], rhs=hT[:, k, s:s + n],
                                start=(k == 0), stop=(k == KD - 1)),
                               R=[B_wr[sC], B_h[k][b]], W=[B_ps[pg]], signal=(k == KD - 1))
                        op("act", lambda e: e.activation(out=sgb[:, cc, :n], in_=psb[pg][:, :n], func=AF.Silu),
                           R=[B_ps[pg]], W=[B_sgb[cc]])
                    keys = [16, 17] if isc else list(range(NT))
                    pso = [psb[2 + cc] for cc in range(4)]
                    B_pso = [B_ps[2 + cc] for cc in range(4)]

                    def emit_scores(j, idx):
                        pi_ = 6 + (idx % 2)
                        for m in range(2):
                            op("pe", lambda e, m=m: e.matmul(
                                psb[pi_][:, :n], lhsT=kr[:, m, j * 128:(j + 1) * 128], rhs=qr[:, qi, m, :n],
                                start=(m == 0), stop=(m == 1)),
                               R=[B_k[m][j], B_q[qi][m]], W=[B_ps[pi_]], signal=(m == 1))
                        ai = rcount["at"] % 3
                        rcount["at"] += 1
                        pst = psb[pi_]
                        if isc:
                            tab = Dg(j - 16, n)
                            op("dve", lambda e: e.tensor_tensor(out=AT[ai][:, :n], in0=pst[:, :n], in1=tab, op=ALU.mult),
                               R=[B_ps[pi_], B_rdec], W=[B_AT[ai]])
                        elif j >= 16:
                            jc = j - 16
                            s1 = float(gf[h] ** (s + 256 - 128 * jc))
                            s2 = float(gb[h] ** (T_LAT - s - 511 + 128 * jc))
                            ci = jc
                            op("pool", lambda e: e.tensor_scalar(
                                out=ctab[ci][:], in0=Ff, scalar1=s1, scalar2=None, op0=ALU.mult),
                               R=[B_rdec], W=[B_ctab[ci]])
                            op("dve", lambda e: e.scalar_tensor_tensor(
                                out=ctab[ci][:], in0=Fb, scalar=s2, in1=ctab[ci][:], op0=ALU.mult, op1=ALU.add),
                               R=[B_rdec, B_ctab[ci]], W=[B_ctab[ci]])
                            op("dve", lambda e: e.tensor_tensor(
                                out=AT[ai][:, :n], in0=pst[:, :n], in1=ctab[ci][:, :n], op=ALU.mult),
                               R=[B_ps[pi_], B_ctab[ci]], W=[B_AT[ai]])
                        else:
                            rel = j - 4 * b
                            if 0 <= rel < 4:
                                tab = Dg(rel, 512)
                                op("dve", lambda e: e.tensor_tensor(out=AT[ai][:], in0=pst[:], in1=tab, op=ALU.mult),
                                   R=[B_ps[pi_], B_rdec], W=[B_AT[ai]])
                            elif rel < 0:
                                sc = float(gf[h] ** (s - 128 * j))
                                op("dve", lambda e: e.scalar_tensor_tensor(
                                    out=AT[ai][:], in0=pst[:], scalar=sc, in1=Ff, op0=ALU.mult, op1=ALU.mult),
                                   R=[B_ps[pi_], B_rdec], W=[B_AT[ai]])
                            else:
                                sc = float(gb[h] ** (128 * j - s - 511))
                                op("dve", lambda e: e.scalar_tensor_tensor(
                                    out=AT[ai][:], in0=pst[:], scalar=sc, in1=Fb, op0=ALU.mult, op1=ALU.mult),
                                   R=[B_ps[pi_], B_rdec], W=[B_AT[ai]])
                        return ai

                    def emit_av(j, ai, first, last):
                        for cc in range(4):
                            op("pe", lambda e, cc=cc: e.matmul(
                                pso[cc][:, :n], lhsT=vv[:, j, cc * 128:(cc + 1) * 128], rhs=AT[ai][:, :n],
                                start=first, stop=last),
                               R=[B_v[j], B_AT[ai]], W=[B_pso[cc]], signal=(cc == 3))

                    pend = None
                    for idx, j in enumerate(keys):
                        ai = emit_scores(j, idx)
                        if pend is not None:
                            emit_av(*pend)
                        pend = (j, ai, idx == 0, idx == len(keys) - 1)
                    emit_av(*pend)

                    yield
                    for cc in range(4):
                        q2 = cc % 2
                        op("act", lambda e, cc=cc, q2=q2: e.activation(out=sq[q2][:, :n], in_=pso[cc][:, :n],
                                                                      func=AF.Square),
                           R=[B_pso[cc]], W=[B_sq[q2]])
                        op("pe", lambda e, cc=cc, q2=q2: e.matmul(psb[6][:, :n], lhsT=ones_b[:], rhs=sq[q2][:, :n],
                                                                 start=(cc == 0), stop=(cc == 3)),
                           R=[B_sq[q2], B_const], W=[B_ps[6]], signal=True)
                    op("act", lambda e: e.activation(out=rstd[:, :n], in_=psb[6][:, :n], func=AF.Sqrt, bias=EPS,
                                                     scale=1.0 / 512), R=[B_ps[6]], W=[B_rstd])
                    op("dve", lambda e: e.reciprocal(out=rstd[:, :n], in_=rstd[:, :n]), R=[B_rstd], W=[B_rstd])
                    for cc in range(4):
                        q2 = cc % 2
                        op("dve", lambda e, cc=cc, q2=q2: e.tensor_tensor(
                            out=tmp[q2][:, :n], in0=pso[cc][:, :n], in1=rstd[:, :n], op=ALU.mult),
                           R=[B_pso[cc], B_rstd], W=[B_tmp[q2]])
                        op("dve", lambda e, cc=cc, q2=q2: e.tensor_tensor(
                            out=ogT[:, cc, :n], in0=tmp[q2][:, :n], in1=sgb[:, cc, :n], op=ALU.mult),
                           R=[B_tmp[q2], B_sgb[cc]], W=[B_og[cc]])
                    for mch in range(KD):
                        pi_ = 6 + (mch % 2)
                        for cc in range(4):
                            op("pe", lambda e, cc=cc, mch=mch, pi_=pi_: e.matmul(
                                psb[pi_][:, :n], lhsT=wD[:, cc, mch * 128:(mch + 1) * 128], rhs=ogT[:, cc, :n],
                                start=(cc == 0), stop=(cc == 3)),
                               R=[B_wr[sD], B_og[cc]], W=[B_ps[pi_]], signal=(cc == 3))
                        g_ap = modv(l, 2, mch, c)
                        op("dve", lambda e, mch=mch, pi_=pi_, g_ap=g_ap: e.scalar_tensor_tensor(
                            out=xT[:, mch, s:s + n], in0=psb[pi_][:, :n], scalar=g_ap, in1=xT[:, mch, s:s + n],
                            op0=ALU.mult, op1=ALU.add),
                           R=[B_ps[pi_], B_mod, B_x[mch][b]], W=[B_x[mch][b]])

                gens = [block_gen(b) for b in range(5)]
                next(gens[0])
                for b in range(5):
                    next(gens[b])
                    if b + 1 < 5:
                        next(gens[b + 1])
                    for _ in gens[b]:
                        pass
            S.barrier()

    iota_d2 = P.din("iota1", [128, 256])

    def moe_layer(l, with_ctx, n_exp_run=N_EXP):
        nblk = 5 if with_ctx else 4
        ntile = NT if with_ctx else 16
        NS = 288 if with_ctx else 256
        nst = 3 if with_ctx else 2
        st_sz = [128, 128, 32][:nst]
        st_off = [0, 128, 256][:nst]
        with ExitStack() as mes:
            h2tok = P.sb(mes, "h2tok", [128, NT, D], BF16)
            B_h2 = [Buf(f"h2t{t}") for t in range(NT)]
            wr_sb = P.sb(mes, "wr_sb", [128, KD, N_EXP], F32)
            B_wrt = Buf("wrt")
            dma("sp", wr_sb[:], router_d[l].rearrange("(k p) e -> p k e", p=128), W=[B_wrt])
            iota1 = P.sb(mes, "iota1_sb", [128, 256], F32)
            dma("sp", iota1[:], iota_d2, W=[B_wrt])
            aff = P.sb(mes, "aff", [128, NT, N_EXP], F32)
            B_aff = Buf("aff")
            aff_hl = P.sb(mes, "aff_hl", [128, NT, N_EXP, 2], BF16)
            posm_tok = P.sb(mes, "posm_tok", [128, NT, N_EXP], F32)
            B_posm = Buf("posm")

            with ExitStack() as r1:
                tmp = [P.sb(r1, f"mtmp{i}", [128, 512], F32) for i in range(2)]
                B_tmp = [Buf("mtmp0"), Buf("mtmp1")]
                rstd = P.sb(r1, "mrstd", [128, 512], F32)
                B_rstd = Buf("mrstd")
                sq = [P.sb(r1, f"msq{i}", [128, 512], BF16) for i in range(2)]
                B_sq = [Buf("msq0"), Buf("msq1")]
                h2f = P.sb(r1, "h2f", [128, KD, 512], F32)
                B_h2f = [Buf(f"h2f{k}") for k in range(KD)]
                h2b = P.sb(r1, "h2b", [128, KD, 512], BF16)
                B_h2b = [Buf(f"h2b{k}") for k in range(KD)]
                for b in range(nblk):
                    s, n, isc = BLOCKS[b]
                    norm_block(l, 1, b, lambda k, n=n: (h2f[:, k, :n], [B_h2f[k]]),
                               tmp, B_tmp, rstd, B_rstd, sq, B_sq, psn_i=7)
                    for k in range(KD):
                        eng_ = "act" if k % 2 == 0 else "dve"
                        if eng_ == "act":
                            op("act", lambda e, k=k: e.copy(out=h2b[:, k, :n], in_=h2f[:, k, :n]), R=[B_h2f[k]], W=[B_h2b[k]])
                        else:
                            op("dve", lambda e, k=k: e.tensor_copy(out=h2b[:, k, :n], in_=h2f[:, k, :n]),
                               R=[B_h2f[k]], W=[B_h2b[k]])
                    for tt in range(n // 128):
                        t = s // 128 + tt
                        for k in range(KD):
                            op("pe", lambda e, k=k: e.matmul(psb[6][:, 0:N_EXP], lhsT=h2f[:, k, tt * 128:(tt + 1) * 128],
                                                            rhs=wr_sb[:, k, :], start=(k == 0), stop=(k == KD - 1)),
                               R=[B_h2f[k], B_wrt], W=[B_ps[6]], signal=(k == KD - 1))
                        op("act", lambda e: e.copy(out=aff[:, t, :], in_=psb[6][:, 0:N_EXP]), R=[B_ps[6]], W=[B_aff])
                        pi_ = t % 2
                        pbf = psb[pi_][:].bitcast(BF16)
                        for k in range(KD):
                            op("pe", lambda e, k=k: e.transpose(out=pbf[:, k * 128:(k + 1) * 128],
                                                               in_=h2b[:, k, tt * 128:(tt + 1) * 128], identity=ident_b[:]),
                               R=[B_h2b[k], B_const], W=[B_ps[pi_]], signal=(k == KD - 1))
                        op("act", lambda e: e.copy(out=h2tok[:, t, :], in_=pbf), R=[B_ps[pi_]], W=[B_h2[t]])
                mx = P.sb(r1, "smx", [128, NT], F32)
                op("dve", lambda e: e.tensor_reduce(out=mx[:, :ntile], in_=aff[:, :ntile, :], axis=AX.X, op=ALU.max),
                   R=[B_aff], W=[B_rstd])
                op("dve", lambda e: e.tensor_tensor(out=aff[:, :ntile, :], in0=aff[:, :ntile, :],
                                                    in1=mx[:, :ntile].unsqueeze(2).to_broadcast([128, ntile, N_EXP]),
                                                    op=ALU.subtract), R=[B_rstd, B_aff], W=[B_aff])
                op("act", lambda e: e.activation(out=aff[:, :ntile, :], in_=aff[:, :ntile, :], func=AF.Exp),
                   R=[B_aff], W=[B_aff])
                op("dve", lambda e: e.tensor_reduce(out=mx[:, :ntile], in_=aff[:, :ntile, :], axis=AX.X, op=ALU.add),
                   R=[B_aff], W=[B_rstd])
                op("dve", lambda e: e.reciprocal(out=mx[:, :ntile], in_=mx[:, :ntile]), R=[B_rstd], W=[B_rstd])
                op("dve", lambda e: e.tensor_tensor(out=aff[:, :ntile, :], in0=aff[:, :ntile, :],
                                                    in1=mx[:, :ntile].unsqueeze(2).to_broadcast([128, ntile, N_EXP]),
                                                    op=ALU.mult), R=[B_rstd, B_aff], W=[B_aff])
                op("dve", lambda e: e.tensor_copy(out=aff_hl[:, :ntile, :, 0], in_=aff[:, :ntile, :]), R=[B_aff], W=[B_posm])
                op("dve", lambda e: e.tensor_tensor(out=aff_hl[:, :ntile, :, 1], in0=aff[:, :ntile, :],
                                                    in1=aff_hl[:, :ntile, :, 0], op=ALU.subtract),
                   R=[B_aff, B_posm], W=[B_posm])
                S.barrier()
            debug_dump(f"aff{l}", lambda o: [dma("sp", o, aff[:], R=[B_aff])])
            debug_dump(f"h2tok{l}", lambda o: [dma("sp", o, h2tok[:], R=B_h2)])

            with ExitStack() as r2:
                affT = P.sb(r2, "affT", [16, T_ALL], F32)
                mk = P.sb(r2, "mkT", [16, T_ALL], F32)
                cum = P.sb(r2, "cumT", [16, T_ALL], F32)
                sm = P.sb(r2, "bis", [16, 16], F32)
                B_affT, B_mk, B_cum, B_sm = Buf("affT"), Buf("mk"), Buf("cum"), Buf("sm")
                for t in range(ntile):
                    pi_ = t % 2
                    op("pe", lambda e: e.transpose(out=psb[pi_][0:16, 0:128], in_=aff[:, t, :], identity=ident_f[:]),
                       R=[B_aff, B_const], W=[B_ps[pi_]])
                    op("act", lambda e: e.copy(out=affT[:, t * 128:(t + 1) * 128], in_=psb[pi_][0:16, 0:128]),
                       R=[B_ps[pi_]], W=[B_affT])
                sets = [(0, T_LAT, 256)] + ([(T_LAT, T_CTX, 32)] if with_ctx else [])
                one = sm[:, 15:16]
                op("dve", lambda e: e.memset(one, 1.0), W=[B_sm])
                B_smx = [Buf("smA"), Buf("smB")]

                def bisect(si, s0, ns, cap):
                    lo, mid, gs = [sm[:, si * 6 + i: si * 6 + i + 1] for i in range(3)]
                    cnts = [sm[:, si * 6 + 3 + i: si * 6 + 4 + i] for i in range(2)]
                    Bs = B_smx[si]
                    B_lo, B_mid, B_gs = Buf("bis_lo"), Buf("bis_mid"), Buf("bis_gs")
                    B_cn = [Buf("bis_c0"), Buf("bis_c1")]
                    a_ap = affT[:, s0:s0 + ns]
                    op("dve", lambda e: e.memset(lo, 0.0), W=[B_lo])
                    op("dve", lambda e: e.memset(mid, 0.5), W=[B_mid])
                    NIT = 28
                    for it in range(NIT):
                        step = 0.5 ** (it + 1)
                        cnt = cnts[it % 2]
                        op("dve", lambda e: e.tensor_scalar(out=mk[:, s0:s0 + ns], in0=a_ap, scalar1=mid, scalar2=0.0,
                                                            op0=ALU.is_ge, op1=ALU.add, accum_out=cnt),
                           R=[B_affT, B_mid], W=[B_mk, B_cn[it % 2]])
                        yield
                        op("dve", lambda e: e.tensor_scalar(out=gs, in0=cnt, scalar1=float(cap), scalar2=step,
                                                            op0=ALU.is_ge, op1=ALU.mult), R=[B_cn[it % 2]], W=[B_gs])
                        yield
                        if it < NIT - 1:
                            op("dve", lambda e: e.scalar_tensor_tensor(out=mid, in0=gs, scalar=step * 0.5, in1=lo,
                                                                       op0=ALU.add, op1=ALU.add), R=[B_gs, B_lo], W=[B_mid])
                            yield
                        op("pool", lambda e: e.tensor_tensor(out=lo, in0=lo, in1=gs, op=ALU.add), R=[B_gs, B_lo], W=[B_lo])
                        yield
                    op("dve", lambda e: e.tensor_scalar(out=mk[:, s0:s0 + ns], in0=a_ap, scalar1=lo, scalar2=None,
                                                        op0=ALU.is_ge), R=[B_affT, B_lo], W=[B_mk])
                    yield
                    op("dve", lambda e: e.tensor_tensor_scan(out=cum[:, s0:s0 + ns],
                                                             data0=one.to_broadcast([16, ns]), data1=mk[:, s0:s0 + ns],
                                                             initial=0.0, op0=ALU.mult, op1=ALU.add),
                       R=[B_mk, B_sm], W=[B_cum])
                    yield
                    op("dve", lambda e: e.tensor_tensor(out=cum[:, s0:s0 + ns], in0=cum[:, s0:s0 + ns],
                                                        in1=mk[:, s0:s0 + ns], op=ALU.mult), R=[B_mk, B_cum], W=[B_cum])
                    yield

                interleave(*[bisect(si, s0, ns, cap) for si, (s0, ns, cap) in enumerate(sets)])
                for t in range(ntile):
                    pi_ = t % 2
                    op("pe", lambda e: e.transpose(out=psb[pi_][:, 0:16], in_=cum[:, t * 128:(t + 1) * 128],
                                                   identity=ident_f[0:16, 0:16]),
                       R=[B_cum, B_const], W=[B_ps[pi_]])
                    op("act", lambda e: e.copy(out=posm_tok[:, t, :], in_=psb[pi_][:, 0:16]), R=[B_ps[pi_]], W=[B_posm])
                S.barrier()
            debug_dump(f"posm{l}", lambda o: [dma("sp", o, posm_tok[:], R=[B_posm])])
            if P.stop_after == f"route{l}":
                return

            with ExitStack() as xs:
                Pm = P.sb(xs, "Pm", [128, NT, NS], BF16)
                B_Pm = Buf("Pm")
                PTl = P.sb(xs, "PTl", [128, 2, T_LAT], BF16)
                PTc = P.sb(xs, "PTc", [128, T_CTX], BF16)
                B_PT = Buf("PT")

                def PTv(st, a, b2, sz):
                    return PTl[0:sz, st, a:b2] if st < 2 else PTc[0:sz, a - T_LAT:b2 - T_LAT]
                xeT = P.sb(xs, "xeT", [128, KD, NS], BF16)
                B_xe = [Buf(f"xe{k}") for k in range(KD)]
                hmid = P.sb(xs, "hmid", [128, 2, NS], BF16)
                B_hm = [Buf("hm0"), Buf("hm1")]
                sil = P.sb(xs, "sil", [128, 2, NS], BF16)
                B_sil = [Buf("sil0"), Buf("sil1")]
                y_sb = P.sb(xs, "y_sb", [128, nst, D], BF16)
                B_y = [Buf(f"y{i}") for i in range(nst)]
                gsl = P.sb(xs, "gsl", [128, 4], F32)
                B_gsl = Buf("gsl")
                NGU = 5
                NDN = 5 if with_ctx else 6
                wgu = wring + [P.sb(xs, f"wgu{i}", [128, KD, 512], BF16) for i in range(NGU - NSLOT)]
                B_gu = [Buf(f"gu{i}") for i in range(NGU)]
                wdn = [P.sb(xs, f"wdn{i}", [128, 2, D], BF16) for i in range(NDN)]
                B_dn = [Buf(f"dn{i}") for i in range(NDN)]
                op("dve", lambda e: e.memset(Pm[:], 0.0), W=[B_Pm])

                NFB = NFC // 2
                sched = [(e_, fb) for e_ in range(n_exp_run) for fb in range(NFB)]
                issued = {"n": 0}

                def issue_weights(upto):
                    while issued["n"] < min(upto, len(sched)):
                        i = issued["n"]
                        e_, fb = sched[i]
                        if f"wgu_{l}_{e_}" not in P.dram:
                            P.din(f"wgu_{l}_{e_}", [NFC // 2, 128, KD, 512])
                            P.din(f"wdt_{l}_{e_}", [NFC // 2, 128, 2, D])
                        wgu_ap, wd_ap = P.dram[f"wgu_{l}_{e_}"], P.dram[f"wdt_{l}_{e_}"]
                        gi, di = i % NGU, i % NDN
                        dma("pool", wgu[gi][:], wgu_ap[fb], W=[B_gu[gi]])
                        dma("pool", wdn[di][:], wd_ap[fb], W=[B_dn[di]])
                        issued["n"] += 1

                LA = 4 if with_ctx else 5
                issue_weights(LA)
                blk_i = 0
                deferred = {"scatter": None}
                xb = (6, 7) if with_ctx else (4, 5)
                for e_ in range(n_exp_run):
                    op("dve", lambda e: e.tensor_tensor(
                        out=Pm[:, 0:16, 0:256], in0=iota1[:, :].unsqueeze(1).to_broadcast([128, 16, 256]),
                        in1=posm_tok[:, 0:16, e_:e_ + 1].to_broadcast([128, 16, 256]), op=ALU.is_equal),
                       R=[B_posm, B_wrt], W=[B_Pm])
                    if with_ctx:
                        op("dve", lambda e: e.tensor_tensor(
                            out=Pm[:, 16:18, 256:288], in0=iota1[:, 0:32].unsqueeze(1).to_broadcast([128, 2, 32]),
                            in1=posm_tok[:, 16:18, e_:e_ + 1].to_broadcast([128, 2, 32]), op=ALU.is_equal),
                           R=[B_posm, B_wrt], W=[B_Pm])
                    for st in range(nst):
                        tiles = range(16) if st < 2 else range(16, 18)
                        tl = list(tiles)
                        for ti, t in enumerate(tl):
                            op("pe", lambda e: e.matmul(psb[6][0:st_sz[st], st * 2:st * 2 + 2],
                                                        lhsT=Pm[:, t, st_off[st]:st_off[st] + st_sz[st]],
                                                        rhs=aff_hl[:, t, e_, :], start=(ti == 0), stop=(ti == len(tl) - 1)),
                               R=[B_Pm, B_posm], W=[B_ps[6]], signal=(ti == len(tl) - 1))
                    for st in range(nst):
                        op("dve", lambda e: e.tensor_reduce(out=gsl[0:st_sz[st], st:st + 1],
                                                            in_=psb[6][0:st_sz[st], st * 2:st * 2 + 2], axis=AX.X, op=ALU.add),
                           R=[B_ps[6]], W=[B_gsl])
                    def do_PT():
                        gcnt = 0
                        for st in range(nst):
                            tl = list(range(16)) if st < 2 else [16, 17]
                            for g0 in range(0, len(tl), 8):
                                grp = tl[g0:g0 + 8]
                                pi_ = xb[gcnt % 2]
                                gcnt += 1
                                pbf = psb[pi_][:].bitcast(BF16)
                                for gi_, t in enumerate(grp):
                                    op("pe", lambda e: e.transpose(
                                        out=pbf[0:st_sz[st], gi_ * 128:(gi_ + 1) * 128],
                                        in_=Pm[:, t, st_off[st]:st_off[st] + st_sz[st]], identity=ident_b[:]),
                                       R=[B_Pm, B_const], W=[B_ps[pi_]], signal=(gi_ == len(grp) - 1))
                                op("act", lambda e: e.copy(out=PTv(st, grp[0] * 128, (grp[-1] + 1) * 128, st_sz[st]),
                                                           in_=pbf[0:st_sz[st], 0:len(grp) * 128]),
                                   R=[B_ps[pi_]], W=[B_PT])
                    for k in range(KD):
                        pi_ = 6 + (k % 2)
                        for t in range(ntile):
                            op("pe", lambda e: e.matmul(psb[pi_][:, 0:NS], lhsT=h2tok[:, t, k * 128:(k + 1) * 128],
                                                        rhs=Pm[:, t, :], start=(t == 0), stop=(t == ntile - 1)),
                               R=[B_h2[t], B_Pm], W=[B_ps[pi_]], signal=(t == ntile - 1))
                        op("act", lambda e: e.copy(out=xeT[:, k, :], in_=psb[pi_][:, 0:NS]), R=[B_ps[pi_]], W=[B_xe[k]])
                    psY = [[psb[st * 2 + nh] for nh in range(2)] for st in range(nst)]
                    B_psY = [[B_ps[st * 2 + nh] for nh in range(2)] for st in range(nst)]

                    def emit_down(fc, hb, di, first, last):
                        for st in range(nst):
                            for nh in range(2):
                                op("pe", lambda e: e.matmul(
                                    psY[st][nh][0:st_sz[st], :], lhsT=hmid[:, hb, st_off[st]:st_off[st] + st_sz[st]],
                                    rhs=wdn[di][:, fc % 2, nh * 512:(nh + 1) * 512], start=first, stop=last),
                                   R=[B_hm[hb], B_dn[di]], W=[B_psY[st][nh]], signal=(st == nst - 1 and nh == 1))

                    pend = None
                    for fb in range(NFB):
                        i = blk_i
                        blk_i += 1
                        issue_weights(i + LA)
                        if fb == 3:
                            if deferred["scatter"] is not None:
                                deferred["scatter"]()
                                deferred["scatter"] = None
                            do_PT()
                        gi, di = i % NGU, i % NDN
                        for f2 in range(2):
                            fc = fb * 2 + f2
                            hb = fc % 2
                            ia, iu = 6, 7
                            pa, pu = psb[ia], psb[iu]
                            for k in range(KD):
                                op("pe", lambda e: e.matmul(pa[:, 0:NS], lhsT=wgu[gi][:, k, f2 * 128:(f2 + 1) * 128],
                                                            rhs=xeT[:, k, :], start=(k == 0), stop=(k == KD - 1)),
                                   R=[B_gu[gi], B_xe[k]], W=[B_ps[ia]], signal=(k == KD - 1))
                            for k in range(KD):
                                op("pe", lambda e: e.matmul(pu[:, 0:NS],
                                                            lhsT=wgu[gi][:, k, 256 + f2 * 128:256 + (f2 + 1) * 128],
                                                            rhs=xeT[:, k, :], start=(k == 0), stop=(k == KD - 1)),
                                   R=[B_gu[gi], B_xe[k]], W=[B_ps[iu]], signal=(k == KD - 1))
                            op("act", lambda e: e.activation(out=sil[:, hb, :], in_=pa[:, 0:NS], func=AF.Silu),
                               R=[B_ps[ia]], W=[B_sil[hb]])
                            op("dve", lambda e: e.tensor_tensor(out=hmid[:, hb, :], in0=pu[:, 0:NS], in1=sil[:, hb, :],
                                                                op=ALU.mult), R=[B_ps[iu], B_sil[hb]], W=[B_hm[hb]])
                            if pend is not None:
                                emit_down(*pend)
                            pend = (fc, hb, di, fc == 0, fc == NFC - 1)
                    emit_down(*pend)
                    for st in range(nst):
                        for nh in range(2):
                            op("act", lambda e: e.activation(out=y_sb[0:st_sz[st], st, nh * 512:(nh + 1) * 512],
                                                             in_=psY[st][nh][0:st_sz[st], :], func=AF.Copy,
                                                             scale=gsl[0:st_sz[st], st:st + 1]),
                               R=[B_psY[st][nh], B_gsl], W=[B_y[st]])
                    def do_scatter():
                        gi_s = 0
                        for b in range(nblk):
                            s, n, isc = BLOCKS[b]
                            c = 1 if isc else 0
                            sts = [2] if isc else [0, 1]
                            for mch in range(KD):
                                pi_ = xb[gi_s % 2]
                                gi_s += 1
                                for si, st in enumerate(sts):
                                    op("pe", lambda e: e.matmul(psb[pi_][:, :n],
                                                                lhsT=y_sb[0:st_sz[st], st, mch * 128:(mch + 1) * 128],
                                                                rhs=PTv(st, s, s + n, st_sz[st]), start=(si == 0),
                                                                stop=(si == len(sts) - 1)),
                                       R=[B_y[st], B_PT], W=[B_ps[pi_]], signal=(si == len(sts) - 1))
                                g_ap = modv(l, 5, mch, c)
                                op("dve", lambda e: e.scalar_tensor_tensor(
                                    out=xT[:, mch, s:s + n], in0=psb[pi_][:, :n], scalar=g_ap, in1=xT[:, mch, s:s + n],
                                    op0=ALU.mult, op1=ALU.add),
                                   R=[B_ps[pi_], B_mod, B_x[mch][b]], W=[B_x[mch][b]])
                    deferred["scatter"] = do_scatter
                if deferred["scatter"] is not None:
                    deferred["scatter"]()
                S.barrier()

    def hgrn2_layer(l):
        hgin_d = P.din("hg_w_in", [D, 5120])
        hgout_d = P.din("hg_w_out", [D, D])
        hgmisc_d = P.din("hg_misc", [128, 1 + 4 * KD])
        hgmask_d = P.din("hgmask", [128, 256])
        PT_ = 256
        NPART = 9
        NCH = 2
        with ExitStack() as les:
            hT = P.sb(les, "hT1", [128, KD, T_ALL], BF16)
            B_h = [[Buf(f"h1{k}_{b}") for b in range(5)] for k in range(KD)]
            B_hT = Buf("hT1all")
            hgm = P.sb(les, "hgm", [128, 1 + 4 * KD], F32)
            msk = P.sb(les, "hgmask_sb", [128, 256], F32)
            lbs = P.sb(les, "lbs", [128, 2, 2, HG_H], F32)
            lbs2 = P.sb(les, "lbs2", [128, 2, 2, HG_H], F32)
            gnh = P.sb(les, "gnh", [128, 1], F32)
            B_hc = Buf("hgconst")
            dma("sp", hgm[:], hgmisc_d, W=[B_hc])
            dma("sp", msk[:], hgmask_d, W=[B_hc])
            AA = P.sb(les, "hgAA", [128, 14 * PT_], F32)
            RG = [AA[:, r * PT_:(r + 1) * PT_] for r in range(14)]
            B_R = [Buf(f"hgR{r}") for r in range(14)]

            def Aregs(d_, par):
                idx = [d_ * 2 + par, 4 + d_ * 2 + par, 8 + d_, 10 + d_, 12 + d_]
                return [RG[i] for i in idx], [B_R[i] for i in idx]
            tmp = [AA[:, 0:512], AA[:, 768:1280]]
            B_tmp = [[B_R[0], B_R[1]], [B_R[3], B_R[4]]]
            rstds = [AA[:, 1536:2048], AA[:, 2304:2816]]
            B_rstds = [[B_R[6], B_R[7]], [B_R[9], B_R[10]]]
            rstd, B_rstd = rstds[0], B_rstds[0]
            sq = [P.sb(les, f"hsq{i}", [128, 512], BF16) for i in range(2)]
            B_sq = [Buf("hsq0"), Buf("hsq1")]

            class _V:
                def __init__(self, ap):
                    self.ap = ap

                def __getitem__(self, key):
                    return self.ap[key]
            for b in range(5):
                s, n, _ = BLOCKS[b]
                norm_block(l, 0, b, lambda k, b=b, s=s, n=n: (hT[:, k, s:s + n], [B_h[k][b]]),
                           [_V(tmp[0]), _V(tmp[1])], B_tmp, _V(rstd), B_rstd, sq, B_sq, psn_i=7)
            for d_ in range(2):
                op("dve", lambda e: e.tensor_tensor(out=lbs[:, 0, d_, :], in0=hgm[:, 1 + (2 + d_) * 8:1 + (3 + d_) * 8],
                                                    in1=hgm[:, 1 + d_ * 8:1 + (d_ + 1) * 8], op=ALU.subtract),
                   R=[B_hc], W=[B_hc])
            op("act", lambda e: e.activation(out=lbs[:, 0, :, :], in_=lbs[:, 0, :, :], func=AF.Sigmoid), R=[B_hc], W=[B_hc])
            op("dve", lambda e: e.tensor_scalar(out=lbs[:, 1, :, :], in0=lbs[:, 0, :, :], scalar1=-1.0, scalar2=1.0,
                                                op0=ALU.mult, op1=ALU.add), R=[B_hc], W=[B_hc])
            op("dve", lambda e: e.tensor_scalar(out=lbs2[:, 0, :, :], in0=lbs[:, 1, :, :], scalar1=0.5, scalar2=None,
                                                op0=ALU.mult), R=[B_hc], W=[B_hc])
            op("dve", lambda e: e.tensor_tensor(out=lbs2[:, 1, :, :], in0=lbs2[:, 0, :, :], in1=lbs[:, 0, :, :], op=ALU.add),
               R=[B_hc], W=[B_hc])
            op("dve", lambda e: e.tensor_scalar(out=gnh[:], in0=hgm[:, 0:1], scalar1=0.5, scalar2=None, op0=ALU.mult),
               R=[B_hc], W=[B_hc])
            S.barrier()
            debug_dump("hT1", lambda o: [dma("sp", o.rearrange("(k p) t -> p k t", p=128)[:, k, :], hT[:, k, :],
                                             R=[B_hT]) for k in range(KD)])

            qs2 = P.sb(les, "hqs", [128, 2, PT_], BF16)
            B_qs2 = [Buf("hqs0"), Buf("hqs1")]
            arr = [[P.sb(les, f"harr{d_}{i}", [128, T_ALL], BF16) for i in range(3)] for d_ in range(2)]
            B_arr = [[Buf(f"harr{d_}{i}") for i in range(3)] for d_ in range(2)]
            sgT, B_sg = arr[0][2], B_arr[0][2]
            ogT, B_og = arr[1][2], B_arr[1][2]
            B_ogb = [[B_og, Buf(f"ogb{b}")] for b in range(4)]
            vv = P.sb(les, "hvv", [128, NT, 128], BF16)
            B_v = Buf("hvv")
            EF = P.sb(les, "hEF", [128, 2, NT], F32)
            EM = P.sb(les, "hEM", [128, 2, NT], F32)
            t6b = P.sb(les, "ht6", [128, 2, 8], F32)
            B_t6 = [Buf("t6a"), Buf("t6b")]
            B_E = Buf("hE")
            Sst = P.sb(les, "hS", [128, 2, 128], F32)
            B_S = [Buf("hS0"), Buf("hS1")]
            Suse = P.sb(les, "hSuse", [128, 2, 16, 128], BF16)
            B_Su = Buf("hSuse")
            kht = [P.sb(les, f"hkht{i}", [128, 128], BF16) for i in range(2)]
            B_kht = [Buf("kht0"), Buf("kht1")]
            ATs = [P.sb(les, f"hAT{i}", [128, 128], BF16) for i in range(8)]
            B_ATs = [Buf(f"hAT{i}") for i in range(8)]
            one_col = hgm[:, 0:1]
            ones_f = P.sb(les, "hones", [128, 1], F32)
            op("dve", lambda e: e.memset(ones_f[:], 1.0), W=[B_hc])
            eps_col = P.sb(les, "heps", [128, 1], F32)
            op("dve", lambda e: e.memset(eps_col[:], EPS), W=[B_hc])

            win_src = hgin_d.rearrange("(k p) f -> p k f", p=128)
            cnt_ = {"ps": 0, "kht": 0, "at": 0}

            def view3(ap):
                return ap.rearrange("p (c i) -> p c i", i=128)

            for h in range(HG_H):
                sX, sY = wslot(), wslot()
                for gi_, off in enumerate((0, 1024, 2048, 3072)):
                    dma("pool", wring[sX][:, :, gi_ * 128:(gi_ + 1) * 128], win_src[:, :, off + h * 128: off + (h + 1) * 128],
                        W=[B_wr[sX]])
                yflat = wring[sY][:].rearrange("p k f -> p (k f)")
                wG = yflat[:, 0:1024].rearrange("p (k f) -> p k f", k=KD)
                wO = yflat[:, 1024:2048]
                dma("pool", wG, win_src[:, :, 4096 + h * 128:4096 + (h + 1) * 128], W=[B_wr[sY]])
                dma("pool", wO, hgout_d[h * 128:(h + 1) * 128, :], W=[B_wr[sY]])

                def proj(gi_, s, n, pi_):
                    for k in range(KD):
                        op("pe", lambda e: e.matmul(psb[pi_][:, :n], lhsT=wring[sX][:, k, gi_ * 128:(gi_ + 1) * 128],
                                                    rhs=hT[:, k, s:s + n], start=(k == 0), stop=(k == KD - 1)),
                           R=[B_wr[sX], B_hT], W=[B_ps[pi_]], signal=(k == KD - 1))

                def stageA(part):
                    p0 = part * PT_
                    c0 = part * NCH
                    par = part % 2
                    qs = qs2[:, par, :]
                    B_qs = B_qs2[par]
                    pi_ = cnt_["ps"] % 2
                    cnt_["ps"] += 1
                    proj(0, p0, PT_, pi_)
                    op("act", lambda e: e.activation(out=qs, in_=psb[pi_][:, :PT_], func=AF.Tanh, scale=0.5),
                       R=[B_ps[pi_]], W=[B_qs])
                    yield
                    op("dve", lambda e: e.scalar_tensor_tensor(out=qs, in0=qs, scalar=1.0, in1=psb[pi_][:, :PT_],
                                                               op0=ALU.add, op1=ALU.mult), R=[B_qs, B_ps[pi_]], W=[B_qs])
                    yield
                    for tt in range(NCH):
                        t = c0 + tt
                        pi_ = cnt_["ps"] % 2
                        cnt_["ps"] += 1
                        for k in range(KD):
                            op("pe", lambda e: e.matmul(psb[pi_][:, 0:128], lhsT=hT[:, k, t * 128:(t + 1) * 128],
                                                        rhs=wring[sX][:, k, 384:512], start=(k == 0), stop=(k == KD - 1)),
                               R=[B_wr[sX], B_hT], W=[B_ps[pi_]], signal=(k == KD - 1))
                        op("dve", lambda e: e.tensor_copy(out=vv[:, t, :], in_=psb[pi_][:, 0:128]), R=[B_ps[pi_]], W=[B_v])
                        yield
                    regs = [Aregs(d_, par) for d_ in range(2)]
                    pis = []
                    for d_ in range(2):
                        pi_ = cnt_["ps"] % 2
                        cnt_["ps"] += 1
                        pis.append(pi_)
                        proj(1 + d_, p0, PT_, pi_)
                    for d_ in range(2):
                        (A1, A2, A3, A4, A5), BA = regs[d_]
                        op("act", lambda e: e.activation(out=A2, in_=psb[pis[d_]][:, :PT_], func=AF.Tanh, scale=0.5),
                           R=[B_ps[pis[d_]]], W=[BA[1]])
                        yield
                    for d_ in range(2):
                        (A1, A2, A3, A4, A5), BA = regs[d_]
                        op("dve", lambda e: e.tensor_scalar(out=A1, in0=A2, scalar1=-0.5, scalar2=0.5, op0=ALU.mult,
                                                            op1=ALU.add), R=[BA[1]], W=[BA[0]])
                        yield
                    for d_ in range(2):
                        (A1, A2, A3, A4, A5), BA = regs[d_]
                        op("act", lambda e: e.activation(out=A2, in_=A2, func=AF.Ln, bias=lbs2[:, 1, d_, h:h + 1],
                                                         scale=lbs2[:, 0, d_, h:h + 1]), R=[BA[1], B_hc], W=[BA[1]])
                        yield

                def stageB_driver(part):
                    p0 = part * PT_
                    c0 = part * NCH
                    par = part % 2
                    qs = qs2[:, par, :]
                    B_qs = B_qs2[par]
                    def chain(d_):
                        si_ = d_
                        (A1, A2, A3, A4, A5), BA = Aregs(d_, par)
                        t6 = t6b[:, si_, :]
                        B_T = B_t6[si_]
                        oml_ap = lbs[:, 1, d_, h:h + 1]
                        op("dve", lambda e: e.tensor_tensor_scan(out=A3, data0=ones_f[:, 0:1].to_broadcast([128, PT_]),
                                                                 data1=A2, initial=0.0, op0=ALU.mult, op1=ALU.add),
                           R=[BA[1], B_hc], W=[BA[2]])
                        G3, g3 = view3(A3), view3(A2)
                        dst = [arr[d_][i][:, p0:p0 + PT_] for i in range(3)]
                        Bd = B_arr[d_]
                        bc = [128, NCH, 128]
                        if d_ == 0:
                            yield
                            op("dve", lambda e: e.tensor_tensor(out=view3(A4), in0=G3, in1=G3[:, :, 63:64].to_broadcast(bc),
                                                                op=ALU.subtract), R=[BA[2]], W=[BA[3]])
                            yield
                            op("act", lambda e: e.activation(out=A5, in_=A4, func=AF.Exp, bias=-0.6931471805599453), R=[BA[3]], W=[BA[4]])
                            yield
                            op("pool", lambda e: e.tensor_tensor(out=dst[0], in0=qs, in1=A5, op=ALU.mult),
                               R=[B_qs, BA[4]], W=[Bd[0]])
                            yield
                            op("dve", lambda e: e.tensor_tensor(out=t6[:, 0:NCH], in0=G3[:, :, 0], in1=g3[:, :, 0], op=ALU.subtract),
                               R=[BA[2], BA[1]], W=[B_T])
                            yield
                            op("dve", lambda e: e.tensor_tensor(out=EF[:, 0, c0:c0 + NCH], in0=G3[:, :, 127], in1=t6[:, 0:NCH],
                                                                op=ALU.subtract), R=[BA[2], B_T], W=[B_E])
                            yield
                            op("dve", lambda e: e.tensor_tensor(out=EM[:, 0, c0:c0 + NCH], in0=G3[:, :, 63], in1=t6[:, 0:NCH],
                                                                op=ALU.subtract), R=[BA[2], B_T], W=[B_E])
                            yield
                            op("act", lambda e: e.activation(out=A2, in_=A4, func=AF.Exp, scale=-1.0), R=[BA[3]], W=[BA[1]])
                            yield
                            op("dve", lambda e: e.scalar_tensor_tensor(out=dst[1], in0=A1, scalar=oml_ap, in1=A2, op0=ALU.mult, op1=ALU.mult),
                               R=[BA[0], BA[1]], W=[Bd[1]])
                            yield
                            op("dve", lambda e: e.tensor_tensor(out=view3(A4), in0=G3, in1=G3[:, :, 127:128].to_broadcast(bc),
                                                                op=ALU.subtract), R=[BA[2]], W=[BA[3]])
                            yield
                            op("act", lambda e: e.activation(out=A5, in_=A4, func=AF.Exp, scale=-1.0), R=[BA[3]], W=[BA[4]])
                            yield
                            op("dve", lambda e: e.scalar_tensor_tensor(out=dst[2], in0=A1, scalar=oml_ap, in1=A5, op0=ALU.mult, op1=ALU.mult),
                               R=[BA[0], BA[4]], W=[Bd[2]])
                        else:
                            yield
                            op("dve", lambda e: e.tensor_copy(out=t6[:, 0:NCH], in_=G3[:, :, 127]), R=[BA[2]], W=[B_T])
                            yield
                            op("pool", lambda e: e.tensor_tensor(out=A2, in0=A3, in1=A2, op=ALU.subtract),
                               R=[BA[2], BA[1]], W=[BA[1]])
                            H3 = view3(A2)
                            yield
                            op("dve", lambda e: e.tensor_tensor(out=view3(A4), in0=H3, in1=H3[:, :, 64:65].to_broadcast(bc),
                                                                op=ALU.subtract), R=[BA[1]], W=[BA[3]])
                            yield
                            op("act", lambda e: e.activation(out=A5, in_=A4, func=AF.Exp, scale=-1.0, bias=-0.6931471805599453), R=[BA[3]], W=[BA[4]])
                            yield
                            op("pool", lambda e: e.tensor_tensor(out=dst[0], in0=qs, in1=A5, op=ALU.mult),
                               R=[B_qs, BA[4]], W=[Bd[0]])
                            yield
                            op("act", lambda e: e.activation(out=A3, in_=A4, func=AF.Exp), R=[BA[3]], W=[BA[2]])
                            yield
                            op("dve", lambda e: e.scalar_tensor_tensor(out=dst[1], in0=A1, scalar=oml_ap, in1=A3, op0=ALU.mult, op1=ALU.mult),
                               R=[BA[0], BA[2]], W=[Bd[1]])
                            yield
                            op("dve", lambda e: e.tensor_tensor(out=EF[:, 1, c0:c0 + NCH], in0=t6[:, 0:NCH], in1=H3[:, :, 0],
                                                                op=ALU.subtract), R=[BA[1], B_T], W=[B_E])
                            yield
                            op("dve", lambda e: e.tensor_tensor(out=EM[:, 1, c0:c0 + NCH], in0=t6[:, 0:NCH], in1=H3[:, :, 64],
                                                                op=ALU.subtract), R=[BA[1], B_T], W=[B_E])
                            yield
                            op("dve", lambda e: e.tensor_tensor(out=view3(A4), in0=H3, in1=H3[:, :, 0:1].to_broadcast(bc),
                                                                op=ALU.subtract), R=[BA[1]], W=[BA[3]])
                            yield
                            op("act", lambda e: e.activation(out=A5, in_=A4, func=AF.Exp), R=[BA[3]], W=[BA[4]])
                            yield
                            op("dve", lambda e: e.scalar_tensor_tensor(out=dst[2], in0=A1, scalar=oml_ap, in1=A5, op0=ALU.mult, op1=ALU.mult),
                               R=[BA[0], BA[4]], W=[Bd[2]])
                        yield
                    return [chain(0), chain(1)]

                for _ in stageA(0):
                    pass
                for part in range(NPART):
                    gens = stageB_driver(part)
                    if part + 1 < NPART:
                        gens.append(stageA(part + 1))
                    interleave(*gens)
                op("act", lambda e: e.activation(out=EF[:], in_=EF[:], func=AF.Exp), R=[B_E], W=[B_E])
                op("act", lambda e: e.activation(out=EM[:], in_=EM[:], func=AF.Exp), R=[B_E], W=[B_E])

                orders = [[16, 17] + list(range(16)), [17, 16] + list(range(15, -1, -1))]
                for d_ in range(2):
                    op("dve", lambda e: e.memset(Sst[:, d_, :], 0.0), W=[B_S[d_]])
                items = [(step, d_) for step in range(NT - 1) for d_ in range(2)]

                def emit_T(i):
                    step, d_ = items[i]
                    c = orders[d_][step]
                    ki = i % 2
                    pq = 4 + (i % 2)
                    pbf = psb[pq][:].bitcast(BF16)
                    op("pe", lambda e: e.transpose(out=pbf[:, 0:128], in_=arr[d_][2][:, c * 128:(c + 1) * 128],
                                                   identity=ident_b[:]), R=[B_arr[d_][2], B_const], W=[B_ps[pq]])
                    op("act", lambda e: e.copy(out=kht[ki][:], in_=pbf[:, 0:128]), R=[B_ps[pq]], W=[B_kht[ki]])

                def emit_suse(step, d_):
                    c = orders[d_][step]
                    if c < 16:
                        op("act", lambda e: e.activation(out=Suse[:, d_, c, :], in_=Sst[:, d_, :], func=AF.Copy,
                                                         scale=EM[:, d_, c:c + 1]), R=[B_S[d_], B_E], W=[B_Su])

                emit_T(0)
                for i, (step, d_) in enumerate(items):
                    c = orders[d_][step]
                    if i + 1 < len(items):
                        emit_T(i + 1)
                    emit_suse(step, d_)
                    ki = i % 2
                    pd = 2 + (i % 2)
                    op("pe", lambda e: e.matmul(psb[pd][:, 0:128], lhsT=kht[ki][:], rhs=vv[:, c, :], start=True, stop=True),
                       R=[B_kht[ki], B_v], W=[B_ps[pd]])
                    op("dve", lambda e: e.scalar_tensor_tensor(out=Sst[:, d_, :], in0=Sst[:, d_, :], scalar=EF[:, d_, c:c + 1],
                                                               in1=psb[pd][:, 0:128], op0=ALU.mult, op1=ALU.add),
                       R=[B_S[d_], B_E, B_ps[pd]], W=[B_S[d_]])
                for d_ in range(2):
                    emit_suse(NT - 1, d_)

                for b in range(4):
                    s, n, _ = BLOCKS[b]
                    pi_ = b % 2
                    for k in range(KD):
                        op("pe", lambda e: e.matmul(psb[pi_][:, :n], lhsT=wG[:, k, :], rhs=hT[:, k, s:s + n],
                                                    start=(k == 0), stop=(k == KD - 1)),
                           R=[B_wr[sY], B_hT], W=[B_ps[pi_]], signal=(k == KD - 1))
                    op("act", lambda e: e.activation(out=sgT[:, s:s + n], in_=psb[pi_][:, :n], func=AF.Tanh, scale=0.5),
                       R=[B_ps[pi_]], W=[B_sg])
                    op("dve", lambda e: e.scalar_tensor_tensor(out=sgT[:, s:s + n], in0=sgT[:, s:s + n], scalar=1.0,
                                                               in1=psb[pi_][:, :n], op0=ALU.add, op1=ALU.mult),
                       R=[B_sg, B_ps[pi_]], W=[B_sg])

                def out_block(b):
                    s, n, _ = BLOCKS[b]
                    bp = b % 2
                    iO = 6 if bp == 0 else 4
                    pO = psb[iO]

                    def scores(cc):
                        c = b * 4 + cc
                        cs = slice(c * 128, (c + 1) * 128)
                        ais = []
                        for d_ in range(2):
                            pa = 2 + (cnt_["at"] % 2)
                            ai = cnt_["at"] % 8
                            cnt_["at"] += 1
                            op("pe", lambda e: e.matmul(psb[pa][:, 0:128], lhsT=arr[d_][1][:, cs], rhs=arr[d_][0][:, cs],
                                                        start=True, stop=True),
                               R=[B_arr[d_][1], B_arr[d_][0]], W=[B_ps[pa]])
                            op("dve", lambda e: e.tensor_tensor(out=ATs[ai][:], in0=psb[pa][:, 0:128],
                                                                in1=msk[:, d_ * 128:(d_ + 1) * 128], op=ALU.mult),
                               R=[B_ps[pa], B_hc], W=[B_ATs[ai]])
                            ais.append(ai)
                        return ais

                    def outs(cc, ais):
                        c = b * 4 + cc
                        cs = slice(c * 128, (c + 1) * 128)
                        oc = pO[:, cc * 128:(cc + 1) * 128]
                        op("pe", lambda e: e.matmul(oc, lhsT=Suse[:, 0, c, :], rhs=arr[0][0][:, cs], start=True, stop=False),
                           R=[B_Su, B_arr[0][0]], W=[B_ps[iO]], signal=False)
                        op("pe", lambda e: e.matmul(oc, lhsT=Suse[:, 1, c, :], rhs=arr[1][0][:, cs], start=False, stop=False),
                           R=[B_Su, B_arr[1][0]], W=[B_ps[iO]], signal=False)
                        op("pe", lambda e: e.matmul(oc, lhsT=vv[:, c, :], rhs=ATs[ais[0]][:], start=False, stop=False),
                           R=[B_v, B_ATs[ais[0]]], W=[B_ps[iO]], signal=False)
                        op("pe", lambda e: e.matmul(oc, lhsT=vv[:, c, :], rhs=ATs[ais[1]][:], start=False, stop=True),
                           R=[B_v, B_ATs[ais[1]]], W=[B_ps[iO]], signal=True)

                    pend = None
                    for cc in range(4):
                        ais = scores(cc)
                        if pend is not None:
                            outs(*pend)
                        pend = (cc, ais)
                    outs(*pend)

                def post_block(b):
                    s, n, _ = BLOCKS[b]
                    bp = b % 2
                    iO, iN = (6, 7) if bp == 0 else (4, 5)
                    pO = psb[iO]
                    rstd, B_rstd = rstds[bp], B_rstds[bp]
                    op("act", lambda e: e.activation(out=sq[bp][:, :n], in_=pO[:, :n], func=AF.Square), R=[B_ps[iO]], W=[B_sq[bp]])
                    op("pe", lambda e: e.matmul(psb[iN][:, :n], lhsT=ones_b[:], rhs=sq[bp][:, :n], start=True, stop=True),
                       R=[B_sq[bp], B_const], W=[B_ps[iN]])
                    op("act", lambda e: e.activation(out=rstd[:, :n], in_=psb[iN][:, :n], func=AF.Ln, bias=eps_col[:, 0:1],
                                                     scale=1.0 / 128), R=[B_ps[iN], B_hc], W=[B_rstd])
                    op("act", lambda e: e.activation(out=rstd[:, :n], in_=rstd[:, :n], func=AF.Exp, scale=-0.5),
                       R=[B_rstd], W=[B_rstd])
                    op("dve", lambda e: e.scalar_tensor_tensor(out=tmp[bp][:, :n], in0=pO[:, :n], scalar=gnh[:, 0:1],
                                                               in1=rstd[:, :n], op0=ALU.mult, op1=ALU.mult),
                       R=[B_ps[iO], B_rstd, B_hc], W=[B_tmp[bp]])
                    op("dve", lambda e: e.tensor_tensor(out=ogT[:, s:s + n], in0=tmp[bp][:, :n], in1=sgT[:, s:s + n],
                                                         op=ALU.mult), R=[B_tmp[bp], B_sg], W=[B_ogb[b][1]])

                def proj_block(b):
                    s, n, _ = BLOCKS[b]
                    for mch in range(KD):
                        pi_ = mch % 2
                        op("pe", lambda e: e.matmul(psb[pi_][:, :n], lhsT=wO[:, mch * 128:(mch + 1) * 128],
                                                    rhs=ogT[:, s:s + n], start=True, stop=True),
                           R=[B_wr[sY], B_ogb[b]], W=[B_ps[pi_]])
                        g_ap = modv(l, 2, mch, 0)
                        op("dve", lambda e: e.scalar_tensor_tensor(
                            out=xT[:, mch, s:s + n], in0=psb[pi_][:, :n], scalar=g_ap, in1=xT[:, mch, s:s + n],
                            op0=ALU.mult, op1=ALU.add),
                           R=[B_ps[pi_], B_mod, B_x[mch][b]], W=[B_x[mch][b]])

                out_block(0)
                out_block(1)
                post_block(0)
                out_block(2)
                proj_block(0)
                post_block(1)
                out_block(3)
                proj_block(1)
                post_block(2)
                post_block(3)
                proj_block(2)
                proj_block(3)
            S.barrier()

    if "ret" in P.parts:
        retention_layer(0)
    debug_dump("x_mix0", lambda o: [dma("sp", o.rearrange("(k p) t -> p k t", p=128)[:, k, :], xT[:, k, :],
                                        R=B_x[k]) for k in range(KD)])

    if "moe0" in P.parts:
        moe_layer(0, True, P.n_exp_run)
    debug_dump("x_ffn0", lambda o: [dma("sp", o.rearrange("(k p) t -> p k t", p=128)[:, k, :], xT[:, k, :],
                                        R=B_x[k]) for k in range(KD)])
    if "hg" in P.parts:
        hgrn2_layer(1)
    debug_dump("x_mix1", lambda o: [dma("sp", o.rearrange("(k p) t -> p k t", p=128)[:, k, :], xT[:, k, 0:T_LAT],
                                        R=B_x[k][:4]) for k in range(KD)])
    if "moe1" in P.parts:
        moe_layer(1, False, P.n_exp_run)
    debug_dump("x_ffn1", lambda o: [dma("sp", o.rearrange("(k p) t -> p k t", p=128)[:, k, :], xT[:, k, 0:T_LAT],
                                        R=B_x[k][:4]) for k in range(KD)])
    if "final" in P.parts:
        with ExitStack() as fes:
            ftmp = [P.sb(fes, f"ftmp{i}", [128, 512], F32) for i in range(2)]
            B_ft = [Buf("ft0"), Buf("ft1")]
            frs = P.sb(fes, "frs", [128, 512], F32)
            B_frs = Buf("frs")
            fsq = [P.sb(fes, f"fsq{i}", [128, 512], BF16) for i in range(2)]
            B_fsq = [Buf("fsq0"), Buf("fsq1")]
            fo = [P.sb(fes, f"fo{i}", [128, 512], F32) for i in range(4)]
            B_fo = [Buf(f"fo{i}") for i in range(4)]
            osrc = out_d.rearrange("(k p) t -> p k t", p=128)
            oc_ = {"n": 0}
            for b in range(4):
                s, n, _ = BLOCKS[b]

                def dst(k):
                    i = oc_["n"] % 4
                    oc_["n"] += 1
                    dst.last = i
                    return fo[i][:, :n], [B_fo[i]]
                norm_block(1, 0, b, dst, ftmp, B_ft, frs, B_frs, fsq, B_fsq, psn_i=7, final=True,
                           after=lambda k, b=b, s=s, n=n: dma("sp", osrc[:, k, s:s + n], fo[dst.last][:, :n], R=[B_fo[dst.last]]))
    if P.stop_after is not None:
        osrc = out_d.rearrange("(k p) t -> p k t", p=128)
        for k in range(KD):
            dma("sp", osrc[:, k, :], xT[:, k, 0:T_LAT], R=B_x[k][:4])
    S.barrier()
    es.close()
    return P


def _prep_inputs(inp, b, consts, names=None):
    f = np.float32
    m = {}
    m["xT"] = np.ascontiguousarray(np.concatenate([inp["x"][b].T, inp["ctx"][b].T], axis=1)).astype(f)
    cv = np.stack([inp["c"][b], inp["c_ctx"]], axis=0)
    m["cvec"] = np.ascontiguousarray(cv.reshape(2, KD, 128).transpose(2, 0, 1).reshape(128, 2 * KD))
    m["w_ada"] = np.ascontiguousarray(inp["w_ada"].reshape(2, KD, 128, 12, 512).transpose(0, 3, 2, 1, 4))
    m["b_ada"] = np.ascontiguousarray(np.tile(inp["b_ada"].reshape(1, 12 * D), (2, 1)))
    nr = np.stack([inp["norm_mix"][0], inp["norm_mix"][1], inp["norm_ffn"][0], inp["norm_ffn"][1],
                   inp["norm_final"]], axis=0)
    m["norms"] = np.ascontiguousarray(nr.reshape(5, KD, 128).transpose(2, 0, 1).reshape(128, 5 * KD))
    m["ret_w_in"] = inp["ret_w_in"][0]
    m["ret_w_out"] = inp["ret_w_out"][0]
    m["hg_w_in"] = inp["hg_w_in"][0]
    m["hg_w_out"] = inp["hg_w_out"][0]
    lb = inp["hg_lower_bounds"].reshape(4, KD, 128).transpose(2, 0, 1).reshape(128, 4 * KD)
    m["hg_misc"] = np.ascontiguousarray(np.concatenate([inp["hg_g_norm"][0].reshape(128, 1), lb], axis=1)).astype(f)
    m["moe_router"] = inp["moe_router"]
    m.update(consts)
    for l in range(2):
        for e in range(N_EXP):
            if names is not None and f"wgu_{l}_{e}" not in names:
                continue
            nfb = NFC // 2
            g = inp["moe_w_gate"][l, e].reshape(KD, 128, nfb, 256).transpose(2, 1, 0, 3)
            u = inp["moe_w_up"][l, e].reshape(KD, 128, nfb, 256).transpose(2, 1, 0, 3)
            m[f"wgu_{l}_{e}"] = np.ascontiguousarray(np.concatenate([g, u], axis=3))
            m[f"wdt_{l}_{e}"] = np.ascontiguousarray(
                inp["moe_w_down"][l, e].reshape(nfb, 2, 128, D).transpose(0, 2, 1, 3))
    if names is not None:
        m = {k: v for k, v in m.items() if k in names}
    return m


def kernel(**inputs):
    inp = {k: np.asarray(v) for k, v in inputs.items()}
    consts = _const_tables()
    P = build_program()
    names = set(P.dram.keys())
    shared = _prep_inputs(inp, 0, consts, names)
    in_maps = []
    for b in range(8):
        mb = dict(shared)
        mb["xT"] = np.ascontiguousarray(np.concatenate([inp["x"][b].T, inp["ctx"][b].T], axis=1)).astype(np.float32)
        cv = np.stack([inp["c"][b], inp["c_ctx"]], axis=0)
        mb["cvec"] = np.ascontiguousarray(cv.reshape(2, KD, 128).transpose(2, 0, 1).reshape(128, 2 * KD))
        in_maps.append(mb)
    res = run_bass_kernel_spmd(P.nc, in_maps, core_ids=list(range(8)))
    out = np.stack([np.ascontiguousarray(res.results[b]["outT"].T) for b in range(8)], axis=0)
    return out.astype(np.float32)
```
